# Optimizing a Trainium2 kernel written in Bass

```python
import jax, jax.numpy as jnp
from jax import lax
import numpy as np

D_MODEL = 1024
BATCH = 16
SEQ = 2048
DEPTH = 2

HEAD_DIM = 64
N_FOX_HEADS = 8
N_SB_HEADS = 8
FOX_WIDTH = N_FOX_HEADS * HEAD_DIM
SB_WIDTH = N_SB_HEADS * HEAD_DIM
LRU_WIDTH = D_MODEL
LRU_BLOCKS = 16
LRU_BLOCK_DIM = LRU_WIDTH // LRU_BLOCKS
CONV_WIDTH = 4
LRU_C = 8.0
N_BRANCHES = 3
N_EXPERTS = 32
TOP_K = 4
D_EXPERT = D_MODEL
SWIGLU_LIMIT = 7.0
SWIGLU_ALPHA = 1.702
Q_BLOCK = 128
EXPERT_BLOCK = 256
LN_EPS = 1e-5
DEEPNORM_ALPHA = (2.0 * DEPTH) ** 0.25
DEEPNORM_BETA = (8.0 * DEPTH) ** -0.25
IN_SIZES = (FOX_WIDTH, FOX_WIDTH, FOX_WIDTH, N_FOX_HEADS, SB_WIDTH, SB_WIDTH, SB_WIDTH, LRU_WIDTH, LRU_WIDTH)
IN_SPLITS = tuple(sum(IN_SIZES[:i + 1]) for i in range(len(IN_SIZES) - 1))
D_IN = sum(IN_SIZES)

kernel_name = 'hybrid_fox_stickbreak_rglru_moe_deepnorm'


def layer_norm(x, g, b):
    xf = x.astype(jnp.float32)
    mu = jnp.mean(xf, axis=-1, keepdims=True)
    var = jnp.mean(jnp.square(xf - mu), axis=-1, keepdims=True)
    y = (xf - mu) * lax.rsqrt(var + LN_EPS)
    return (y * g + b).astype(x.dtype)


def _to_blocks(t):
    b, s = t.shape[0], t.shape[1]
    return jnp.moveaxis(t.reshape((b, s // Q_BLOCK, Q_BLOCK) + t.shape[2:]), 1, 0)


def _from_blocks(o):
    o = jnp.moveaxis(o, 0, 1)
    b, nb, qb, h, d = o.shape
    return o.reshape(b, nb * qb, h * d)


def fox_attention(q, k, v, logf):
    s_len, dh = q.shape[1], q.shape[3]
    nb = s_len // Q_BLOCK
    F = jnp.cumsum(logf.astype(jnp.float32), axis=1)
    Fk = jnp.transpose(F, (0, 2, 1))
    key_pos = jnp.arange(s_len)
    scale = dh ** -0.5

    def block(args):
        qb, Fq, i = args
        q_pos = i * Q_BLOCK + jnp.arange(Q_BLOCK)
        s = jnp.einsum('bqhd,bkhd->bhqk', qb, k, preferred_element_type=jnp.float32) * scale
        s = s + jnp.transpose(Fq, (0, 2, 1))[..., None] - Fk[:, :, None, :]
        s = jnp.where(key_pos[None, :] <= q_pos[:, None], s, -jnp.inf)
        p = jax.nn.softmax(s, axis=-1)
        return jnp.einsum('bhqk,bkhd->bqhd', p.astype(v.dtype), v)

    out = lax.map(block, (_to_blocks(q), _to_blocks(F), jnp.arange(nb)))
    return _from_blocks(out)


def stick_breaking_attention(q, k, v):
    s_len, dh = q.shape[1], q.shape[3]
    nb = s_len // Q_BLOCK
    key_pos = jnp.arange(s_len)
    scale = dh ** -0.5

    def block(args):
        qb, i = args
        q_pos = i * Q_BLOCK + jnp.arange(Q_BLOCK)
        z = jnp.einsum('bqhd,bkhd->bhqk', qb, k, preferred_element_type=jnp.float32) * scale
        strict = key_pos[None, :] < q_pos[:, None]
        log_keep = jnp.where(strict, jax.nn.log_sigmoid(-z), 0.0)
        cs = jnp.cumsum(log_keep, axis=-1)
        after = jnp.minimum(cs[..., -1:] - cs, 0.0)
        w = jnp.where(strict, jnp.exp(jax.nn.log_sigmoid(z) + after), 0.0)
        return jnp.einsum('bhqk,bkhd->bqhd', w.astype(v.dtype), v)

    out = lax.map(block, (_to_blocks(q), jnp.arange(nb)))
    return _from_blocks(out)


def rg_lru_branch(xc, gc, conv_w, conv_b, wa, ba, wx, bx, lam):
    b, s, w = xc.shape
    xconv = lax.conv_general_dilated(
        xc, conv_w[:, None, :], window_strides=(1,), padding=[(CONV_WIDTH - 1, 0)],
        dimension_numbers=('NWC', 'WIO', 'NWC'), feature_group_count=w) + conv_b
    xf = xconv.astype(jnp.float32)
    xb = xf.reshape(b, s, LRU_BLOCKS, LRU_BLOCK_DIM)
    r = jax.nn.sigmoid(jnp.einsum('bsnc,ncd->bsnd', xb, wa.astype(jnp.float32)).reshape(b, s, w) + ba.astype(jnp.float32))
    i_gate = jax.nn.sigmoid(jnp.einsum('bsnc,ncd->bsnd', xb, wx.astype(jnp.float32)).reshape(b, s, w) + bx.astype(jnp.float32))
    log_a = -LRU_C * jax.nn.softplus(-lam.astype(jnp.float32)) * r
    a = jnp.exp(log_a)
    u = jnp.sqrt(-jnp.expm1(2.0 * log_a)) * (i_gate * xf)

    def combine(left, right):
        a_l, b_l = left
        a_r, b_r = right
        return a_l * a_r, a_r * b_l + b_r

    _, h = lax.associative_scan(combine, (a, u), axis=1)
    return (h * jax.nn.gelu(gc.astype(jnp.float32))).astype(xc.dtype)


def token_mixer(h, w_in, b_f, conv_w, conv_b, lru_wa, lru_ba, lru_wx, lru_bx, lru_lambda,
                w_gate, b_gate, w_pa, w_pb, w_pc, w_o):
    b, s, d = h.shape
    z = h @ w_in
    qa, ka, va, fa, qb, kb, vb, xc, gc = jnp.split(z, IN_SPLITS, axis=-1)
    fox_h = lambda t: t.reshape(b, s, N_FOX_HEADS, HEAD_DIM)
    sb_h = lambda t: t.reshape(b, s, N_SB_HEADS, HEAD_DIM)
    logf = jax.nn.log_sigmoid(fa.astype(jnp.float32) + b_f.astype(jnp.float32))
    o_a = fox_attention(fox_h(qa), fox_h(ka), fox_h(va), logf)
    o_b = stick_breaking_attention(sb_h(qb), sb_h(kb), sb_h(vb))
    o_c = rg_lru_branch(xc, gc, conv_w, conv_b, lru_wa, lru_ba, lru_wx, lru_bx, lru_lambda)
    g = jax.nn.sigmoid((h @ w_gate + b_gate).astype(jnp.float32)).astype(h.dtype)
    g = g.reshape(b, s, N_BRANCHES, d)
    merged = g[:, :, 0] * (o_a @ w_pa) + g[:, :, 1] * (o_b @ w_pb) + g[:, :, 2] * (o_c @ w_pc)
    return merged @ w_o


def moe_ffn(h, w_router, b_router, w_gu, b_gu, w_down, b_down):
    b, s, d = h.shape
    t = b * s
    hf = h.reshape(t, d)
    logits = (hf @ w_router).astype(jnp.float32) + b_router.astype(jnp.float32)
    top_v, top_i = lax.top_k(logits, TOP_K)
    gates = jax.nn.softmax(top_v, axis=-1)
    n = t * TOP_K
    flat_e = top_i.reshape(n)
    order = jnp.argsort(flat_e)
    e_sorted = flat_e[order]
    tok_sorted = order // TOP_K
    gate_sorted = gates.reshape(n)[order]
    counts = jnp.bincount(flat_e, length=N_EXPERTS)
    padded = (counts + EXPERT_BLOCK - 1) // EXPERT_BLOCK * EXPERT_BLOCK
    pad_end = jnp.cumsum(padded)
    pad_start = pad_end - padded
    start = jnp.cumsum(counts) - counts
    dest = pad_start[e_sorted] + jnp.arange(n) - start[e_sorted]
    n_blocks = -(-n // EXPERT_BLOCK) + N_EXPERTS
    x_buf = jnp.zeros((n_blocks * EXPERT_BLOCK, d), hf.dtype).at[dest].set(hf[tok_sorted])
    block_e = jnp.minimum(jnp.searchsorted(pad_end, jnp.arange(n_blocks) * EXPERT_BLOCK, side='right'),
                          N_EXPERTS - 1)

    def expert_block(args):
        xb, e = args
        gu = xb @ w_gu[e] + b_gu[e]
        gate, up = jnp.split(gu, 2, axis=-1)
        gate = jnp.minimum(gate, SWIGLU_LIMIT)
        up = jnp.clip(up, -SWIGLU_LIMIT, SWIGLU_LIMIT)
        act = (up + 1.0) * (gate * jax.nn.sigmoid(SWIGLU_ALPHA * gate))
        return act @ w_down[e] + b_down[e]

    y_buf = lax.map(expert_block, (x_buf.reshape(n_blocks, EXPERT_BLOCK, d), block_e)).reshape(-1, d)
    y_tok = y_buf[dest] * gate_sorted[:, None].astype(y_buf.dtype)
    y = jnp.zeros((t, d), h.dtype).at[tok_sorted].add(y_tok.astype(h.dtype))
    return y.reshape(b, s, d)


def setup_inputs(seed: int = 0) -> dict:
    key = jax.random.key(seed)
    ks = jax.random.split(key, 32)
    f32 = jnp.float32
    nrm = lambda k, shape, sc: jax.random.normal(k, shape, f32) * sc
    L, D = DEPTH, D_MODEL
    a0 = jax.random.uniform(ks[14], (L, LRU_WIDTH), f32, minval=0.9, maxval=0.999)
    p0 = a0 ** (1.0 / LRU_C)
    lru_lambda = jnp.log(p0) - jnp.log1p(-p0)
    return {
        'x': nrm(ks[0], (BATCH, SEQ, D), 1.0),
        'c': nrm(ks[1], (BATCH, D), 1.0),
        'w_ada': nrm(ks[2], (L, D, 6 * D), 0.2 * D ** -0.5),
        'b_ada': nrm(ks[3], (L, 6 * D), 0.01),
        'ln1_g': 1.0 + nrm(ks[4], (L, D), 0.02),
        'ln1_b': nrm(ks[5], (L, D), 0.02),
        'w_in': nrm(ks[6], (L, D, D_IN), D ** -0.5),
        'b_f': jax.random.uniform(ks[7], (L, N_FOX_HEADS), f32, minval=1.0, maxval=4.0),
        'conv_w': nrm(ks[8], (L, CONV_WIDTH, LRU_WIDTH), CONV_WIDTH ** -0.5),
        'conv_b': nrm(ks[9], (L, LRU_WIDTH), 0.01),
        'lru_wa': nrm(ks[10], (L, LRU_BLOCKS, LRU_BLOCK_DIM, LRU_BLOCK_DIM), LRU_BLOCK_DIM ** -0.5),
        'lru_ba': nrm(ks[11], (L, LRU_WIDTH), 0.01),
        'lru_wx': nrm(ks[12], (L, LRU_BLOCKS, LRU_BLOCK_DIM, LRU_BLOCK_DIM), LRU_BLOCK_DIM ** -0.5),
        'lru_bx': nrm(ks[13], (L, LRU_WIDTH), 0.01),
        'lru_lambda': lru_lambda,
        'w_gate': nrm(ks[15], (L, D, N_BRANCHES * D), D ** -0.5),
        'b_gate': nrm(ks[16], (L, N_BRANCHES * D), 0.01),
        'w_pa': nrm(ks[17], (L, FOX_WIDTH, D), FOX_WIDTH ** -0.5),
        'w_pb': nrm(ks[18], (L, SB_WIDTH, D), SB_WIDTH ** -0.5),
        'w_pc': nrm(ks[19], (L, LRU_WIDTH, D), LRU_WIDTH ** -0.5),
        'w_o': nrm(ks[20], (L, D, D), DEEPNORM_BETA * D ** -0.5),
        'ln2_g': 1.0 + nrm(ks[21], (L, D), 0.02),
        'ln2_b': nrm(ks[22], (L, D), 0.02),
        'w_router': nrm(ks[23], (L, D, N_EXPERTS), D ** -0.5),
        'b_router': nrm(ks[24], (L, N_EXPERTS), 0.01),
        'w_gu': nrm(ks[25], (L, N_EXPERTS, D, 2 * D_EXPERT), D ** -0.5),
        'b_gu': nrm(ks[26], (L, N_EXPERTS, 2 * D_EXPERT), 0.01),
        'w_down': nrm(ks[27], (L, N_EXPERTS, D_EXPERT, D), DEEPNORM_BETA * D_EXPERT ** -0.5),
        'b_down': nrm(ks[28], (L, N_EXPERTS, D), 0.01),
    }


def reference(x, c, w_ada, b_ada, ln1_g, ln1_b, w_in, b_f, conv_w, conv_b, lru_wa, lru_ba, lru_wx,
              lru_bx, lru_lambda, w_gate, b_gate, w_pa, w_pb, w_pc, w_o, ln2_g, ln2_b, w_router,
              b_router, w_gu, b_gu, w_down, b_down):
    cond = jax.nn.silu(c)
    for l in range(DEPTH):
        ada = (cond @ w_ada[l] + b_ada[l])[:, None, :]
        shift1, scale1, gate1, shift2, scale2, gate2 = jnp.split(ada, 6, axis=-1)
        h = x * (1.0 + scale1) + shift1
        y = token_mixer(h, w_in[l], b_f[l], conv_w[l], conv_b[l], lru_wa[l], lru_ba[l], lru_wx[l],
                        lru_bx[l], lru_lambda[l], w_gate[l], b_gate[l], w_pa[l], w_pb[l], w_pc[l], w_o[l])
        x = layer_norm(DEEPNORM_ALPHA * x + (1.0 + gate1) * y, ln1_g[l], ln1_b[l])
        h = x * (1.0 + scale2) + shift2
        y = moe_ffn(h, w_router[l], b_router[l], w_gu[l], b_gu[l], w_down[l], b_down[l])
        x = layer_norm(DEEPNORM_ALPHA * x + (1.0 + gate2) * y, ln2_g[l], ln2_b[l])
    return x
```

```python
from contextlib import ExitStack
import numpy as np
import concourse.bass as bass
import concourse.mybir as mybir
from concourse.bass_utils import run_bass_kernel_spmd

F32 = mybir.dt.float32
F32R = mybir.dt.float32r
BF16 = mybir.dt.bfloat16
I32 = mybir.dt.int32
U32 = mybir.dt.uint32
AF = mybir.ActivationFunctionType
ALU = mybir.AluOpType

NCORES = 8
D = 1024
SEQ = 2048
NSEQ = 2
T = NSEQ * SEQ
NT = T // 128
NCH = T // 512
DEPTH = 2
H = 8
DH = 64
D_IN = 5128
NE = 32
CAP = 1024
ALPHA = (2.0 * DEPTH) ** 0.25
LN_EPS = 1e-5
NEG = -30000.0


class Slot:
    def __init__(self, sem):
        self.sem = sem
        self.count = 0


class Buf:
    def __init__(self, name, t=None):
        self.name = name
        self.t = t
        self.w = {}
        self.r = {}
        self.ds = None

    def __getitem__(self, k):
        return self.t[k]


class EngState:
    def __init__(self, name, eng, sem):
        self.name = name
        self.eng = eng
        self.sem = sem
        self.count = 0
        self.waited = {}


class Ctx:
    def __init__(self, nc, stack, n_dma_sems=72):
        self.nc = nc
        self.stack = stack
        self.E = {}
        for name, eng in (("pe", nc.tensor), ("act", nc.scalar), ("dve", nc.vector),
                          ("pool", nc.gpsimd), ("sp", nc.sync)):
            sem = stack.enter_context(nc.semaphore("s_" + name))
            self.E[name] = EngState(name, eng, sem)
        self.free_slots = [Slot(stack.enter_context(nc.semaphore("d%d" % i))) for i in range(n_dma_sems)]
        self.used_slots = []
        self.n_ins = 0
        self.n_wait = 0
        self.uid = 0

    def sbuf(self, ph, name, shape, dtype=F32):
        self.uid += 1
        t = ph.enter_context(self.nc.sbuf_tensor("%s_%d" % (name, self.uid), list(shape), dtype))
        return Buf(name, t)

    def psum(self, name, shape, dtype=F32):
        t = self.stack.enter_context(self.nc.psum_tensor(name, list(shape), dtype))
        return Buf(name, t)

    def dram(self, name, shape, dtype=F32, kind="Internal"):
        t = self.nc.dram_tensor(name, list(shape), dtype, kind=kind)
        return Buf(name, t.ap())

    def _wait(self, E, deps):
        for sid, (sem, val) in deps.items():
            if E.waited.get(sid, 0) >= val:
                continue
            E.eng.wait_ge(sem, val)
            E.waited[sid] = val
            self.n_wait += 1

    @staticmethod
    def _merge(d, src, skip=None):
        for sid, (sem, val) in src.items():
            if skip is not None and sid == skip:
                continue
            if sid not in d or d[sid][1] < val:
                d[sid] = (sem, val)

    def op(self, en, fn, reads=(), writes=()):
        E = self.E[en]
        own = id(E.sem)
        deps = {}
        for b in reads:
            self._merge(deps, b.w, skip=own if en == "pe" else None)
        for b in writes:
            self._merge(deps, b.w, skip=own)
            self._merge(deps, b.r, skip=own)
        self._wait(E, deps)
        ins = fn()
        E.count += 1
        ins.then_inc(E.sem, 1)
        tok = (E.sem, E.count)
        for b in reads:
            b.r[own] = tok
        for b in writes:
            b.w = {own: tok}
            b.r = {}
        self.n_ins += 1
        return ins

    def dma(self, qn, out, in_, reads=(), writes=(), sb=None, fn=None):
        E = self.E[qn]
        if sb.ds is None:
            sb.ds = self.free_slots.pop()
            self.used_slots.append(sb.ds)
        ds = sb.ds
        deps = {}
        if ds.count:
            deps[id(ds.sem)] = (ds.sem, ds.count)
        for b in reads:
            self._merge(deps, b.w)
        for b in writes:
            self._merge(deps, b.w)
            self._merge(deps, b.r)
        self._wait(E, deps)
        ins = E.eng.dma_start(out=out, in_=in_) if fn is None else fn()
        ds.count += 16
        ins.then_inc(ds.sem, 16)
        tok = (ds.sem, ds.count)
        sid = id(ds.sem)
        for b in reads:
            b.r[sid] = tok
        for b in writes:
            b.w = {sid: tok}
            b.r = {}
        self.n_ins += 1
        return ins

    def barrier(self, only=None):
        deps = {}
        for E in self.E.values():
            if E.count:
                deps[id(E.sem)] = (E.sem, E.count)
        for s in self.used_slots:
            if s.count:
                deps[id(s.sem)] = (s.sem, s.count)
        for name, E in self.E.items():
            if only is not None and name not in only:
                continue
            d = {k: v for k, v in deps.items() if k != id(E.sem)}
            self._wait(E, d)

    def end_phase(self):
        self.barrier()
        self.free_slots.extend(self.used_slots)
        self.used_slots = []


def build_program(n_layers=DEPTH, stop_after=None, debug=()):
    nc = bass.Bass("TRN2", target_bir_lowering=False)
    st = ExitStack()
    cx = Ctx(nc, st)
    V, S, P = nc.vector, nc.scalar, nc.gpsimd

    def din(name, shape, dtype=F32):
        return cx.dram(name, shape, dtype, kind="ExternalInput")

    x_in = din("x", [T, D]); c_in = din("c", [NSEQ, D])
    w_ada = din("w_ada", [DEPTH, D, 6 * D]); b_ada = din("b_ada", [DEPTH, 6 * D])
    ln1_g = din("ln1_g", [DEPTH, D]); ln1_b = din("ln1_b", [DEPTH, D])
    w_in = din("w_in", [DEPTH, D, D_IN]); b_f = din("b_f", [DEPTH, H])
    conv_w = din("conv_w", [DEPTH, 4, D]); conv_b = din("conv_b", [DEPTH, D])
    lru_wa = din("lru_wa", [DEPTH, 16, 64, 64]); lru_ba = din("lru_ba", [DEPTH, D])
    lru_wx = din("lru_wx", [DEPTH, 16, 64, 64]); lru_bx = din("lru_bx", [DEPTH, D])
    lru_lambda = din("lru_lambda", [DEPTH, D])
    w_gate = din("w_gate", [DEPTH, D, 3 * D]); b_gate = din("b_gate", [DEPTH, 3 * D])
    w_pa = din("w_pa", [DEPTH, 512, D]); w_pb = din("w_pb", [DEPTH, 512, D])
    w_pc = din("w_pc", [DEPTH, D, D]); w_o = din("w_o", [DEPTH, D, D])
    ln2_g = din("ln2_g", [DEPTH, D]); ln2_b = din("ln2_b", [DEPTH, D])
    w_router = din("w_router", [DEPTH, D, NE]); b_router = din("b_router", [DEPTH, NE])
    w_gu = din("w_gu", [DEPTH, NE, D, 2 * D]); b_gu = din("b_gu", [DEPTH, NE, 2 * D])
    w_down = din("w_down", [DEPTH, NE, D, D]); b_down = din("b_down", [DEPTH, NE, D])
    k_identb = din("k_identb", [128, 128], BF16)
    k_identf = din("k_identf", [128, 128])
    k_masks = din("k_masks", [128, 8, 128])
    k_ebase = din("k_ebase", [128, NE])
    out_d = cx.dram("out", [T, D], F32, kind="ExternalOutput")

    adaB = cx.dram("adaB", [DEPTH, NSEQ, 6, D])
    xres = [cx.dram("xresA", [T, D]), cx.dram("xresB", [T, D])]
    qkT = cx.dram("qkT", [4, 512, T], BF16)
    vv = cx.dram("vv", [2, T, 512], BF16)
    faT = cx.dram("faT", [H, T])
    Fd = cx.dram("Fd", [6, H, T], BF16)
    xgT = cx.dram("xgT", [2, D, T])
    gT = cx.dram("gT", [3 * D, T])
    oT = cx.dram("oT", [2 * D, T], BF16)
    xbuf = cx.dram("xbuf", [(NE + 1) * CAP, D], BF16)
    ybuf = cx.dram("ybuf", [(NE + 1) * CAP, D])
    dbg = {}
    for name, shape in debug:
        dbg[name] = cx.dram("dbg_" + name, shape, F32, kind="ExternalOutput")

    pb = [cx.psum("pb%d" % i, [128, 512]) for i in range(7)]
    pbh = cx.psum("pbh", [128, 1024], BF16)

    gl = st
    identb = cx.sbuf(gl, "identb", [128, 128], BF16)
    identf = cx.sbuf(gl, "identf", [128, 128])
    masks = cx.sbuf(gl, "masks", [128, 8, 128])
    masksb = cx.sbuf(gl, "masksb", [128, 8, 128], BF16)
    masksr = cx.sbuf(gl, "masksr", [128, 8, 128], F32R)
    ebase = cx.sbuf(gl, "ebase", [128, NE])
    IDX = cx.sbuf(gl, "IDX", [128, NT, 4], I32)
    GK = cx.sbuf(gl, "GK", [128, NT, 4])
    onesb = cx.sbuf(gl, "onesb", [128, 128], BF16)
    cx.dma("sp", identb[:], k_identb[:], reads=[k_identb], writes=[identb], sb=identb)
    cx.dma("sp", identf[:], k_identf[:], reads=[k_identf], writes=[identf], sb=identf)
    cx.dma("sp", masks[:], k_masks[:], reads=[k_masks], writes=[masks], sb=masks)
    cx.dma("sp", ebase[:], k_ebase[:], reads=[k_ebase], writes=[ebase], sb=ebase)
    cx.op("dve", lambda: V.tensor_copy(masksb[:], masks[:]), [masks], [masksb])
    cx.op("dve", lambda: V.tensor_copy(masksr[:], masks[:]), [masks], [masksr])
    cx.op("dve", lambda: V.memset(onesb[:], 1.0), [], [onesb])
    M_SUT, M_TRI, M_ONES, M_NEGC, M_NEGNS, M_NSTRICT, M_NTRII, M_NONES = range(8)

    def row_bc(ap_row, n):
        return ap_row.partition_broadcast(n)

    def phase_ada():
        ph = ExitStack()
        c_col = cx.sbuf(ph, "c_col", [128, NSEQ, 8])
        cond = cx.sbuf(ph, "cond", [128, NSEQ, 8])
        with nc.allow_non_contiguous_dma(reason="tiny transposed load of c"):
            cx.dma("sp", c_col[:], c_in.t.rearrange("s (kc p) -> p s kc", p=128), reads=[c_in], writes=[c_col], sb=c_col)
        cx.op("act", lambda: S.activation(cond[:], c_col[:], AF.Silu), [c_col], [cond])
        condB = cx.sbuf(ph, "condB", [128, NSEQ, 8, 128], BF16)
        for s in range(NSEQ):
            cx.op("dve", lambda: V.tensor_copy(condB[:, s], cond[:, s, :].unsqueeze(2).to_broadcast([128, 8, 128])),
                  [cond], [condB])
        wring = [cx.sbuf(ph, "wada%d" % i, [128, 8, 512], BF16) for i in range(3)]
        brow = [cx.sbuf(ph, "brow%d" % i, [128, 512]) for i in range(2)]
        res = [cx.sbuf(ph, "ares%d" % i, [128, 512]) for i in range(3)]
        k = 0
        for l in range(n_layers):
            for n in range(12):
                wt = wring[k % 3]; br = brow[k % 2]
                cx.dma("pool", None, None, reads=[w_ada], writes=[wt], sb=wt,
                       fn=lambda: P.dma_start(out=wt[:], in_=w_ada[l, :, n * 512:(n + 1) * 512].rearrange("(kc p) n -> p kc n", p=128)))
                cx.dma("sp", br[:], row_bc(b_ada[l, n * 512:(n + 1) * 512], 128), reads=[b_ada], writes=[br], sb=br)
                which = n // 2
                for s in range(NSEQ):
                    ps = pb[(k * NSEQ + s) % 4]
                    for kc in range(8):
                        cx.op("pe", lambda: nc.tensor.matmul(ps[:], condB[:, s, kc, :], wt[:, kc, :], start=(kc == 0), stop=(kc == 7)),
                              [condB, wt], [ps])
                    r = res[(k * NSEQ + s) % 3]
                    cx.op("dve", lambda: V.tensor_tensor(r[:], ps[:], br[:], ALU.add), [ps, br], [r])
                    if which not in (0, 3):
                        cx.op("dve", lambda: V.tensor_scalar_add(r[:], r[:], 1.0), [r], [r])
                    c0_ = (n % 2) * 512
                    cx.dma("sp", adaB[l, s, which:which + 1, c0_:c0_ + 512], r[0:1, :], reads=[r], writes=[adaB], sb=r)
                k += 1
        cx.end_phase()
        ph.close()

    def load_mod(ph, l, sub, names=("sh", "sc", "gt")):
        tiles = {}
        for s in range(NSEQ):
            for nm, idx in (("sh", 3 * sub), ("sc", 3 * sub + 1), ("gt", 3 * sub + 2)):
                if nm not in names:
                    continue
                t = cx.sbuf(ph, "mod_%s%d" % (nm, s), [128, D])
                cx.dma("sp", t[:], row_bc(adaB[l, s, idx, :], 128), reads=[adaB], writes=[t], sb=t)
                tiles[(nm, s)] = t
        return tiles

    def phase_proj(l, xsrc):
        ph = ExitStack()
        mod = load_mod(ph, l, 0, ("sh", "sc"))
        bgate = cx.sbuf(ph, "bgate", [128, 24])
        with nc.allow_non_contiguous_dma(reason="tiny bias relayout"):
            cx.dma("sp", bgate[:], b_gate[l].rearrange("(m p) -> p m", p=128), reads=[b_gate], writes=[bgate], sb=bgate)
        xt = [cx.sbuf(ph, "xt%d" % i, [128, D]) for i in range(3)]
        hf = [cx.sbuf(ph, "hf%d" % i, [128, D]) for i in range(2)]
        hb = [cx.sbuf(ph, "hb%d" % i, [128, D], BF16) for i in range(2)]
        hT = [cx.sbuf(ph, "hT%d" % i, [128, 8, 512], BF16) for i in range(2)]
        wring = [cx.sbuf(ph, "win%d" % i, [128, 8, 512], BF16) for i in range(3)]
        wfa = cx.sbuf(ph, "wfa", [128, 8, 8], BF16)
        evf = [cx.sbuf(ph, "evf%d" % i, [128, 512]) for i in range(4)]
        evb = [cx.sbuf(ph, "evb%d" % i, [128, 512], BF16) for i in range(4)]
        cx.dma("pool", None, None, reads=[w_in], writes=[wfa], sb=wfa,
               fn=lambda: P.dma_start(out=wfa[:], in_=w_in[l, :, 1536:1544].rearrange("(kc p) n -> p kc n", p=128)))
        wk = 0; ek = 0; pk = 0
        pieces = [("qa", 0), ("ka", 512), ("va", 1024), ("qb", 1544), ("kb", 2056), ("vb", 2568),
                  ("xc0", 3080), ("xc1", 3592), ("gc0", 4104), ("gc1", 4616)] + [("g%d" % i, i * 512) for i in range(6)]
        for ch in range(NCH):
            s = ch // (SEQ // 512)
            hTc = hT[ch % 2]
            for tt in range(4):
                ti = ch * 4 + tt
                x_t = xt[ti % 3]; h_f = hf[ti % 2]; h_b = hb[ti % 2]
                cx.dma("sp", x_t[:], xsrc[ti * 128:(ti + 1) * 128, :], reads=[xsrc], writes=[x_t], sb=x_t)
                cx.op("dve", lambda: V.tensor_tensor(h_f[:], x_t[:], mod[("sc", s)][:], ALU.mult), [x_t, mod[("sc", s)]], [h_f])
                cx.op("pool", lambda: P.tensor_tensor(h_b[:], h_f[:], mod[("sh", s)][:], ALU.add), [h_f, mod[("sh", s)]], [h_b])
                for kc in range(8):
                    cx.op("pe", lambda: nc.tensor.transpose(pbh[:, kc * 128:(kc + 1) * 128], h_b[:, kc * 128:(kc + 1) * 128], identb[:]),
                          [h_b, identb], [pbh])
                cx.op("act", lambda: S.copy(hTc[:, :, tt * 128:(tt + 1) * 128], pbh[:].rearrange("p (kc t) -> p kc t", kc=8)),
                      [pbh], [hTc])
            tok0 = ch * 512
            ps = pb[pk % 6]; pk += 1
            for kc in range(8):
                cx.op("pe", lambda: nc.tensor.matmul(ps[0:8, :], wfa[:, kc, :], hTc[:, kc, :], start=(kc == 0), stop=(kc == 7)),
                      [wfa, hTc], [ps])
            ev = evf[ek % 4]; ek += 1
            cx.op("dve", lambda: V.tensor_copy(ev[0:8, :], ps[0:8, :]), [ps], [ev])
            cx.dma("sp", faT[:, tok0:tok0 + 512], ev[0:8, :], reads=[ev], writes=[faT], sb=ev)
            for (nm, c0) in pieces:
                wt = wring[wk % 3]; wk += 1
                wsrc = (w_gate if nm[0] == "g" and nm[1].isdigit() else w_in)
                cx.dma("pool", None, None, reads=[wsrc], writes=[wt], sb=wt,
                       fn=lambda: P.dma_start(out=wt[:], in_=wsrc[l, :, c0:c0 + 512].rearrange("(kc p) n -> p kc n", p=128)))
                if nm in ("va", "vb"):
                    for tt in range(4):
                        ps = pb[pk % 6]; pk += 1
                        for kc in range(8):
                            cx.op("pe", lambda: nc.tensor.matmul(ps[:], hTc[:, kc, tt * 128:(tt + 1) * 128], wt[:, kc, :], start=(kc == 0), stop=(kc == 7)),
                                  [hTc, wt], [ps])
                        ev = evb[ek % 4]; ek += 1
                        eng = "act" if ek % 2 else "dve"
                        if eng == "act":
                            cx.op("act", lambda: S.copy(ev[:], ps[:]), [ps], [ev])
                        else:
                            cx.op("dve", lambda: V.tensor_copy(ev[:], ps[:]), [ps], [ev])
                        cx.dma("sp", vv[0 if nm == "va" else 1, tok0 + tt * 128:tok0 + (tt + 1) * 128, :], ev[:], reads=[ev], writes=[vv], sb=ev)
                    continue
                for m in range(4):
                    ps = pb[pk % 6]; pk += 1
                    for kc in range(8):
                        cx.op("pe", lambda: nc.tensor.matmul(ps[:], wt[:, kc, m * 128:(m + 1) * 128], hTc[:, kc, :], start=(kc == 0), stop=(kc == 7)),
                              [wt, hTc], [ps])
                    if nm in ("qa", "ka", "qb", "kb"):
                        ev = evb[ek % 4]; ek += 1
                        sc = 0.125 if nm[0] == "q" else 1.0
                        if ek % 2:
                            cx.op("act", lambda: S.mul(ev[:], ps[:], sc), [ps], [ev])
                        else:
                            cx.op("dve", lambda: V.tensor_scalar_mul(ev[:], ps[:], sc), [ps], [ev])
                        qi = ("qa", "ka", "qb", "kb").index(nm)
                        cx.dma("sp", qkT[qi, m * 128:(m + 1) * 128, tok0:tok0 + 512], ev[:], reads=[ev], writes=[qkT], sb=ev)
                    elif nm[0] in ("x",) or nm[:2] == "gc":
                        ev = evf[ek % 4]; ek += 1
                        if ek % 2:
                            cx.op("act", lambda: S.copy(ev[:], ps[:]), [ps], [ev])
                        else:
                            cx.op("dve", lambda: V.tensor_copy(ev[:], ps[:]), [ps], [ev])
                        r0 = int(nm[2]) * 512 + m * 128
                        cx.dma("sp", xgT[0 if nm[0] == "x" else 1, r0:r0 + 128, tok0:tok0 + 512], ev[:], reads=[ev], writes=[xgT], sb=ev)
                    else:
                        ev = evf[ek % 4]; ek += 1
                        mi = int(nm[1]) * 4 + m
                        cx.op("act", lambda: S.activation(ev[:], ps[:], AF.Sigmoid, bias=bgate[:, mi:mi + 1], scale=1.0), [ps, bgate], [ev])
                        cx.dma("sp", gT[mi * 128:(mi + 1) * 128, tok0:tok0 + 512], ev[:], reads=[ev], writes=[gT], sb=ev)
        cx.end_phase()
        ph.close()

    def phase_fprep(l):
        ph = ExitStack()
        bf = cx.sbuf(ph, "bf", [H, 1]); nbf = cx.sbuf(ph, "nbf", [H, 1])
        with nc.allow_non_contiguous_dma(reason="tiny"):
            cx.dma("sp", bf[:], b_f[l].rearrange("(h o) -> h o", o=1), reads=[b_f], writes=[bf], sb=bf)
        cx.op("dve", lambda: V.tensor_scalar_mul(nbf[:], bf[:], -1.0), [bf], [nbf])
        ones = cx.sbuf(ph, "ones", [H, SEQ]); cx.op("dve", lambda: V.memset(ones[:], 1.0), [], [ones])
        for s in range(NSEQ):
            fa = cx.sbuf(ph, "fa%d" % s, [H, SEQ]); e = cx.sbuf(ph, "fe%d" % s, [H, SEQ]); sp_ = cx.sbuf(ph, "fs%d" % s, [H, SEQ])
            F = cx.sbuf(ph, "F%d" % s, [H, SEQ]); r1 = cx.sbuf(ph, "r1%d" % s, [H, SEQ]); r2 = cx.sbuf(ph, "r2%d" % s, [H, SEQ])
            parts = cx.sbuf(ph, "parts%d" % s, [H, 6, SEQ], BF16)
            cx.dma("sp", fa[:], faT[:, s * SEQ:(s + 1) * SEQ], reads=[faT], writes=[fa], sb=fa)
            cx.op("act", lambda: S.activation(e[:], fa[:], AF.Exp, bias=nbf[:, 0:1], scale=-1.0), [fa, nbf], [e])
            cx.op("act", lambda: S.activation(sp_[:], e[:], AF.Ln, bias=1.0, scale=1.0), [e], [sp_])
            cx.op("dve", lambda: V.tensor_tensor_scan(F[:], ones[:], sp_[:], 0.0, ALU.mult, ALU.subtract), [ones, sp_], [F])
            cx.op("dve", lambda: V.tensor_copy(parts[:, 0, :], F[:]), [F], [parts])
            cx.op("dve", lambda: V.tensor_tensor(r1[:], F[:], parts[:, 0, :], ALU.subtract), [F, parts], [r1])
            cx.op("dve", lambda: V.tensor_copy(parts[:, 1, :], r1[:]), [r1], [parts])
            cx.op("dve", lambda: V.tensor_tensor(r2[:], r1[:], parts[:, 1, :], ALU.subtract), [r1, parts], [r2])
            cx.op("dve", lambda: V.tensor_copy(parts[:, 2, :], r2[:]), [r2], [parts])
            cx.op("dve", lambda: V.tensor_scalar_mul(parts[:, 3:6, :], parts[:, 0:3, :], -1.0), [parts], [parts])
            cx.dma("sp", Fd[:, :, s * SEQ:(s + 1) * SEQ].rearrange("v h t -> h v t"), parts[:], reads=[parts], writes=[Fd], sb=parts)
        cx.end_phase()
        ph.close()

    def phase_fox():
        ph = ExitStack()
        NB = 2
        kT = [cx.sbuf(ph, "kT%d" % i, [64, SEQ], BF16) for i in range(NB)]
        qT = [cx.sbuf(ph, "qT%d" % i, [64, SEQ], BF16) for i in range(NB)]
        A6 = [cx.sbuf(ph, "A6%d" % i, [6, SEQ], BF16) for i in range(NB)]
        B6 = [cx.sbuf(ph, "B6%d" % i, [6, SEQ], BF16) for i in range(NB)]
        Va = [cx.sbuf(ph, "Va%d" % i, [128, 16, 128], BF16) for i in range(NB)]
        Pt = [cx.sbuf(ph, "Pt%d" % i, [128, 512], BF16) for i in range(3)]
        rec = [cx.sbuf(ph, "rec%d" % i, [128, 512]) for i in range(2)]
        ob = [cx.sbuf(ph, "ob%d" % i, [64, 512], BF16) for i in range(2)]
        for i in range(NB):
            cx.op("dve", lambda: V.memset(A6[i][:], 1.0), [], [A6[i]])
            cx.op("dve", lambda: V.memset(B6[i][:], 1.0), [], [B6[i]])
            cx.op("dve", lambda: V.memset(Va[i][:], 1.0), [], [Va[i]])
        it = 0; pk = 0; ck = 0
        for s in range(NSEQ):
            for h in range(H):
                b = it % NB; it += 1
                t0 = s * SEQ
                cx.dma("sp", qT[b][:], qkT[0, h * 64:(h + 1) * 64, t0:t0 + SEQ], reads=[qkT], writes=[qT[b]], sb=qT[b])
                cx.dma("sp", kT[b][:], qkT[1, h * 64:(h + 1) * 64, t0:t0 + SEQ], reads=[qkT], writes=[kT[b]], sb=kT[b])
                cx.dma("sp", B6[b][0:3, :], Fd[0:3, h, t0:t0 + SEQ], reads=[Fd], writes=[B6[b]], sb=B6[b])
                cx.dma("sp", A6[b][3:6, :], Fd[3:6, h, t0:t0 + SEQ], reads=[Fd], writes=[A6[b]], sb=A6[b])
                with nc.allow_non_contiguous_dma(reason="v head slice, 128B runs"):
                    cx.dma("sp", Va[b][:, :, 0:64], vv[0, t0:t0 + SEQ, h * 64:(h + 1) * 64].rearrange("(j p) d -> p j d", p=128),
                           reads=[vv], writes=[Va[b]], sb=Va[b])
                for c in range(4):
                    nJ = 4 * c + 4
                    O = pb[4 + ck % 2]; ck += 1
                    q0 = c * 512
                    sq = []
                    for i in range(nJ + 1):
                        if i < nJ:
                            J = i
                            lo = 128 * max(0, J - 4 * c)
                            Sb = pb[pk % 3]; Pb = Pt[pk % 3]; pk += 1
                            diag = J >= 4 * c
                            cx.op("pe", lambda: nc.tensor.matmul(Sb[:, lo:512], kT[b][:, J * 128:(J + 1) * 128], qT[b][:, q0 + lo:q0 + 512], start=True, stop=False),
                                  [kT[b], qT[b]], [Sb])
                            cx.op("pe", lambda: nc.tensor.matmul(Sb[:, lo:512], A6[b][:, J * 128:(J + 1) * 128], B6[b][:, q0 + lo:q0 + 512], start=False, stop=not diag),
                                  [A6[b], B6[b]], [Sb])
                            if diag:
                                cx.op("pe", lambda: nc.tensor.matmul(Sb[:, lo:lo + 128], identb[:], masksb[:, M_NEGC, :], start=False, stop=True),
                                      [identb, masksb], [Sb])
                            cx.op("act", lambda: S.activation(Pb[:, lo:512], Sb[:, lo:512], AF.Exp), [Sb], [Pb])
                            sq.append((J, lo, Pb))
                        if i >= 1:
                            J, lo, Pb = sq[i - 1]
                            cx.op("pe", lambda: nc.tensor.matmul(O[:, lo:512], Va[b][:, J, :], Pb[:, lo:512], start=(J == 0), stop=(J == nJ - 1)),
                                  [Va[b], Pb], [O])
                    rc = rec[ck % 2]; o_ = ob[ck % 2]
                    cx.op("dve", lambda: V.reciprocal(rc[64:128, :], O[64:128, :]), [O], [rc])
                    cx.op("dve", lambda: V.tensor_tensor(o_[:], O[0:64, :], rc[64:128, :], ALU.mult), [O, rc], [o_])
                    cx.dma("sp", oT[h * 64:(h + 1) * 64, t0 + q0:t0 + q0 + 512], o_[:], reads=[o_], writes=[oT], sb=o_)
        cx.end_phase()
        ph.close()

    def phase_sb():
        ph = ExitStack()
        NB = 2
        kT = [cx.sbuf(ph, "kT%d" % i, [64, SEQ], BF16) for i in range(NB)]
        qT = [cx.sbuf(ph, "qT%d" % i, [64, SEQ], BF16) for i in range(NB)]
        Vb = [cx.sbuf(ph, "Vb%d" % i, [128, 16, 64], BF16) for i in range(NB)]
        Et = [cx.sbuf(ph, "Et%d" % i, [128, 512]) for i in range(3)]
        SPt = [cx.sbuf(ph, "SPt%d" % i, [128, 512], F32R) for i in range(4)]
        R = [cx.sbuf(ph, "R%d" % i, [128, 512], F32R) for i in range(2)]
        Wt = [cx.sbuf(ph, "Wt%d" % i, [128, 512], BF16) for i in range(3)]
        ob = [cx.sbuf(ph, "ob%d" % i, [64, 512], BF16) for i in range(2)]
        Zf = cx.sbuf(ph, "Zf", [128, 512])
        cx.op("dve", lambda: V.memset(Zf[:], 0.0), [], [Zf])
        it = 0; zk = 0; ak = 0; ck = 0; k3 = 0; wk = 0
        for s in range(NSEQ):
            for h in range(H):
                b = it % NB; it += 1
                t0 = s * SEQ
                cx.dma("sp", qT[b][:], qkT[2, h * 64:(h + 1) * 64, t0:t0 + SEQ], reads=[qkT], writes=[qT[b]], sb=qT[b])
                cx.dma("sp", kT[b][:], qkT[3, h * 64:(h + 1) * 64, t0:t0 + SEQ], reads=[qkT], writes=[kT[b]], sb=kT[b])
                with nc.allow_non_contiguous_dma(reason="v head slice, 128B runs"):
                    cx.dma("sp", Vb[b][:], vv[1, t0:t0 + SEQ, h * 64:(h + 1) * 64].rearrange("(j p) d -> p j d", p=128),
                           reads=[vv], writes=[Vb[b]], sb=Vb[b])
                for c in range(4):
                    nJ = 4 * c + 4
                    O = pb[4 + ck % 2]; Rc = R[ck % 2]; o_ = ob[ck % 2]; ck += 1
                    q0 = c * 512
                    cx.op("dve", lambda: V.tensor_copy(Rc[:], Zf[:]), [Zf], [Rc])
                    st_ = {}
                    for step in range(nJ + 2):
                        if step < nJ:
                            J = nJ - 1 - step
                            lo = 128 * max(0, J - 4 * c)
                            diag = J >= 4 * c
                            Z = pb[zk % 2]; zk += 1
                            e_ = Et[k3 % 3]; sp_ = SPt[k3 % 4]; k3 += 1
                            cx.op("pe", lambda: nc.tensor.matmul(Z[:, lo:512], kT[b][:, J * 128:(J + 1) * 128], qT[b][:, q0 + lo:q0 + 512], start=True, stop=True),
                                  [kT[b], qT[b]], [Z])
                            cx.op("act", lambda: S.activation(e_[:, lo:512], Z[:, lo:512], AF.Exp), [Z], [e_])
                            cx.op("act", lambda: S.activation(sp_[:, lo:512], e_[:, lo:512], AF.Ln, bias=1.0, scale=1.0), [e_], [sp_])
                            if diag:
                                cx.op("dve", lambda: V.tensor_tensor(sp_[:, lo:lo + 128], sp_[:, lo:lo + 128].bitcast(F32), masks[:, M_SUT, :], ALU.mult), [sp_, masks], [sp_])
                            st_[step] = (J, lo, diag, sp_)
                        if 1 <= step <= nJ:
                            J, lo, diag, sp_ = st_[step - 1]
                            top = (step - 1 == 0)
                            Ab = pb[2 + ak % 2]; ak += 1
                            w_ = Wt[wk % 3]; wk += 1
                            cx.op("pe", lambda: nc.tensor.matmul(Ab[:, lo:512], kT[b][:, J * 128:(J + 1) * 128], qT[b][:, q0 + lo:q0 + 512], start=True, stop=False),
                                  [kT[b], qT[b]], [Ab])
                            cx.op("pe", lambda: nc.tensor.matmul(Ab[:, lo:512], masksr[:, M_NTRII, :], sp_[:, lo:512], start=False, stop=(top and not diag)),
                                  [masksr, sp_], [Ab])
                            if not top:
                                cx.op("pe", lambda: nc.tensor.matmul(Ab[:, lo:512], masksr[:, M_NONES, :], Rc[:, lo:512], start=False, stop=not diag),
                                      [masksr, Rc], [Ab])
                            if diag:
                                cx.op("pe", lambda: nc.tensor.matmul(Ab[:, lo:lo + 128], identb[:], masksb[:, M_NEGNS, :], start=False, stop=True),
                                      [identb, masksb], [Ab])
                            cx.op("act", lambda: S.activation(w_[:, lo:512], Ab[:, lo:512], AF.Exp), [Ab], [w_])
                            if J > 0:
                                cx.op("dve", lambda: V.tensor_tensor(Rc[:, lo:512], Rc[:, lo:512].bitcast(F32), sp_[:, lo:512].bitcast(F32), ALU.add), [Rc, sp_], [Rc])
                            st_[step - 1] = (J, lo, diag, sp_, w_)
                        if step >= 2:
                            J, lo, diag, sp_, w_ = st_[step - 2]
                            cx.op("pe", lambda: nc.tensor.matmul(O[0:64, lo:512], Vb[b][:, J, :], w_[:, lo:512], start=(step - 2 == 0), stop=(J == 0)),
                                  [Vb[b], w_], [O])
                    cx.op("act", lambda: S.copy(o_[:], O[0:64, :]), [O], [o_])
                    cx.dma("sp", oT[512 + h * 64:512 + (h + 1) * 64, t0 + q0:t0 + q0 + 512], o_[:], reads=[o_], writes=[oT], sb=o_)
        cx.end_phase()
        ph.close()

    def phase_lru(l):
        ph = ExitStack()
        def colvec(name, src_row):
            t = cx.sbuf(ph, name, [128, 8])
            with nc.allow_non_contiguous_dma(reason="tiny"):
                cx.dma("sp", t[:], src_row.rearrange("(m p) -> p m", p=128), reads=[], writes=[t], sb=t)
            return t
        cb = colvec("cb", conv_b[l]); ba = colvec("ba", lru_ba[l]); bx = colvec("bx", lru_bx[l]); lam = colvec("lam", lru_lambda[l])
        cw = cx.sbuf(ph, "cw", [128, 4, 8])
        with nc.allow_non_contiguous_dma(reason="tiny"):
            cx.dma("sp", cw[:], conv_w[l].rearrange("i (m p) -> p i m", p=128), reads=[], writes=[cw], sb=cw)
        el = cx.sbuf(ph, "el", [128, 8]); cA = cx.sbuf(ph, "cA", [128, 8]); cA2 = cx.sbuf(ph, "cA2", [128, 8])
        cx.op("act", lambda: S.activation(el[:], lam[:], AF.Exp, scale=-1.0), [lam], [el])
        cx.op("act", lambda: S.activation(cA[:], el[:], AF.Ln, bias=1.0, scale=1.0), [el], [cA])
        cx.op("dve", lambda: V.tensor_scalar_mul(cA2[:], cA[:], -16.0), [cA], [cA2])
        cx.op("dve", lambda: V.tensor_scalar_mul(cA[:], cA[:], -8.0), [cA], [cA])
        BDf = cx.sbuf(ph, "BDf", [128, 2, 128]); BD = [cx.sbuf(ph, "BD%d" % i, [128, 2, 128], F32R) for i in range(2)]
        cx.op("dve", lambda: V.memset(BDf[:], 0.0), [], [BDf])
        N = SEQ
        xc = [cx.sbuf(ph, "xc%d" % i, [128, 3 + N]) for i in range(2)]
        gc = [cx.sbuf(ph, "gc%d" % i, [128, N]) for i in range(2)]
        xv = cx.sbuf(ph, "xv", [128, N], F32R); t_a = cx.sbuf(ph, "t_a", [128, N]); t_b = cx.sbuf(ph, "t_b", [128, N])
        t_c = cx.sbuf(ph, "t_c", [128, N]); t_d = cx.sbuf(ph, "t_d", [128, N]); hh = cx.sbuf(ph, "hh", [128, N])
        oc = [cx.sbuf(ph, "oc%d" % i, [128, N], BF16) for i in range(2)]
        it = 0; pk = 0
        for m in range(8):
            bd = BD[m % 2]
            for g_, wsrc in enumerate((lru_wa, lru_wx)):
                cx.dma("sp", BDf[0:64, g_, 0:64], wsrc[l, 2 * m], reads=[], writes=[BDf], sb=BDf)
                cx.dma("sp", BDf[64:128, g_, 64:128], wsrc[l, 2 * m + 1], reads=[], writes=[BDf], sb=BDf)
            cx.op("dve", lambda: V.tensor_copy(bd[:], BDf[:]), [BDf], [bd])
            for s in range(NSEQ):
                x_ = xc[it % 2]; g = gc[it % 2]; o_ = oc[it % 2]; it += 1
                t0 = s * SEQ
                cx.op("pool", lambda: P.memset(x_[:, 0:3], 0.0), [], [x_])
                cx.dma("sp", x_[:, 3:3 + N], xgT[0, m * 128:(m + 1) * 128, t0:t0 + N], reads=[xgT], writes=[x_], sb=x_)
                cx.dma("sp", g[:], xgT[1, m * 128:(m + 1) * 128, t0:t0 + N], reads=[xgT], writes=[g], sb=g)
                cx.op("dve", lambda: V.tensor_scalar(t_a[:], x_[:, 0:N], cw[:, 0, m:m + 1], cb[:, m:m + 1], ALU.mult, ALU.add), [x_, cw, cb], [t_a])
                for i in (1, 2):
                    cx.op("dve", lambda: V.scalar_tensor_tensor(t_a[:], x_[:, i:i + N], cw[:, i, m:m + 1], t_a[:], ALU.mult, ALU.add), [x_, cw, t_a], [t_a])
                cx.op("dve", lambda: V.scalar_tensor_tensor(t_a[:], x_[:, 3:3 + N], cw[:, 3, m:m + 1], t_a[:], ALU.mult, ALU.add), [x_, cw, t_a], [t_a])
                cx.op("act", lambda: S.copy(xv[:], t_a[:]), [t_a], [xv])
                for q in range(N // 512):
                    pr = pb[pk % 6]; pk += 1; pi = pb[pk % 6]; pk += 1
                    cs = slice(q * 512, (q + 1) * 512)
                    cx.op("pe", lambda: nc.tensor.matmul(pr[:], bd[:, 0, :], xv[:, cs], start=True, stop=True), [bd, xv], [pr])
                    cx.op("pe", lambda: nc.tensor.matmul(pi[:], bd[:, 1, :], xv[:, cs], start=True, stop=True), [bd, xv], [pi])
                    cx.op("act", lambda: S.activation(t_b[:, cs], pr[:], AF.Sigmoid, bias=ba[:, m:m + 1], scale=1.0), [pr, ba], [t_b])
                    cx.op("act", lambda: S.activation(t_c[:, cs], pi[:], AF.Sigmoid, bias=bx[:, m:m + 1], scale=1.0), [pi, bx], [t_c])
                cx.op("pool", lambda: P.tensor_tensor(t_d[:], g[:], g[:], ALU.mult), [g], [t_d])
                cx.op("pool", lambda: P.tensor_scalar(t_d[:], t_d[:], 0.044715, 1.0, ALU.mult, ALU.add), [t_d], [t_d])
                cx.op("pool", lambda: P.tensor_tensor(t_d[:], t_d[:], g[:], ALU.mult), [t_d, g], [t_d])
                cx.op("act", lambda: S.activation(t_d[:], t_d[:], AF.Sigmoid, scale=1.5957691216057308), [t_d], [t_d])
                cx.op("pool", lambda: P.tensor_tensor(g[:], t_d[:], g[:], ALU.mult), [t_d, g], [g])
                cx.op("dve", lambda: V.tensor_tensor(t_c[:], t_c[:], t_a[:], ALU.mult), [t_c, t_a], [t_c])
                cx.op("act", lambda: S.activation(t_a[:], t_b[:], AF.Exp, scale=cA[:, m:m + 1]), [t_b, cA], [t_a])
                cx.op("act", lambda: S.activation(t_b[:], t_b[:], AF.Exp, scale=cA2[:, m:m + 1]), [t_b, cA2], [t_b])
                cx.op("act", lambda: S.activation(t_b[:], t_b[:], AF.Ln, bias=1.0, scale=-1.0), [t_b], [t_b])
                cx.op("act", lambda: S.activation(t_b[:], t_b[:], AF.Exp, scale=0.5), [t_b], [t_b])
                cx.op("dve", lambda: V.tensor_tensor(t_c[:], t_c[:], t_b[:], ALU.mult), [t_c, t_b], [t_c])
                cx.op("dve", lambda: V.tensor_tensor_scan(hh[:], t_a[:], t_c[:], 0.0, ALU.mult, ALU.add), [t_a, t_c], [hh])
                cx.op("dve", lambda: V.tensor_tensor(o_[:], hh[:], g[:], ALU.mult), [hh, g], [o_])
                cx.dma("sp", oT[1024 + m * 128:1024 + (m + 1) * 128, t0:t0 + N], o_[:], reads=[o_], writes=[oT], sb=o_)
        cx.end_phase()
        ph.close()

    def ln_tile(ph_bufs, y_src_bufs, y_ap_halves, x_t, gtile, lg, lb, dst_ap, dst_buf, k):
        tt_, st6, mv, rs, res_ = ph_bufs
        t = tt_[k % 2]; r = res_[k % 2]; s6 = st6[k % 2]; mv_ = mv[k % 2]; rs_ = rs[k % 2]
        for hf_ in range(2):
            cs = slice(hf_ * 512, (hf_ + 1) * 512)
            cx.op("dve", lambda: V.tensor_tensor(t[:, cs], y_ap_halves[hf_], gtile[:, cs], ALU.mult), list(y_src_bufs) + [gtile], [t])
        cx.op("dve", lambda: V.scalar_tensor_tensor(t[:], x_t[:], ALPHA, t[:], ALU.mult, ALU.add), [x_t, t], [t])
        for hf_ in range(2):
            cx.op("dve", lambda: V.bn_stats(s6[:, hf_, :], t[:, hf_ * 512:(hf_ + 1) * 512]), [t], [s6])
        cx.op("dve", lambda: V.bn_aggr(mv_[:], s6[:].rearrange("p a b -> p (a b)")), [s6], [mv_])
        cx.op("dve", lambda: V.tensor_scalar_add(rs_[:], mv_[:, 1:2], LN_EPS), [mv_], [rs_])
        cx.op("act", lambda: S.activation(rs_[:], rs_[:], AF.Ln), [rs_], [rs_])
        cx.op("act", lambda: S.activation(rs_[:], rs_[:], AF.Exp, scale=-0.5), [rs_], [rs_])
        cx.op("dve", lambda: V.tensor_scalar(t[:], t[:], mv_[:, 0:1], rs_[:, 0:1], ALU.subtract, ALU.mult), [t, mv_, rs_], [t])
        cx.op("pool", lambda: P.tensor_tensor(t[:], t[:], lg[:], ALU.mult), [t, lg], [t])
        cx.op("pool", lambda: P.tensor_tensor(r[:], t[:], lb[:], ALU.add), [t, lb], [r])
        cx.dma("sp", dst_ap, r[:], reads=[r], writes=[dst_buf], sb=r)

    def ln_bufs(ph):
        return ([cx.sbuf(ph, "lnt%d" % i, [128, D]) for i in range(2)],
                [cx.sbuf(ph, "lns%d" % i, [128, 2, 6]) for i in range(2)],
                [cx.sbuf(ph, "lnm%d" % i, [128, 2]) for i in range(2)],
                [cx.sbuf(ph, "lnr%d" % i, [128, 1]) for i in range(2)],
                [cx.sbuf(ph, "lno%d" % i, [128, D]) for i in range(2)])

    def phase_merge(l, xsrc, xdst):
        ph = ExitStack()
        mod = load_mod(ph, l, 0, ("gt",))
        lg = cx.sbuf(ph, "lg", [128, D]); lb = cx.sbuf(ph, "lb", [128, D])
        cx.dma("sp", lg[:], row_bc(ln1_g[l, :], 128), reads=[], writes=[lg], sb=lg)
        cx.dma("sp", lb[:], row_bc(ln1_b[l, :], 128), reads=[], writes=[lb], sb=lb)
        wpa = cx.sbuf(ph, "wpa", [128, 4, D], BF16); wpb = cx.sbuf(ph, "wpb", [128, 4, D], BF16)
        wpc = cx.sbuf(ph, "wpc", [128, 8, D], BF16); wo = cx.sbuf(ph, "wo", [128, 8, D], BF16)
        for t_, src in ((wpa, w_pa), (wpb, w_pb), (wpc, w_pc), (wo, w_o)):
            cx.dma("pool", None, None, reads=[], writes=[t_], sb=t_,
                   fn=lambda: P.dma_start(out=t_[:], in_=src[l].rearrange("(kc p) n -> p kc n", p=128)))
        oTc = [cx.sbuf(ph, "oTc%d" % i, [128, 16, 512], BF16) for i in range(1)]
        gg = [cx.sbuf(ph, "gg%d" % i, [128, 3, 512]) for i in range(2)]
        ta = [cx.sbuf(ph, "ta%d" % i, [128, 512]) for i in range(2)]
        tb = [cx.sbuf(ph, "tb%d" % i, [128, 512]) for i in range(2)]
        mT = [cx.sbuf(ph, "mT%d" % i, [128, 8, 512], BF16) for i in range(1)]
        xt = [cx.sbuf(ph, "xt%d" % i, [128, D]) for i in range(2)]
        lnb = ln_bufs(ph)
        gk = 0; k = 0
        for ch in range(NCH):
            s = ch // (SEQ // 512)
            tok0 = ch * 512
            oc_ = oTc[0]; mT_ = mT[0]
            cx.dma("sp", oc_[:], oT[:, tok0:tok0 + 512].rearrange("(kc p) t -> p kc t", p=128), reads=[oT], writes=[oc_], sb=oc_)
            for m in range(8):
                g_ = gg[gk % 2]; a_ = ta[gk % 2]; b_ = tb[gk % 2]; gk += 1
                cx.dma("sp", g_[:], gT[:, tok0:tok0 + 512].rearrange("(j q p) t -> q p j t", j=3, p=128)[m], reads=[gT], writes=[g_], sb=g_)
                pa, pb_, pc = pb[0 + 3 * (m % 2)], pb[1 + 3 * (m % 2)], pb[2 + 3 * (m % 2)]
                for kc in range(4):
                    cx.op("pe", lambda: nc.tensor.matmul(pa[:], wpa[:, kc, m * 128:(m + 1) * 128], oc_[:, kc, :], start=(kc == 0), stop=(kc == 3)), [wpa, oc_], [pa])
                for kc in range(4):
                    cx.op("pe", lambda: nc.tensor.matmul(pb_[:], wpb[:, kc, m * 128:(m + 1) * 128], oc_[:, 4 + kc, :], start=(kc == 0), stop=(kc == 3)), [wpb, oc_], [pb_])
                for kc in range(8):
                    cx.op("pe", lambda: nc.tensor.matmul(pc[:], wpc[:, kc, m * 128:(m + 1) * 128], oc_[:, 8 + kc, :], start=(kc == 0), stop=(kc == 7)), [wpc, oc_], [pc])
                cx.op("dve", lambda: V.tensor_tensor(a_[:], pa[:], g_[:, 0, :], ALU.mult), [pa, g_], [a_])
                cx.op("dve", lambda: V.tensor_tensor(b_[:], pb_[:], g_[:, 1, :], ALU.mult), [pb_, g_], [b_])
                cx.op("pool", lambda: P.tensor_tensor(a_[:], a_[:], b_[:], ALU.add), [a_, b_], [a_])
                cx.op("dve", lambda: V.tensor_tensor(b_[:], pc[:], g_[:, 2, :], ALU.mult), [pc, g_], [b_])
                cx.op("pool", lambda: P.tensor_tensor(mT_[:, m, :], a_[:], b_[:], ALU.add), [a_, b_], [mT_])
            for tt in range(4):
                ti = ch * 4 + tt
                x_t = xt[k % 2]
                cx.dma("sp", x_t[:], xsrc[ti * 128:(ti + 1) * 128, :], reads=[xsrc], writes=[x_t], sb=x_t)
                ys = []
                for hf_ in range(2):
                    yb = pb[(k * 2 + hf_) % 7]
                    for m in range(8):
                        cx.op("pe", lambda: nc.tensor.matmul(yb[:], mT_[:, m, tt * 128:(tt + 1) * 128], wo[:, m, hf_ * 512:(hf_ + 1) * 512], start=(m == 0), stop=(m == 7)),
                              [mT_, wo], [yb])
                    ys.append(yb)
                ln_tile(lnb, ys, [ys[0][:], ys[1][:]], x_t, mod[("gt", s)], lg, lb, xdst[ti * 128:(ti + 1) * 128, :], xdst, k)
                k += 1
        cx.end_phase()
        ph.close()

    def phase_route(l, xsrc):
        ph = ExitStack()
        mod = load_mod(ph, l, 1, ("sh", "sc"))
        wr = cx.sbuf(ph, "wr", [128, 8, NE])
        with nc.allow_non_contiguous_dma(reason="router weights 128B runs"):
            cx.dma("sp", wr[:], w_router[l].rearrange("(kc p) e -> p kc e", p=128), reads=[], writes=[wr], sb=wr)
        brt = cx.sbuf(ph, "brt", [128, NE])
        cx.dma("sp", brt[:], row_bc(b_router[l, :], 128), reads=[], writes=[brt], sb=brt)
        cnt = cx.sbuf(ph, "cnt", [128, NE]); cx.op("dve", lambda: V.memset(cnt[:], 0.0), [], [cnt])
        xt = [cx.sbuf(ph, "xt%d" % i, [128, D]) for i in range(2)]
        hf = [cx.sbuf(ph, "hf%d" % i, [128, D]) for i in range(2)]
        hb = [cx.sbuf(ph, "hb%d" % i, [128, D], BF16) for i in range(2)]
        hT = [cx.sbuf(ph, "hT%d" % i, [128, 8, 128]) for i in range(2)]
        def sm(name, w=NE):
            return [cx.sbuf(ph, "%s%d" % (name, i), [128, w]) for i in range(2)]
        lgt = sm("lgt"); m8 = sm("m8", 8); msk = sm("msk"); ex = sm("ex"); em = sm("em"); ssum = sm("ssum", 1)
        gte = sm("gte"); slv = sm("slv"); s8 = sm("s8", 8); nmx = sm("nmx", 1); tmp = sm("tmp")
        for ti in range(NT):
            s = ti // (SEQ // 128)
            b = ti % 2
            x_t = xt[b]; h_f = hf[b]; h_b = hb[b]; hT_ = hT[b]
            cx.dma("sp", x_t[:], xsrc[ti * 128:(ti + 1) * 128, :], reads=[xsrc], writes=[x_t], sb=x_t)
            cx.op("dve", lambda: V.tensor_tensor(h_f[:], x_t[:], mod[("sc", s)][:], ALU.mult), [x_t, mod[("sc", s)]], [h_f])
            cx.op("pool", lambda: P.tensor_tensor(h_f[:], h_f[:], mod[("sh", s)][:], ALU.add), [h_f, mod[("sh", s)]], [h_f])
            cx.op("act", lambda: S.copy(h_b[:], h_f[:]), [h_f], [h_b])
            for half in range(2):
                pt = pb[half]
                for q in range(4):
                    kc = half * 4 + q
                    cx.op("pe", lambda: nc.tensor.transpose(pt[:, q * 128:(q + 1) * 128], h_f[:, kc * 128:(kc + 1) * 128], identf[:]), [h_f, identf], [pt])
                cx.op("dve" if half else "act",
                      (lambda: V.tensor_copy(hT_[:, half * 4:half * 4 + 4, :], pt[:].rearrange("p (q t) -> p q t", q=4))) if half else
                      (lambda: S.copy(hT_[:, half * 4:half * 4 + 4, :], pt[:].rearrange("p (q t) -> p q t", q=4))), [pt], [hT_])
            pl = pb[2 + b]
            for kc in range(8):
                cx.op("pe", lambda: nc.tensor.matmul(pl[:, 0:NE], hT_[:, kc, :], wr[:, kc, :], start=(kc == 0), stop=(kc == 7)), [hT_, wr], [pl])
            L_ = lgt[b]; M8 = m8[b]; MK = msk[b]
            cx.op("dve", lambda: V.tensor_tensor(L_[:], pl[:, 0:NE], brt[:], ALU.add), [pl, brt], [L_])
            cx.op("dve", lambda: V.max(M8[:], L_[:]), [L_], [M8])
            cx.op("dve", lambda: V.tensor_scalar(MK[:], L_[:], M8[:, 3:4], None, ALU.is_ge), [L_, M8], [MK])
            cx.op("dve", lambda: V.tensor_scalar_mul(nmx[b][:], M8[:, 0:1], -1.0), [M8], [nmx[b]])
            cx.op("act", lambda: S.activation(ex[b][:], L_[:], AF.Exp, bias=nmx[b][:, 0:1], scale=1.0), [L_, nmx[b]], [ex[b]])
            cx.op("dve", lambda: V.tensor_tensor(em[b][:], ex[b][:], MK[:], ALU.mult), [ex[b], MK], [em[b]])
            cx.op("dve", lambda: V.reduce_sum(ssum[b][:], em[b][:], mybir.AxisListType.X), [em[b]], [ssum[b]])
            cx.op("dve", lambda: V.reciprocal(ssum[b][:], ssum[b][:]), [ssum[b]], [ssum[b]])
            cx.op("dve", lambda: V.tensor_scalar_mul(gte[b][:], em[b][:], ssum[b][:, 0:1]), [em[b], ssum[b]], [gte[b]])
            pp = pb[4 + b]
            cx.op("pe", lambda: nc.tensor.matmul(pp[:, 0:NE], masks[:, M_SUT, :], MK[:], start=True, stop=True), [masks, MK], [pp])
            cx.op("pe", lambda: nc.tensor.matmul(pp[:, 64:64 + NE], masks[:, M_ONES, :], MK[:], start=True, stop=True), [masks, MK], [pp])
            SL = slv[b]
            cx.op("dve", lambda: V.tensor_tensor(SL[:], pp[:, 0:NE], cnt[:], ALU.add), [pp, cnt], [SL])
            cx.op("dve", lambda: V.tensor_tensor(cnt[:], pp[:, 64:64 + NE], cnt[:], ALU.add), [pp, cnt], [cnt])
            cx.op("dve", lambda: V.tensor_tensor(SL[:], SL[:], ebase[:], ALU.add), [SL, ebase], [SL])
            cx.op("dve", lambda: V.tensor_tensor(SL[:], SL[:], MK[:], ALU.mult), [SL, MK], [SL])
            cx.op("dve", lambda: V.tensor_scalar_add(SL[:], SL[:], -1.0), [SL], [SL])
            cx.op("dve", lambda: V.max(s8[b][:], SL[:]), [SL], [s8[b]])
            cx.op("dve", lambda: V.tensor_copy(IDX[:, ti, :], s8[b][:, 0:4]), [s8[b]], [IDX])
            for k in range(4):
                cx.op("dve", lambda: V.tensor_scalar(tmp[b][:], SL[:], s8[b][:, k:k + 1], None, ALU.is_equal), [SL, s8[b]], [tmp[b]])
                cx.op("dve", lambda: V.tensor_tensor(tmp[b][:], tmp[b][:], gte[b][:], ALU.mult), [tmp[b], gte[b]], [tmp[b]])
                cx.op("dve", lambda: V.reduce_sum(GK[:, ti, k:k + 1], tmp[b][:], mybir.AxisListType.X), [tmp[b]], [GK])
            for k in range(4):
                cx.dma("pool", None, None, reads=[h_b, IDX], writes=[xbuf], sb=h_b,
                       fn=lambda: P.indirect_dma_start(out=xbuf[:], out_offset=bass.IndirectOffsetOnAxis(ap=IDX[:, ti, k:k + 1], axis=0),
                                                       in_=h_b[:], in_offset=None))
        if "cnt" in dbg:
            cx.dma("sp", dbg["cnt"][:], cnt[:], reads=[cnt], writes=[dbg["cnt"]], sb=cnt)
        cx.end_phase()
        ph.close()

    def phase_experts(l):
        ph = ExitStack()
        wgu = [cx.sbuf(ph, "wgu%d" % i, [128, 8, 2 * D], BF16) for i in range(2)]
        wdn = [cx.sbuf(ph, "wdn%d" % i, [128, 8, D], BF16) for i in range(2)]
        bgu = [cx.sbuf(ph, "bgu%d" % i, [128, 16]) for i in range(2)]
        bdn = [cx.sbuf(ph, "bdn%d" % i, [1, D], BF16) for i in range(2)]
        xr = [cx.sbuf(ph, "xr%d" % i, [128, 4, D], BF16) for i in range(2)]
        xTs = [cx.sbuf(ph, "xT%d" % i, [128, 8, 512], BF16) for i in range(2)]
        aTs = [cx.sbuf(ph, "aT%d" % i, [128, 8, 512], BF16) for i in range(2)]
        g1 = [cx.sbuf(ph, "g1%d" % i, [128, 512]) for i in range(2)]
        sg = [cx.sbuf(ph, "sg%d" % i, [128, 512]) for i in range(2)]
        u1 = [cx.sbuf(ph, "u1%d" % i, [128, 512]) for i in range(2)]
        yt = [cx.sbuf(ph, "yt%d" % i, [128, D]) for i in range(2)]
        xk = 0; jk = 0; yk = 0
        for e in range(NE):
            b = e % 2
            cx.dma("pool", None, None, reads=[], writes=[wgu[b]], sb=wgu[b],
                   fn=lambda: P.dma_start(out=wgu[b][:], in_=w_gu[l, e].rearrange("(kc p) n -> p kc n", p=128)))
            cx.dma("pool", None, None, reads=[], writes=[wdn[b]], sb=wdn[b],
                   fn=lambda: P.dma_start(out=wdn[b][:], in_=w_down[l, e].rearrange("(kc p) n -> p kc n", p=128)))
            cx.dma("pool", None, None, reads=[], writes=[bdn[b]], sb=bdn[b],
                   fn=lambda: P.dma_start(out=bdn[b][:], in_=b_down[l, e:e + 1, :]))
            with nc.allow_non_contiguous_dma(reason="tiny"):
                cx.dma("sp", bgu[b][:], b_gu[l, e].rearrange("(m p) -> p m", p=128), reads=[], writes=[bgu[b]], sb=bgu[b])
            for sc_ in range(CAP // 512):
                r0 = e * CAP + sc_ * 512
                xr_ = xr[xk % 2]; xT = xTs[xk % 2]; aT = aTs[xk % 2]; xk += 1
                cx.dma("sp", xr_[:], xbuf[r0:r0 + 512, :].rearrange("(t p) d -> p t d", p=128), reads=[xbuf], writes=[xr_], sb=xr_)
                for t_ in range(4):
                    for kc in range(8):
                        cx.op("pe", lambda: nc.tensor.transpose(pbh[:, kc * 128:(kc + 1) * 128], xr_[:, t_, kc * 128:(kc + 1) * 128], identb[:]), [xr_, identb], [pbh])
                    if t_ % 2:
                        cx.op("act", lambda: S.copy(xT[:, :, t_ * 128:(t_ + 1) * 128], pbh[:].rearrange("p (kc t) -> p kc t", kc=8)), [pbh], [xT])
                    else:
                        cx.op("dve", lambda: V.tensor_copy(xT[:, :, t_ * 128:(t_ + 1) * 128], pbh[:].rearrange("p (kc t) -> p kc t", kc=8)), [pbh], [xT])
                for j in range(8):
                    pg = pb[(jk % 2) * 2]; pu = pb[(jk % 2) * 2 + 1]
                    g_ = g1[jk % 2]; s_ = sg[jk % 2]; u_ = u1[jk % 2]; jk += 1
                    for kc in range(8):
                        cx.op("pe", lambda: nc.tensor.matmul(pg[:], wgu[b][:, kc, j * 128:(j + 1) * 128], xT[:, kc, :], start=(kc == 0), stop=(kc == 7)), [wgu[b], xT], [pg])
                    for kc in range(8):
                        cx.op("pe", lambda: nc.tensor.matmul(pu[:], wgu[b][:, kc, D + j * 128:D + (j + 1) * 128], xT[:, kc, :], start=(kc == 0), stop=(kc == 7)), [wgu[b], xT], [pu])
                    cx.op("dve", lambda: V.tensor_scalar(g_[:], pg[:], bgu[b][:, j:j + 1], 7.0, ALU.add, ALU.min), [pg, bgu[b]], [g_])
                    cx.op("act", lambda: S.activation(u_[:], pu[:], AF.Identity, bias=bgu[b][:, 8 + j:9 + j], scale=1.0), [pu, bgu[b]], [u_])
                    cx.op("act", lambda: S.activation(s_[:], g_[:], AF.Sigmoid, scale=1.702), [g_], [s_])
                    cx.op("dve", lambda: V.tensor_scalar(u_[:], u_[:], 7.0, -7.0, ALU.min, ALU.max), [u_], [u_])
                    cx.op("dve", lambda: V.tensor_tensor(g_[:], g_[:], s_[:], ALU.mult), [g_, s_], [g_])
                    cx.op("dve", lambda: V.scalar_tensor_tensor(aT[:, j, :], u_[:], 1.0, g_[:], ALU.add, ALU.mult), [u_, g_], [aT])
                for t_ in range(4):
                    y_ = yt[yk % 2]; yk += 1
                    for hf_ in range(2):
                        py = pb[4 + (yk * 2 + hf_) % 3]
                        for j in range(8):
                            cx.op("pe", lambda: nc.tensor.matmul(py[:], aT[:, j, t_ * 128:(t_ + 1) * 128], wdn[b][:, j, hf_ * 512:(hf_ + 1) * 512], start=(j == 0), stop=False), [aT, wdn[b]], [py])
                        cx.op("pe", lambda: nc.tensor.matmul(py[:], onesb[0:1, :], bdn[b][0:1, hf_ * 512:(hf_ + 1) * 512], start=False, stop=True), [onesb, bdn[b]], [py])
                        if hf_:
                            cx.op("act", lambda: S.copy(y_[:, 512:1024], py[:]), [py], [y_])
                        else:
                            cx.op("dve", lambda: V.tensor_copy(y_[:, 0:512], py[:]), [py], [y_])
                    cx.dma("sp", ybuf[r0 + t_ * 128:r0 + (t_ + 1) * 128, :], y_[:], reads=[y_], writes=[ybuf], sb=y_)
        cx.end_phase()
        ph.close()

    def phase_combine(l, xsrc, xdst):
        ph = ExitStack()
        mod = load_mod(ph, l, 1, ("gt",))
        lg = cx.sbuf(ph, "lg", [128, D]); lb = cx.sbuf(ph, "lb", [128, D])
        cx.dma("sp", lg[:], row_bc(ln2_g[l, :], 128), reads=[], writes=[lg], sb=lg)
        cx.dma("sp", lb[:], row_bc(ln2_b[l, :], 128), reads=[], writes=[lb], sb=lb)
        xt = [cx.sbuf(ph, "xt%d" % i, [128, D]) for i in range(2)]
        yg = [cx.sbuf(ph, "yg%d" % i, [128, D]) for i in range(8)]
        acc = [cx.sbuf(ph, "acc%d" % i, [128, D]) for i in range(2)]
        lnb = ln_bufs(ph)
        for ti in range(NT):
            s = ti // (SEQ // 128)
            b = ti % 2
            x_t = xt[b]; a_ = acc[b]
            cx.dma("sp", x_t[:], xsrc[ti * 128:(ti + 1) * 128, :], reads=[xsrc], writes=[x_t], sb=x_t)
            ys = []
            for k in range(4):
                y_ = yg[b * 4 + k]
                cx.dma("pool", None, None, reads=[ybuf, IDX], writes=[y_], sb=y_,
                       fn=lambda: P.indirect_dma_start(out=y_[:], out_offset=None, in_=ybuf[:],
                                                       in_offset=bass.IndirectOffsetOnAxis(ap=IDX[:, ti, k:k + 1], axis=0)))
                ys.append(y_)
            cx.op("dve", lambda: V.tensor_scalar_mul(a_[:], ys[0][:], GK[:, ti, 0:1]), [ys[0], GK], [a_])
            for k in range(1, 4):
                cx.op("dve", lambda: V.scalar_tensor_tensor(a_[:], ys[k][:], GK[:, ti, k:k + 1], a_[:], ALU.mult, ALU.add), [ys[k], GK, a_], [a_])
            ln_tile(lnb, [a_], [a_[:, 0:512], a_[:, 512:1024]], x_t, mod[("gt", s)], lg, lb, xdst[ti * 128:(ti + 1) * 128, :], xdst, ti)
        cx.end_phase()
        ph.close()

    def done(tag):
        return stop_after == tag

    phase_ada()
    cur = x_in
    finished = False
    for l in range(n_layers):
        if done("ada"):
            break
        phase_proj(l, cur)
        if done("proj"): break
        phase_fprep(l)
        phase_fox()
        if done("fox"): break
        phase_sb()
        if done("sb"): break
        phase_lru(l)
        if done("lru"): break
        x1 = xres[0]
        phase_merge(l, cur, x1)
        if done("merge"): break
        phase_route(l, x1)
        if done("route"): break
        phase_experts(l)
        if done("experts"): break
        last = (l == n_layers - 1)
        x2 = out_d if last else xres[1]
        phase_combine(l, x1, x2)
        cur = x2
    for name, src in (("oT", oT), ("x1", xres[0]), ("gT", gT), ("faT", faT), ("qkT", qkT[0]), ("xbuf", xbuf), ("ybuf", ybuf)):
        if name in dbg:
            ph = ExitStack()
            d = dbg[name]
            rows, cols = d.t.shape
            cw_ = min(cols, 1024)
            tmpb = [cx.sbuf(ph, "dump%d" % i, [128, cw_], src.t.dtype if hasattr(src, "t") else src.dtype) for i in range(2)]
            tmpf = [cx.sbuf(ph, "dumpf%d" % i, [128, cw_]) for i in range(2)]
            srcb = src if hasattr(src, "t") else qkT
            i = 0
            for r0 in range(0, rows, 128):
                for c0 in range(0, cols, cw_):
                    i += 1
                    n = min(128, rows - r0)
                    tb_, tf_ = tmpb[i % 2], tmpf[i % 2]
                    cx.dma("sp", tb_[0:n, :], src[r0:r0 + n, c0:c0 + cw_], reads=[srcb], writes=[tb_], sb=tb_)
                    cx.op("dve", lambda: V.tensor_copy(tf_[0:n, :], tb_[0:n, :]), [tb_], [tf_])
                    cx.dma("sp", d[r0:r0 + n, c0:c0 + cw_], tf_[0:n, :], reads=[tf_], writes=[d], sb=tf_)
            cx.end_phase()
            ph.close()
    ph = ExitStack()
    for name in ("IDX", "GK"):
        if name in dbg:
            srcb = IDX if name == "IDX" else GK
            tmpf = cx.sbuf(ph, "dumpg" + name, [128, NT * 4])
            cx.op("dve", lambda: V.tensor_copy(tmpf[:], srcb[:].rearrange("p a b -> p (a b)")), [srcb], [tmpf])
            cx.dma("sp", dbg[name][:], tmpf[:], reads=[tmpf], writes=[dbg[name]], sb=tmpf)
    cx.end_phase()
    ph.close()
    st.close()
    return nc, cx


def make_consts():
    import ml_dtypes
    k = np.arange(128)[:, None]
    q = np.arange(128)[None, :]
    masks = np.zeros((128, 8, 128), np.float32)
    masks[:, 0, :] = (k < q)
    masks[:, 1, :] = (k > q)
    masks[:, 2, :] = 1.0
    masks[:, 3, :] = np.where(k > q, NEG, 0.0)
    masks[:, 4, :] = np.where(k >= q, NEG, 0.0)
    masks[:, 5, :] = np.where(k < q, -1.0, 0.0)
    masks[:, 6, :] = np.where(k >= q, -1.0, 0.0)
    masks[:, 7, :] = -1.0
    ebase = np.broadcast_to((np.arange(NE) * CAP + 1).astype(np.float32)[None, :], (128, NE)).copy()
    return {
        "k_identb": np.eye(128, dtype=np.float32).astype(ml_dtypes.bfloat16),
        "k_identf": np.eye(128, dtype=np.float32),
        "k_masks": masks,
        "k_ebase": ebase,
    }


WEIGHT_KEYS = ["w_ada", "b_ada", "ln1_g", "ln1_b", "w_in", "b_f", "conv_w", "conv_b", "lru_wa", "lru_ba", "lru_wx",
               "lru_bx", "lru_lambda", "w_gate", "b_gate", "w_pa", "w_pb", "w_pc", "w_o", "ln2_g", "ln2_b", "w_router",
               "b_router", "w_gu", "b_gu", "w_down", "b_down"]


def make_in_maps(inputs):
    consts = make_consts()
    x = np.ascontiguousarray(np.asarray(inputs["x"], dtype=np.float32))
    c = np.ascontiguousarray(np.asarray(inputs["c"], dtype=np.float32))
    shared = {k: np.ascontiguousarray(np.asarray(inputs[k], dtype=np.float32)) for k in WEIGHT_KEYS}
    shared.update(consts)
    in_maps = []
    for i in range(NCORES):
        m = dict(shared)
        m["x"] = x[NSEQ * i:NSEQ * (i + 1)].reshape(T, D)
        m["c"] = c[NSEQ * i:NSEQ * (i + 1)]
        in_maps.append(m)
    return in_maps


def kernel(**inputs):
    nc, cx = build_program()
    in_maps = make_in_maps(inputs)
    res = run_bass_kernel_spmd(nc, in_maps, core_ids=list(range(NCORES)))
    out = np.stack([np.asarray(r["out"]).reshape(NSEQ, SEQ, D) for r in res.results], axis=0)
    return out.reshape(NCORES * NSEQ, SEQ, D).astype(np.float32)
```

```python
from contextlib import ExitStack
import numpy as np
import concourse.bass as bass
import concourse.mybir as mybir
from concourse.bass_utils import run_bass_kernel_spmd

F32 = mybir.dt.float32
F32R = mybir.dt.float32r
BF16 = mybir.dt.bfloat16
I32 = mybir.dt.int32
U32 = mybir.dt.uint32
AF = mybir.ActivationFunctionType
ALU = mybir.AluOpType

NCORES = 8
D = 1024
SEQ = 2048
NSEQ = 2
T = NSEQ * SEQ
NT = T // 128
NCH = T // 512
DEPTH = 2
H = 8
DH = 64
D_IN = 5128
NE = 32
CAP = 1024
ALPHA = (2.0 * DEPTH) ** 0.25
LN_EPS = 1e-5
NEG = -30000.0


class Slot:
    def __init__(self, sem):
        self.sem = sem
        self.count = 0


class Buf:
    def __init__(self, name, t=None):
        self.name = name
        self.t = t
        self.w = {}
        self.r = {}
        self.ds = None

    def __getitem__(self, k):
        return self.t[k]


class EngState:
    def __init__(self, name, eng, sem):
        self.name = name
        self.eng = eng
        self.sem = sem
        self.count = 0
        self.waited = {}


class Ctx:
    def __init__(self, nc, stack, n_dma_sems=72):
        self.nc = nc
        self.stack = stack
        self.E = {}
        for name, eng in (("pe", nc.tensor), ("act", nc.scalar), ("dve", nc.vector),
                          ("pool", nc.gpsimd), ("sp", nc.sync)):
            sem = stack.enter_context(nc.semaphore("s_" + name))
            self.E[name] = EngState(name, eng, sem)
        self.free_slots = [Slot(stack.enter_context(nc.semaphore("d%d" % i))) for i in range(n_dma_sems)]
        self.used_slots = []
        self.n_ins = 0
        self.n_wait = 0
        self.uid = 0

    def sbuf(self, ph, name, shape, dtype=F32):
        self.uid += 1
        t = ph.enter_context(self.nc.sbuf_tensor("%s_%d" % (name, self.uid), list(shape), dtype))
        return Buf(name, t)

    def psum(self, name, shape, dtype=F32):
        t = self.stack.enter_context(self.nc.psum_tensor(name, list(shape), dtype))
        return Buf(name, t)

    def dram(self, name, shape, dtype=F32, kind="Internal"):
        t = self.nc.dram_tensor(name, list(shape), dtype, kind=kind)
        return Buf(name, t.ap())

    def _wait(self, E, deps):
        for sid, (sem, val) in deps.items():
            if E.waited.get(sid, 0) >= val:
                continue
            E.eng.wait_ge(sem, val)
            E.waited[sid] = val
            self.n_wait += 1

    @staticmethod
    def _merge(d, src, skip=None):
        for sid, (sem, val) in src.items():
            if skip is not None and sid == skip:
                continue
            if sid not in d or d[sid][1] < val:
                d[sid] = (sem, val)

    def op(self, en, fn, reads=(), writes=()):
        E = self.E[en]
        own = id(E.sem)
        deps = {}
        for b in reads:
            self._merge(deps, b.w, skip=own if en == "pe" else None)
        for b in writes:
            self._merge(deps, b.w, skip=own)
            self._merge(deps, b.r, skip=own)
        self._wait(E, deps)
        ins = fn()
        E.count += 1
        ins.then_inc(E.sem, 1)
        tok = (E.sem, E.count)
        for b in reads:
            b.r[own] = tok
        for b in writes:
            b.w = {own: tok}
            b.r = {}
        self.n_ins += 1
        return ins

    def dma(self, qn, out, in_, reads=(), writes=(), sb=None, fn=None):
        E = self.E[qn]
        if sb.ds is None:
            sb.ds = self.free_slots.pop()
            self.used_slots.append(sb.ds)
        ds = sb.ds
        deps = {}
        if ds.count:
            deps[id(ds.sem)] = (ds.sem, ds.count)
        for b in reads:
            self._merge(deps, b.w)
        for b in writes:
            self._merge(deps, b.w)
            self._merge(deps, b.r)
        self._wait(E, deps)
        ins = E.eng.dma_start(out=out, in_=in_) if fn is None else fn()
        ds.count += 16
        ins.then_inc(ds.sem, 16)
        tok = (ds.sem, ds.count)
        sid = id(ds.sem)
        for b in reads:
            b.r[sid] = tok
        for b in writes:
            b.w = {sid: tok}
            b.r = {}
        self.n_ins += 1
        return ins

    def barrier(self, only=None):
        deps = {}
        for E in self.E.values():
            if E.count:
                deps[id(E.sem)] = (E.sem, E.count)
        for s in self.used_slots:
            if s.count:
                deps[id(s.sem)] = (s.sem, s.count)
        for name, E in self.E.items():
            if only is not None and name not in only:
                continue
            d = {k: v for k, v in deps.items() if k != id(E.sem)}
            self._wait(E, d)

    def end_phase(self):
        self.barrier()
        self.free_slots.extend(self.used_slots)
        self.used_slots = []


def build_program(n_layers=DEPTH, stop_after=None, debug=()):
    nc = bass.Bass("TRN2", target_bir_lowering=False)
    st = ExitStack()
    cx = Ctx(nc, st)
    V, S, P = nc.vector, nc.scalar, nc.gpsimd

    def din(name, shape, dtype=F32):
        return cx.dram(name, shape, dtype, kind="ExternalInput")

    x_in = din("x", [T, D]); c_in = din("c", [NSEQ, D])
    w_ada = din("w_ada", [DEPTH, D, 6 * D]); b_ada = din("b_ada", [DEPTH, 6 * D])
    ln1_g = din("ln1_g", [DEPTH, D]); ln1_b = din("ln1_b", [DEPTH, D])
    w_in = din("w_in", [DEPTH, D, D_IN]); b_f = din("b_f", [DEPTH, H])
    conv_w = din("conv_w", [DEPTH, 4, D]); conv_b = din("conv_b", [DEPTH, D])
    lru_wa = din("lru_wa", [DEPTH, 16, 64, 64]); lru_ba = din("lru_ba", [DEPTH, D])
    lru_wx = din("lru_wx", [DEPTH, 16, 64, 64]); lru_bx = din("lru_bx", [DEPTH, D])
    lru_lambda = din("lru_lambda", [DEPTH, D])
    w_gate = din("w_gate", [DEPTH, D, 3 * D]); b_gate = din("b_gate", [DEPTH, 3 * D])
    w_pa = din("w_pa", [DEPTH, 512, D]); w_pb = din("w_pb", [DEPTH, 512, D])
    w_pc = din("w_pc", [DEPTH, D, D]); w_o = din("w_o", [DEPTH, D, D])
    ln2_g = din("ln2_g", [DEPTH, D]); ln2_b = din("ln2_b", [DEPTH, D])
    w_router = din("w_router", [DEPTH, D, NE]); b_router = din("b_router", [DEPTH, NE])
    w_gu = din("w_gu", [DEPTH, NE, D, 2 * D]); b_gu = din("b_gu", [DEPTH, NE, 2 * D])
    w_down = din("w_down", [DEPTH, NE, D, D]); b_down = din("b_down", [DEPTH, NE, D])
    k_identb = din("k_identb", [128, 128], BF16)
    k_identf = din("k_identf", [128, 128])
    k_masks = din("k_masks", [128, 8, 128])
    k_ebase = din("k_ebase", [128, NE])
    out_d = cx.dram("out", [T, D], F32, kind="ExternalOutput")

    adaB = cx.dram("adaB", [DEPTH, NSEQ, 6, D])
    xres = [cx.dram("xresA", [T, D]), cx.dram("xresB", [T, D])]
    qkT = cx.dram("qkT", [4, 512, T], BF16)
    vv = cx.dram("vv", [2, T, 512], BF16)
    faT = cx.dram("faT", [H, T])
    Fd = cx.dram("Fd", [6, H, T], BF16)
    xgT = cx.dram("xgT", [2, D, T])
    gT = cx.dram("gT", [3 * D, T])
    oT = cx.dram("oT", [2 * D, T], BF16)
    xbuf = cx.dram("xbuf", [(NE + 1) * CAP, D], BF16)
    ybuf = cx.dram("ybuf", [(NE + 1) * CAP, D])
    dbg = {}
    for name, shape in debug:
        dbg[name] = cx.dram("dbg_" + name, shape, F32, kind="ExternalOutput")

    pb = [cx.psum("pb%d" % i, [128, 512]) for i in range(7)]
    pbh = cx.psum("pbh", [128, 1024], BF16)

    gl = st
    identb = cx.sbuf(gl, "identb", [128, 128], BF16)
    identf = cx.sbuf(gl, "identf", [128, 128])
    masks = cx.sbuf(gl, "masks", [128, 8, 128])
    masksb = cx.sbuf(gl, "masksb", [128, 8, 128], BF16)
    masksr = cx.sbuf(gl, "masksr", [128, 8, 128], F32R)
    ebase = cx.sbuf(gl, "ebase", [128, NE])
    IDX = cx.sbuf(gl, "IDX", [128, NT, 4], I32)
    GK = cx.sbuf(gl, "GK", [128, NT, 4])
    onesb = cx.sbuf(gl, "onesb", [128, 128], BF16)
    cx.dma("sp", identb[:], k_identb[:], reads=[k_identb], writes=[identb], sb=identb)
    cx.dma("sp", identf[:], k_identf[:], reads=[k_identf], writes=[identf], sb=identf)
    cx.dma("sp", masks[:], k_masks[:], reads=[k_masks], writes=[masks], sb=masks)
    cx.dma("sp", ebase[:], k_ebase[:], reads=[k_ebase], writes=[ebase], sb=ebase)
    cx.op("dve", lambda: V.tensor_copy(masksb[:], masks[:]), [masks], [masksb])
    cx.op("dve", lambda: V.tensor_copy(masksr[:], masks[:]), [masks], [masksr])
    cx.op("dve", lambda: V.memset(onesb[:], 1.0), [], [onesb])
    M_SUT, M_TRI, M_ONES, M_NEGC, M_NEGNS, M_NSTRICT, M_NTRII, M_NONES = range(8)

    def row_bc(ap_row, n):
        return ap_row.partition_broadcast(n)

    def phase_ada():
        ph = ExitStack()
        c_col = cx.sbuf(ph, "c_col", [128, NSEQ, 8])
        cond = cx.sbuf(ph, "cond", [128, NSEQ, 8])
        with nc.allow_non_contiguous_dma(reason="tiny transposed load of c"):
            cx.dma("sp", c_col[:], c_in.t.rearrange("s (kc p) -> p s kc", p=128), reads=[c_in], writes=[c_col], sb=c_col)
        cx.op("act", lambda: S.activation(cond[:], c_col[:], AF.Silu), [c_col], [cond])
        condB = cx.sbuf(ph, "condB", [128, NSEQ, 8, 128], BF16)
        for s in range(NSEQ):
            cx.op("dve", lambda: V.tensor_copy(condB[:, s], cond[:, s, :].unsqueeze(2).to_broadcast([128, 8, 128])),
                  [cond], [condB])
        wring = [cx.sbuf(ph, "wada%d" % i, [128, 8, 512], BF16) for i in range(3)]
        brow = [cx.sbuf(ph, "brow%d" % i, [128, 512]) for i in range(2)]
        res = [cx.sbuf(ph, "ares%d" % i, [128, 512]) for i in range(3)]
        k = 0
        for l in range(n_layers):
            for n in range(12):
                wt = wring[k % 3]; br = brow[k % 2]
                cx.dma("pool", None, None, reads=[w_ada], writes=[wt], sb=wt,
                       fn=lambda: P.dma_start(out=wt[:], in_=w_ada[l, :, n * 512:(n + 1) * 512].rearrange("(kc p) n -> p kc n", p=128)))
                cx.dma("sp", br[:], row_bc(b_ada[l, n * 512:(n + 1) * 512], 128), reads=[b_ada], writes=[br], sb=br)
                which = n // 2
                for s in range(NSEQ):
                    ps = pb[(k * NSEQ + s) % 4]
                    for kc in range(8):
                        cx.op("pe", lambda: nc.tensor.matmul(ps[:], condB[:, s, kc, :], wt[:, kc, :], start=(kc == 0), stop=(kc == 7)),
                              [condB, wt], [ps])
                    r = res[(k * NSEQ + s) % 3]
                    cx.op("dve", lambda: V.tensor_tensor(r[:], ps[:], br[:], ALU.add), [ps, br], [r])
                    if which not in (0, 3):
                        cx.op("dve", lambda: V.tensor_scalar_add(r[:], r[:], 1.0), [r], [r])
                    c0_ = (n % 2) * 512
                    cx.dma("sp", adaB[l, s, which:which + 1, c0_:c0_ + 512], r[0:1, :], reads=[r], writes=[adaB], sb=r)
                k += 1
        cx.end_phase()
        ph.close()

    def load_mod(ph, l, sub, names=("sh", "sc", "gt")):
        tiles = {}
        for s in range(NSEQ):
            for nm, idx in (("sh", 3 * sub), ("sc", 3 * sub + 1), ("gt", 3 * sub + 2)):
                if nm not in names:
                    continue
                t = cx.sbuf(ph, "mod_%s%d" % (nm, s), [128, D])
                cx.dma("sp", t[:], row_bc(adaB[l, s, idx, :], 128), reads=[adaB], writes=[t], sb=t)
                tiles[(nm, s)] = t
        return tiles

    def phase_proj(l, xsrc):
        ph = ExitStack()
        mod = load_mod(ph, l, 0, ("sh", "sc"))
        bgate = cx.sbuf(ph, "bgate", [128, 24])
        with nc.allow_non_contiguous_dma(reason="tiny bias relayout"):
            cx.dma("sp", bgate[:], b_gate[l].rearrange("(m p) -> p m", p=128), reads=[b_gate], writes=[bgate], sb=bgate)
        xt = [cx.sbuf(ph, "xt%d" % i, [128, D]) for i in range(3)]
        hf = [cx.sbuf(ph, "hf%d" % i, [128, D]) for i in range(2)]
        hb = [cx.sbuf(ph, "hb%d" % i, [128, D], BF16) for i in range(2)]
        hT = [cx.sbuf(ph, "hT%d" % i, [128, 8, 512], BF16) for i in range(2)]
        wring = [cx.sbuf(ph, "win%d" % i, [128, 8, 512], BF16) for i in range(3)]
        wfa = cx.sbuf(ph, "wfa", [128, 8, 8], BF16)
        evf = [cx.sbuf(ph, "evf%d" % i, [128, 512]) for i in range(4)]
        evb = [cx.sbuf(ph, "evb%d" % i, [128, 512], BF16) for i in range(4)]
        cx.dma("pool", None, None, reads=[w_in], writes=[wfa], sb=wfa,
               fn=lambda: P.dma_start(out=wfa[:], in_=w_in[l, :, 1536:1544].rearrange("(kc p) n -> p kc n", p=128)))
        wk = 0; ek = 0; pk = 0
        pieces = [("qa", 0), ("ka", 512), ("va", 1024), ("qb", 1544), ("kb", 2056), ("vb", 2568),
                  ("xc0", 3080), ("xc1", 3592), ("gc0", 4104), ("gc1", 4616)] + [("g%d" % i, i * 512) for i in range(6)]
        def prologue(ch):
            s = ch // (SEQ // 512)
            hTc = hT[ch % 2]
            for tt in range(4):
                ti = ch * 4 + tt
                x_t = xt[ti % 3]; h_f = hf[ti % 2]; h_b = hb[ti % 2]
                cx.dma("sp", x_t[:], xsrc[ti * 128:(ti + 1) * 128, :], reads=[xsrc], writes=[x_t], sb=x_t)
                cx.op("dve", lambda: V.tensor_tensor(h_f[:], x_t[:], mod[("sc", s)][:], ALU.mult), [x_t, mod[("sc", s)]], [h_f])
                cx.op("dve", lambda: V.tensor_tensor(h_b[:], h_f[:], mod[("sh", s)][:], ALU.add), [h_f, mod[("sh", s)]], [h_b])
                for kc in range(8):
                    cx.op("pe", lambda: nc.tensor.transpose(pbh[:, kc * 128:(kc + 1) * 128], h_b[:, kc * 128:(kc + 1) * 128], identb[:]),
                          [h_b, identb], [pbh])
                cx.op("act", lambda: S.copy(hTc[:, :, tt * 128:(tt + 1) * 128], pbh[:].rearrange("p (kc t) -> p kc t", kc=8)),
                      [pbh], [hTc])
        prologue(0)
        for ch in range(NCH):
            s = ch // (SEQ // 512)
            hTc = hT[ch % 2]
            tok0 = ch * 512
            ps = pb[pk % 6]; pk += 1
            for kc in range(8):
                cx.op("pe", lambda: nc.tensor.matmul(ps[0:8, :], wfa[:, kc, :], hTc[:, kc, :], start=(kc == 0), stop=(kc == 7)),
                      [wfa, hTc], [ps])
            ev = evf[ek % 4]; ek += 1
            cx.op("dve", lambda: V.tensor_copy(ev[0:8, :], ps[0:8, :]), [ps], [ev])
            cx.dma("sp", faT[:, tok0:tok0 + 512], ev[0:8, :], reads=[ev], writes=[faT], sb=ev)
            for pi_, (nm, c0) in enumerate(pieces):
                if pi_ == 6 and ch + 1 < NCH:
                    prologue(ch + 1)
                wt = wring[wk % 3]; wk += 1
                wsrc = (w_gate if nm[0] == "g" and nm[1].isdigit() else w_in)
                cx.dma("pool", None, None, reads=[wsrc], writes=[wt], sb=wt,
                       fn=lambda: P.dma_start(out=wt[:], in_=wsrc[l, :, c0:c0 + 512].rearrange("(kc p) n -> p kc n", p=128)))
                if nm in ("va", "vb"):
                    for tt in range(4):
                        ps = pb[pk % 6]; pk += 1
                        for kc in range(8):
                            cx.op("pe", lambda: nc.tensor.matmul(ps[:], hTc[:, kc, tt * 128:(tt + 1) * 128], wt[:, kc, :], start=(kc == 0), stop=(kc == 7)),
                                  [hTc, wt], [ps])
                        ev = evb[ek % 4]; ek += 1
                        eng = "act" if ek % 2 else "dve"
                        if eng == "act":
                            cx.op("act", lambda: S.copy(ev[:], ps[:]), [ps], [ev])
                        else:
                            cx.op("dve", lambda: V.tensor_copy(ev[:], ps[:]), [ps], [ev])
                        cx.dma("sp", vv[0 if nm == "va" else 1, tok0 + tt * 128:tok0 + (tt + 1) * 128, :], ev[:], reads=[ev], writes=[vv], sb=ev)
                    continue
                for m in range(4):
                    ps = pb[pk % 6]; pk += 1
                    for kc in range(8):
                        cx.op("pe", lambda: nc.tensor.matmul(ps[:], wt[:, kc, m * 128:(m + 1) * 128], hTc[:, kc, :], start=(kc == 0), stop=(kc == 7)),
                              [wt, hTc], [ps])
                    if nm in ("qa", "ka", "qb", "kb"):
                        ev = evb[ek % 4]; ek += 1
                        sc = 0.125 if nm[0] == "q" else 1.0
                        if ek % 2:
                            cx.op("act", lambda: S.mul(ev[:], ps[:], sc), [ps], [ev])
                        else:
                            cx.op("dve", lambda: V.tensor_scalar_mul(ev[:], ps[:], sc), [ps], [ev])
                        qi = ("qa", "ka", "qb", "kb").index(nm)
                        cx.dma("sp", qkT[qi, m * 128:(m + 1) * 128, tok0:tok0 + 512], ev[:], reads=[ev], writes=[qkT], sb=ev)
                    elif nm[0] in ("x",) or nm[:2] == "gc":
                        ev = evf[ek % 4]; ek += 1
                        if ek % 2:
                            cx.op("act", lambda: S.copy(ev[:], ps[:]), [ps], [ev])
                        else:
                            cx.op("dve", lambda: V.tensor_copy(ev[:], ps[:]), [ps], [ev])
                        r0 = int(nm[2]) * 512 + m * 128
                        cx.dma("sp", xgT[0 if nm[0] == "x" else 1, r0:r0 + 128, tok0:tok0 + 512], ev[:], reads=[ev], writes=[xgT], sb=ev)
                    else:
                        ev = evf[ek % 4]; ek += 1
                        mi = int(nm[1]) * 4 + m
                        cx.op("act", lambda: S.activation(ev[:], ps[:], AF.Sigmoid, bias=bgate[:, mi:mi + 1], scale=1.0), [ps, bgate], [ev])
                        cx.dma("sp", gT[mi * 128:(mi + 1) * 128, tok0:tok0 + 512], ev[:], reads=[ev], writes=[gT], sb=ev)
        cx.end_phase()
        ph.close()

    def phase_fprep(l):
        ph = ExitStack()
        bf = cx.sbuf(ph, "bf", [H, 1]); nbf = cx.sbuf(ph, "nbf", [H, 1])
        with nc.allow_non_contiguous_dma(reason="tiny"):
            cx.dma("sp", bf[:], b_f[l].rearrange("(h o) -> h o", o=1), reads=[b_f], writes=[bf], sb=bf)
        cx.op("dve", lambda: V.tensor_scalar_mul(nbf[:], bf[:], -1.0), [bf], [nbf])
        ones = cx.sbuf(ph, "ones", [H, SEQ]); cx.op("dve", lambda: V.memset(ones[:], 1.0), [], [ones])
        for s in range(NSEQ):
            fa = cx.sbuf(ph, "fa%d" % s, [H, SEQ]); e = cx.sbuf(ph, "fe%d" % s, [H, SEQ]); sp_ = cx.sbuf(ph, "fs%d" % s, [H, SEQ])
            F = cx.sbuf(ph, "F%d" % s, [H, SEQ]); r1 = cx.sbuf(ph, "r1%d" % s, [H, SEQ]); r2 = cx.sbuf(ph, "r2%d" % s, [H, SEQ])
            parts = cx.sbuf(ph, "parts%d" % s, [H, 6, SEQ], BF16)
            cx.dma("sp", fa[:], faT[:, s * SEQ:(s + 1) * SEQ], reads=[faT], writes=[fa], sb=fa)
            cx.op("act", lambda: S.activation(e[:], fa[:], AF.Exp, bias=nbf[:, 0:1], scale=-1.0), [fa, nbf], [e])
            cx.op("act", lambda: S.activation(sp_[:], e[:], AF.Ln, bias=1.0, scale=1.0), [e], [sp_])
            cx.op("dve", lambda: V.tensor_tensor_scan(F[:], ones[:], sp_[:], 0.0, ALU.mult, ALU.subtract), [ones, sp_], [F])
            cx.op("dve", lambda: V.tensor_copy(parts[:, 0, :], F[:]), [F], [parts])
            cx.op("dve", lambda: V.tensor_tensor(r1[:], F[:], parts[:, 0, :], ALU.subtract), [F, parts], [r1])
            cx.op("dve", lambda: V.tensor_copy(parts[:, 1, :], r1[:]), [r1], [parts])
            cx.op("dve", lambda: V.tensor_tensor(r2[:], r1[:], parts[:, 1, :], ALU.subtract), [r1, parts], [r2])
            cx.op("dve", lambda: V.tensor_copy(parts[:, 2, :], r2[:]), [r2], [parts])
            cx.op("dve", lambda: V.tensor_scalar_mul(parts[:, 3:6, :], parts[:, 0:3, :], -1.0), [parts], [parts])
            cx.dma("sp", Fd[:, :, s * SEQ:(s + 1) * SEQ].rearrange("v h t -> h v t"), parts[:], reads=[parts], writes=[Fd], sb=parts)
        cx.end_phase()
        ph.close()

    def gen_fox(ph):
        NB = 2
        kT = [cx.sbuf(ph, "fkT%d" % i, [70, SEQ], BF16) for i in range(NB)]
        qT = [cx.sbuf(ph, "fqT%d" % i, [70, SEQ], BF16) for i in range(NB)]
        Va = [cx.sbuf(ph, "Va%d" % i, [128, 16, 128], BF16) for i in range(NB)]
        Pt = [cx.sbuf(ph, "Pt%d" % i, [128, 512], BF16) for i in range(3)]
        rec = [cx.sbuf(ph, "rec%d" % i, [128, 512]) for i in range(2)]
        ob = [cx.sbuf(ph, "fob%d" % i, [64, 512], BF16) for i in range(2)]
        for i in range(NB):
            cx.op("dve", lambda: V.memset(kT[i][64:70, :], 1.0), [], [kT[i]])
            cx.op("dve", lambda: V.memset(qT[i][64:70, :], 1.0), [], [qT[i]])
            cx.op("dve", lambda: V.memset(Va[i][:], 1.0), [], [Va[i]])
        it = 0; pk = 0; ck = 0
        for s in range(NSEQ):
            for h in range(H):
                b = it % NB; it += 1
                t0 = s * SEQ
                cx.dma("sp", qT[b][0:64, :], qkT[0, h * 64:(h + 1) * 64, t0:t0 + SEQ], reads=[qkT], writes=[qT[b]], sb=qT[b])
                cx.dma("sp", qT[b][64:67, :], Fd[0:3, h, t0:t0 + SEQ], reads=[Fd], writes=[qT[b]], sb=qT[b])
                cx.dma("sp", kT[b][0:64, :], qkT[1, h * 64:(h + 1) * 64, t0:t0 + SEQ], reads=[qkT], writes=[kT[b]], sb=kT[b])
                cx.dma("sp", kT[b][67:70, :], Fd[3:6, h, t0:t0 + SEQ], reads=[Fd], writes=[kT[b]], sb=kT[b])
                with nc.allow_non_contiguous_dma(reason="v head slice, 128B runs"):
                    cx.dma("sp", Va[b][:, :, 0:64], vv[0, t0:t0 + SEQ, h * 64:(h + 1) * 64].rearrange("(j p) d -> p j d", p=128),
                           reads=[vv], writes=[Va[b]], sb=Va[b])
                for c in range(4):
                    nJ = 4 * c + 4
                    O = pb[4 + ck % 2]; ck += 1
                    q0 = c * 512
                    sq = []
                    for i in range(nJ + 2):
                        if i < nJ:
                            J = i
                            lo = 128 * max(0, J - 4 * c)
                            Sb = pb[pk % 4]; Pb = Pt[pk % 3]; pk += 1
                            diag = J >= 4 * c
                            cx.op("pe", lambda: nc.tensor.matmul(Sb[:, lo:512], kT[b][:, J * 128:(J + 1) * 128], qT[b][:, q0 + lo:q0 + 512], start=True, stop=not diag),
                                  [kT[b], qT[b]], [Sb])
                            if diag:
                                cx.op("pe", lambda: nc.tensor.matmul(Sb[:, lo:lo + 128], identb[:], masksb[:, M_NEGC, :], start=False, stop=True),
                                      [identb, masksb], [Sb])
                            cx.op("act", lambda: S.activation(Pb[:, lo:512], Sb[:, lo:512], AF.Exp), [Sb], [Pb])
                            sq.append((J, lo, Pb))
                        if i >= 2:
                            J, lo, Pb = sq[i - 2]
                            cx.op("pe", lambda: nc.tensor.matmul(O[:, lo:512], Va[b][:, J, :], Pb[:, lo:512], start=(J == 0), stop=(J == nJ - 1)),
                                  [Va[b], Pb], [O])
                    rc = rec[ck % 2]; o_ = ob[ck % 2]
                    cx.op("dve", lambda: V.reciprocal(rc[64:128, :], O[64:128, :]), [O], [rc])
                    cx.op("dve", lambda: V.tensor_tensor(o_[:], O[0:64, :], rc[64:128, :], ALU.mult), [O, rc], [o_])
                    cx.dma("sp", oT[h * 64:(h + 1) * 64, t0 + q0:t0 + q0 + 512], o_[:], reads=[o_], writes=[oT], sb=o_)
                yield

    def gen_sb(ph):
        NB = 2
        kT = [cx.sbuf(ph, "kT%d" % i, [64, SEQ], BF16) for i in range(NB)]
        qT = [cx.sbuf(ph, "qT%d" % i, [64, SEQ], BF16) for i in range(NB)]
        Vb = [cx.sbuf(ph, "Vb%d" % i, [128, 16, 64], BF16) for i in range(NB)]
        Et = [cx.sbuf(ph, "Et%d" % i, [128, 512]) for i in range(3)]
        SPt = [cx.sbuf(ph, "SPt%d" % i, [128, 512], F32R) for i in range(4)]
        R = [cx.sbuf(ph, "R%d" % i, [128, 512], F32R) for i in range(2)]
        Wt = [cx.sbuf(ph, "Wt%d" % i, [128, 512], BF16) for i in range(3)]
        ob = [cx.sbuf(ph, "ob%d" % i, [64, 512], BF16) for i in range(2)]
        Zf = cx.sbuf(ph, "Zf", [128, 512])
        cx.op("dve", lambda: V.memset(Zf[:], 0.0), [], [Zf])
        it = 0; zk = 0; ak = 0; ck = 0; k3 = 0; wk = 0
        for s in range(NSEQ):
            for h in range(H):
                b = it % NB; it += 1
                t0 = s * SEQ
                cx.dma("sp", qT[b][:], qkT[2, h * 64:(h + 1) * 64, t0:t0 + SEQ], reads=[qkT], writes=[qT[b]], sb=qT[b])
                cx.dma("sp", kT[b][:], qkT[3, h * 64:(h + 1) * 64, t0:t0 + SEQ], reads=[qkT], writes=[kT[b]], sb=kT[b])
                with nc.allow_non_contiguous_dma(reason="v head slice, 128B runs"):
                    cx.dma("sp", Vb[b][:], vv[1, t0:t0 + SEQ, h * 64:(h + 1) * 64].rearrange("(j p) d -> p j d", p=128),
                           reads=[vv], writes=[Vb[b]], sb=Vb[b])
                for c in range(4):
                    nJ = 4 * c + 4
                    O = pb[4 + ck % 2]; Rc = R[ck % 2]; o_ = ob[ck % 2]; ck += 1
                    q0 = c * 512
                    cx.op("dve", lambda: V.tensor_copy(Rc[:], Zf[:]), [Zf], [Rc])
                    st_ = {}
                    for step in range(nJ + 2):
                        if step < nJ:
                            J = nJ - 1 - step
                            lo = 128 * max(0, J - 4 * c)
                            diag = J >= 4 * c
                            Z = pb[zk % 2]; zk += 1
                            e_ = Et[k3 % 3]; sp_ = SPt[k3 % 4]; k3 += 1
                            cx.op("pe", lambda: nc.tensor.matmul(Z[:, lo:512], kT[b][:, J * 128:(J + 1) * 128], qT[b][:, q0 + lo:q0 + 512], start=True, stop=True),
                                  [kT[b], qT[b]], [Z])
                            cx.op("act", lambda: S.activation(e_[:, lo:512], Z[:, lo:512], AF.Exp), [Z], [e_])
                            cx.op("act", lambda: S.activation(sp_[:, lo:512], e_[:, lo:512], AF.Ln, bias=1.0, scale=1.0), [e_], [sp_])
                            if diag:
                                cx.op("dve", lambda: V.tensor_tensor(sp_[:, lo:lo + 128], sp_[:, lo:lo + 128].bitcast(F32), masks[:, M_SUT, :], ALU.mult), [sp_, masks], [sp_])
                            st_[step] = (J, lo, diag, sp_)
                        if 1 <= step <= nJ:
                            J, lo, diag, sp_ = st_[step - 1]
                            top = (step - 1 == 0)
                            Ab = pb[2 + ak % 2]; ak += 1
                            w_ = Wt[wk % 3]; wk += 1
                            cx.op("pe", lambda: nc.tensor.matmul(Ab[:, lo:512], kT[b][:, J * 128:(J + 1) * 128], qT[b][:, q0 + lo:q0 + 512], start=True, stop=False),
                                  [kT[b], qT[b]], [Ab])
                            cx.op("pe", lambda: nc.tensor.matmul(Ab[:, lo:512], masksr[:, M_NTRII, :], sp_[:, lo:512], start=False, stop=(top and not diag)),
                                  [masksr, sp_], [Ab])
                            if not top:
                                cx.op("pe", lambda: nc.tensor.matmul(Ab[:, lo:512], masksr[:, M_NONES, :], Rc[:, lo:512], start=False, stop=not diag),
                                      [masksr, Rc], [Ab])
                            if diag:
                                cx.op("pe", lambda: nc.tensor.matmul(Ab[:, lo:lo + 128], identb[:], masksb[:, M_NEGNS, :], start=False, stop=True),
                                      [identb, masksb], [Ab])
                            cx.op("act", lambda: S.activation(w_[:, lo:512], Ab[:, lo:512], AF.Exp), [Ab], [w_])
                            if J > 0:
                                cx.op("dve", lambda: V.tensor_tensor(Rc[:, lo:512], Rc[:, lo:512].bitcast(F32), sp_[:, lo:512].bitcast(F32), ALU.add), [Rc, sp_], [Rc])
                            st_[step - 1] = (J, lo, diag, sp_, w_)
                        if step >= 2:
                            J, lo, diag, sp_, w_ = st_[step - 2]
                            cx.op("pe", lambda: nc.tensor.matmul(O[0:64, lo:512], Vb[b][:, J, :], w_[:, lo:512], start=(step - 2 == 0), stop=(J == 0)),
                                  [Vb[b], w_], [O])
                    cx.op("act", lambda: S.copy(o_[:], O[0:64, :]), [O], [o_])
                    cx.dma("sp", oT[512 + h * 64:512 + (h + 1) * 64, t0 + q0:t0 + q0 + 512], o_[:], reads=[o_], writes=[oT], sb=o_)
                yield

    def gen_lru(ph, l):
        def colvec(name, src_row):
            t = cx.sbuf(ph, name, [128, 8])
            with nc.allow_non_contiguous_dma(reason="tiny"):
                cx.dma("sp", t[:], src_row.rearrange("(m p) -> p m", p=128), reads=[], writes=[t], sb=t)
            return t
        cb = colvec("cb", conv_b[l]); ba = colvec("ba", lru_ba[l]); bx = colvec("bx", lru_bx[l]); lam = colvec("lam", lru_lambda[l])
        cw = cx.sbuf(ph, "cw", [128, 4, 8])
        with nc.allow_non_contiguous_dma(reason="tiny"):
            cx.dma("sp", cw[:], conv_w[l].rearrange("i (m p) -> p i m", p=128), reads=[], writes=[cw], sb=cw)
        el = cx.sbuf(ph, "el", [128, 8]); cA = cx.sbuf(ph, "cA", [128, 8]); cA2 = cx.sbuf(ph, "cA2", [128, 8])
        cx.op("act", lambda: S.activation(el[:], lam[:], AF.Exp, scale=-1.0), [lam], [el])
        cx.op("act", lambda: S.activation(cA[:], el[:], AF.Ln, bias=1.0, scale=1.0), [el], [cA])
        cx.op("dve", lambda: V.tensor_scalar_mul(cA2[:], cA[:], -16.0), [cA], [cA2])
        cx.op("dve", lambda: V.tensor_scalar_mul(cA[:], cA[:], -8.0), [cA], [cA])
        BDf = cx.sbuf(ph, "BDf", [128, 2, 128]); BD = [cx.sbuf(ph, "BD%d" % i, [128, 2, 128], F32R) for i in range(2)]
        cx.op("dve", lambda: V.memset(BDf[:], 0.0), [], [BDf])
        N = SEQ
        xc = [cx.sbuf(ph, "xc%d" % i, [128, 3 + N]) for i in range(2)]
        gc = [cx.sbuf(ph, "gc%d" % i, [128, N]) for i in range(2)]
        xv = cx.sbuf(ph, "xv", [128, N], F32R); t_a = cx.sbuf(ph, "t_a", [128, N]); t_b = cx.sbuf(ph, "t_b", [128, N])
        t_c = cx.sbuf(ph, "t_c", [128, N]); t_d = cx.sbuf(ph, "t_d", [128, N]); hh = cx.sbuf(ph, "hh", [128, N])
        oc = [cx.sbuf(ph, "oc%d" % i, [128, N], BF16) for i in range(2)]
        it = 0; pk = 0
        for m in range(8):
            bd = BD[m % 2]
            for g_, wsrc in enumerate((lru_wa, lru_wx)):
                cx.dma("sp", BDf[0:64, g_, 0:64], wsrc[l, 2 * m], reads=[], writes=[BDf], sb=BDf)
                cx.dma("sp", BDf[64:128, g_, 64:128], wsrc[l, 2 * m + 1], reads=[], writes=[BDf], sb=BDf)
            cx.op("dve", lambda: V.tensor_copy(bd[:], BDf[:]), [BDf], [bd])
            for s in range(NSEQ):
                x_ = xc[it % 2]; g = gc[it % 2]; o_ = oc[it % 2]; it += 1
                t0 = s * SEQ
                cx.op("pool", lambda: P.memset(x_[:, 0:3], 0.0), [], [x_])
                cx.dma("sp", x_[:, 3:3 + N], xgT[0, m * 128:(m + 1) * 128, t0:t0 + N], reads=[xgT], writes=[x_], sb=x_)
                cx.dma("sp", g[:], xgT[1, m * 128:(m + 1) * 128, t0:t0 + N], reads=[xgT], writes=[g], sb=g)
                cx.op("dve", lambda: V.tensor_scalar(t_a[:], x_[:, 0:N], cw[:, 0, m:m + 1], cb[:, m:m + 1], ALU.mult, ALU.add), [x_, cw, cb], [t_a])
                for i in (1, 2):
                    cx.op("dve", lambda: V.scalar_tensor_tensor(t_a[:], x_[:, i:i + N], cw[:, i, m:m + 1], t_a[:], ALU.mult, ALU.add), [x_, cw, t_a], [t_a])
                cx.op("dve", lambda: V.scalar_tensor_tensor(t_a[:], x_[:, 3:3 + N], cw[:, 3, m:m + 1], t_a[:], ALU.mult, ALU.add), [x_, cw, t_a], [t_a])
                cx.op("act", lambda: S.copy(xv[:], t_a[:]), [t_a], [xv])
                for q in range(N // 512):
                    pr = pb[6]; pi = pb[6]
                    cs = slice(q * 512, (q + 1) * 512)
                    cx.op("pe", lambda: nc.tensor.matmul(pr[:], bd[:, 0, :], xv[:, cs], start=True, stop=True), [bd, xv], [pr])
                    cx.op("act", lambda: S.activation(t_b[:, cs], pr[:], AF.Sigmoid, bias=ba[:, m:m + 1], scale=1.0), [pr, ba], [t_b])
                    cx.op("pe", lambda: nc.tensor.matmul(pi[:], bd[:, 1, :], xv[:, cs], start=True, stop=True), [bd, xv], [pi])
                    cx.op("act", lambda: S.activation(t_c[:, cs], pi[:], AF.Sigmoid, bias=bx[:, m:m + 1], scale=1.0), [pi, bx], [t_c])
                cx.op("pool", lambda: P.tensor_tensor(t_d[:], g[:], g[:], ALU.mult), [g], [t_d])
                cx.op("pool", lambda: P.tensor_scalar(t_d[:], t_d[:], 0.044715, 1.0, ALU.mult, ALU.add), [t_d], [t_d])
                cx.op("pool", lambda: P.tensor_tensor(t_d[:], t_d[:], g[:], ALU.mult), [t_d, g], [t_d])
                cx.op("act", lambda: S.activation(t_d[:], t_d[:], AF.Sigmoid, scale=1.5957691216057308), [t_d], [t_d])
                cx.op("pool", lambda: P.tensor_tensor(g[:], t_d[:], g[:], ALU.mult), [t_d, g], [g])
                cx.op("dve", lambda: V.tensor_tensor(t_c[:], t_c[:], t_a[:], ALU.mult), [t_c, t_a], [t_c])
                cx.op("act", lambda: S.activation(t_a[:], t_b[:], AF.Exp, scale=cA[:, m:m + 1]), [t_b, cA], [t_a])
                cx.op("act", lambda: S.activation(t_b[:], t_b[:], AF.Exp, scale=cA2[:, m:m + 1]), [t_b, cA2], [t_b])
                cx.op("act", lambda: S.activation(t_b[:], t_b[:], AF.Ln, bias=1.0, scale=-1.0), [t_b], [t_b])
                cx.op("act", lambda: S.activation(t_b[:], t_b[:], AF.Exp, scale=0.5), [t_b], [t_b])
                cx.op("dve", lambda: V.tensor_tensor(t_c[:], t_c[:], t_b[:], ALU.mult), [t_c, t_b], [t_c])
                cx.op("dve", lambda: V.tensor_tensor_scan(hh[:], t_a[:], t_c[:], 0.0, ALU.mult, ALU.add), [t_a, t_c], [hh])
                cx.op("dve", lambda: V.tensor_tensor(o_[:], hh[:], g[:], ALU.mult), [hh, g], [o_])
                cx.dma("sp", oT[1024 + m * 128:1024 + (m + 1) * 128, t0:t0 + N], o_[:], reads=[o_], writes=[oT], sb=o_)
                yield

    def phase_mix(l):
        ph = ExitStack()
        def attn():
            yield from gen_fox(ph)
            yield from gen_sb(ph)
        ga = attn(); gr = gen_lru(ph, l)
        done_a = done_r = False
        while not (done_a and done_r):
            for _ in range(2):
                if not done_a:
                    try:
                        next(ga)
                    except StopIteration:
                        done_a = True
            if not done_r:
                try:
                    next(gr)
                except StopIteration:
                    done_r = True
        cx.end_phase()
        ph.close()

    def ln_tile(ph_bufs, y_src_bufs, y_ap_halves, x_t, gtile, lg, lb, dst_ap, dst_buf, k):
        tt_, st6, mv, rs, res_ = ph_bufs
        t = tt_[k % 2]; r = res_[k % 2]; s6 = st6[k % 2]; mv_ = mv[k % 2]; rs_ = rs[k % 2]
        for hf_ in range(2):
            cs = slice(hf_ * 512, (hf_ + 1) * 512)
            cx.op("dve", lambda: V.tensor_tensor(t[:, cs], y_ap_halves[hf_], gtile[:, cs], ALU.mult), list(y_src_bufs) + [gtile], [t])
        cx.op("dve", lambda: V.scalar_tensor_tensor(t[:], x_t[:], ALPHA, t[:], ALU.mult, ALU.add), [x_t, t], [t])
        for hf_ in range(2):
            cx.op("dve", lambda: V.bn_stats(s6[:, hf_, :], t[:, hf_ * 512:(hf_ + 1) * 512]), [t], [s6])
        cx.op("dve", lambda: V.bn_aggr(mv_[:], s6[:].rearrange("p a b -> p (a b)")), [s6], [mv_])
        cx.op("dve", lambda: V.tensor_scalar_add(rs_[:], mv_[:, 1:2], LN_EPS), [mv_], [rs_])
        cx.op("act", lambda: S.activation(rs_[:], rs_[:], AF.Ln), [rs_], [rs_])
        cx.op("act", lambda: S.activation(rs_[:], rs_[:], AF.Exp, scale=-0.5), [rs_], [rs_])
        cx.op("dve", lambda: V.tensor_scalar(t[:], t[:], mv_[:, 0:1], rs_[:, 0:1], ALU.subtract, ALU.mult), [t, mv_, rs_], [t])
        cx.op("pool", lambda: P.tensor_tensor(t[:], t[:], lg[:], ALU.mult), [t, lg], [t])
        cx.op("dve", lambda: V.tensor_tensor(r[:], t[:], lb[:], ALU.add), [t, lb], [r])
        cx.dma("sp", dst_ap, r[:], reads=[r], writes=[dst_buf], sb=r)
        return r

    def ln_bufs(ph):
        return ([cx.sbuf(ph, "lnt%d" % i, [128, D]) for i in range(2)],
                [cx.sbuf(ph, "lns%d" % i, [128, 2, 6]) for i in range(2)],
                [cx.sbuf(ph, "lnm%d" % i, [128, 2]) for i in range(2)],
                [cx.sbuf(ph, "lnr%d" % i, [128, 1]) for i in range(2)],
                [cx.sbuf(ph, "lno%d" % i, [128, D]) for i in range(2)])

    def phase_merge(l, xsrc, xdst):
        ph = ExitStack()
        mod = load_mod(ph, l, 0, ("gt",))
        lg = cx.sbuf(ph, "lg", [128, D]); lb = cx.sbuf(ph, "lb", [128, D])
        cx.dma("sp", lg[:], row_bc(ln1_g[l, :], 128), reads=[], writes=[lg], sb=lg)
        cx.dma("sp", lb[:], row_bc(ln1_b[l, :], 128), reads=[], writes=[lb], sb=lb)
        wpa = cx.sbuf(ph, "wpa", [128, 4, D], BF16); wpb = cx.sbuf(ph, "wpb", [128, 4, D], BF16)
        wpc = cx.sbuf(ph, "wpc", [128, 8, D], BF16); wo = cx.sbuf(ph, "wo", [128, 8, D], BF16)
        for t_, src in ((wpa, w_pa), (wpb, w_pb), (wpc, w_pc), (wo, w_o)):
            cx.dma("pool", None, None, reads=[], writes=[t_], sb=t_,
                   fn=lambda: P.dma_start(out=t_[:], in_=src[l].rearrange("(kc p) n -> p kc n", p=128)))
        oTc = [cx.sbuf(ph, "oTc%d" % i, [128, 16, 512], BF16) for i in range(1)]
        gg = [cx.sbuf(ph, "gg%d" % i, [128, 3, 512]) for i in range(2)]
        ta = [cx.sbuf(ph, "ta%d" % i, [128, 512]) for i in range(2)]
        tb = [cx.sbuf(ph, "tb%d" % i, [128, 512]) for i in range(2)]
        mT = [cx.sbuf(ph, "mT%d" % i, [128, 8, 512], BF16) for i in range(1)]
        xt = [cx.sbuf(ph, "xt%d" % i, [128, D]) for i in range(2)]
        lnb = ln_bufs(ph)
        rs = route_setup(ph, l)
        pend = None
        gk = 0; k = 0
        for ch in range(NCH):
            s = ch // (SEQ // 512)
            tok0 = ch * 512
            oc_ = oTc[0]; mT_ = mT[0]
            cx.dma("sp", oc_[:], oT[:, tok0:tok0 + 512].rearrange("(kc p) t -> p kc t", p=128), reads=[oT], writes=[oc_], sb=oc_)
            for m in range(8):
                g_ = gg[gk % 2]; a_ = ta[gk % 2]; b_ = tb[gk % 2]; gk += 1
                cx.dma("sp", g_[:], gT[:, tok0:tok0 + 512].rearrange("(j q p) t -> q p j t", j=3, p=128)[m], reads=[gT], writes=[g_], sb=g_)
                pa, pb_, pc = pb[0 + 3 * (m % 2)], pb[1 + 3 * (m % 2)], pb[2 + 3 * (m % 2)]
                for kc in range(4):
                    cx.op("pe", lambda: nc.tensor.matmul(pa[:], wpa[:, kc, m * 128:(m + 1) * 128], oc_[:, kc, :], start=(kc == 0), stop=(kc == 3)), [wpa, oc_], [pa])
                for kc in range(4):
                    cx.op("pe", lambda: nc.tensor.matmul(pb_[:], wpb[:, kc, m * 128:(m + 1) * 128], oc_[:, 4 + kc, :], start=(kc == 0), stop=(kc == 3)), [wpb, oc_], [pb_])
                for kc in range(8):
                    cx.op("pe", lambda: nc.tensor.matmul(pc[:], wpc[:, kc, m * 128:(m + 1) * 128], oc_[:, 8 + kc, :], start=(kc == 0), stop=(kc == 7)), [wpc, oc_], [pc])
                cx.op("dve", lambda: V.tensor_tensor(a_[:], pa[:], g_[:, 0, :], ALU.mult), [pa, g_], [a_])
                cx.op("dve", lambda: V.tensor_tensor(b_[:], pb_[:], g_[:, 1, :], ALU.mult), [pb_, g_], [b_])
                cx.op("dve", lambda: V.tensor_tensor(a_[:], a_[:], b_[:], ALU.add), [a_, b_], [a_])
                cx.op("dve", lambda: V.tensor_tensor(b_[:], pc[:], g_[:, 2, :], ALU.mult), [pc, g_], [b_])
                cx.op("dve", lambda: V.tensor_tensor(mT_[:, m, :], a_[:], b_[:], ALU.add), [a_, b_], [mT_])
            for tt in range(4):
                ti = ch * 4 + tt
                x_t = xt[k % 2]
                cx.dma("sp", x_t[:], xsrc[ti * 128:(ti + 1) * 128, :], reads=[xsrc], writes=[x_t], sb=x_t)
                ys = []
                for hf_ in range(2):
                    yb = pb[(k * 2 + hf_) % 6]
                    for m in range(8):
                        cx.op("pe", lambda: nc.tensor.matmul(yb[:], mT_[:, m, tt * 128:(tt + 1) * 128], wo[:, m, hf_ * 512:(hf_ + 1) * 512], start=(m == 0), stop=(m == 7)),
                              [mT_, wo], [yb])
                    ys.append(yb)
                r_ = ln_tile(lnb, ys, [ys[0][:], ys[1][:]], x_t, mod[("gt", s)], lg, lb, xdst[ti * 128:(ti + 1) * 128, :], xdst, k)
                if pend is not None:
                    route_tile(rs, *pend)
                pend = (ti, r_)
                k += 1
        route_tile(rs, *pend)
        if "cnt" in dbg:
            cx.dma("sp", dbg["cnt"][:], rs["cnt"][:], reads=[rs["cnt"]], writes=[dbg["cnt"]], sb=rs["cnt"])
        cx.end_phase()
        ph.close()

    def route_setup(ph, l):
        rs = {}
        rs["mod"] = load_mod(ph, l, 1, ("sh", "sc"))
        wr = cx.sbuf(ph, "wr", [128, 8, NE])
        with nc.allow_non_contiguous_dma(reason="router weights 128B runs"):
            cx.dma("sp", wr[:], w_router[l].rearrange("(kc p) e -> p kc e", p=128), reads=[], writes=[wr], sb=wr)
        brt = cx.sbuf(ph, "brt", [128, NE])
        cx.dma("sp", brt[:], row_bc(b_router[l, :], 128), reads=[], writes=[brt], sb=brt)
        cnt = cx.sbuf(ph, "cnt", [128, NE]); cx.op("dve", lambda: V.memset(cnt[:], 0.0), [], [cnt])
        rs.update(wr=wr, brt=brt, cnt=cnt)
        rs["hf"] = [cx.sbuf(ph, "rhf%d" % i, [128, D]) for i in range(2)]
        rs["hb"] = [cx.sbuf(ph, "rhb%d" % i, [128, D], BF16) for i in range(2)]
        rs["hT"] = [cx.sbuf(ph, "rhT%d" % i, [128, 8, 128]) for i in range(2)]
        def sm(name, w=NE):
            return [cx.sbuf(ph, "%s%d" % (name, i), [128, w]) for i in range(2)]
        for nm, w in (("lgt", NE), ("m8", 8), ("msk", NE), ("ex", NE), ("em", NE), ("ssum", 1), ("gte", NE), ("slv", NE),
                      ("s8", 8), ("nmx", 1), ("tmp", NE)):
            rs[nm] = sm(nm, w)
        return rs

    def route_tile(rs, ti, x_t):
        mod = rs["mod"]; wr = rs["wr"]; brt = rs["brt"]; cnt = rs["cnt"]
        s = ti // (SEQ // 128)
        b = ti % 2
        h_f = rs["hf"][b]; h_b = rs["hb"][b]; hT_ = rs["hT"][b]
        lgt, m8, msk, ex, em, ssum, gte, slv, s8, nmx, tmp = (rs[k] for k in ("lgt", "m8", "msk", "ex", "em", "ssum", "gte", "slv", "s8", "nmx", "tmp"))
        cx.op("dve", lambda: V.tensor_tensor(h_f[:], x_t[:], mod[("sc", s)][:], ALU.mult), [x_t, mod[("sc", s)]], [h_f])
        cx.op("dve", lambda: V.tensor_tensor(h_f[:], h_f[:], mod[("sh", s)][:], ALU.add), [h_f, mod[("sh", s)]], [h_f])
        cx.op("act", lambda: S.copy(h_b[:], h_f[:]), [h_f], [h_b])
        pt = pb[6]
        for half in range(2):
            for q in range(4):
                kc = half * 4 + q
                cx.op("pe", lambda: nc.tensor.transpose(pt[:, q * 128:(q + 1) * 128], h_f[:, kc * 128:(kc + 1) * 128], identf[:]), [h_f, identf], [pt])
            cx.op("act", lambda: S.copy(hT_[:, half * 4:half * 4 + 4, :], pt[:].rearrange("p (q t) -> p q t", q=4)), [pt], [hT_])
        pl = pb[6]
        for kc in range(8):
            cx.op("pe", lambda: nc.tensor.matmul(pl[:, 0:NE], hT_[:, kc, :], wr[:, kc, :], start=(kc == 0), stop=(kc == 7)), [hT_, wr], [pl])
        L_ = lgt[b]; M8 = m8[b]; MK = msk[b]
        cx.op("dve", lambda: V.tensor_tensor(L_[:], pl[:, 0:NE], brt[:], ALU.add), [pl, brt], [L_])
        cx.op("dve", lambda: V.max(M8[:], L_[:]), [L_], [M8])
        cx.op("dve", lambda: V.tensor_scalar(MK[:], L_[:], M8[:, 3:4], None, ALU.is_ge), [L_, M8], [MK])
        cx.op("dve", lambda: V.tensor_scalar_mul(nmx[b][:], M8[:, 0:1], -1.0), [M8], [nmx[b]])
        cx.op("act", lambda: S.activation(ex[b][:], L_[:], AF.Exp, bias=nmx[b][:, 0:1], scale=1.0), [L_, nmx[b]], [ex[b]])
        cx.op("dve", lambda: V.tensor_tensor(em[b][:], ex[b][:], MK[:], ALU.mult), [ex[b], MK], [em[b]])
        cx.op("dve", lambda: V.reduce_sum(ssum[b][:], em[b][:], mybir.AxisListType.X), [em[b]], [ssum[b]])
        cx.op("dve", lambda: V.reciprocal(ssum[b][:], ssum[b][:]), [ssum[b]], [ssum[b]])
        cx.op("dve", lambda: V.tensor_scalar_mul(gte[b][:], em[b][:], ssum[b][:, 0:1]), [em[b], ssum[b]], [gte[b]])
        pp = pb[6]
        cx.op("pe", lambda: nc.tensor.matmul(pp[:, 64:64 + NE], masks[:, M_SUT, :], MK[:], start=True, stop=True), [masks, MK], [pp])
        cx.op("pe", lambda: nc.tensor.matmul(pp[:, 128:128 + NE], masks[:, M_ONES, :], MK[:], start=True, stop=True), [masks, MK], [pp])
        SL = slv[b]
        cx.op("dve", lambda: V.tensor_tensor(SL[:], pp[:, 64:64 + NE], cnt[:], ALU.add), [pp, cnt], [SL])
        cx.op("dve", lambda: V.tensor_tensor(cnt[:], pp[:, 128:128 + NE], cnt[:], ALU.add), [pp, cnt], [cnt])
        cx.op("dve", lambda: V.tensor_tensor(SL[:], SL[:], ebase[:], ALU.add), [SL, ebase], [SL])
        cx.op("dve", lambda: V.tensor_tensor(SL[:], SL[:], MK[:], ALU.mult), [SL, MK], [SL])
        cx.op("dve", lambda: V.tensor_scalar_add(SL[:], SL[:], -1.0), [SL], [SL])
        cx.op("dve", lambda: V.max(s8[b][:], SL[:]), [SL], [s8[b]])
        cx.op("dve", lambda: V.tensor_copy(IDX[:, ti, :], s8[b][:, 0:4]), [s8[b]], [IDX])
        for k in range(4):
            cx.op("dve", lambda: V.tensor_scalar(tmp[b][:], SL[:], s8[b][:, k:k + 1], None, ALU.is_equal), [SL, s8[b]], [tmp[b]])
            cx.op("dve", lambda: V.tensor_tensor(tmp[b][:], tmp[b][:], gte[b][:], ALU.mult), [tmp[b], gte[b]], [tmp[b]])
            cx.op("dve", lambda: V.reduce_sum(GK[:, ti, k:k + 1], tmp[b][:], mybir.AxisListType.X), [tmp[b]], [GK])
        for k in range(4):
            cx.dma("pool", None, None, reads=[h_b, IDX], writes=[xbuf], sb=h_b,
                   fn=lambda: P.indirect_dma_start(out=xbuf[:], out_offset=bass.IndirectOffsetOnAxis(ap=IDX[:, ti, k:k + 1], axis=0),
                                                   in_=h_b[:], in_offset=None))

    def phase_experts(l):
        ph = ExitStack()
        wgu = [cx.sbuf(ph, "wgu%d" % i, [128, 8, 2 * D], BF16) for i in range(2)]
        wdn = [cx.sbuf(ph, "wdn%d" % i, [128, 8, D], BF16) for i in range(2)]
        bgu = [cx.sbuf(ph, "bgu%d" % i, [128, 16]) for i in range(2)]
        bdn = [cx.sbuf(ph, "bdn%d" % i, [1, D], BF16) for i in range(2)]
        xr = [cx.sbuf(ph, "xr%d" % i, [128, 4, D], BF16) for i in range(2)]
        xTs = [cx.sbuf(ph, "xT%d" % i, [128, 8, 512], BF16) for i in range(2)]
        aTs = [cx.sbuf(ph, "aT%d" % i, [128, 8, 512], BF16) for i in range(2)]
        g1 = [cx.sbuf(ph, "g1%d" % i, [128, 512]) for i in range(2)]
        sg = [cx.sbuf(ph, "sg%d" % i, [128, 512]) for i in range(2)]
        u1 = [cx.sbuf(ph, "u1%d" % i, [128, 512]) for i in range(2)]
        yt = [cx.sbuf(ph, "yt%d" % i, [128, D]) for i in range(2)]
        jk = 0; yk = 0
        chunks = [(e, sc_) for e in range(NE) for sc_ in range(CAP // 512)]

        def load_w(e):
            b = e % 2
            cx.dma("pool", None, None, reads=[], writes=[wgu[b]], sb=wgu[b],
                   fn=lambda: P.dma_start(out=wgu[b][:], in_=w_gu[l, e].rearrange("(kc p) n -> p kc n", p=128)))
            cx.dma("pool", None, None, reads=[], writes=[wdn[b]], sb=wdn[b],
                   fn=lambda: P.dma_start(out=wdn[b][:], in_=w_down[l, e].rearrange("(kc p) n -> p kc n", p=128)))
            cx.dma("pool", None, None, reads=[], writes=[bdn[b]], sb=bdn[b],
                   fn=lambda: P.dma_start(out=bdn[b][:], in_=b_down[l, e:e + 1, :]))
            with nc.allow_non_contiguous_dma(reason="tiny"):
                cx.dma("sp", bgu[b][:], b_gu[l, e].rearrange("(m p) -> p m", p=128), reads=[], writes=[bgu[b]], sb=bgu[b])

        def load_x(ci):
            e, sc_ = chunks[ci]
            r0 = e * CAP + sc_ * 512
            xr_ = xr[ci % 2]
            cx.dma("sp", xr_[:], xbuf[r0:r0 + 512, :].rearrange("(t p) d -> p t d", p=128), reads=[xbuf], writes=[xr_], sb=xr_)

        def transp(ci, t_):
            xr_ = xr[ci % 2]; xT = xTs[ci % 2]
            for kc in range(8):
                cx.op("pe", lambda: nc.tensor.transpose(pbh[:, kc * 128:(kc + 1) * 128], xr_[:, t_, kc * 128:(kc + 1) * 128], identb[:]), [xr_, identb], [pbh])
            if t_ % 2:
                cx.op("act", lambda: S.copy(xT[:, :, t_ * 128:(t_ + 1) * 128], pbh[:].rearrange("p (kc t) -> p kc t", kc=8)), [pbh], [xT])
            else:
                cx.op("dve", lambda: V.tensor_copy(xT[:, :, t_ * 128:(t_ + 1) * 128], pbh[:].rearrange("p (kc t) -> p kc t", kc=8)), [pbh], [xT])

        load_w(0)
        load_x(0)
        for t_ in range(4):
            transp(0, t_)
        for ci, (e, sc_) in enumerate(chunks):
            b = e % 2
            r0 = e * CAP + sc_ * 512
            xT = xTs[ci % 2]; aT = aTs[ci % 2]
            if sc_ == 0 and e + 1 < NE:
                load_w(e + 1)
            if ci + 1 < len(chunks):
                load_x(ci + 1)
            for j in range(8):
                pg = pb[(jk % 2) * 2]; pu = pb[(jk % 2) * 2 + 1]
                g_ = g1[jk % 2]; s_ = sg[jk % 2]; u_ = u1[jk % 2]; jk += 1
                for kc in range(8):
                    cx.op("pe", lambda: nc.tensor.matmul(pg[:], wgu[b][:, kc, j * 128:(j + 1) * 128], xT[:, kc, :], start=(kc == 0), stop=(kc == 7)), [wgu[b], xT], [pg])
                for kc in range(8):
                    cx.op("pe", lambda: nc.tensor.matmul(pu[:], wgu[b][:, kc, D + j * 128:D + (j + 1) * 128], xT[:, kc, :], start=(kc == 0), stop=(kc == 7)), [wgu[b], xT], [pu])
                cx.op("dve", lambda: V.tensor_scalar(g_[:], pg[:], bgu[b][:, j:j + 1], 7.0, ALU.add, ALU.min), [pg, bgu[b]], [g_])
                cx.op("act", lambda: S.activation(u_[:], pu[:], AF.Identity, bias=bgu[b][:, 8 + j:9 + j], scale=1.0), [pu, bgu[b]], [u_])
                cx.op("act", lambda: S.activation(s_[:], g_[:], AF.Sigmoid, scale=1.702), [g_], [s_])
                cx.op("dve", lambda: V.tensor_scalar(u_[:], u_[:], 7.0, -7.0, ALU.min, ALU.max), [u_], [u_])
                cx.op("dve", lambda: V.tensor_tensor(g_[:], g_[:], s_[:], ALU.mult), [g_, s_], [g_])
                cx.op("dve", lambda: V.scalar_tensor_tensor(aT[:, j, :], u_[:], 1.0, g_[:], ALU.add, ALU.mult), [u_, g_], [aT])
            for t_ in range(4):
                if ci + 1 < len(chunks):
                    transp(ci + 1, t_)
                y_ = yt[yk % 2]; yk += 1
                for hf_ in range(2):
                    py = pb[4 + (yk * 2 + hf_) % 3]
                    for j in range(8):
                        cx.op("pe", lambda: nc.tensor.matmul(py[:], aT[:, j, t_ * 128:(t_ + 1) * 128], wdn[b][:, j, hf_ * 512:(hf_ + 1) * 512], start=(j == 0), stop=False), [aT, wdn[b]], [py])
                    cx.op("pe", lambda: nc.tensor.matmul(py[:], onesb[0:1, :], bdn[b][0:1, hf_ * 512:(hf_ + 1) * 512], start=False, stop=True), [onesb, bdn[b]], [py])
                    if hf_:
                        cx.op("act", lambda: S.copy(y_[:, 512:1024], py[:]), [py], [y_])
                    else:
                        cx.op("dve", lambda: V.tensor_copy(y_[:, 0:512], py[:]), [py], [y_])
                cx.dma("sp", ybuf[r0 + t_ * 128:r0 + (t_ + 1) * 128, :], y_[:], reads=[y_], writes=[ybuf], sb=y_)
        cx.end_phase()
        ph.close()

    def phase_combine(l, xsrc, xdst):
        ph = ExitStack()
        mod = load_mod(ph, l, 1, ("gt",))
        lg = cx.sbuf(ph, "lg", [128, D]); lb = cx.sbuf(ph, "lb", [128, D])
        cx.dma("sp", lg[:], row_bc(ln2_g[l, :], 128), reads=[], writes=[lg], sb=lg)
        cx.dma("sp", lb[:], row_bc(ln2_b[l, :], 128), reads=[], writes=[lb], sb=lb)
        xt = [cx.sbuf(ph, "xt%d" % i, [128, D]) for i in range(2)]
        yg = [cx.sbuf(ph, "yg%d" % i, [128, D]) for i in range(8)]
        acc = [cx.sbuf(ph, "acc%d" % i, [128, D]) for i in range(2)]
        lnb = ln_bufs(ph)
        for ti in range(NT):
            s = ti // (SEQ // 128)
            b = ti % 2
            x_t = xt[b]; a_ = acc[b]
            cx.dma("sp", x_t[:], xsrc[ti * 128:(ti + 1) * 128, :], reads=[xsrc], writes=[x_t], sb=x_t)
            ys = []
            for k in range(4):
                y_ = yg[b * 4 + k]
                cx.dma("pool", None, None, reads=[ybuf, IDX], writes=[y_], sb=y_,
                       fn=lambda: P.indirect_dma_start(out=y_[:], out_offset=None, in_=ybuf[:],
                                                       in_offset=bass.IndirectOffsetOnAxis(ap=IDX[:, ti, k:k + 1], axis=0)))
                ys.append(y_)
            cx.op("dve", lambda: V.tensor_scalar_mul(a_[:], ys[0][:], GK[:, ti, 0:1]), [ys[0], GK], [a_])
            for k in range(1, 4):
                cx.op("dve", lambda: V.scalar_tensor_tensor(a_[:], ys[k][:], GK[:, ti, k:k + 1], a_[:], ALU.mult, ALU.add), [ys[k], GK, a_], [a_])
            ln_tile(lnb, [a_], [a_[:, 0:512], a_[:, 512:1024]], x_t, mod[("gt", s)], lg, lb, xdst[ti * 128:(ti + 1) * 128, :], xdst, ti)
        cx.end_phase()
        ph.close()

    def done(tag):
        return stop_after == tag

    phase_ada()
    cur = x_in
    finished = False
    for l in range(n_layers):
        if done("ada"):
            break
        phase_proj(l, cur)
        if done("proj"): break
        phase_fprep(l)
        phase_mix(l)
        if done("mix"): break
        x1 = xres[0]
        phase_merge(l, cur, x1)
        if done("merge"): break
        phase_experts(l)
        if done("experts"): break
        last = (l == n_layers - 1)
        x2 = out_d if last else xres[1]
        phase_combine(l, x1, x2)
        cur = x2
    for name, src in (("oT", oT), ("x1", xres[0]), ("gT", gT), ("faT", faT), ("qkT", qkT[0]), ("xbuf", xbuf), ("ybuf", ybuf)):
        if name in dbg:
            ph = ExitStack()
            d = dbg[name]
            rows, cols = d.t.shape
            cw_ = min(cols, 1024)
            tmpb = [cx.sbuf(ph, "dump%d" % i, [128, cw_], src.t.dtype if hasattr(src, "t") else src.dtype) for i in range(2)]
            tmpf = [cx.sbuf(ph, "dumpf%d" % i, [128, cw_]) for i in range(2)]
            srcb = src if hasattr(src, "t") else qkT
            i = 0
            for r0 in range(0, rows, 128):
                for c0 in range(0, cols, cw_):
                    i += 1
                    n = min(128, rows - r0)
                    tb_, tf_ = tmpb[i % 2], tmpf[i % 2]
                    cx.dma("sp", tb_[0:n, :], src[r0:r0 + n, c0:c0 + cw_], reads=[srcb], writes=[tb_], sb=tb_)
                    cx.op("dve", lambda: V.tensor_copy(tf_[0:n, :], tb_[0:n, :]), [tb_], [tf_])
                    cx.dma("sp", d[r0:r0 + n, c0:c0 + cw_], tf_[0:n, :], reads=[tf_], writes=[d], sb=tf_)
            cx.end_phase()
            ph.close()
    ph = ExitStack()
    for name in ("IDX", "GK"):
        if name in dbg:
            srcb = IDX if name == "IDX" else GK
            tmpf = cx.sbuf(ph, "dumpg" + name, [128, NT * 4])
            cx.op("dve", lambda: V.tensor_copy(tmpf[:], srcb[:].rearrange("p a b -> p (a b)")), [srcb], [tmpf])
            cx.dma("sp", dbg[name][:], tmpf[:], reads=[tmpf], writes=[dbg[name]], sb=tmpf)
    cx.end_phase()
    ph.close()
    st.close()
    return nc, cx


def make_consts():
    import ml_dtypes
    k = np.arange(128)[:, None]
    q = np.arange(128)[None, :]
    masks = np.zeros((128, 8, 128), np.float32)
    masks[:, 0, :] = (k < q)
    masks[:, 1, :] = (k > q)
    masks[:, 2, :] = 1.0
    masks[:, 3, :] = np.where(k > q, NEG, 0.0)
    masks[:, 4, :] = np.where(k >= q, NEG, 0.0)
    masks[:, 5, :] = np.where(k < q, -1.0, 0.0)
    masks[:, 6, :] = np.where(k >= q, -1.0, 0.0)
    masks[:, 7, :] = -1.0
    ebase = np.broadcast_to((np.arange(NE) * CAP + 1).astype(np.float32)[None, :], (128, NE)).copy()
    return {
        "k_identb": np.eye(128, dtype=np.float32).astype(ml_dtypes.bfloat16),
        "k_identf": np.eye(128, dtype=np.float32),
        "k_masks": masks,
        "k_ebase": ebase,
    }


WEIGHT_KEYS = ["w_ada", "b_ada", "ln1_g", "ln1_b", "w_in", "b_f", "conv_w", "conv_b", "lru_wa", "lru_ba", "lru_wx",
               "lru_bx", "lru_lambda", "w_gate", "b_gate", "w_pa", "w_pb", "w_pc", "w_o", "ln2_g", "ln2_b", "w_router",
               "b_router", "w_gu", "b_gu", "w_down", "b_down"]


def make_in_maps(inputs):
    consts = make_consts()
    x = np.ascontiguousarray(np.asarray(inputs["x"], dtype=np.float32))
    c = np.ascontiguousarray(np.asarray(inputs["c"], dtype=np.float32))
    shared = {k: np.ascontiguousarray(np.asarray(inputs[k], dtype=np.float32)) for k in WEIGHT_KEYS}
    shared.update(consts)
    in_maps = []
    for i in range(NCORES):
        m = dict(shared)
        m["x"] = x[NSEQ * i:NSEQ * (i + 1)].reshape(T, D)
        m["c"] = c[NSEQ * i:NSEQ * (i + 1)]
        in_maps.append(m)
    return in_maps


def kernel(**inputs):
    nc, cx = build_program()
    in_maps = make_in_maps(inputs)
    res = run_bass_kernel_spmd(nc, in_maps, core_ids=list(range(NCORES)))
    out = np.stack([np.asarray(r["out"]).reshape(NSEQ, SEQ, D) for r in res.results], axis=0)
    return out.reshape(NCORES * NSEQ, SEQ, D).astype(np.float32)
```

```python
from contextlib import ExitStack
import numpy as np
import concourse.bass as bass
import concourse.mybir as mybir
from concourse.bass_utils import run_bass_kernel_spmd

F32 = mybir.dt.float32
F32R = mybir.dt.float32r
BF16 = mybir.dt.bfloat16
I32 = mybir.dt.int32
U32 = mybir.dt.uint32
AF = mybir.ActivationFunctionType
ALU = mybir.AluOpType

NCORES = 8
D = 1024
SEQ = 2048
NSEQ = 2
T = NSEQ * SEQ
NT = T // 128
NCH = T // 512
DEPTH = 2
H = 8
DH = 64
D_IN = 5128
NE = 32
CAP = 1024
ALPHA = (2.0 * DEPTH) ** 0.25
LN_EPS = 1e-5
NEG = -30000.0


class Slot:
    def __init__(self, sem):
        self.sem = sem
        self.count = 0


class Buf:
    def __init__(self, name, t=None):
        self.name = name
        self.t = t
        self.w = {}
        self.r = {}
        self.ds = None

    def __getitem__(self, k):
        return self.t[k]


class EngState:
    def __init__(self, name, eng, sem):
        self.name = name
        self.eng = eng
        self.sem = sem
        self.count = 0
        self.waited = {}


class Ctx:
    def __init__(self, nc, stack, n_dma_sems=72):
        self.nc = nc
        self.stack = stack
        self.E = {}
        for name, eng in (("pe", nc.tensor), ("act", nc.scalar), ("dve", nc.vector),
                          ("pool", nc.gpsimd), ("sp", nc.sync)):
            sem = stack.enter_context(nc.semaphore("s_" + name))
            self.E[name] = EngState(name, eng, sem)
        self.free_slots = [Slot(stack.enter_context(nc.semaphore("d%d" % i))) for i in range(n_dma_sems)]
        self.used_slots = []
        self.n_ins = 0
        self.n_wait = 0
        self.uid = 0

    def sbuf(self, ph, name, shape, dtype=F32):
        self.uid += 1
        t = ph.enter_context(self.nc.sbuf_tensor("%s_%d" % (name, self.uid), list(shape), dtype))
        return Buf(name, t)

    def psum(self, name, shape, dtype=F32):
        t = self.stack.enter_context(self.nc.psum_tensor(name, list(shape), dtype))
        return Buf(name, t)

    def dram(self, name, shape, dtype=F32, kind="Internal"):
        t = self.nc.dram_tensor(name, list(shape), dtype, kind=kind)
        return Buf(name, t.ap())

    def _wait(self, E, deps):
        for sid, (sem, val) in deps.items():
            if E.waited.get(sid, 0) >= val:
                continue
            E.eng.wait_ge(sem, val)
            E.waited[sid] = val
            self.n_wait += 1

    @staticmethod
    def _merge(d, src, skip=None):
        for sid, (sem, val) in src.items():
            if skip is not None and sid == skip:
                continue
            if sid not in d or d[sid][1] < val:
                d[sid] = (sem, val)

    def op(self, en, fn, reads=(), writes=()):
        E = self.E[en]
        own = id(E.sem)
        deps = {}
        for b in reads:
            self._merge(deps, b.w, skip=own if en == "pe" else None)
        for b in writes:
            self._merge(deps, b.w, skip=own)
            self._merge(deps, b.r, skip=own)
        self._wait(E, deps)
        ins = fn()
        E.count += 1
        ins.then_inc(E.sem, 1)
        tok = (E.sem, E.count)
        for b in reads:
            b.r[own] = tok
        for b in writes:
            b.w = {own: tok}
            b.r = {}
        self.n_ins += 1
        return ins

    def dma(self, qn, out, in_, reads=(), writes=(), sb=None, fn=None):
        E = self.E[qn]
        if sb.ds is None:
            sb.ds = self.free_slots.pop()
            self.used_slots.append(sb.ds)
        ds = sb.ds
        deps = {}
        if ds.count:
            deps[id(ds.sem)] = (ds.sem, ds.count)
        for b in reads:
            self._merge(deps, b.w)
        for b in writes:
            self._merge(deps, b.w)
            self._merge(deps, b.r)
        self._wait(E, deps)
        ins = E.eng.dma_start(out=out, in_=in_) if fn is None else fn()
        ds.count += 16
        ins.then_inc(ds.sem, 16)
        tok = (ds.sem, ds.count)
        sid = id(ds.sem)
        for b in reads:
            b.r[sid] = tok
        for b in writes:
            b.w = {sid: tok}
            b.r = {}
        self.n_ins += 1
        return ins

    def barrier(self, only=None):
        deps = {}
        for E in self.E.values():
            if E.count:
                deps[id(E.sem)] = (E.sem, E.count)
        for s in self.used_slots:
            if s.count:
                deps[id(s.sem)] = (s.sem, s.count)
        for name, E in self.E.items():
            if only is not None and name not in only:
                continue
            d = {k: v for k, v in deps.items() if k != id(E.sem)}
            self._wait(E, d)

    def end_phase(self):
        self.barrier()
        self.free_slots.extend(self.used_slots)
        self.used_slots = []


def build_program(n_layers=DEPTH, stop_after=None, debug=()):
    nc = bass.Bass("TRN2", target_bir_lowering=False)
    st = ExitStack()
    cx = Ctx(nc, st)
    V, S, P = nc.vector, nc.scalar, nc.gpsimd

    def din(name, shape, dtype=F32):
        return cx.dram(name, shape, dtype, kind="ExternalInput")

    x_in = din("x", [T, D]); c_in = din("c", [NSEQ, D])
    w_ada = din("w_ada", [DEPTH, D, 6 * D]); b_ada = din("b_ada", [DEPTH, 6 * D])
    ln1_g = din("ln1_g", [DEPTH, D]); ln1_b = din("ln1_b", [DEPTH, D])
    w_in = din("w_in", [DEPTH, D, D_IN]); b_f = din("b_f", [DEPTH, H])
    conv_w = din("conv_w", [DEPTH, 4, D]); conv_b = din("conv_b", [DEPTH, D])
    lru_wa = din("lru_wa", [DEPTH, 16, 64, 64]); lru_ba = din("lru_ba", [DEPTH, D])
    lru_wx = din("lru_wx", [DEPTH, 16, 64, 64]); lru_bx = din("lru_bx", [DEPTH, D])
    lru_lambda = din("lru_lambda", [DEPTH, D])
    w_gate = din("w_gate", [DEPTH, D, 3 * D]); b_gate = din("b_gate", [DEPTH, 3 * D])
    w_pa = din("w_pa", [DEPTH, 512, D]); w_pb = din("w_pb", [DEPTH, 512, D])
    w_pc = din("w_pc", [DEPTH, D, D]); w_o = din("w_o", [DEPTH, D, D])
    ln2_g = din("ln2_g", [DEPTH, D]); ln2_b = din("ln2_b", [DEPTH, D])
    w_router = din("w_router", [DEPTH, D, NE]); b_router = din("b_router", [DEPTH, NE])
    w_gu = din("w_gu", [DEPTH, NE, D, 2 * D]); b_gu = din("b_gu", [DEPTH, NE, 2 * D])
    w_down = din("w_down", [DEPTH, NE, D, D]); b_down = din("b_down", [DEPTH, NE, D])
    k_identb = din("k_identb", [128, 128], BF16)
    k_identf = din("k_identf", [128, 128])
    k_masks = din("k_masks", [128, 8, 128])
    k_ebase = din("k_ebase", [128, NE])
    out_d = cx.dram("out", [T, D], F32, kind="ExternalOutput")

    adaB = cx.dram("adaB", [DEPTH, NSEQ, 6, D])
    xres = [cx.dram("xresA", [T, D]), cx.dram("xresB", [T, D])]
    qkT = cx.dram("qkT", [4, 512, T], BF16)
    vv = cx.dram("vv", [2, T, 512], BF16)
    faT = cx.dram("faT", [H, T])
    Fd = cx.dram("Fd", [6, H, T], BF16)
    xgT = cx.dram("xgT", [2, D, T])
    gT = cx.dram("gT", [3 * D, T])
    oT = cx.dram("oT", [2 * D, T], BF16)
    xbuf = cx.dram("xbuf", [(NE + 1) * CAP, D], BF16)
    ybuf = cx.dram("ybuf", [(NE + 1) * CAP, D])
    dbg = {}
    for name, shape in debug:
        dbg[name] = cx.dram("dbg_" + name, shape, F32, kind="ExternalOutput")

    pb = [cx.psum("pb%d" % i, [128, 512]) for i in range(7)]
    pbh = cx.psum("pbh", [128, 1024], BF16)

    gl = st
    identb = cx.sbuf(gl, "identb", [128, 128], BF16)
    identf = cx.sbuf(gl, "identf", [128, 128])
    masks = cx.sbuf(gl, "masks", [128, 8, 128])
    masksb = cx.sbuf(gl, "masksb", [128, 8, 128], BF16)
    masksr = cx.sbuf(gl, "masksr", [128, 8, 128], F32R)
    ebase = cx.sbuf(gl, "ebase", [128, NE])
    IDX = cx.sbuf(gl, "IDX", [128, NT, 4], I32)
    GK = cx.sbuf(gl, "GK", [128, NT, 4])
    onesb = cx.sbuf(gl, "onesb", [128, 128], BF16)
    cx.dma("sp", identb[:], k_identb[:], reads=[k_identb], writes=[identb], sb=identb)
    cx.dma("sp", identf[:], k_identf[:], reads=[k_identf], writes=[identf], sb=identf)
    cx.dma("sp", masks[:], k_masks[:], reads=[k_masks], writes=[masks], sb=masks)
    cx.dma("sp", ebase[:], k_ebase[:], reads=[k_ebase], writes=[ebase], sb=ebase)
    cx.op("dve", lambda: V.tensor_copy(masksb[:], masks[:]), [masks], [masksb])
    cx.op("dve", lambda: V.tensor_copy(masksr[:], masks[:]), [masks], [masksr])
    cx.op("dve", lambda: V.memset(onesb[:], 1.0), [], [onesb])
    M_SUT, M_TRI, M_ONES, M_NEGC, M_NEGNS, M_NSTRICT, M_NTRII, M_NONES = range(8)

    def row_bc(ap_row, n):
        return ap_row.partition_broadcast(n)

    def phase_ada():
        ph = ExitStack()
        c_col = cx.sbuf(ph, "c_col", [128, NSEQ, 8])
        cond = cx.sbuf(ph, "cond", [128, NSEQ, 8])
        with nc.allow_non_contiguous_dma(reason="tiny transposed load of c"):
            cx.dma("sp", c_col[:], c_in.t.rearrange("s (kc p) -> p s kc", p=128), reads=[c_in], writes=[c_col], sb=c_col)
        cx.op("act", lambda: S.activation(cond[:], c_col[:], AF.Silu), [c_col], [cond])
        condB = cx.sbuf(ph, "condB", [128, NSEQ, 8, 128], BF16)
        for s in range(NSEQ):
            cx.op("dve", lambda: V.tensor_copy(condB[:, s], cond[:, s, :].unsqueeze(2).to_broadcast([128, 8, 128])),
                  [cond], [condB])
        wring = [cx.sbuf(ph, "wada%d" % i, [128, 8, 512], BF16) for i in range(3)]
        brow = [cx.sbuf(ph, "brow%d" % i, [128, 512]) for i in range(2)]
        res = [cx.sbuf(ph, "ares%d" % i, [128, 512]) for i in range(3)]
        k = 0
        for l in range(n_layers):
            for n in range(12):
                wt = wring[k % 3]; br = brow[k % 2]
                cx.dma("pool", None, None, reads=[w_ada], writes=[wt], sb=wt,
                       fn=lambda: P.dma_start(out=wt[:], in_=w_ada[l, :, n * 512:(n + 1) * 512].rearrange("(kc p) n -> p kc n", p=128)))
                cx.dma("sp", br[:], row_bc(b_ada[l, n * 512:(n + 1) * 512], 128), reads=[b_ada], writes=[br], sb=br)
                which = n // 2
                for s in range(NSEQ):
                    ps = pb[(k * NSEQ + s) % 4]
                    for kc in range(8):
                        cx.op("pe", lambda: nc.tensor.matmul(ps[:], condB[:, s, kc, :], wt[:, kc, :], start=(kc == 0), stop=(kc == 7)),
                              [condB, wt], [ps])
                    r = res[(k * NSEQ + s) % 3]
                    cx.op("dve", lambda: V.tensor_tensor(r[:], ps[:], br[:], ALU.add), [ps, br], [r])
                    if which not in (0, 3):
                        cx.op("dve", lambda: V.tensor_scalar_add(r[:], r[:], 1.0), [r], [r])
                    c0_ = (n % 2) * 512
                    cx.dma("sp", adaB[l, s, which:which + 1, c0_:c0_ + 512], r[0:1, :], reads=[r], writes=[adaB], sb=r)
                k += 1
        cx.end_phase()
        ph.close()

    def load_mod(ph, l, sub, names=("sh", "sc", "gt")):
        tiles = {}
        for s in range(NSEQ):
            for nm, idx in (("sh", 3 * sub), ("sc", 3 * sub + 1), ("gt", 3 * sub + 2)):
                if nm not in names:
                    continue
                t = cx.sbuf(ph, "mod_%s%d" % (nm, s), [128, D])
                cx.dma("sp", t[:], row_bc(adaB[l, s, idx, :], 128), reads=[adaB], writes=[t], sb=t)
                tiles[(nm, s)] = t
        return tiles

    def phase_proj(l, xsrc):
        ph = ExitStack()
        TC = 1024
        NC2 = T // TC
        TPC = TC // 128
        mod = load_mod(ph, l, 0, ("sh", "sc"))
        bgate = cx.sbuf(ph, "bgate", [128, 24])
        with nc.allow_non_contiguous_dma(reason="tiny bias relayout"):
            cx.dma("sp", bgate[:], b_gate[l].rearrange("(m p) -> p m", p=128), reads=[b_gate], writes=[bgate], sb=bgate)
        xt = [cx.sbuf(ph, "xt%d" % i, [128, D]) for i in range(TPC)]
        hf = [cx.sbuf(ph, "hf%d" % i, [128, D]) for i in range(2)]
        hb = [cx.sbuf(ph, "hb%d" % i, [128, D], BF16) for i in range(2)]
        hT = [cx.sbuf(ph, "hT%d" % i, [128, 8, TC], BF16) for i in range(2)]
        wring = [cx.sbuf(ph, "win%d" % i, [128, 8, 512], BF16) for i in range(3)]
        wfa = cx.sbuf(ph, "wfa", [128, 8, 8], BF16)
        evf = [cx.sbuf(ph, "evf%d" % i, [128, 512]) for i in range(4)]
        evb = [cx.sbuf(ph, "evb%d" % i, [128, 512], BF16) for i in range(4)]
        cx.dma("pool", None, None, reads=[w_in], writes=[wfa], sb=wfa,
               fn=lambda: P.dma_start(out=wfa[:], in_=w_in[l, :, 1536:1544].rearrange("(kc p) n -> p kc n", p=128)))
        cnt = {"wk": 0, "ek": 0, "pk": 0}
        pieces = [("qa", 0), ("ka", 512), ("va", 1024), ("qb", 1544), ("kb", 2056), ("vb", 2568),
                  ("xc0", 3080), ("xc1", 3592), ("gc0", 4104), ("gc1", 4616)] + [("g%d" % i, i * 512) for i in range(6)]

        def load_x(ch):
            for tt in range(TPC):
                ti = ch * TPC + tt
                cx.dma("sp", xt[tt][:], xsrc[ti * 128:(ti + 1) * 128, :], reads=[xsrc], writes=[xt[tt]], sb=xt[tt])

        def compute_h(ch):
            s = (ch * TC) // SEQ
            hTc = hT[ch % 2]
            for tt in range(TPC):
                x_t = xt[tt]; h_f = hf[tt % 2]; h_b = hb[tt % 2]
                cx.op("dve", lambda: V.tensor_tensor(h_f[:], x_t[:], mod[("sc", s)][:], ALU.mult), [x_t, mod[("sc", s)]], [h_f])
                cx.op("dve", lambda: V.tensor_tensor(h_b[:], h_f[:], mod[("sh", s)][:], ALU.add), [h_f, mod[("sh", s)]], [h_b])
                for kc in range(8):
                    cx.op("pe", lambda: nc.tensor.transpose(pbh[:, kc * 128:(kc + 1) * 128], h_b[:, kc * 128:(kc + 1) * 128], identb[:]),
                          [h_b, identb], [pbh])
                cx.op("act", lambda: S.copy(hTc[:, :, tt * 128:(tt + 1) * 128], pbh[:].rearrange("p (kc t) -> p kc t", kc=8)),
                      [pbh], [hTc])

        def evac(kind, ps, arg=None):
            ek = cnt["ek"]; cnt["ek"] += 1
            if kind == "bf":
                ev = evb[ek % 4]
                if ek % 2:
                    cx.op("act", lambda: S.mul(ev[:], ps[:], arg), [ps], [ev])
                else:
                    cx.op("dve", lambda: V.tensor_scalar_mul(ev[:], ps[:], arg), [ps], [ev])
            elif kind == "f":
                ev = evf[ek % 4]
                if ek % 2:
                    cx.op("act", lambda: S.copy(ev[:], ps[:]), [ps], [ev])
                else:
                    cx.op("dve", lambda: V.tensor_copy(ev[:], ps[:]), [ps], [ev])
            else:
                ev = evf[ek % 4]
                cx.op("act", lambda: S.activation(ev[:], ps[:], AF.Sigmoid, bias=bgate[:, arg:arg + 1], scale=1.0), [ps, bgate], [ev])
            return ev

        def next_ps():
            ps = pb[cnt["pk"] % 6]; cnt["pk"] += 1
            return ps

        load_x(0)
        compute_h(0)
        for ch in range(NC2):
            hTc = hT[ch % 2]
            tok0 = ch * TC
            if ch + 1 < NC2:
                load_x(ch + 1)
            for half in range(TC // 512):
                ps = next_ps()
                hs = slice(half * 512, (half + 1) * 512)
                for kc in range(8):
                    cx.op("pe", lambda: nc.tensor.matmul(ps[0:8, :], wfa[:, kc, :], hTc[:, kc, hs], start=(kc == 0), stop=(kc == 7)),
                          [wfa, hTc], [ps])
                ev = evf[cnt["ek"] % 4]; cnt["ek"] += 1
                cx.op("dve", lambda: V.tensor_copy(ev[0:8, :], ps[0:8, :]), [ps], [ev])
                cx.dma("sp", faT[:, tok0 + half * 512:tok0 + (half + 1) * 512], ev[0:8, :], reads=[ev], writes=[faT], sb=ev)
            for pi_, (nm, c0) in enumerate(pieces):
                if pi_ == 8 and ch + 1 < NC2:
                    compute_h(ch + 1)
                wt = wring[cnt["wk"] % 3]; cnt["wk"] += 1
                wsrc = (w_gate if nm[0] == "g" and nm[1].isdigit() else w_in)
                cx.dma("pool", None, None, reads=[wsrc], writes=[wt], sb=wt,
                       fn=lambda: P.dma_start(out=wt[:], in_=wsrc[l, :, c0:c0 + 512].rearrange("(kc p) n -> p kc n", p=128)))
                if nm in ("va", "vb"):
                    for tt in range(TPC):
                        ps = next_ps()
                        for kc in range(8):
                            cx.op("pe", lambda: nc.tensor.matmul(ps[:], hTc[:, kc, tt * 128:(tt + 1) * 128], wt[:, kc, :], start=(kc == 0), stop=(kc == 7)),
                                  [hTc, wt], [ps])
                        ev = evac("bf", ps, 1.0)
                        cx.dma("sp", vv[0 if nm == "va" else 1, tok0 + tt * 128:tok0 + (tt + 1) * 128, :], ev[:], reads=[ev], writes=[vv], sb=ev)
                    continue
                for m in range(4):
                    for half in range(TC // 512):
                        hs = slice(half * 512, (half + 1) * 512)
                        tk = tok0 + half * 512
                        ps = next_ps()
                        for kc in range(8):
                            cx.op("pe", lambda: nc.tensor.matmul(ps[:], wt[:, kc, m * 128:(m + 1) * 128], hTc[:, kc, hs], start=(kc == 0), stop=(kc == 7)),
                                  [wt, hTc], [ps])
                        if nm in ("qa", "ka", "qb", "kb"):
                            ev = evac("bf", ps, 0.125 if nm[0] == "q" else 1.0)
                            qi = ("qa", "ka", "qb", "kb").index(nm)
                            cx.dma("sp", qkT[qi, m * 128:(m + 1) * 128, tk:tk + 512], ev[:], reads=[ev], writes=[qkT], sb=ev)
                        elif nm[0] == "x" or nm[:2] == "gc":
                            ev = evac("f", ps)
                            r0 = int(nm[2]) * 512 + m * 128
                            cx.dma("sp", xgT[0 if nm[0] == "x" else 1, r0:r0 + 128, tk:tk + 512], ev[:], reads=[ev], writes=[xgT], sb=ev)
                        else:
                            mi = int(nm[1]) * 4 + m
                            ev = evac("g", ps, mi)
                            cx.dma("sp", gT[mi * 128:(mi + 1) * 128, tk:tk + 512], ev[:], reads=[ev], writes=[gT], sb=ev)
        cx.end_phase()
        ph.close()

    def phase_fprep(l):
        ph = ExitStack()
        bf = cx.sbuf(ph, "bf", [H, 1]); nbf = cx.sbuf(ph, "nbf", [H, 1])
        with nc.allow_non_contiguous_dma(reason="tiny"):
            cx.dma("sp", bf[:], b_f[l].rearrange("(h o) -> h o", o=1), reads=[b_f], writes=[bf], sb=bf)
        cx.op("dve", lambda: V.tensor_scalar_mul(nbf[:], bf[:], -1.0), [bf], [nbf])
        ones = cx.sbuf(ph, "ones", [H, SEQ]); cx.op("dve", lambda: V.memset(ones[:], 1.0), [], [ones])
        for s in range(NSEQ):
            fa = cx.sbuf(ph, "fa%d" % s, [H, SEQ]); e = cx.sbuf(ph, "fe%d" % s, [H, SEQ]); sp_ = cx.sbuf(ph, "fs%d" % s, [H, SEQ])
            F = cx.sbuf(ph, "F%d" % s, [H, SEQ]); r1 = cx.sbuf(ph, "r1%d" % s, [H, SEQ]); r2 = cx.sbuf(ph, "r2%d" % s, [H, SEQ])
            parts = cx.sbuf(ph, "parts%d" % s, [H, 6, SEQ], BF16)
            cx.dma("sp", fa[:], faT[:, s * SEQ:(s + 1) * SEQ], reads=[faT], writes=[fa], sb=fa)
            cx.op("act", lambda: S.activation(e[:], fa[:], AF.Exp, bias=nbf[:, 0:1], scale=-1.0), [fa, nbf], [e])
            cx.op("act", lambda: S.activation(sp_[:], e[:], AF.Ln, bias=1.0, scale=1.0), [e], [sp_])
            cx.op("dve", lambda: V.tensor_tensor_scan(F[:], ones[:], sp_[:], 0.0, ALU.mult, ALU.subtract), [ones, sp_], [F])
            cx.op("dve", lambda: V.tensor_copy(parts[:, 0, :], F[:]), [F], [parts])
            cx.op("dve", lambda: V.tensor_tensor(r1[:], F[:], parts[:, 0, :], ALU.subtract), [F, parts], [r1])
            cx.op("dve", lambda: V.tensor_copy(parts[:, 1, :], r1[:]), [r1], [parts])
            cx.op("dve", lambda: V.tensor_tensor(r2[:], r1[:], parts[:, 1, :], ALU.subtract), [r1, parts], [r2])
            cx.op("dve", lambda: V.tensor_copy(parts[:, 2, :], r2[:]), [r2], [parts])
            cx.op("dve", lambda: V.tensor_scalar_mul(parts[:, 3:6, :], parts[:, 0:3, :], -1.0), [parts], [parts])
            cx.dma("sp", Fd[:, :, s * SEQ:(s + 1) * SEQ].rearrange("v h t -> h v t"), parts[:], reads=[parts], writes=[Fd], sb=parts)
        cx.end_phase()
        ph.close()

    def gen_fox(ph):
        NB = 2
        kT = [cx.sbuf(ph, "fkT%d" % i, [70, SEQ], BF16) for i in range(NB)]
        qT = [cx.sbuf(ph, "fqT%d" % i, [70, SEQ], BF16) for i in range(NB)]
        Va = [cx.sbuf(ph, "Va%d" % i, [128, 16, 128], BF16) for i in range(NB)]
        Pt = [cx.sbuf(ph, "Pt%d" % i, [128, 512], BF16) for i in range(3)]
        rec = [cx.sbuf(ph, "rec%d" % i, [128, 512]) for i in range(2)]
        ob = [cx.sbuf(ph, "fob%d" % i, [64, 512], BF16) for i in range(2)]
        for i in range(NB):
            cx.op("dve", lambda: V.memset(kT[i][64:70, :], 1.0), [], [kT[i]])
            cx.op("dve", lambda: V.memset(qT[i][64:70, :], 1.0), [], [qT[i]])
            cx.op("dve", lambda: V.memset(Va[i][:], 1.0), [], [Va[i]])
        it = 0; pk = 0; ck = 0
        units = [(s, h) for s in range(NSEQ) for h in range(H)]

        def loads(ui):
            s, h = units[ui]
            b = ui % NB
            t0 = s * SEQ
            cx.dma("sp", qT[b][0:64, :], qkT[0, h * 64:(h + 1) * 64, t0:t0 + SEQ], reads=[qkT], writes=[qT[b]], sb=qT[b])
            cx.dma("sp", qT[b][64:67, :], Fd[0:3, h, t0:t0 + SEQ], reads=[Fd], writes=[qT[b]], sb=qT[b])
            cx.dma("sp", kT[b][0:64, :], qkT[1, h * 64:(h + 1) * 64, t0:t0 + SEQ], reads=[qkT], writes=[kT[b]], sb=kT[b])
            cx.dma("sp", kT[b][67:70, :], Fd[3:6, h, t0:t0 + SEQ], reads=[Fd], writes=[kT[b]], sb=kT[b])
            with nc.allow_non_contiguous_dma(reason="v head slice, 128B runs"):
                cx.dma("sp", Va[b][:, :, 0:64], vv[0, t0:t0 + SEQ, h * 64:(h + 1) * 64].rearrange("(j p) d -> p j d", p=128),
                       reads=[vv], writes=[Va[b]], sb=Va[b])

        loads(0); loads(1)
        steps = [(ui, c, J) for ui in range(len(units)) for c in range(4) for J in range(4 * c + 4)]
        pend = {}
        for k in range(len(steps) + 2):
            if k < len(steps):
                ui, c, J = steps[k]
                s, h = units[ui]; b = ui % NB
                q0 = c * 512
                lo = 128 * max(0, J - 4 * c)
                Sb = pb[pk % 4]; Pb = Pt[pk % 3]; pk += 1
                diag = J >= 4 * c
                cx.op("pe", lambda: nc.tensor.matmul(Sb[:, lo:512], kT[b][:, J * 128:(J + 1) * 128], qT[b][:, q0 + lo:q0 + 512], start=True, stop=not diag),
                      [kT[b], qT[b]], [Sb])
                if diag:
                    cx.op("pe", lambda: nc.tensor.matmul(Sb[:, lo:lo + 128], identb[:], masksb[:, M_NEGC, :], start=False, stop=True),
                          [identb, masksb], [Sb])
                cx.op("act", lambda: S.activation(Pb[:, lo:512], Sb[:, lo:512], AF.Exp), [Sb], [Pb])
                pend[k] = (lo, Pb)
            if k >= 2:
                ui, c, J = steps[k - 2]
                s, h = units[ui]; b = ui % NB
                lo, Pb = pend.pop(k - 2)
                nJ = 4 * c + 4
                gci = ui * 4 + c
                O = pb[4 + gci % 2]
                cx.op("pe", lambda: nc.tensor.matmul(O[:, lo:512], Va[b][:, J, :], Pb[:, lo:512], start=(J == 0), stop=(J == nJ - 1)),
                      [Va[b], Pb], [O])
                if J == nJ - 1:
                    rc = rec[gci % 2]; o_ = ob[gci % 2]
                    t0 = s * SEQ; q0 = c * 512
                    cx.op("dve", lambda: V.reciprocal(rc[64:128, :], O[64:128, :]), [O], [rc])
                    cx.op("dve", lambda: V.tensor_tensor(o_[:], O[0:64, :], rc[64:128, :], ALU.mult), [O, rc], [o_])
                    cx.dma("sp", oT[h * 64:(h + 1) * 64, t0 + q0:t0 + q0 + 512], o_[:], reads=[o_], writes=[oT], sb=o_)
                    if c == 3 and ui + 2 < len(units):
                        loads(ui + 2)
            yield

    def gen_sb(ph):
        NB = 2
        kT = [cx.sbuf(ph, "kT%d" % i, [64, SEQ], BF16) for i in range(NB)]
        qT = [cx.sbuf(ph, "qT%d" % i, [64, SEQ], BF16) for i in range(NB)]
        Vb = [cx.sbuf(ph, "Vb%d" % i, [128, 16, 64], BF16) for i in range(NB)]
        Et = [cx.sbuf(ph, "Et%d" % i, [128, 512]) for i in range(3)]
        SPt = [cx.sbuf(ph, "SPt%d" % i, [128, 512], F32R) for i in range(4)]
        R = [cx.sbuf(ph, "R%d" % i, [128, 512], F32R) for i in range(2)]
        Wt = [cx.sbuf(ph, "Wt%d" % i, [128, 512], BF16) for i in range(3)]
        ob = [cx.sbuf(ph, "ob%d" % i, [64, 512], BF16) for i in range(2)]
        Zf = cx.sbuf(ph, "Zf", [128, 512])
        cx.op("dve", lambda: V.memset(Zf[:], 0.0), [], [Zf])
        it = 0; zk = 0; ak = 0; ck = 0; k3 = 0; wk = 0
        units = [(s, h) for s in range(NSEQ) for h in range(H)]

        def loads(ui):
            s, h = units[ui]
            b = ui % NB
            t0 = s * SEQ
            cx.dma("sp", qT[b][:], qkT[2, h * 64:(h + 1) * 64, t0:t0 + SEQ], reads=[qkT], writes=[qT[b]], sb=qT[b])
            cx.dma("sp", kT[b][:], qkT[3, h * 64:(h + 1) * 64, t0:t0 + SEQ], reads=[qkT], writes=[kT[b]], sb=kT[b])
            with nc.allow_non_contiguous_dma(reason="v head slice, 128B runs"):
                cx.dma("sp", Vb[b][:], vv[1, t0:t0 + SEQ, h * 64:(h + 1) * 64].rearrange("(j p) d -> p j d", p=128),
                       reads=[vv], writes=[Vb[b]], sb=Vb[b])

        loads(0); loads(1)
        steps = [(ui, c, 4 * c + 3 - i) for ui in range(len(units)) for c in range(4) for i in range(4 * c + 4)]
        st_ = {}
        for k in range(len(steps) + 2):
            if k < len(steps):
                ui, c, J = steps[k]
                s, h = units[ui]; b = ui % NB
                gci = ui * 4 + c
                q0 = c * 512
                top = (J == 4 * c + 3)
                if top:
                    cx.op("dve", lambda: V.tensor_copy(R[gci % 2][:], Zf[:]), [Zf], [R[gci % 2]])
                lo = 128 * max(0, J - 4 * c)
                diag = J >= 4 * c
                Z = pb[zk % 2]; zk += 1
                e_ = Et[k3 % 3]; sp_ = SPt[k3 % 4]; k3 += 1
                cx.op("pe", lambda: nc.tensor.matmul(Z[:, lo:512], kT[b][:, J * 128:(J + 1) * 128], qT[b][:, q0 + lo:q0 + 512], start=True, stop=True),
                      [kT[b], qT[b]], [Z])
                cx.op("act", lambda: S.activation(e_[:, lo:512], Z[:, lo:512], AF.Exp), [Z], [e_])
                cx.op("act", lambda: S.activation(sp_[:, lo:512], e_[:, lo:512], AF.Ln, bias=1.0, scale=1.0), [e_], [sp_])
                if diag:
                    cx.op("dve", lambda: V.tensor_tensor(sp_[:, lo:lo + 128], sp_[:, lo:lo + 128].bitcast(F32), masks[:, M_SUT, :], ALU.mult), [sp_, masks], [sp_])
                st_[k] = (lo, diag, top, sp_)
            if 1 <= k <= len(steps):
                ui, c, J = steps[k - 1]
                s, h = units[ui]; b = ui % NB
                gci = ui * 4 + c
                q0 = c * 512
                Rc = R[gci % 2]
                lo, diag, top, sp_ = st_[k - 1]
                Ab = pb[2 + ak % 2]; ak += 1
                w_ = Wt[wk % 3]; wk += 1
                cx.op("pe", lambda: nc.tensor.matmul(Ab[:, lo:512], kT[b][:, J * 128:(J + 1) * 128], qT[b][:, q0 + lo:q0 + 512], start=True, stop=False),
                      [kT[b], qT[b]], [Ab])
                cx.op("pe", lambda: nc.tensor.matmul(Ab[:, lo:512], masksr[:, M_NTRII, :], sp_[:, lo:512], start=False, stop=(top and not diag)),
                      [masksr, sp_], [Ab])
                if not top:
                    cx.op("pe", lambda: nc.tensor.matmul(Ab[:, lo:512], masksr[:, M_NONES, :], Rc[:, lo:512], start=False, stop=not diag),
                          [masksr, Rc], [Ab])
                if diag:
                    cx.op("pe", lambda: nc.tensor.matmul(Ab[:, lo:lo + 128], identb[:], masksb[:, M_NEGNS, :], start=False, stop=True),
                          [identb, masksb], [Ab])
                cx.op("act", lambda: S.activation(w_[:, lo:512], Ab[:, lo:512], AF.Exp), [Ab], [w_])
                if J > 0:
                    cx.op("dve", lambda: V.tensor_tensor(Rc[:, lo:512], Rc[:, lo:512].bitcast(F32), sp_[:, lo:512].bitcast(F32), ALU.add), [Rc, sp_], [Rc])
                st_[k - 1] = (lo, diag, top, sp_, w_)
            if k >= 2:
                ui, c, J = steps[k - 2]
                s, h = units[ui]; b = ui % NB
                gci = ui * 4 + c
                O = pb[4 + gci % 2]; o_ = ob[gci % 2]
                lo, diag, top, sp_, w_ = st_.pop(k - 2)
                cx.op("pe", lambda: nc.tensor.matmul(O[0:64, lo:512], Vb[b][:, J, :], w_[:, lo:512], start=top, stop=(J == 0)),
                      [Vb[b], w_], [O])
                if J == 0:
                    t0 = s * SEQ; q0 = c * 512
                    cx.op("act", lambda: S.copy(o_[:], O[0:64, :]), [O], [o_])
                    cx.dma("sp", oT[512 + h * 64:512 + (h + 1) * 64, t0 + q0:t0 + q0 + 512], o_[:], reads=[o_], writes=[oT], sb=o_)
                    if c == 3 and ui + 2 < len(units):
                        loads(ui + 2)
            yield

    def gen_lru(ph, l):
        def colvec(name, src_row):
            t = cx.sbuf(ph, name, [128, 8])
            with nc.allow_non_contiguous_dma(reason="tiny"):
                cx.dma("sp", t[:], src_row.rearrange("(m p) -> p m", p=128), reads=[], writes=[t], sb=t)
            return t
        cb = colvec("cb", conv_b[l]); ba = colvec("ba", lru_ba[l]); bx = colvec("bx", lru_bx[l]); lam = colvec("lam", lru_lambda[l])
        cw = cx.sbuf(ph, "cw", [128, 4, 8])
        with nc.allow_non_contiguous_dma(reason="tiny"):
            cx.dma("sp", cw[:], conv_w[l].rearrange("i (m p) -> p i m", p=128), reads=[], writes=[cw], sb=cw)
        el = cx.sbuf(ph, "el", [128, 8]); cA = cx.sbuf(ph, "cA", [128, 8]); cA2 = cx.sbuf(ph, "cA2", [128, 8])
        cx.op("act", lambda: S.activation(el[:], lam[:], AF.Exp, scale=-1.0), [lam], [el])
        cx.op("act", lambda: S.activation(cA[:], el[:], AF.Ln, bias=1.0, scale=1.0), [el], [cA])
        cx.op("dve", lambda: V.tensor_scalar_mul(cA2[:], cA[:], -16.0), [cA], [cA2])
        cx.op("dve", lambda: V.tensor_scalar_mul(cA[:], cA[:], -8.0), [cA], [cA])
        BDf = cx.sbuf(ph, "BDf", [128, 2, 128]); BD = [cx.sbuf(ph, "BD%d" % i, [128, 2, 128], F32R) for i in range(2)]
        cx.op("dve", lambda: V.memset(BDf[:], 0.0), [], [BDf])
        N = SEQ
        xc = [cx.sbuf(ph, "xc%d" % i, [128, 3 + N]) for i in range(2)]
        gc = [cx.sbuf(ph, "gc%d" % i, [128, N]) for i in range(2)]
        xvs = [cx.sbuf(ph, "xv%d" % i, [128, N], F32R) for i in range(2)]
        tas = [cx.sbuf(ph, "t_a%d" % i, [128, N]) for i in range(2)]
        t_b = cx.sbuf(ph, "t_b", [128, N])
        t_c = cx.sbuf(ph, "t_c", [128, N]); t_d = cx.sbuf(ph, "t_d", [128, N]); hh = cx.sbuf(ph, "hh", [128, N])
        oc = [cx.sbuf(ph, "oc%d" % i, [128, N], BF16) for i in range(2)]
        units = [(m, s) for m in range(8) for s in range(NSEQ)]

        def stage1(ui):
            m, s = units[ui]
            bd = BD[m % 2]
            if s == 0:
                for g_, wsrc in enumerate((lru_wa, lru_wx)):
                    cx.dma("sp", BDf[0:64, g_, 0:64], wsrc[l, 2 * m], reads=[], writes=[BDf], sb=BDf)
                    yield
                    cx.dma("sp", BDf[64:128, g_, 64:128], wsrc[l, 2 * m + 1], reads=[], writes=[BDf], sb=BDf)
                    yield
                cx.op("dve", lambda: V.tensor_copy(bd[:], BDf[:]), [BDf], [bd])
                yield
            x_ = xc[ui % 2]; g = gc[ui % 2]; xv = xvs[ui % 2]; t_a = tas[ui % 2]
            t0 = s * SEQ
            cx.op("pool", lambda: P.memset(x_[:, 0:3], 0.0), [], [x_])
            yield
            cx.dma("sp", x_[:, 3:3 + N], xgT[0, m * 128:(m + 1) * 128, t0:t0 + N], reads=[xgT], writes=[x_], sb=x_)
            yield
            cx.dma("sp", g[:], xgT[1, m * 128:(m + 1) * 128, t0:t0 + N], reads=[xgT], writes=[g], sb=g)
            yield
            cx.op("dve", lambda: V.tensor_scalar(t_a[:], x_[:, 0:N], cw[:, 0, m:m + 1], cb[:, m:m + 1], ALU.mult, ALU.add), [x_, cw, cb], [t_a])
            yield
            for i in (1, 2):
                cx.op("dve", lambda: V.scalar_tensor_tensor(t_a[:], x_[:, i:i + N], cw[:, i, m:m + 1], t_a[:], ALU.mult, ALU.add), [x_, cw, t_a], [t_a])
                yield
            cx.op("dve", lambda: V.scalar_tensor_tensor(t_a[:], x_[:, 3:3 + N], cw[:, 3, m:m + 1], t_a[:], ALU.mult, ALU.add), [x_, cw, t_a], [t_a])
            yield
            cx.op("act", lambda: S.copy(xv[:], t_a[:]), [t_a], [xv])
            yield
            cx.op("pool", lambda: P.tensor_tensor(t_d[:], g[:], g[:], ALU.mult), [g], [t_d])
            yield
            cx.op("pool", lambda: P.tensor_scalar(t_d[:], t_d[:], 0.044715, 1.0, ALU.mult, ALU.add), [t_d], [t_d])
            yield
            cx.op("pool", lambda: P.tensor_tensor(t_d[:], t_d[:], g[:], ALU.mult), [t_d, g], [t_d])
            yield
            cx.op("act", lambda: S.activation(t_d[:], t_d[:], AF.Sigmoid, scale=1.5957691216057308), [t_d], [t_d])
            yield
            cx.op("pool", lambda: P.tensor_tensor(g[:], t_d[:], g[:], ALU.mult), [t_d, g], [g])
            yield

        def stage2(ui):
            m, s = units[ui]
            bd = BD[m % 2]
            g = gc[ui % 2]; xv = xvs[ui % 2]; t_a = tas[ui % 2]; o_ = oc[ui % 2]
            t0 = s * SEQ
            for q in range(N // 512):
                pr = pb[6]; pi = pb[6]
                cs = slice(q * 512, (q + 1) * 512)
                cx.op("pe", lambda: nc.tensor.matmul(pr[:], bd[:, 0, :], xv[:, cs], start=True, stop=True), [bd, xv], [pr])
                yield
                cx.op("act", lambda: S.activation(t_b[:, cs], pr[:], AF.Sigmoid, bias=ba[:, m:m + 1], scale=1.0), [pr, ba], [t_b])
                cx.op("pe", lambda: nc.tensor.matmul(pi[:], bd[:, 1, :], xv[:, cs], start=True, stop=True), [bd, xv], [pi])
                yield
                cx.op("act", lambda: S.activation(t_c[:, cs], pi[:], AF.Sigmoid, bias=bx[:, m:m + 1], scale=1.0), [pi, bx], [t_c])
            cx.op("dve", lambda: V.tensor_tensor(t_c[:], t_c[:], t_a[:], ALU.mult), [t_c, t_a], [t_c])
            yield
            cx.op("act", lambda: S.activation(t_a[:], t_b[:], AF.Exp, scale=cA[:, m:m + 1]), [t_b, cA], [t_a])
            yield
            cx.op("act", lambda: S.activation(t_b[:], t_b[:], AF.Exp, scale=cA2[:, m:m + 1]), [t_b, cA2], [t_b])
            yield
            cx.op("act", lambda: S.activation(t_b[:], t_b[:], AF.Ln, bias=1.0, scale=-1.0), [t_b], [t_b])
            yield
            cx.op("act", lambda: S.activation(t_b[:], t_b[:], AF.Exp, scale=0.5), [t_b], [t_b])
            yield
            cx.op("dve", lambda: V.tensor_tensor(t_c[:], t_c[:], t_b[:], ALU.mult), [t_c, t_b], [t_c])
            cx.op("dve", lambda: V.tensor_tensor_scan(hh[:], t_a[:], t_c[:], 0.0, ALU.mult, ALU.add), [t_a, t_c], [hh])
            yield
            cx.op("dve", lambda: V.tensor_tensor(o_[:], hh[:], g[:], ALU.mult), [hh, g], [o_])
            yield
            cx.dma("sp", oT[1024 + m * 128:1024 + (m + 1) * 128, t0:t0 + N], o_[:], reads=[o_], writes=[oT], sb=o_)
            yield

        yield from stage1(0)
        for ui in range(len(units)):
            if ui + 1 < len(units):
                yield from stage1(ui + 1)
            yield from stage2(ui)

    def phase_mix(l):
        ph = ExitStack()
        def attn():
            yield from gen_fox(ph)
            yield from gen_sb(ph)
        ga = attn(); gr = gen_lru(ph, l)
        done_a = done_r = False
        while not (done_a and done_r):
            for _ in range(2):
                if not done_a:
                    try:
                        next(ga)
                    except StopIteration:
                        done_a = True
            if not done_r:
                try:
                    next(gr)
                except StopIteration:
                    done_r = True
        cx.end_phase()
        ph.close()

    def ln_tile(ph_bufs, y_src_bufs, y_ap_halves, x_t, gtile, lg, lb, dst_ap, dst_buf, k):
        tt_, st6, mv, rs, res_ = ph_bufs
        t = tt_[k % 2]; r = res_[k % 2]; s6 = st6[k % 2]; mv_ = mv[k % 2]; rs_ = rs[k % 2]
        for hf_ in range(2):
            cs = slice(hf_ * 512, (hf_ + 1) * 512)
            cx.op("dve", lambda: V.tensor_tensor(t[:, cs], y_ap_halves[hf_], gtile[:, cs], ALU.mult), list(y_src_bufs) + [gtile], [t])
        cx.op("dve", lambda: V.scalar_tensor_tensor(t[:], x_t[:], ALPHA, t[:], ALU.mult, ALU.add), [x_t, t], [t])
        for hf_ in range(2):
            cx.op("dve", lambda: V.bn_stats(s6[:, hf_, :], t[:, hf_ * 512:(hf_ + 1) * 512]), [t], [s6])
        cx.op("dve", lambda: V.bn_aggr(mv_[:], s6[:].rearrange("p a b -> p (a b)")), [s6], [mv_])
        cx.op("dve", lambda: V.tensor_scalar_add(rs_[:], mv_[:, 1:2], LN_EPS), [mv_], [rs_])
        cx.op("act", lambda: S.activation(rs_[:], rs_[:], AF.Ln), [rs_], [rs_])
        cx.op("act", lambda: S.activation(rs_[:], rs_[:], AF.Exp, scale=-0.5), [rs_], [rs_])
        cx.op("dve", lambda: V.tensor_scalar(t[:], t[:], mv_[:, 0:1], rs_[:, 0:1], ALU.subtract, ALU.mult), [t, mv_, rs_], [t])
        cx.op("pool", lambda: P.tensor_tensor(t[:], t[:], lg[:], ALU.mult), [t, lg], [t])
        cx.op("dve", lambda: V.tensor_tensor(r[:], t[:], lb[:], ALU.add), [t, lb], [r])
        cx.dma("sp", dst_ap, r[:], reads=[r], writes=[dst_buf], sb=r)
        return r

    def ln_bufs(ph):
        return ([cx.sbuf(ph, "lnt%d" % i, [128, D]) for i in range(2)],
                [cx.sbuf(ph, "lns%d" % i, [128, 2, 6]) for i in range(2)],
                [cx.sbuf(ph, "lnm%d" % i, [128, 2]) for i in range(2)],
                [cx.sbuf(ph, "lnr%d" % i, [128, 1]) for i in range(2)],
                [cx.sbuf(ph, "lno%d" % i, [128, D]) for i in range(2)])

    def phase_merge(l, xsrc, xdst):
        ph = ExitStack()
        mod = load_mod(ph, l, 0, ("gt",))
        lg = cx.sbuf(ph, "lg", [128, D]); lb = cx.sbuf(ph, "lb", [128, D])
        cx.dma("sp", lg[:], row_bc(ln1_g[l, :], 128), reads=[], writes=[lg], sb=lg)
        cx.dma("sp", lb[:], row_bc(ln1_b[l, :], 128), reads=[], writes=[lb], sb=lb)
        wpa = cx.sbuf(ph, "wpa", [128, 4, D], BF16); wpb = cx.sbuf(ph, "wpb", [128, 4, D], BF16)
        wpc = cx.sbuf(ph, "wpc", [128, 8, D], BF16); wo = cx.sbuf(ph, "wo", [128, 8, D], BF16)
        for t_, src in ((wpa, w_pa), (wpb, w_pb), (wpc, w_pc), (wo, w_o)):
            cx.dma("pool", None, None, reads=[], writes=[t_], sb=t_,
                   fn=lambda: P.dma_start(out=t_[:], in_=src[l].rearrange("(kc p) n -> p kc n", p=128)))
        oTc = [cx.sbuf(ph, "oTc%d" % i, [128, 16, 512], BF16) for i in range(1)]
        gg = [cx.sbuf(ph, "gg%d" % i, [128, 3, 512]) for i in range(2)]
        ta = [cx.sbuf(ph, "ta%d" % i, [128, 512]) for i in range(2)]
        tb = [cx.sbuf(ph, "tb%d" % i, [128, 512]) for i in range(2)]
        mT = [cx.sbuf(ph, "mT%d" % i, [128, 8, 512], BF16) for i in range(1)]
        xt = [cx.sbuf(ph, "xt%d" % i, [128, D]) for i in range(2)]
        lnb = ln_bufs(ph)
        rs = route_setup(ph, l)
        pend = None
        gk = 0; k = 0
        for ch in range(NCH):
            s = ch // (SEQ // 512)
            tok0 = ch * 512
            oc_ = oTc[0]; mT_ = mT[0]
            cx.dma("sp", oc_[:], oT[:, tok0:tok0 + 512].rearrange("(kc p) t -> p kc t", p=128), reads=[oT], writes=[oc_], sb=oc_)
            for m in range(8):
                g_ = gg[gk % 2]; a_ = ta[gk % 2]; b_ = tb[gk % 2]; gk += 1
                cx.dma("sp", g_[:], gT[:, tok0:tok0 + 512].rearrange("(j q p) t -> q p j t", j=3, p=128)[m], reads=[gT], writes=[g_], sb=g_)
                pa, pb_, pc = pb[0 + 3 * (m % 2)], pb[1 + 3 * (m % 2)], pb[2 + 3 * (m % 2)]
                for kc in range(4):
                    cx.op("pe", lambda: nc.tensor.matmul(pa[:], wpa[:, kc, m * 128:(m + 1) * 128], oc_[:, kc, :], start=(kc == 0), stop=(kc == 3)), [wpa, oc_], [pa])
                for kc in range(4):
                    cx.op("pe", lambda: nc.tensor.matmul(pb_[:], wpb[:, kc, m * 128:(m + 1) * 128], oc_[:, 4 + kc, :], start=(kc == 0), stop=(kc == 3)), [wpb, oc_], [pb_])
                for kc in range(8):
                    cx.op("pe", lambda: nc.tensor.matmul(pc[:], wpc[:, kc, m * 128:(m + 1) * 128], oc_[:, 8 + kc, :], start=(kc == 0), stop=(kc == 7)), [wpc, oc_], [pc])
                cx.op("dve", lambda: V.tensor_tensor(a_[:], pa[:], g_[:, 0, :], ALU.mult), [pa, g_], [a_])
                cx.op("dve", lambda: V.tensor_tensor(b_[:], pb_[:], g_[:, 1, :], ALU.mult), [pb_, g_], [b_])
                cx.op("dve", lambda: V.tensor_tensor(a_[:], a_[:], b_[:], ALU.add), [a_, b_], [a_])
                cx.op("dve", lambda: V.tensor_tensor(b_[:], pc[:], g_[:, 2, :], ALU.mult), [pc, g_], [b_])
                cx.op("dve", lambda: V.tensor_tensor(mT_[:, m, :], a_[:], b_[:], ALU.add), [a_, b_], [mT_])
            for tt in range(4):
                ti = ch * 4 + tt
                x_t = xt[k % 2]
                cx.dma("sp", x_t[:], xsrc[ti * 128:(ti + 1) * 128, :], reads=[xsrc], writes=[x_t], sb=x_t)
                ys = []
                for hf_ in range(2):
                    yb = pb[(k * 2 + hf_) % 6]
                    for m in range(8):
                        cx.op("pe", lambda: nc.tensor.matmul(yb[:], mT_[:, m, tt * 128:(tt + 1) * 128], wo[:, m, hf_ * 512:(hf_ + 1) * 512], start=(m == 0), stop=(m == 7)),
                              [mT_, wo], [yb])
                    ys.append(yb)
                r_ = ln_tile(lnb, ys, [ys[0][:], ys[1][:]], x_t, mod[("gt", s)], lg, lb, xdst[ti * 128:(ti + 1) * 128, :], xdst, k)
                if pend is not None:
                    route_tile(rs, *pend)
                pend = (ti, r_)
                k += 1
        route_tile(rs, *pend)
        if "cnt" in dbg:
            cx.dma("sp", dbg["cnt"][:], rs["cnt"][:], reads=[rs["cnt"]], writes=[dbg["cnt"]], sb=rs["cnt"])
        cx.end_phase()
        ph.close()

    def route_setup(ph, l):
        rs = {}
        rs["mod"] = load_mod(ph, l, 1, ("sh", "sc"))
        wr = cx.sbuf(ph, "wr", [128, 8, NE])
        with nc.allow_non_contiguous_dma(reason="router weights 128B runs"):
            cx.dma("sp", wr[:], w_router[l].rearrange("(kc p) e -> p kc e", p=128), reads=[], writes=[wr], sb=wr)
        brt = cx.sbuf(ph, "brt", [128, NE])
        cx.dma("sp", brt[:], row_bc(b_router[l, :], 128), reads=[], writes=[brt], sb=brt)
        cnt = cx.sbuf(ph, "cnt", [128, NE]); cx.op("dve", lambda: V.memset(cnt[:], 0.0), [], [cnt])
        rs.update(wr=wr, brt=brt, cnt=cnt)
        rs["hf"] = [cx.sbuf(ph, "rhf%d" % i, [128, D]) for i in range(2)]
        rs["hb"] = [cx.sbuf(ph, "rhb%d" % i, [128, D], BF16) for i in range(2)]
        rs["hT"] = [cx.sbuf(ph, "rhT%d" % i, [128, 8, 128]) for i in range(2)]
        def sm(name, w=NE):
            return [cx.sbuf(ph, "%s%d" % (name, i), [128, w]) for i in range(2)]
        for nm, w in (("lgt", NE), ("m8", 8), ("msk", NE), ("ex", NE), ("em", NE), ("ssum", 1), ("gte", NE), ("slv", NE),
                      ("s8", 8), ("nmx", 1), ("tmp", NE)):
            rs[nm] = sm(nm, w)
        return rs

    def route_tile(rs, ti, x_t):
        mod = rs["mod"]; wr = rs["wr"]; brt = rs["brt"]; cnt = rs["cnt"]
        s = ti // (SEQ // 128)
        b = ti % 2
        h_f = rs["hf"][b]; h_b = rs["hb"][b]; hT_ = rs["hT"][b]
        lgt, m8, msk, ex, em, ssum, gte, slv, s8, nmx, tmp = (rs[k] for k in ("lgt", "m8", "msk", "ex", "em", "ssum", "gte", "slv", "s8", "nmx", "tmp"))
        cx.op("dve", lambda: V.tensor_tensor(h_f[:], x_t[:], mod[("sc", s)][:], ALU.mult), [x_t, mod[("sc", s)]], [h_f])
        cx.op("dve", lambda: V.tensor_tensor(h_f[:], h_f[:], mod[("sh", s)][:], ALU.add), [h_f, mod[("sh", s)]], [h_f])
        cx.op("act", lambda: S.copy(h_b[:], h_f[:]), [h_f], [h_b])
        pt = pb[6]
        for half in range(2):
            for q in range(4):
                kc = half * 4 + q
                cx.op("pe", lambda: nc.tensor.transpose(pt[:, q * 128:(q + 1) * 128], h_f[:, kc * 128:(kc + 1) * 128], identf[:]), [h_f, identf], [pt])
            cx.op("act", lambda: S.copy(hT_[:, half * 4:half * 4 + 4, :], pt[:].rearrange("p (q t) -> p q t", q=4)), [pt], [hT_])
        pl = pb[6]
        for kc in range(8):
            cx.op("pe", lambda: nc.tensor.matmul(pl[:, 0:NE], hT_[:, kc, :], wr[:, kc, :], start=(kc == 0), stop=(kc == 7)), [hT_, wr], [pl])
        L_ = lgt[b]; M8 = m8[b]; MK = msk[b]
        cx.op("dve", lambda: V.tensor_tensor(L_[:], pl[:, 0:NE], brt[:], ALU.add), [pl, brt], [L_])
        cx.op("dve", lambda: V.max(M8[:], L_[:]), [L_], [M8])
        cx.op("dve", lambda: V.tensor_scalar(MK[:], L_[:], M8[:, 3:4], None, ALU.is_ge), [L_, M8], [MK])
        cx.op("dve", lambda: V.tensor_scalar_mul(nmx[b][:], M8[:, 0:1], -1.0), [M8], [nmx[b]])
        cx.op("act", lambda: S.activation(ex[b][:], L_[:], AF.Exp, bias=nmx[b][:, 0:1], scale=1.0), [L_, nmx[b]], [ex[b]])
        cx.op("dve", lambda: V.tensor_tensor(em[b][:], ex[b][:], MK[:], ALU.mult), [ex[b], MK], [em[b]])
        cx.op("dve", lambda: V.reduce_sum(ssum[b][:], em[b][:], mybir.AxisListType.X), [em[b]], [ssum[b]])
        cx.op("dve", lambda: V.reciprocal(ssum[b][:], ssum[b][:]), [ssum[b]], [ssum[b]])
        cx.op("dve", lambda: V.tensor_scalar_mul(gte[b][:], em[b][:], ssum[b][:, 0:1]), [em[b], ssum[b]], [gte[b]])
        pp = pb[6]
        cx.op("pe", lambda: nc.tensor.matmul(pp[:, 64:64 + NE], masks[:, M_SUT, :], MK[:], start=True, stop=True), [masks, MK], [pp])
        cx.op("pe", lambda: nc.tensor.matmul(pp[:, 128:128 + NE], masks[:, M_ONES, :], MK[:], start=True, stop=True), [masks, MK], [pp])
        SL = slv[b]
        cx.op("dve", lambda: V.tensor_tensor(SL[:], pp[:, 64:64 + NE], cnt[:], ALU.add), [pp, cnt], [SL])
        cx.op("dve", lambda: V.tensor_tensor(cnt[:], pp[:, 128:128 + NE], cnt[:], ALU.add), [pp, cnt], [cnt])
        cx.op("dve", lambda: V.tensor_tensor(SL[:], SL[:], ebase[:], ALU.add), [SL, ebase], [SL])
        cx.op("dve", lambda: V.tensor_tensor(SL[:], SL[:], MK[:], ALU.mult), [SL, MK], [SL])
        cx.op("dve", lambda: V.tensor_scalar_add(SL[:], SL[:], -1.0), [SL], [SL])
        cx.op("dve", lambda: V.max(s8[b][:], SL[:]), [SL], [s8[b]])
        cx.op("dve", lambda: V.tensor_copy(IDX[:, ti, :], s8[b][:, 0:4]), [s8[b]], [IDX])
        for k in range(4):
            cx.op("dve", lambda: V.tensor_scalar(tmp[b][:], SL[:], s8[b][:, k:k + 1], None, ALU.is_equal), [SL, s8[b]], [tmp[b]])
            cx.op("dve", lambda: V.tensor_tensor(tmp[b][:], tmp[b][:], gte[b][:], ALU.mult), [tmp[b], gte[b]], [tmp[b]])
            cx.op("dve", lambda: V.reduce_sum(GK[:, ti, k:k + 1], tmp[b][:], mybir.AxisListType.X), [tmp[b]], [GK])
        for k in range(4):
            cx.dma("pool", None, None, reads=[h_b, IDX], writes=[xbuf], sb=h_b,
                   fn=lambda: P.indirect_dma_start(out=xbuf[:], out_offset=bass.IndirectOffsetOnAxis(ap=IDX[:, ti, k:k + 1], axis=0),
                                                   in_=h_b[:], in_offset=None))

    def phase_experts(l):
        ph = ExitStack()
        wgu = [cx.sbuf(ph, "wgu%d" % i, [128, 8, 2 * D], BF16) for i in range(2)]
        wdn = [cx.sbuf(ph, "wdn%d" % i, [128, 8, D], BF16) for i in range(2)]
        bgu = [cx.sbuf(ph, "bgu%d" % i, [128, 16]) for i in range(2)]
        bdn = [cx.sbuf(ph, "bdn%d" % i, [1, D], BF16) for i in range(2)]
        xr = [cx.sbuf(ph, "xr%d" % i, [128, 4, D], BF16) for i in range(2)]
        xTs = [cx.sbuf(ph, "xT%d" % i, [128, 8, 512], BF16) for i in range(2)]
        aTs = [cx.sbuf(ph, "aT%d" % i, [128, 8, 512], BF16) for i in range(2)]
        g1 = [cx.sbuf(ph, "g1%d" % i, [128, 512]) for i in range(2)]
        sg = [cx.sbuf(ph, "sg%d" % i, [128, 512]) for i in range(2)]
        u1 = [cx.sbuf(ph, "u1%d" % i, [128, 512]) for i in range(2)]
        yt = [cx.sbuf(ph, "yt%d" % i, [128, D]) for i in range(2)]
        jk = 0; yk = 0
        chunks = [(e, sc_) for e in range(NE) for sc_ in range(CAP // 512)]

        def load_w(e):
            b = e % 2
            cx.dma("pool", None, None, reads=[], writes=[wgu[b]], sb=wgu[b],
                   fn=lambda: P.dma_start(out=wgu[b][:], in_=w_gu[l, e].rearrange("(kc p) n -> p kc n", p=128)))
            cx.dma("pool", None, None, reads=[], writes=[wdn[b]], sb=wdn[b],
                   fn=lambda: P.dma_start(out=wdn[b][:], in_=w_down[l, e].rearrange("(kc p) n -> p kc n", p=128)))
            cx.dma("pool", None, None, reads=[], writes=[bdn[b]], sb=bdn[b],
                   fn=lambda: P.dma_start(out=bdn[b][:], in_=b_down[l, e:e + 1, :]))
            with nc.allow_non_contiguous_dma(reason="tiny"):
                cx.dma("sp", bgu[b][:], b_gu[l, e].rearrange("(m p) -> p m", p=128), reads=[], writes=[bgu[b]], sb=bgu[b])

        def load_x(ci):
            e, sc_ = chunks[ci]
            r0 = e * CAP + sc_ * 512
            xr_ = xr[ci % 2]
            cx.dma("sp", xr_[:], xbuf[r0:r0 + 512, :].rearrange("(t p) d -> p t d", p=128), reads=[xbuf], writes=[xr_], sb=xr_)

        def transp(ci, t_):
            xr_ = xr[ci % 2]; xT = xTs[ci % 2]
            for kc in range(8):
                cx.op("pe", lambda: nc.tensor.transpose(pbh[:, kc * 128:(kc + 1) * 128], xr_[:, t_, kc * 128:(kc + 1) * 128], identb[:]), [xr_, identb], [pbh])
            if t_ % 2:
                cx.op("act", lambda: S.copy(xT[:, :, t_ * 128:(t_ + 1) * 128], pbh[:].rearrange("p (kc t) -> p kc t", kc=8)), [pbh], [xT])
            else:
                cx.op("dve", lambda: V.tensor_copy(xT[:, :, t_ * 128:(t_ + 1) * 128], pbh[:].rearrange("p (kc t) -> p kc t", kc=8)), [pbh], [xT])

        load_w(0)
        load_x(0)
        for t_ in range(4):
            transp(0, t_)
        for ci, (e, sc_) in enumerate(chunks):
            b = e % 2
            r0 = e * CAP + sc_ * 512
            xT = xTs[ci % 2]; aT = aTs[ci % 2]
            if sc_ == 0 and e + 1 < NE:
                load_w(e + 1)
            if ci + 1 < len(chunks):
                load_x(ci + 1)
            for j in range(8):
                pg = pb[(jk % 2) * 2]; pu = pb[(jk % 2) * 2 + 1]
                g_ = g1[jk % 2]; s_ = sg[jk % 2]; u_ = u1[jk % 2]; jk += 1
                for kc in range(8):
                    cx.op("pe", lambda: nc.tensor.matmul(pg[:], wgu[b][:, kc, j * 128:(j + 1) * 128], xT[:, kc, :], start=(kc == 0), stop=(kc == 7)), [wgu[b], xT], [pg])
                for kc in range(8):
                    cx.op("pe", lambda: nc.tensor.matmul(pu[:], wgu[b][:, kc, D + j * 128:D + (j + 1) * 128], xT[:, kc, :], start=(kc == 0), stop=(kc == 7)), [wgu[b], xT], [pu])
                cx.op("dve", lambda: V.tensor_scalar(g_[:], pg[:], bgu[b][:, j:j + 1], 7.0, ALU.add, ALU.min), [pg, bgu[b]], [g_])
                cx.op("act", lambda: S.activation(u_[:], pu[:], AF.Identity, bias=bgu[b][:, 8 + j:9 + j], scale=1.0), [pu, bgu[b]], [u_])
                cx.op("act", lambda: S.activation(s_[:], g_[:], AF.Sigmoid, scale=1.702), [g_], [s_])
                cx.op("dve", lambda: V.tensor_scalar(u_[:], u_[:], 7.0, -7.0, ALU.min, ALU.max), [u_], [u_])
                cx.op("dve", lambda: V.tensor_tensor(g_[:], g_[:], s_[:], ALU.mult), [g_, s_], [g_])
                cx.op("dve", lambda: V.scalar_tensor_tensor(aT[:, j, :], u_[:], 1.0, g_[:], ALU.add, ALU.mult), [u_, g_], [aT])
            for t_ in range(4):
                if ci + 1 < len(chunks):
                    transp(ci + 1, t_)
                y_ = yt[yk % 2]; yk += 1
                for hf_ in range(2):
                    py = pb[4 + (yk * 2 + hf_) % 3]
                    for j in range(8):
                        cx.op("pe", lambda: nc.tensor.matmul(py[:], aT[:, j, t_ * 128:(t_ + 1) * 128], wdn[b][:, j, hf_ * 512:(hf_ + 1) * 512], start=(j == 0), stop=False), [aT, wdn[b]], [py])
                    cx.op("pe", lambda: nc.tensor.matmul(py[:], onesb[0:1, :], bdn[b][0:1, hf_ * 512:(hf_ + 1) * 512], start=False, stop=True), [onesb, bdn[b]], [py])
                    if hf_:
                        cx.op("act", lambda: S.copy(y_[:, 512:1024], py[:]), [py], [y_])
                    else:
                        cx.op("dve", lambda: V.tensor_copy(y_[:, 0:512], py[:]), [py], [y_])
                cx.dma("sp", ybuf[r0 + t_ * 128:r0 + (t_ + 1) * 128, :], y_[:], reads=[y_], writes=[ybuf], sb=y_)
        cx.end_phase()
        ph.close()

    def phase_combine(l, xsrc, xdst):
        ph = ExitStack()
        mod = load_mod(ph, l, 1, ("gt",))
        lg = cx.sbuf(ph, "lg", [128, D]); lb = cx.sbuf(ph, "lb", [128, D])
        cx.dma("sp", lg[:], row_bc(ln2_g[l, :], 128), reads=[], writes=[lg], sb=lg)
        cx.dma("sp", lb[:], row_bc(ln2_b[l, :], 128), reads=[], writes=[lb], sb=lb)
        xt = [cx.sbuf(ph, "xt%d" % i, [128, D]) for i in range(2)]
        yg = [cx.sbuf(ph, "yg%d" % i, [128, D]) for i in range(12)]
        acc = [cx.sbuf(ph, "acc%d" % i, [128, D]) for i in range(2)]
        lnb = ln_bufs(ph)
        def gathers(ti):
            for k in range(4):
                y_ = yg[(ti % 3) * 4 + k]
                cx.dma("pool", None, None, reads=[ybuf, IDX], writes=[y_], sb=y_,
                       fn=lambda: P.indirect_dma_start(out=y_[:], out_offset=None, in_=ybuf[:],
                                                       in_offset=bass.IndirectOffsetOnAxis(ap=IDX[:, ti, k:k + 1], axis=0)))
        gathers(0); gathers(1)
        for ti in range(NT):
            s = ti // (SEQ // 128)
            b = ti % 2
            x_t = xt[b]; a_ = acc[b]
            cx.dma("sp", x_t[:], xsrc[ti * 128:(ti + 1) * 128, :], reads=[xsrc], writes=[x_t], sb=x_t)
            if ti + 2 < NT:
                gathers(ti + 2)
            ys = [yg[(ti % 3) * 4 + k] for k in range(4)]
            cx.op("dve", lambda: V.tensor_scalar_mul(a_[:], ys[0][:], GK[:, ti, 0:1]), [ys[0], GK], [a_])
            for k in range(1, 4):
                cx.op("dve", lambda: V.scalar_tensor_tensor(a_[:], ys[k][:], GK[:, ti, k:k + 1], a_[:], ALU.mult, ALU.add), [ys[k], GK, a_], [a_])
            ln_tile(lnb, [a_], [a_[:, 0:512], a_[:, 512:1024]], x_t, mod[("gt", s)], lg, lb, xdst[ti * 128:(ti + 1) * 128, :], xdst, ti)
        cx.end_phase()
        ph.close()

    def done(tag):
        return stop_after == tag

    phase_ada()
    cur = x_in
    finished = False
    for l in range(n_layers):
        if done("ada"):
            break
        phase_proj(l, cur)
        if done("proj"): break
        phase_fprep(l)
        phase_mix(l)
        if done("mix"): break
        x1 = xres[0]
        phase_merge(l, cur, x1)
        if done("merge"): break
        phase_experts(l)
        if done("experts"): break
        last = (l == n_layers - 1)
        x2 = out_d if last else xres[1]
        phase_combine(l, x1, x2)
        cur = x2
    for name, src in (("oT", oT), ("x1", xres[0]), ("gT", gT), ("faT", faT), ("qkT", qkT[0]), ("xbuf", xbuf), ("ybuf", ybuf)):
        if name in dbg:
            ph = ExitStack()
            d = dbg[name]
            rows, cols = d.t.shape
            cw_ = min(cols, 1024)
            tmpb = [cx.sbuf(ph, "dump%d" % i, [128, cw_], src.t.dtype if hasattr(src, "t") else src.dtype) for i in range(2)]
            tmpf = [cx.sbuf(ph, "dumpf%d" % i, [128, cw_]) for i in range(2)]
            srcb = src if hasattr(src, "t") else qkT
            i = 0
            for r0 in range(0, rows, 128):
                for c0 in range(0, cols, cw_):
                    i += 1
                    n = min(128, rows - r0)
                    tb_, tf_ = tmpb[i % 2], tmpf[i % 2]
                    cx.dma("sp", tb_[0:n, :], src[r0:r0 + n, c0:c0 + cw_], reads=[srcb], writes=[tb_], sb=tb_)
                    cx.op("dve", lambda: V.tensor_copy(tf_[0:n, :], tb_[0:n, :]), [tb_], [tf_])
                    cx.dma("sp", d[r0:r0 + n, c0:c0 + cw_], tf_[0:n, :], reads=[tf_], writes=[d], sb=tf_)
            cx.end_phase()
            ph.close()
    ph = ExitStack()
    for name in ("IDX", "GK"):
        if name in dbg:
            srcb = IDX if name == "IDX" else GK
            tmpf = cx.sbuf(ph, "dumpg" + name, [128, NT * 4])
            cx.op("dve", lambda: V.tensor_copy(tmpf[:], srcb[:].rearrange("p a b -> p (a b)")), [srcb], [tmpf])
            cx.dma("sp", dbg[name][:], tmpf[:], reads=[tmpf], writes=[dbg[name]], sb=tmpf)
    cx.end_phase()
    ph.close()
    st.close()
    return nc, cx


def make_consts():
    import ml_dtypes
    k = np.arange(128)[:, None]
    q = np.arange(128)[None, :]
    masks = np.zeros((128, 8, 128), np.float32)
    masks[:, 0, :] = (k < q)
    masks[:, 1, :] = (k > q)
    masks[:, 2, :] = 1.0
    masks[:, 3, :] = np.where(k > q, NEG, 0.0)
    masks[:, 4, :] = np.where(k >= q, NEG, 0.0)
    masks[:, 5, :] = np.where(k < q, -1.0, 0.0)
    masks[:, 6, :] = np.where(k >= q, -1.0, 0.0)
    masks[:, 7, :] = -1.0
    ebase = np.broadcast_to((np.arange(NE) * CAP + 1).astype(np.float32)[None, :], (128, NE)).copy()
    return {
        "k_identb": np.eye(128, dtype=np.float32).astype(ml_dtypes.bfloat16),
        "k_identf": np.eye(128, dtype=np.float32),
        "k_masks": masks,
        "k_ebase": ebase,
    }


WEIGHT_KEYS = ["w_ada", "b_ada", "ln1_g", "ln1_b", "w_in", "b_f", "conv_w", "conv_b", "lru_wa", "lru_ba", "lru_wx",
               "lru_bx", "lru_lambda", "w_gate", "b_gate", "w_pa", "w_pb", "w_pc", "w_o", "ln2_g", "ln2_b", "w_router",
               "b_router", "w_gu", "b_gu", "w_down", "b_down"]


def make_in_maps(inputs):
    consts = make_consts()
    x = np.ascontiguousarray(np.asarray(inputs["x"], dtype=np.float32))
    c = np.ascontiguousarray(np.asarray(inputs["c"], dtype=np.float32))
    shared = {k: np.ascontiguousarray(np.asarray(inputs[k], dtype=np.float32)) for k in WEIGHT_KEYS}
    shared.update(consts)
    in_maps = []
    for i in range(NCORES):
        m = dict(shared)
        m["x"] = x[NSEQ * i:NSEQ * (i + 1)].reshape(T, D)
        m["c"] = c[NSEQ * i:NSEQ * (i + 1)]
        in_maps.append(m)
    return in_maps


def kernel(**inputs):
    nc, cx = build_program()
    in_maps = make_in_maps(inputs)
    res = run_bass_kernel_spmd(nc, in_maps, core_ids=list(range(NCORES)))
    out = np.stack([np.asarray(r["out"]).reshape(NSEQ, SEQ, D) for r in res.results], axis=0)
    return out.reshape(NCORES * NSEQ, SEQ, D).astype(np.float32)
```

```python
from contextlib import ExitStack
import numpy as np
import concourse.bass as bass
import concourse.mybir as mybir
from concourse.bass_utils import run_bass_kernel_spmd

F32 = mybir.dt.float32
F32R = mybir.dt.float32r
BF16 = mybir.dt.bfloat16
I32 = mybir.dt.int32
U32 = mybir.dt.uint32
AF = mybir.ActivationFunctionType
ALU = mybir.AluOpType

NCORES = 8
D = 1024
SEQ = 2048
NSEQ = 2
T = NSEQ * SEQ
NT = T // 128
NCH = T // 512
DEPTH = 2
H = 8
DH = 64
D_IN = 5128
NE = 32
CAP = 1024
ALPHA = (2.0 * DEPTH) ** 0.25
LN_EPS = 1e-5
NEG = -30000.0


class Slot:
    def __init__(self, sem):
        self.sem = sem
        self.count = 0


class Buf:
    def __init__(self, name, t=None):
        self.name = name
        self.t = t
        self.w = {}
        self.r = {}
        self.ds = None

    def __getitem__(self, k):
        return self.t[k]


class EngState:
    def __init__(self, name, eng, sem):
        self.name = name
        self.eng = eng
        self.sem = sem
        self.count = 0
        self.waited = {}


class Ctx:
    def __init__(self, nc, stack, n_dma_sems=72):
        self.nc = nc
        self.stack = stack
        self.E = {}
        for name, eng in (("pe", nc.tensor), ("act", nc.scalar), ("dve", nc.vector),
                          ("pool", nc.gpsimd), ("sp", nc.sync)):
            sem = stack.enter_context(nc.semaphore("s_" + name))
            self.E[name] = EngState(name, eng, sem)
        self.free_slots = [Slot(stack.enter_context(nc.semaphore("d%d" % i))) for i in range(n_dma_sems)]
        self.used_slots = []
        self.n_ins = 0
        self.n_wait = 0
        self.uid = 0

    def sbuf(self, ph, name, shape, dtype=F32):
        self.uid += 1
        t = ph.enter_context(self.nc.sbuf_tensor("%s_%d" % (name, self.uid), list(shape), dtype))
        return Buf(name, t)

    def psum(self, name, shape, dtype=F32):
        t = self.stack.enter_context(self.nc.psum_tensor(name, list(shape), dtype))
        return Buf(name, t)

    def dram(self, name, shape, dtype=F32, kind="Internal"):
        t = self.nc.dram_tensor(name, list(shape), dtype, kind=kind)
        return Buf(name, t.ap())

    def _wait(self, E, deps):
        for sid, (sem, val) in deps.items():
            if E.waited.get(sid, 0) >= val:
                continue
            E.eng.wait_ge(sem, val)
            E.waited[sid] = val
            self.n_wait += 1

    @staticmethod
    def _merge(d, src, skip=None):
        for sid, (sem, val) in src.items():
            if skip is not None and sid == skip:
                continue
            if sid not in d or d[sid][1] < val:
                d[sid] = (sem, val)

    def op(self, en, fn, reads=(), writes=()):
        E = self.E[en]
        own = id(E.sem)
        deps = {}
        for b in reads:
            self._merge(deps, b.w, skip=own if en == "pe" else None)
        for b in writes:
            self._merge(deps, b.w, skip=own)
            self._merge(deps, b.r, skip=own)
        self._wait(E, deps)
        ins = fn()
        E.count += 1
        ins.then_inc(E.sem, 1)
        tok = (E.sem, E.count)
        for b in reads:
            b.r[own] = tok
        for b in writes:
            b.w = {own: tok}
            b.r = {}
        self.n_ins += 1
        return ins

    def dma(self, qn, out, in_, reads=(), writes=(), sb=None, fn=None):
        E = self.E[qn]
        if sb.ds is None:
            sb.ds = self.free_slots.pop()
            self.used_slots.append(sb.ds)
        ds = sb.ds
        deps = {}
        if ds.count:
            deps[id(ds.sem)] = (ds.sem, ds.count)
        for b in reads:
            self._merge(deps, b.w)
        for b in writes:
            self._merge(deps, b.w)
            self._merge(deps, b.r)
        self._wait(E, deps)
        ins = E.eng.dma_start(out=out, in_=in_) if fn is None else fn()
        ds.count += 16
        ins.then_inc(ds.sem, 16)
        tok = (ds.sem, ds.count)
        sid = id(ds.sem)
        for b in reads:
            b.r[sid] = tok
        for b in writes:
            b.w = {sid: tok}
            b.r = {}
        self.n_ins += 1
        return ins

    def cond_region(self, cond, body):
        snap_c = {n: E.count for n, E in self.E.items()}
        all_slots = self.used_slots + self.free_slots
        snap_s = {id(sl): sl.count for sl in all_slots}
        snap_w = {n: dict(E.waited) for n, E in self.E.items()}
        with self.nc.If(cond):
            body()
        with self.nc.Else():
            for n, E in self.E.items():
                d = E.count - snap_c[n]
                if d:
                    if snap_c[n]:
                        E.eng.wait_ge(E.sem, snap_c[n])
                    E.eng.sem_inc(E.sem, d)
            sp = self.E["sp"]
            for sl in self.used_slots + self.free_slots:
                d = sl.count - snap_s.get(id(sl), 0)
                if d:
                    if snap_s.get(id(sl), 0):
                        sp.eng.wait_ge(sl.sem, snap_s[id(sl)])
                    sp.eng.sem_inc(sl.sem, d)
        for n, E in self.E.items():
            E.waited = snap_w[n]

    def barrier(self, only=None):
        deps = {}
        for E in self.E.values():
            if E.count:
                deps[id(E.sem)] = (E.sem, E.count)
        for s in self.used_slots:
            if s.count:
                deps[id(s.sem)] = (s.sem, s.count)
        for name, E in self.E.items():
            if only is not None and name not in only:
                continue
            d = {k: v for k, v in deps.items() if k != id(E.sem)}
            self._wait(E, d)

    def end_phase(self):
        self.barrier()
        self.free_slots.extend(self.used_slots)
        self.used_slots = []


def build_program(n_layers=DEPTH, stop_after=None, debug=()):
    nc = bass.Bass("TRN2", target_bir_lowering=False)
    st = ExitStack()
    cx = Ctx(nc, st)
    V, S, P = nc.vector, nc.scalar, nc.gpsimd

    def din(name, shape, dtype=F32):
        return cx.dram(name, shape, dtype, kind="ExternalInput")

    x_in = din("x", [T, D]); c_in = din("c", [NSEQ, D])
    w_ada = din("w_ada", [DEPTH, D, 6 * D]); b_ada = din("b_ada", [DEPTH, 6 * D])
    ln1_g = din("ln1_g", [DEPTH, D]); ln1_b = din("ln1_b", [DEPTH, D])
    w_in = din("w_in", [DEPTH, D, D_IN]); b_f = din("b_f", [DEPTH, H])
    conv_w = din("conv_w", [DEPTH, 4, D]); conv_b = din("conv_b", [DEPTH, D])
    lru_wa = din("lru_wa", [DEPTH, 16, 64, 64]); lru_ba = din("lru_ba", [DEPTH, D])
    lru_wx = din("lru_wx", [DEPTH, 16, 64, 64]); lru_bx = din("lru_bx", [DEPTH, D])
    lru_lambda = din("lru_lambda", [DEPTH, D])
    w_gate = din("w_gate", [DEPTH, D, 3 * D]); b_gate = din("b_gate", [DEPTH, 3 * D])
    w_pa = din("w_pa", [DEPTH, 512, D]); w_pb = din("w_pb", [DEPTH, 512, D])
    w_pc = din("w_pc", [DEPTH, D, D]); w_o = din("w_o", [DEPTH, D, D])
    ln2_g = din("ln2_g", [DEPTH, D]); ln2_b = din("ln2_b", [DEPTH, D])
    w_router = din("w_router", [DEPTH, D, NE]); b_router = din("b_router", [DEPTH, NE])
    w_gu = din("w_gu", [DEPTH, NE, D, 2 * D]); b_gu = din("b_gu", [DEPTH, NE, 2 * D])
    w_down = din("w_down", [DEPTH, NE, D, D]); b_down = din("b_down", [DEPTH, NE, D])
    k_identb = din("k_identb", [128, 128], BF16)
    k_identf = din("k_identf", [128, 128])
    k_masks = din("k_masks", [128, 8, 128])
    k_ebase = din("k_ebase", [128, NE])
    out_d = cx.dram("out", [T, D], F32, kind="ExternalOutput")

    adaB = cx.dram("adaB", [DEPTH, NSEQ, 6, D])
    xres = [cx.dram("xresA", [T, D]), cx.dram("xresB", [T, D])]
    qkT = cx.dram("qkT", [4, 512, T], BF16)
    vv = cx.dram("vv", [2, T, 512], BF16)
    faT = cx.dram("faT", [H, T])
    Fd = cx.dram("Fd", [6, H, T], BF16)
    xgT = cx.dram("xgT", [2, D, T])
    gT = cx.dram("gT", [3 * D, T])
    oT = cx.dram("oT", [2 * D, T], BF16)
    xbuf = cx.dram("xbuf", [(NE + 1) * CAP, D], BF16)
    ybuf = cx.dram("ybuf", [(NE + 1) * CAP, D])
    dbg = {}
    for name, shape in debug:
        dbg[name] = cx.dram("dbg_" + name, shape, F32, kind="ExternalOutput")

    pb = [cx.psum("pb%d" % i, [128, 512]) for i in range(7)]
    pbh = cx.psum("pbh", [128, 1024], BF16)

    gl = st
    identb = cx.sbuf(gl, "identb", [128, 128], BF16)
    identf = cx.sbuf(gl, "identf", [128, 128])
    masks = cx.sbuf(gl, "masks", [128, 8, 128])
    masksb = cx.sbuf(gl, "masksb", [128, 8, 128], BF16)
    masksr = cx.sbuf(gl, "masksr", [128, 8, 128], F32R)
    ebase = cx.sbuf(gl, "ebase", [128, NE])
    IDX = cx.sbuf(gl, "IDX", [128, NT, 4], I32)
    GK = cx.sbuf(gl, "GK", [128, NT, 4])
    onesb = cx.sbuf(gl, "onesb", [128, 128], BF16)
    cx.dma("sp", identb[:], k_identb[:], reads=[k_identb], writes=[identb], sb=identb)
    cx.dma("sp", identf[:], k_identf[:], reads=[k_identf], writes=[identf], sb=identf)
    cx.dma("sp", masks[:], k_masks[:], reads=[k_masks], writes=[masks], sb=masks)
    cx.dma("sp", ebase[:], k_ebase[:], reads=[k_ebase], writes=[ebase], sb=ebase)
    cx.op("dve", lambda: V.tensor_copy(masksb[:], masks[:]), [masks], [masksb])
    cx.op("dve", lambda: V.tensor_copy(masksr[:], masks[:]), [masks], [masksr])
    cx.op("dve", lambda: V.memset(onesb[:], 1.0), [], [onesb])
    M_SUT, M_TRI, M_ONES, M_NEGC, M_NEGNS, M_NSTRICT, M_NTRII, M_NONES = range(8)

    def row_bc(ap_row, n):
        return ap_row.partition_broadcast(n)

    def phase_ada():
        ph = ExitStack()
        c_col = cx.sbuf(ph, "c_col", [128, NSEQ, 8])
        cond = cx.sbuf(ph, "cond", [128, NSEQ, 8])
        with nc.allow_non_contiguous_dma(reason="tiny transposed load of c"):
            cx.dma("sp", c_col[:], c_in.t.rearrange("s (kc p) -> p s kc", p=128), reads=[c_in], writes=[c_col], sb=c_col)
        cx.op("act", lambda: S.activation(cond[:], c_col[:], AF.Silu), [c_col], [cond])
        condB = cx.sbuf(ph, "condB", [128, NSEQ, 8, 128], BF16)
        for s in range(NSEQ):
            cx.op("dve", lambda: V.tensor_copy(condB[:, s], cond[:, s, :].unsqueeze(2).to_broadcast([128, 8, 128])),
                  [cond], [condB])
        wring = [cx.sbuf(ph, "wada%d" % i, [128, 8, 512], BF16) for i in range(3)]
        brow = [cx.sbuf(ph, "brow%d" % i, [128, 512]) for i in range(2)]
        res = [cx.sbuf(ph, "ares%d" % i, [128, 512]) for i in range(3)]
        k = 0
        for l in range(n_layers):
            for n in range(12):
                wt = wring[k % 3]; br = brow[k % 2]
                cx.dma("pool", None, None, reads=[w_ada], writes=[wt], sb=wt,
                       fn=lambda: P.dma_start(out=wt[:], in_=w_ada[l, :, n * 512:(n + 1) * 512].rearrange("(kc p) n -> p kc n", p=128)))
                cx.dma("sp", br[:], row_bc(b_ada[l, n * 512:(n + 1) * 512], 128), reads=[b_ada], writes=[br], sb=br)
                which = n // 2
                for s in range(NSEQ):
                    ps = pb[(k * NSEQ + s) % 4]
                    for kc in range(8):
                        cx.op("pe", lambda: nc.tensor.matmul(ps[:], condB[:, s, kc, :], wt[:, kc, :], start=(kc == 0), stop=(kc == 7)),
                              [condB, wt], [ps])
                    r = res[(k * NSEQ + s) % 3]
                    cx.op("dve", lambda: V.tensor_tensor(r[:], ps[:], br[:], ALU.add), [ps, br], [r])
                    if which not in (0, 3):
                        cx.op("dve", lambda: V.tensor_scalar_add(r[:], r[:], 1.0), [r], [r])
                    c0_ = (n % 2) * 512
                    cx.dma("sp", adaB[l, s, which:which + 1, c0_:c0_ + 512], r[0:1, :], reads=[r], writes=[adaB], sb=r)
                k += 1
        cx.end_phase()
        ph.close()

    def load_mod(ph, l, sub, names=("sh", "sc", "gt")):
        tiles = {}
        for s in range(NSEQ):
            for nm, idx in (("sh", 3 * sub), ("sc", 3 * sub + 1), ("gt", 3 * sub + 2)):
                if nm not in names:
                    continue
                t = cx.sbuf(ph, "mod_%s%d" % (nm, s), [128, D])
                cx.dma("sp", t[:], row_bc(adaB[l, s, idx, :], 128), reads=[adaB], writes=[t], sb=t)
                tiles[(nm, s)] = t
        return tiles

    def phase_proj(l, xsrc):
        ph = ExitStack()
        TC = 1024
        NC2 = T // TC
        TPC = TC // 128
        mod = load_mod(ph, l, 0, ("sh", "sc"))
        bgate = cx.sbuf(ph, "bgate", [128, 24])
        with nc.allow_non_contiguous_dma(reason="tiny bias relayout"):
            cx.dma("sp", bgate[:], b_gate[l].rearrange("(m p) -> p m", p=128), reads=[b_gate], writes=[bgate], sb=bgate)
        xt = [cx.sbuf(ph, "xt%d" % i, [128, D]) for i in range(TPC)]
        hf = [cx.sbuf(ph, "hf%d" % i, [128, D]) for i in range(2)]
        hb = [cx.sbuf(ph, "hb%d" % i, [128, D], BF16) for i in range(2)]
        hT = [cx.sbuf(ph, "hT%d" % i, [128, 8, TC], BF16) for i in range(2)]
        wring = [cx.sbuf(ph, "win%d" % i, [128, 8, 512], BF16) for i in range(3)]
        wfa = cx.sbuf(ph, "wfa", [128, 8, 8], BF16)
        evf = [cx.sbuf(ph, "evf%d" % i, [128, 512]) for i in range(8)]
        evb = [cx.sbuf(ph, "evb%d" % i, [128, 512], BF16) for i in range(8)]
        cx.dma("pool", None, None, reads=[w_in], writes=[wfa], sb=wfa,
               fn=lambda: P.dma_start(out=wfa[:], in_=w_in[l, :, 1536:1544].rearrange("(kc p) n -> p kc n", p=128)))
        cnt = {"wk": 0, "ek": 0, "pk": 0}
        pieces = [("qa", 0), ("ka", 512), ("va", 1024), ("qb", 1544), ("kb", 2056), ("vb", 2568),
                  ("xc0", 3080), ("xc1", 3592), ("gc0", 4104), ("gc1", 4616)] + [("g%d" % i, i * 512) for i in range(6)]

        def load_x(ch):
            for tt in range(TPC):
                ti = ch * TPC + tt
                cx.dma("sp", xt[tt][:], xsrc[ti * 128:(ti + 1) * 128, :], reads=[xsrc], writes=[xt[tt]], sb=xt[tt])

        def compute_h(ch):
            s = (ch * TC) // SEQ
            hTc = hT[ch % 2]
            for tt in range(TPC):
                x_t = xt[tt]; h_f = hf[tt % 2]; h_b = hb[tt % 2]
                cx.op("dve", lambda: V.tensor_tensor(h_f[:], x_t[:], mod[("sc", s)][:], ALU.mult), [x_t, mod[("sc", s)]], [h_f])
                cx.op("dve", lambda: V.tensor_tensor(h_b[:], h_f[:], mod[("sh", s)][:], ALU.add), [h_f, mod[("sh", s)]], [h_b])
                for kc in range(8):
                    cx.op("pe", lambda: nc.tensor.transpose(pbh[:, kc * 128:(kc + 1) * 128], h_b[:, kc * 128:(kc + 1) * 128], identb[:]),
                          [h_b, identb], [pbh])
                cx.op("act", lambda: S.copy(hTc[:, :, tt * 128:(tt + 1) * 128], pbh[:].rearrange("p (kc t) -> p kc t", kc=8)),
                      [pbh], [hTc])

        def evac(kind, ps, arg=None):
            ek = cnt["ek"]; cnt["ek"] += 1
            if kind == "bf":
                ev = evb[ek % 8]
                if ek % 2:
                    cx.op("act", lambda: S.mul(ev[:], ps[:], arg), [ps], [ev])
                else:
                    cx.op("dve", lambda: V.tensor_scalar_mul(ev[:], ps[:], arg), [ps], [ev])
            elif kind == "f":
                ev = evf[ek % 8]
                if ek % 2:
                    cx.op("act", lambda: S.copy(ev[:], ps[:]), [ps], [ev])
                else:
                    cx.op("dve", lambda: V.tensor_copy(ev[:], ps[:]), [ps], [ev])
            else:
                ev = evf[ek % 8]
                cx.op("act", lambda: S.activation(ev[:], ps[:], AF.Sigmoid, bias=bgate[:, arg:arg + 1], scale=1.0), [ps, bgate], [ev])
            return ev

        def next_ps():
            ps = pb[cnt["pk"] % 6]; cnt["pk"] += 1
            return ps

        load_x(0)
        compute_h(0)
        for ch in range(NC2):
            hTc = hT[ch % 2]
            tok0 = ch * TC
            if ch + 1 < NC2:
                load_x(ch + 1)
            for half in range(TC // 512):
                ps = next_ps()
                hs = slice(half * 512, (half + 1) * 512)
                for kc in range(8):
                    cx.op("pe", lambda: nc.tensor.matmul(ps[0:8, :], wfa[:, kc, :], hTc[:, kc, hs], start=(kc == 0), stop=(kc == 7)),
                          [wfa, hTc], [ps])
                ev = evf[cnt["ek"] % 8]; cnt["ek"] += 1
                cx.op("dve", lambda: V.tensor_copy(ev[0:8, :], ps[0:8, :]), [ps], [ev])
                cx.dma("sp", faT[:, tok0 + half * 512:tok0 + (half + 1) * 512], ev[0:8, :], reads=[ev], writes=[faT], sb=ev)
            for pi_, (nm, c0) in enumerate(pieces):
                if pi_ == 8 and ch + 1 < NC2:
                    compute_h(ch + 1)
                wt = wring[cnt["wk"] % 3]; cnt["wk"] += 1
                wsrc = (w_gate if nm[0] == "g" and nm[1].isdigit() else w_in)
                cx.dma("pool", None, None, reads=[wsrc], writes=[wt], sb=wt,
                       fn=lambda: P.dma_start(out=wt[:], in_=wsrc[l, :, c0:c0 + 512].rearrange("(kc p) n -> p kc n", p=128)))
                if nm in ("va", "vb"):
                    for tt in range(TPC):
                        ps = next_ps()
                        for kc in range(8):
                            cx.op("pe", lambda: nc.tensor.matmul(ps[:], hTc[:, kc, tt * 128:(tt + 1) * 128], wt[:, kc, :], start=(kc == 0), stop=(kc == 7)),
                                  [hTc, wt], [ps])
                        ev = evac("bf", ps, 1.0)
                        cx.dma("sp", vv[0 if nm == "va" else 1, tok0 + tt * 128:tok0 + (tt + 1) * 128, :], ev[:], reads=[ev], writes=[vv], sb=ev)
                    continue
                for m in range(4):
                    for half in range(TC // 512):
                        hs = slice(half * 512, (half + 1) * 512)
                        tk = tok0 + half * 512
                        ps = next_ps()
                        for kc in range(8):
                            cx.op("pe", lambda: nc.tensor.matmul(ps[:], wt[:, kc, m * 128:(m + 1) * 128], hTc[:, kc, hs], start=(kc == 0), stop=(kc == 7)),
                                  [wt, hTc], [ps])
                        if nm in ("qa", "ka", "qb", "kb"):
                            ev = evac("bf", ps, 0.125 if nm[0] == "q" else 1.0)
                            qi = ("qa", "ka", "qb", "kb").index(nm)
                            cx.dma("sp", qkT[qi, m * 128:(m + 1) * 128, tk:tk + 512], ev[:], reads=[ev], writes=[qkT], sb=ev)
                        elif nm[0] == "x" or nm[:2] == "gc":
                            ev = evac("f", ps)
                            r0 = int(nm[2]) * 512 + m * 128
                            cx.dma("sp", xgT[0 if nm[0] == "x" else 1, r0:r0 + 128, tk:tk + 512], ev[:], reads=[ev], writes=[xgT], sb=ev)
                        else:
                            mi = int(nm[1]) * 4 + m
                            ev = evac("g", ps, mi)
                            cx.dma("sp", gT[mi * 128:(mi + 1) * 128, tk:tk + 512], ev[:], reads=[ev], writes=[gT], sb=ev)
        cx.end_phase()
        ph.close()

    def phase_fprep(l):
        ph = ExitStack()
        bf = cx.sbuf(ph, "bf", [H, 1]); nbf = cx.sbuf(ph, "nbf", [H, 1])
        with nc.allow_non_contiguous_dma(reason="tiny"):
            cx.dma("sp", bf[:], b_f[l].rearrange("(h o) -> h o", o=1), reads=[b_f], writes=[bf], sb=bf)
        cx.op("dve", lambda: V.tensor_scalar_mul(nbf[:], bf[:], -1.0), [bf], [nbf])
        ones = cx.sbuf(ph, "ones", [H, SEQ]); cx.op("dve", lambda: V.memset(ones[:], 1.0), [], [ones])
        for s in range(NSEQ):
            fa = cx.sbuf(ph, "fa%d" % s, [H, SEQ]); e = cx.sbuf(ph, "fe%d" % s, [H, SEQ]); sp_ = cx.sbuf(ph, "fs%d" % s, [H, SEQ])
            F = cx.sbuf(ph, "F%d" % s, [H, SEQ]); r1 = cx.sbuf(ph, "r1%d" % s, [H, SEQ]); r2 = cx.sbuf(ph, "r2%d" % s, [H, SEQ])
            parts = cx.sbuf(ph, "parts%d" % s, [H, 6, SEQ], BF16)
            cx.dma("sp", fa[:], faT[:, s * SEQ:(s + 1) * SEQ], reads=[faT], writes=[fa], sb=fa)
            cx.op("act", lambda: S.activation(e[:], fa[:], AF.Exp, bias=nbf[:, 0:1], scale=-1.0), [fa, nbf], [e])
            cx.op("act", lambda: S.activation(sp_[:], e[:], AF.Ln, bias=1.0, scale=1.0), [e], [sp_])
            cx.op("dve", lambda: V.tensor_tensor_scan(F[:], ones[:], sp_[:], 0.0, ALU.mult, ALU.subtract), [ones, sp_], [F])
            cx.op("dve", lambda: V.tensor_copy(parts[:, 0, :], F[:]), [F], [parts])
            cx.op("dve", lambda: V.tensor_tensor(r1[:], F[:], parts[:, 0, :], ALU.subtract), [F, parts], [r1])
            cx.op("dve", lambda: V.tensor_copy(parts[:, 1, :], r1[:]), [r1], [parts])
            cx.op("dve", lambda: V.tensor_tensor(r2[:], r1[:], parts[:, 1, :], ALU.subtract), [r1, parts], [r2])
            cx.op("dve", lambda: V.tensor_copy(parts[:, 2, :], r2[:]), [r2], [parts])
            cx.op("dve", lambda: V.tensor_scalar_mul(parts[:, 3:6, :], parts[:, 0:3, :], -1.0), [parts], [parts])
            cx.dma("sp", Fd[:, :, s * SEQ:(s + 1) * SEQ].rearrange("v h t -> h v t"), parts[:], reads=[parts], writes=[Fd], sb=parts)
        cx.end_phase()
        ph.close()

    def gen_fox(ph):
        NB = 2
        kT = [cx.sbuf(ph, "fkT%d" % i, [70, SEQ], BF16) for i in range(NB)]
        qT = [cx.sbuf(ph, "fqT%d" % i, [70, SEQ], BF16) for i in range(NB)]
        Va = [cx.sbuf(ph, "Va%d" % i, [128, 16, 128], BF16) for i in range(NB)]
        Pt = [cx.sbuf(ph, "Pt%d" % i, [128, 512], BF16) for i in range(3)]
        rec = [cx.sbuf(ph, "rec%d" % i, [128, 512]) for i in range(2)]
        ob = [cx.sbuf(ph, "fob%d" % i, [64, 512], BF16) for i in range(2)]
        for i in range(NB):
            cx.op("dve", lambda: V.memset(kT[i][64:70, :], 1.0), [], [kT[i]])
            cx.op("dve", lambda: V.memset(qT[i][64:70, :], 1.0), [], [qT[i]])
            cx.op("dve", lambda: V.memset(Va[i][:], 1.0), [], [Va[i]])
        it = 0; pk = 0; ck = 0
        units = [(s, h) for s in range(NSEQ) for h in range(H)]

        def loads(ui):
            s, h = units[ui]
            b = ui % NB
            t0 = s * SEQ
            cx.dma("sp", qT[b][0:64, :], qkT[0, h * 64:(h + 1) * 64, t0:t0 + SEQ], reads=[qkT], writes=[qT[b]], sb=qT[b])
            cx.dma("sp", qT[b][64:67, :], Fd[0:3, h, t0:t0 + SEQ], reads=[Fd], writes=[qT[b]], sb=qT[b])
            cx.dma("sp", kT[b][0:64, :], qkT[1, h * 64:(h + 1) * 64, t0:t0 + SEQ], reads=[qkT], writes=[kT[b]], sb=kT[b])
            cx.dma("sp", kT[b][67:70, :], Fd[3:6, h, t0:t0 + SEQ], reads=[Fd], writes=[kT[b]], sb=kT[b])
            with nc.allow_non_contiguous_dma(reason="v head slice, 128B runs"):
                cx.dma("sp", Va[b][:, :, 0:64], vv[0, t0:t0 + SEQ, h * 64:(h + 1) * 64].rearrange("(j p) d -> p j d", p=128),
                       reads=[vv], writes=[Va[b]], sb=Va[b])

        loads(0); loads(1)
        steps = [(ui, c, J) for ui in range(len(units)) for c in range(4) for J in range(4 * c + 4)]
        pend = {}
        for k in range(len(steps) + 2):
            if k < len(steps):
                ui, c, J = steps[k]
                s, h = units[ui]; b = ui % NB
                q0 = c * 512
                lo = 128 * max(0, J - 4 * c)
                Sb = pb[pk % 4]; Pb = Pt[pk % 3]; pk += 1
                diag = J >= 4 * c
                cx.op("pe", lambda: nc.tensor.matmul(Sb[:, lo:512], kT[b][:, J * 128:(J + 1) * 128], qT[b][:, q0 + lo:q0 + 512], start=True, stop=not diag),
                      [kT[b], qT[b]], [Sb])
                if diag:
                    cx.op("pe", lambda: nc.tensor.matmul(Sb[:, lo:lo + 128], identb[:], masksb[:, M_NEGC, :], start=False, stop=True),
                          [identb, masksb], [Sb])
                cx.op("act", lambda: S.activation(Pb[:, lo:512], Sb[:, lo:512], AF.Exp), [Sb], [Pb])
                pend[k] = (lo, Pb)
            if k >= 2:
                ui, c, J = steps[k - 2]
                s, h = units[ui]; b = ui % NB
                lo, Pb = pend.pop(k - 2)
                nJ = 4 * c + 4
                gci = ui * 4 + c
                O = pb[4 + gci % 2]
                cx.op("pe", lambda: nc.tensor.matmul(O[:, lo:512], Va[b][:, J, :], Pb[:, lo:512], start=(J == 0), stop=(J == nJ - 1)),
                      [Va[b], Pb], [O])
                if J == nJ - 1:
                    rc = rec[gci % 2]; o_ = ob[gci % 2]
                    t0 = s * SEQ; q0 = c * 512
                    cx.op("dve", lambda: V.reciprocal(rc[64:128, :], O[64:128, :]), [O], [rc])
                    cx.op("dve", lambda: V.tensor_tensor(o_[:], O[0:64, :], rc[64:128, :], ALU.mult), [O, rc], [o_])
                    cx.dma("sp", oT[h * 64:(h + 1) * 64, t0 + q0:t0 + q0 + 512], o_[:], reads=[o_], writes=[oT], sb=o_)
                    if c == 3 and ui + 2 < len(units):
                        loads(ui + 2)
            yield

    def gen_sb(ph):
        NB = 2
        kT = [cx.sbuf(ph, "kT%d" % i, [64, SEQ], BF16) for i in range(NB)]
        qT = [cx.sbuf(ph, "qT%d" % i, [64, SEQ], BF16) for i in range(NB)]
        Vb = [cx.sbuf(ph, "Vb%d" % i, [128, 16, 64], BF16) for i in range(NB)]
        Et = [cx.sbuf(ph, "Et%d" % i, [128, 512]) for i in range(3)]
        SPt = [cx.sbuf(ph, "SPt%d" % i, [128, 512], F32R) for i in range(4)]
        R = [cx.sbuf(ph, "R%d" % i, [128, 512], F32R) for i in range(2)]
        Wt = [cx.sbuf(ph, "Wt%d" % i, [128, 512], BF16) for i in range(3)]
        ob = [cx.sbuf(ph, "ob%d" % i, [64, 512], BF16) for i in range(2)]
        Zf = cx.sbuf(ph, "Zf", [128, 512])
        cx.op("dve", lambda: V.memset(Zf[:], 0.0), [], [Zf])
        it = 0; zk = 0; ak = 0; ck = 0; k3 = 0; wk = 0
        units = [(s, h) for s in range(NSEQ) for h in range(H)]

        def loads(ui):
            s, h = units[ui]
            b = ui % NB
            t0 = s * SEQ
            cx.dma("sp", qT[b][:], qkT[2, h * 64:(h + 1) * 64, t0:t0 + SEQ], reads=[qkT], writes=[qT[b]], sb=qT[b])
            cx.dma("sp", kT[b][:], qkT[3, h * 64:(h + 1) * 64, t0:t0 + SEQ], reads=[qkT], writes=[kT[b]], sb=kT[b])
            with nc.allow_non_contiguous_dma(reason="v head slice, 128B runs"):
                cx.dma("sp", Vb[b][:], vv[1, t0:t0 + SEQ, h * 64:(h + 1) * 64].rearrange("(j p) d -> p j d", p=128),
                       reads=[vv], writes=[Vb[b]], sb=Vb[b])

        loads(0); loads(1)
        steps = [(ui, c, 4 * c + 3 - i) for ui in range(len(units)) for c in range(4) for i in range(4 * c + 4)]
        st_ = {}
        for k in range(len(steps) + 2):
            if k < len(steps):
                ui, c, J = steps[k]
                s, h = units[ui]; b = ui % NB
                gci = ui * 4 + c
                q0 = c * 512
                top = (J == 4 * c + 3)
                if top:
                    cx.op("dve", lambda: V.tensor_copy(R[gci % 2][:], Zf[:]), [Zf], [R[gci % 2]])
                lo = 128 * max(0, J - 4 * c)
                diag = J >= 4 * c
                Z = pb[zk % 2]; zk += 1
                e_ = Et[k3 % 3]; sp_ = SPt[k3 % 4]; k3 += 1
                cx.op("pe", lambda: nc.tensor.matmul(Z[:, lo:512], kT[b][:, J * 128:(J + 1) * 128], qT[b][:, q0 + lo:q0 + 512], start=True, stop=True),
                      [kT[b], qT[b]], [Z])
                cx.op("act", lambda: S.activation(e_[:, lo:512], Z[:, lo:512], AF.Exp), [Z], [e_])
                cx.op("act", lambda: S.activation(sp_[:, lo:512], e_[:, lo:512], AF.Ln, bias=1.0, scale=1.0), [e_], [sp_])
                if diag:
                    cx.op("dve", lambda: V.tensor_tensor(sp_[:, lo:lo + 128], sp_[:, lo:lo + 128].bitcast(F32), masks[:, M_SUT, :], ALU.mult), [sp_, masks], [sp_])
                st_[k] = (lo, diag, top, sp_)
            if 1 <= k <= len(steps):
                ui, c, J = steps[k - 1]
                s, h = units[ui]; b = ui % NB
                gci = ui * 4 + c
                q0 = c * 512
                Rc = R[gci % 2]
                lo, diag, top, sp_ = st_[k - 1]
                Ab = pb[2 + ak % 2]; ak += 1
                w_ = Wt[wk % 3]; wk += 1
                cx.op("pe", lambda: nc.tensor.matmul(Ab[:, lo:512], kT[b][:, J * 128:(J + 1) * 128], qT[b][:, q0 + lo:q0 + 512], start=True, stop=False),
                      [kT[b], qT[b]], [Ab])
                cx.op("pe", lambda: nc.tensor.matmul(Ab[:, lo:512], masksr[:, M_NTRII, :], sp_[:, lo:512], start=False, stop=(top and not diag)),
                      [masksr, sp_], [Ab])
                if not top:
                    cx.op("pe", lambda: nc.tensor.matmul(Ab[:, lo:512], masksr[:, M_NONES, :], Rc[:, lo:512], start=False, stop=not diag),
                          [masksr, Rc], [Ab])
                if diag:
                    cx.op("pe", lambda: nc.tensor.matmul(Ab[:, lo:lo + 128], identb[:], masksb[:, M_NEGNS, :], start=False, stop=True),
                          [identb, masksb], [Ab])
                cx.op("act", lambda: S.activation(w_[:, lo:512], Ab[:, lo:512], AF.Exp), [Ab], [w_])
                if J > 0:
                    cx.op("dve", lambda: V.tensor_tensor(Rc[:, lo:512], Rc[:, lo:512].bitcast(F32), sp_[:, lo:512].bitcast(F32), ALU.add), [Rc, sp_], [Rc])
                st_[k - 1] = (lo, diag, top, sp_, w_)
            if k >= 2:
                ui, c, J = steps[k - 2]
                s, h = units[ui]; b = ui % NB
                gci = ui * 4 + c
                O = pb[4 + gci % 2]; o_ = ob[gci % 2]
                lo, diag, top, sp_, w_ = st_.pop(k - 2)
                cx.op("pe", lambda: nc.tensor.matmul(O[0:64, lo:512], Vb[b][:, J, :], w_[:, lo:512], start=top, stop=(J == 0)),
                      [Vb[b], w_], [O])
                if J == 0:
                    t0 = s * SEQ; q0 = c * 512
                    cx.op("act", lambda: S.copy(o_[:], O[0:64, :]), [O], [o_])
                    cx.dma("sp", oT[512 + h * 64:512 + (h + 1) * 64, t0 + q0:t0 + q0 + 512], o_[:], reads=[o_], writes=[oT], sb=o_)
                    if c == 3 and ui + 2 < len(units):
                        loads(ui + 2)
            yield

    def gen_lru(ph, l):
        def colvec(name, src_row):
            t = cx.sbuf(ph, name, [128, 8])
            with nc.allow_non_contiguous_dma(reason="tiny"):
                cx.dma("sp", t[:], src_row.rearrange("(m p) -> p m", p=128), reads=[], writes=[t], sb=t)
            return t
        cb = colvec("cb", conv_b[l]); ba = colvec("ba", lru_ba[l]); bx = colvec("bx", lru_bx[l]); lam = colvec("lam", lru_lambda[l])
        cw = cx.sbuf(ph, "cw", [128, 4, 8])
        with nc.allow_non_contiguous_dma(reason="tiny"):
            cx.dma("sp", cw[:], conv_w[l].rearrange("i (m p) -> p i m", p=128), reads=[], writes=[cw], sb=cw)
        el = cx.sbuf(ph, "el", [128, 8]); cA = cx.sbuf(ph, "cA", [128, 8]); cA2 = cx.sbuf(ph, "cA2", [128, 8])
        cx.op("act", lambda: S.activation(el[:], lam[:], AF.Exp, scale=-1.0), [lam], [el])
        cx.op("act", lambda: S.activation(cA[:], el[:], AF.Ln, bias=1.0, scale=1.0), [el], [cA])
        cx.op("dve", lambda: V.tensor_scalar_mul(cA2[:], cA[:], -16.0), [cA], [cA2])
        cx.op("dve", lambda: V.tensor_scalar_mul(cA[:], cA[:], -8.0), [cA], [cA])
        BDf = cx.sbuf(ph, "BDf", [128, 2, 128]); BD = [cx.sbuf(ph, "BD%d" % i, [128, 2, 128], F32R) for i in range(2)]
        cx.op("dve", lambda: V.memset(BDf[:], 0.0), [], [BDf])
        N = SEQ
        xc = [cx.sbuf(ph, "xc%d" % i, [128, 3 + N]) for i in range(2)]
        gc = [cx.sbuf(ph, "gc%d" % i, [128, N]) for i in range(2)]
        xvs = [cx.sbuf(ph, "xv%d" % i, [128, N], F32R) for i in range(2)]
        tas = [cx.sbuf(ph, "t_a%d" % i, [128, N]) for i in range(2)]
        t_b = cx.sbuf(ph, "t_b", [128, N])
        t_c = cx.sbuf(ph, "t_c", [128, N]); t_d = cx.sbuf(ph, "t_d", [128, N]); hh = cx.sbuf(ph, "hh", [128, N])
        oc = [cx.sbuf(ph, "oc%d" % i, [128, N], BF16) for i in range(2)]
        units = [(m, s) for m in range(8) for s in range(NSEQ)]

        def stage1(ui):
            m, s = units[ui]
            bd = BD[m % 2]
            if s == 0:
                for g_, wsrc in enumerate((lru_wa, lru_wx)):
                    cx.dma("sp", BDf[0:64, g_, 0:64], wsrc[l, 2 * m], reads=[], writes=[BDf], sb=BDf)
                    yield
                    cx.dma("sp", BDf[64:128, g_, 64:128], wsrc[l, 2 * m + 1], reads=[], writes=[BDf], sb=BDf)
                    yield
                cx.op("dve", lambda: V.tensor_copy(bd[:], BDf[:]), [BDf], [bd])
                yield
            x_ = xc[ui % 2]; g = gc[ui % 2]; xv = xvs[ui % 2]; t_a = tas[ui % 2]
            t0 = s * SEQ
            cx.op("pool", lambda: P.memset(x_[:, 0:3], 0.0), [], [x_])
            yield
            cx.dma("sp", x_[:, 3:3 + N], xgT[0, m * 128:(m + 1) * 128, t0:t0 + N], reads=[xgT], writes=[x_], sb=x_)
            yield
            cx.dma("sp", g[:], xgT[1, m * 128:(m + 1) * 128, t0:t0 + N], reads=[xgT], writes=[g], sb=g)
            yield
            cx.op("dve", lambda: V.tensor_scalar(t_a[:], x_[:, 0:N], cw[:, 0, m:m + 1], cb[:, m:m + 1], ALU.mult, ALU.add), [x_, cw, cb], [t_a])
            yield
            for i in (1, 2):
                cx.op("dve", lambda: V.scalar_tensor_tensor(t_a[:], x_[:, i:i + N], cw[:, i, m:m + 1], t_a[:], ALU.mult, ALU.add), [x_, cw, t_a], [t_a])
                yield
            cx.op("dve", lambda: V.scalar_tensor_tensor(t_a[:], x_[:, 3:3 + N], cw[:, 3, m:m + 1], t_a[:], ALU.mult, ALU.add), [x_, cw, t_a], [t_a])
            yield
            cx.op("act", lambda: S.copy(xv[:], t_a[:]), [t_a], [xv])
            yield
            cx.op("pool", lambda: P.tensor_tensor(t_d[:], g[:], g[:], ALU.mult), [g], [t_d])
            yield
            cx.op("pool", lambda: P.tensor_scalar(t_d[:], t_d[:], 0.044715, 1.0, ALU.mult, ALU.add), [t_d], [t_d])
            yield
            cx.op("pool", lambda: P.tensor_tensor(t_d[:], t_d[:], g[:], ALU.mult), [t_d, g], [t_d])
            yield
            cx.op("act", lambda: S.activation(t_d[:], t_d[:], AF.Sigmoid, scale=1.5957691216057308), [t_d], [t_d])
            yield
            cx.op("pool", lambda: P.tensor_tensor(g[:], t_d[:], g[:], ALU.mult), [t_d, g], [g])
            yield

        def stage2(ui):
            m, s = units[ui]
            bd = BD[m % 2]
            g = gc[ui % 2]; xv = xvs[ui % 2]; t_a = tas[ui % 2]; o_ = oc[ui % 2]
            t0 = s * SEQ
            for q in range(N // 512):
                pr = pb[6]; pi = pb[6]
                cs = slice(q * 512, (q + 1) * 512)
                cx.op("pe", lambda: nc.tensor.matmul(pr[:], bd[:, 0, :], xv[:, cs], start=True, stop=True), [bd, xv], [pr])
                yield
                cx.op("act", lambda: S.activation(t_b[:, cs], pr[:], AF.Sigmoid, bias=ba[:, m:m + 1], scale=1.0), [pr, ba], [t_b])
                cx.op("pe", lambda: nc.tensor.matmul(pi[:], bd[:, 1, :], xv[:, cs], start=True, stop=True), [bd, xv], [pi])
                yield
                cx.op("act", lambda: S.activation(t_c[:, cs], pi[:], AF.Sigmoid, bias=bx[:, m:m + 1], scale=1.0), [pi, bx], [t_c])
            cx.op("dve", lambda: V.tensor_tensor(t_c[:], t_c[:], t_a[:], ALU.mult), [t_c, t_a], [t_c])
            yield
            cx.op("act", lambda: S.activation(t_a[:], t_b[:], AF.Exp, scale=cA[:, m:m + 1]), [t_b, cA], [t_a])
            yield
            cx.op("act", lambda: S.activation(t_b[:], t_b[:], AF.Exp, scale=cA2[:, m:m + 1]), [t_b, cA2], [t_b])
            yield
            cx.op("act", lambda: S.activation(t_b[:], t_b[:], AF.Ln, bias=1.0, scale=-1.0), [t_b], [t_b])
            yield
            cx.op("act", lambda: S.activation(t_b[:], t_b[:], AF.Exp, scale=0.5), [t_b], [t_b])
            yield
            cx.op("dve", lambda: V.tensor_tensor(t_c[:], t_c[:], t_b[:], ALU.mult), [t_c, t_b], [t_c])
            cx.op("dve", lambda: V.tensor_tensor_scan(hh[:], t_a[:], t_c[:], 0.0, ALU.mult, ALU.add), [t_a, t_c], [hh])
            yield
            cx.op("dve", lambda: V.tensor_tensor(o_[:], hh[:], g[:], ALU.mult), [hh, g], [o_])
            yield
            cx.dma("sp", oT[1024 + m * 128:1024 + (m + 1) * 128, t0:t0 + N], o_[:], reads=[o_], writes=[oT], sb=o_)
            yield

        yield from stage1(0)
        for ui in range(len(units)):
            if ui + 1 < len(units):
                yield from stage1(ui + 1)
            yield from stage2(ui)

    def phase_mix(l):
        ph = ExitStack()
        def attn():
            yield from gen_fox(ph)
            yield from gen_sb(ph)
        ga = attn(); gr = gen_lru(ph, l)
        done_a = done_r = False
        while not (done_a and done_r):
            for _ in range(2):
                if not done_a:
                    try:
                        next(ga)
                    except StopIteration:
                        done_a = True
            if not done_r:
                try:
                    next(gr)
                except StopIteration:
                    done_r = True
        cx.end_phase()
        ph.close()

    def ln_tile(ph_bufs, y_src_bufs, y_ap_halves, x_t, gtile, lg, lb, dst_ap, dst_buf, k):
        tt_, st6, mv, rs, res_ = ph_bufs
        t = tt_[k % 2]; r = res_[k % 4]; s6 = st6[k % 2]; mv_ = mv[k % 2]; rs_ = rs[k % 2]
        for hf_ in range(2):
            cs = slice(hf_ * 512, (hf_ + 1) * 512)
            cx.op("dve", lambda: V.tensor_tensor(t[:, cs], y_ap_halves[hf_], gtile[:, cs], ALU.mult), list(y_src_bufs) + [gtile], [t])
        cx.op("dve", lambda: V.scalar_tensor_tensor(t[:], x_t[:], ALPHA, t[:], ALU.mult, ALU.add), [x_t, t], [t])
        for hf_ in range(2):
            cx.op("dve", lambda: V.bn_stats(s6[:, hf_, :], t[:, hf_ * 512:(hf_ + 1) * 512]), [t], [s6])
        cx.op("dve", lambda: V.bn_aggr(mv_[:], s6[:].rearrange("p a b -> p (a b)")), [s6], [mv_])
        cx.op("dve", lambda: V.tensor_scalar_add(rs_[:], mv_[:, 1:2], LN_EPS), [mv_], [rs_])
        cx.op("act", lambda: S.activation(rs_[:], rs_[:], AF.Ln), [rs_], [rs_])
        cx.op("act", lambda: S.activation(rs_[:], rs_[:], AF.Exp, scale=-0.5), [rs_], [rs_])
        cx.op("dve", lambda: V.tensor_scalar(t[:], t[:], mv_[:, 0:1], rs_[:, 0:1], ALU.subtract, ALU.mult), [t, mv_, rs_], [t])
        cx.op("pool", lambda: P.tensor_tensor(t[:], t[:], lg[:], ALU.mult), [t, lg], [t])
        cx.op("dve", lambda: V.tensor_tensor(r[:], t[:], lb[:], ALU.add), [t, lb], [r])
        cx.dma("sp", dst_ap, r[:], reads=[r], writes=[dst_buf], sb=r)
        return r

    def ln_bufs(ph):
        return ([cx.sbuf(ph, "lnt%d" % i, [128, D]) for i in range(2)],
                [cx.sbuf(ph, "lns%d" % i, [128, 2, 6]) for i in range(2)],
                [cx.sbuf(ph, "lnm%d" % i, [128, 2]) for i in range(2)],
                [cx.sbuf(ph, "lnr%d" % i, [128, 1]) for i in range(2)],
                [cx.sbuf(ph, "lno%d" % i, [128, D]) for i in range(4)])

    def phase_merge(l, xsrc, xdst):
        ph = ExitStack()
        mod = load_mod(ph, l, 0, ("gt",))
        lg = cx.sbuf(ph, "lg", [128, D]); lb = cx.sbuf(ph, "lb", [128, D])
        cx.dma("sp", lg[:], row_bc(ln1_g[l, :], 128), reads=[], writes=[lg], sb=lg)
        cx.dma("sp", lb[:], row_bc(ln1_b[l, :], 128), reads=[], writes=[lb], sb=lb)
        wpa = cx.sbuf(ph, "wpa", [128, 4, D], BF16); wpb = cx.sbuf(ph, "wpb", [128, 4, D], BF16)
        wpc = cx.sbuf(ph, "wpc", [128, 8, D], BF16); wo = cx.sbuf(ph, "wo", [128, 8, D], BF16)
        for t_, src in ((wpa, w_pa), (wpb, w_pb), (wpc, w_pc), (wo, w_o)):
            cx.dma("pool", None, None, reads=[], writes=[t_], sb=t_,
                   fn=lambda: P.dma_start(out=t_[:], in_=src[l].rearrange("(kc p) n -> p kc n", p=128)))
        oTc = [cx.sbuf(ph, "oTc%d" % i, [128, 16, 512], BF16) for i in range(1)]
        gg = [cx.sbuf(ph, "gg%d" % i, [128, 3, 512]) for i in range(2)]
        ta = [cx.sbuf(ph, "ta%d" % i, [128, 512]) for i in range(2)]
        tb = [cx.sbuf(ph, "tb%d" % i, [128, 512]) for i in range(2)]
        mT = [cx.sbuf(ph, "mT%d" % i, [128, 8, 512], BF16) for i in range(1)]
        xt = [cx.sbuf(ph, "xt%d" % i, [128, D]) for i in range(2)]
        lnb = ln_bufs(ph)
        rs = route_setup(ph, l)
        hist = []
        gk = 0; k = 0
        for ch in range(NCH):
            s = ch // (SEQ // 512)
            tok0 = ch * 512
            oc_ = oTc[0]; mT_ = mT[0]
            cx.dma("sp", oc_[:], oT[:, tok0:tok0 + 512].rearrange("(kc p) t -> p kc t", p=128), reads=[oT], writes=[oc_], sb=oc_)
            for m in range(8):
                g_ = gg[gk % 2]; a_ = ta[gk % 2]; b_ = tb[gk % 2]; gk += 1
                cx.dma("sp", g_[:], gT[:, tok0:tok0 + 512].rearrange("(j q p) t -> q p j t", j=3, p=128)[m], reads=[gT], writes=[g_], sb=g_)
                pa, pb_, pc = pb[0 + 3 * (m % 2)], pb[1 + 3 * (m % 2)], pb[2 + 3 * (m % 2)]
                for kc in range(4):
                    cx.op("pe", lambda: nc.tensor.matmul(pa[:], wpa[:, kc, m * 128:(m + 1) * 128], oc_[:, kc, :], start=(kc == 0), stop=(kc == 3)), [wpa, oc_], [pa])
                for kc in range(4):
                    cx.op("pe", lambda: nc.tensor.matmul(pb_[:], wpb[:, kc, m * 128:(m + 1) * 128], oc_[:, 4 + kc, :], start=(kc == 0), stop=(kc == 3)), [wpb, oc_], [pb_])
                for kc in range(8):
                    cx.op("pe", lambda: nc.tensor.matmul(pc[:], wpc[:, kc, m * 128:(m + 1) * 128], oc_[:, 8 + kc, :], start=(kc == 0), stop=(kc == 7)), [wpc, oc_], [pc])
                cx.op("dve", lambda: V.tensor_tensor(a_[:], pa[:], g_[:, 0, :], ALU.mult), [pa, g_], [a_])
                cx.op("dve", lambda: V.tensor_tensor(b_[:], pb_[:], g_[:, 1, :], ALU.mult), [pb_, g_], [b_])
                cx.op("dve", lambda: V.tensor_tensor(a_[:], a_[:], b_[:], ALU.add), [a_, b_], [a_])
                cx.op("dve", lambda: V.tensor_tensor(b_[:], pc[:], g_[:, 2, :], ALU.mult), [pc, g_], [b_])
                cx.op("dve", lambda: V.tensor_tensor(mT_[:, m, :], a_[:], b_[:], ALU.add), [a_, b_], [mT_])
            for tt in range(4):
                ti = ch * 4 + tt
                x_t = xt[k % 2]
                cx.dma("sp", x_t[:], xsrc[ti * 128:(ti + 1) * 128, :], reads=[xsrc], writes=[x_t], sb=x_t)
                ys = []
                for hf_ in range(2):
                    yb = pb[(k * 2 + hf_) % 6]
                    for m in range(8):
                        cx.op("pe", lambda: nc.tensor.matmul(yb[:], mT_[:, m, tt * 128:(tt + 1) * 128], wo[:, m, hf_ * 512:(hf_ + 1) * 512], start=(m == 0), stop=(m == 7)),
                              [mT_, wo], [yb])
                    ys.append(yb)
                if len(hist) >= 1:
                    route_stage(rs, *hist[-1], 1)
                if len(hist) >= 2:
                    route_stage(rs, *hist[-2], 3)
                r_ = ln_tile(lnb, ys, [ys[0][:], ys[1][:]], x_t, mod[("gt", s)], lg, lb, xdst[ti * 128:(ti + 1) * 128, :], xdst, k)
                route_stage(rs, ti, r_, 0)
                if len(hist) >= 1:
                    route_stage(rs, *hist[-1], 2)
                if len(hist) >= 2:
                    route_stage(rs, *hist[-2], 4)
                hist.append((ti, r_))
                k += 1
        route_stage(rs, *hist[-1], 1)
        route_stage(rs, *hist[-2], 3)
        route_stage(rs, *hist[-1], 2)
        route_stage(rs, *hist[-2], 4)
        route_stage(rs, *hist[-1], 3)
        route_stage(rs, *hist[-1], 4)
        if "cnt" in dbg:
            cx.dma("sp", dbg["cnt"][:], rs["cnt"][:], reads=[rs["cnt"]], writes=[dbg["cnt"]], sb=rs["cnt"])
        cx.end_phase()
        ph.close()

    def route_setup(ph, l):
        rs = {}
        rs["mod"] = load_mod(ph, l, 1, ("sh", "sc"))
        wr = cx.sbuf(ph, "wr", [128, 8, NE])
        with nc.allow_non_contiguous_dma(reason="router weights 128B runs"):
            cx.dma("sp", wr[:], w_router[l].rearrange("(kc p) e -> p kc e", p=128), reads=[], writes=[wr], sb=wr)
        brt = cx.sbuf(ph, "brt", [128, NE])
        cx.dma("sp", brt[:], row_bc(b_router[l, :], 128), reads=[], writes=[brt], sb=brt)
        cnt = cx.sbuf(ph, "cnt", [128, NE]); cx.op("dve", lambda: V.memset(cnt[:], 0.0), [], [cnt])
        rs.update(wr=wr, brt=brt, cnt=cnt)
        rs["hf"] = [cx.sbuf(ph, "rhf%d" % i, [128, D]) for i in range(3)]
        rs["hb"] = [cx.sbuf(ph, "rhb%d" % i, [128, D], BF16) for i in range(3)]
        rs["hT"] = [cx.sbuf(ph, "rhT%d" % i, [128, 8, 128]) for i in range(3)]
        rs["pp"] = pbh[:].bitcast(F32)
        rs["ppb"] = pbh
        def sm(name, w=NE):
            return [cx.sbuf(ph, "%s%d" % (name, i), [128, w]) for i in range(3)]
        for nm, w in (("lgt", NE), ("m8", 8), ("msk", NE), ("ex", NE), ("em", NE), ("ssum", 1), ("gte", NE), ("slv", NE),
                      ("s8", 8), ("nmx", 1), ("tmp", NE)):
            rs[nm] = sm(nm, w)
        return rs

    def route_stage(rs, ti, x_t, stage):
        mod = rs["mod"]; wr = rs["wr"]; brt = rs["brt"]; cnt = rs["cnt"]
        s = ti // (SEQ // 128)
        b = ti % 3
        h_f = rs["hf"][b]; h_b = rs["hb"][b]; hT_ = rs["hT"][b]
        lgt, m8, msk, ex, em, ssum, gte, slv, s8, nmx, tmp = (rs[k] for k in ("lgt", "m8", "msk", "ex", "em", "ssum", "gte", "slv", "s8", "nmx", "tmp"))
        L_ = lgt[b]; M8 = m8[b]; MK = msk[b]; SL = slv[b]
        pp = rs["pp"]
        if stage == 0:
            cx.op("dve", lambda: V.tensor_tensor(h_f[:], x_t[:], mod[("sc", s)][:], ALU.mult), [x_t, mod[("sc", s)]], [h_f])
            cx.op("dve", lambda: V.tensor_tensor(h_f[:], h_f[:], mod[("sh", s)][:], ALU.add), [h_f, mod[("sh", s)]], [h_f])
            cx.op("act", lambda: S.copy(h_b[:], h_f[:]), [h_f], [h_b])
        elif stage == 1:
            pt = pb[6]
            for half in range(2):
                for q in range(4):
                    kc = half * 4 + q
                    cx.op("pe", lambda: nc.tensor.transpose(pt[:, q * 128:(q + 1) * 128], h_f[:, kc * 128:(kc + 1) * 128], identf[:]), [h_f, identf], [pt])
                cx.op("act", lambda: S.copy(hT_[:, half * 4:half * 4 + 4, :], pt[:].rearrange("p (q t) -> p q t", q=4)), [pt], [hT_])
            for kc in range(8):
                cx.op("pe", lambda: nc.tensor.matmul(pp[:, 0:NE], hT_[:, kc, :], wr[:, kc, :], start=(kc == 0), stop=(kc == 7)), [hT_, wr], [rs["ppb"]])
        elif stage == 2:
            cx.op("dve", lambda: V.tensor_tensor(L_[:], pp[:, 0:NE], brt[:], ALU.add), [rs["ppb"], brt], [L_])
            cx.op("dve", lambda: V.max(M8[:], L_[:]), [L_], [M8])
            cx.op("dve", lambda: V.tensor_scalar(MK[:], L_[:], M8[:, 3:4], None, ALU.is_ge), [L_, M8], [MK])
            cx.op("dve", lambda: V.tensor_scalar_mul(nmx[b][:], M8[:, 0:1], -1.0), [M8], [nmx[b]])
            cx.op("act", lambda: S.activation(ex[b][:], L_[:], AF.Exp, bias=nmx[b][:, 0:1], scale=1.0), [L_, nmx[b]], [ex[b]])
            cx.op("dve", lambda: V.tensor_tensor(em[b][:], ex[b][:], MK[:], ALU.mult), [ex[b], MK], [em[b]])
            cx.op("dve", lambda: V.reduce_sum(ssum[b][:], em[b][:], mybir.AxisListType.X), [em[b]], [ssum[b]])
            cx.op("dve", lambda: V.reciprocal(ssum[b][:], ssum[b][:]), [ssum[b]], [ssum[b]])
            cx.op("dve", lambda: V.tensor_scalar_mul(gte[b][:], em[b][:], ssum[b][:, 0:1]), [em[b], ssum[b]], [gte[b]])
        elif stage == 3:
            cx.op("pe", lambda: nc.tensor.matmul(pp[:, 64:64 + NE], masks[:, M_SUT, :], MK[:], start=True, stop=True), [masks, MK], [rs["ppb"]])
            cx.op("pe", lambda: nc.tensor.matmul(pp[:, 128:128 + NE], masks[:, M_ONES, :], MK[:], start=True, stop=True), [masks, MK], [rs["ppb"]])
        else:
            cx.op("dve", lambda: V.tensor_tensor(SL[:], pp[:, 64:64 + NE], cnt[:], ALU.add), [rs["ppb"], cnt], [SL])
            cx.op("dve", lambda: V.tensor_tensor(cnt[:], pp[:, 128:128 + NE], cnt[:], ALU.add), [rs["ppb"], cnt], [cnt])
            cx.op("dve", lambda: V.tensor_tensor(SL[:], SL[:], ebase[:], ALU.add), [SL, ebase], [SL])
            cx.op("dve", lambda: V.tensor_tensor(SL[:], SL[:], MK[:], ALU.mult), [SL, MK], [SL])
            cx.op("dve", lambda: V.tensor_scalar_add(SL[:], SL[:], -1.0), [SL], [SL])
            cx.op("dve", lambda: V.max(s8[b][:], SL[:]), [SL], [s8[b]])
            cx.op("dve", lambda: V.tensor_copy(IDX[:, ti, :], s8[b][:, 0:4]), [s8[b]], [IDX])
            for k in range(4):
                cx.op("dve", lambda: V.tensor_scalar(tmp[b][:], SL[:], s8[b][:, k:k + 1], None, ALU.is_equal), [SL, s8[b]], [tmp[b]])
                cx.op("dve", lambda: V.tensor_tensor(tmp[b][:], tmp[b][:], gte[b][:], ALU.mult), [tmp[b], gte[b]], [tmp[b]])
                cx.op("dve", lambda: V.reduce_sum(GK[:, ti, k:k + 1], tmp[b][:], mybir.AxisListType.X), [tmp[b]], [GK])
            for k in range(4):
                cx.dma("pool", None, None, reads=[h_b, IDX], writes=[xbuf], sb=h_b,
                       fn=lambda: P.indirect_dma_start(out=xbuf[:], out_offset=bass.IndirectOffsetOnAxis(ap=IDX[:, ti, k:k + 1], axis=0),
                                                       in_=h_b[:], in_offset=None))

    def phase_experts(l):
        ph = ExitStack()
        wgu = [cx.sbuf(ph, "wgu%d" % i, [128, 8, 2 * D], BF16) for i in range(2)]
        wdn = [cx.sbuf(ph, "wdn%d" % i, [128, 8, D], BF16) for i in range(2)]
        bgu = [cx.sbuf(ph, "bgu%d" % i, [128, 16]) for i in range(2)]
        bdn = [cx.sbuf(ph, "bdn%d" % i, [1, D], BF16) for i in range(2)]
        xr = [cx.sbuf(ph, "xr%d" % i, [128, 4, D], BF16) for i in range(2)]
        xTs = [cx.sbuf(ph, "xT%d" % i, [128, 8, 512], BF16) for i in range(2)]
        aTs = [cx.sbuf(ph, "aT%d" % i, [128, 8, 512], BF16) for i in range(2)]
        g1 = [cx.sbuf(ph, "g1%d" % i, [128, 512]) for i in range(2)]
        sg = [cx.sbuf(ph, "sg%d" % i, [128, 512]) for i in range(2)]
        u1 = [cx.sbuf(ph, "u1%d" % i, [128, 512]) for i in range(2)]
        yt = [cx.sbuf(ph, "yt%d" % i, [128, D]) for i in range(2)]
        jk = 0; yk = 0
        chunks = [(e, sc_) for e in range(NE) for sc_ in range(CAP // 512)]

        def load_w(e):
            b = e % 2
            cx.dma("pool", None, None, reads=[], writes=[wgu[b]], sb=wgu[b],
                   fn=lambda: P.dma_start(out=wgu[b][:], in_=w_gu[l, e].rearrange("(kc p) n -> p kc n", p=128)))
            cx.dma("pool", None, None, reads=[], writes=[wdn[b]], sb=wdn[b],
                   fn=lambda: P.dma_start(out=wdn[b][:], in_=w_down[l, e].rearrange("(kc p) n -> p kc n", p=128)))
            cx.dma("pool", None, None, reads=[], writes=[bdn[b]], sb=bdn[b],
                   fn=lambda: P.dma_start(out=bdn[b][:], in_=b_down[l, e:e + 1, :]))
            with nc.allow_non_contiguous_dma(reason="tiny"):
                cx.dma("sp", bgu[b][:], b_gu[l, e].rearrange("(m p) -> p m", p=128), reads=[], writes=[bgu[b]], sb=bgu[b])

        def load_x(ci):
            e, sc_ = chunks[ci]
            r0 = e * CAP + sc_ * 512
            xr_ = xr[ci % 2]
            cx.dma("sp", xr_[:], xbuf[r0:r0 + 512, :].rearrange("(t p) d -> p t d", p=128), reads=[xbuf], writes=[xr_], sb=xr_)

        def transp(ci, t_):
            xr_ = xr[ci % 2]; xT = xTs[ci % 2]
            for kc in range(8):
                cx.op("pe", lambda: nc.tensor.transpose(pbh[:, kc * 128:(kc + 1) * 128], xr_[:, t_, kc * 128:(kc + 1) * 128], identb[:]), [xr_, identb], [pbh])
            if t_ % 2:
                cx.op("act", lambda: S.copy(xT[:, :, t_ * 128:(t_ + 1) * 128], pbh[:].rearrange("p (kc t) -> p kc t", kc=8)), [pbh], [xT])
            else:
                cx.op("dve", lambda: V.tensor_copy(xT[:, :, t_ * 128:(t_ + 1) * 128], pbh[:].rearrange("p (kc t) -> p kc t", kc=8)), [pbh], [xT])

        load_w(0)
        load_x(0)
        for t_ in range(4):
            transp(0, t_)
        for ci, (e, sc_) in enumerate(chunks):
            b = e % 2
            r0 = e * CAP + sc_ * 512
            xT = xTs[ci % 2]; aT = aTs[ci % 2]
            if sc_ == 0 and e + 1 < NE:
                load_w(e + 1)
            if ci + 1 < len(chunks):
                load_x(ci + 1)
            for j in range(8):
                pg = pb[(jk % 2) * 2]; pu = pb[(jk % 2) * 2 + 1]
                g_ = g1[jk % 2]; s_ = sg[jk % 2]; u_ = u1[jk % 2]; jk += 1
                for kc in range(8):
                    cx.op("pe", lambda: nc.tensor.matmul(pg[:], wgu[b][:, kc, j * 128:(j + 1) * 128], xT[:, kc, :], start=(kc == 0), stop=(kc == 7)), [wgu[b], xT], [pg])
                for kc in range(8):
                    cx.op("pe", lambda: nc.tensor.matmul(pu[:], wgu[b][:, kc, D + j * 128:D + (j + 1) * 128], xT[:, kc, :], start=(kc == 0), stop=(kc == 7)), [wgu[b], xT], [pu])
                cx.op("dve", lambda: V.tensor_scalar(g_[:], pg[:], bgu[b][:, j:j + 1], 7.0, ALU.add, ALU.min), [pg, bgu[b]], [g_])
                cx.op("act", lambda: S.activation(u_[:], pu[:], AF.Identity, bias=bgu[b][:, 8 + j:9 + j], scale=1.0), [pu, bgu[b]], [u_])
                cx.op("act", lambda: S.activation(s_[:], g_[:], AF.Sigmoid, scale=1.702), [g_], [s_])
                cx.op("dve", lambda: V.tensor_scalar(u_[:], u_[:], 7.0, -7.0, ALU.min, ALU.max), [u_], [u_])
                cx.op("dve", lambda: V.tensor_tensor(g_[:], g_[:], s_[:], ALU.mult), [g_, s_], [g_])
                cx.op("dve", lambda: V.scalar_tensor_tensor(aT[:, j, :], u_[:], 1.0, g_[:], ALU.add, ALU.mult), [u_, g_], [aT])
            for t_ in range(4):
                if ci + 1 < len(chunks):
                    transp(ci + 1, t_)
                y_ = yt[yk % 2]; yk += 1
                for hf_ in range(2):
                    py = pb[4 + (yk * 2 + hf_) % 3]
                    for j in range(8):
                        cx.op("pe", lambda: nc.tensor.matmul(py[:], aT[:, j, t_ * 128:(t_ + 1) * 128], wdn[b][:, j, hf_ * 512:(hf_ + 1) * 512], start=(j == 0), stop=False), [aT, wdn[b]], [py])
                    cx.op("pe", lambda: nc.tensor.matmul(py[:], onesb[0:1, :], bdn[b][0:1, hf_ * 512:(hf_ + 1) * 512], start=False, stop=True), [onesb, bdn[b]], [py])
                    if hf_:
                        cx.op("act", lambda: S.copy(y_[:, 512:1024], py[:]), [py], [y_])
                    else:
                        cx.op("dve", lambda: V.tensor_copy(y_[:, 0:512], py[:]), [py], [y_])
                cx.dma("sp", ybuf[r0 + t_ * 128:r0 + (t_ + 1) * 128, :], y_[:], reads=[y_], writes=[ybuf], sb=y_)
        cx.end_phase()
        ph.close()

    def phase_combine(l, xsrc, xdst):
        ph = ExitStack()
        mod = load_mod(ph, l, 1, ("gt",))
        lg = cx.sbuf(ph, "lg", [128, D]); lb = cx.sbuf(ph, "lb", [128, D])
        cx.dma("sp", lg[:], row_bc(ln2_g[l, :], 128), reads=[], writes=[lg], sb=lg)
        cx.dma("sp", lb[:], row_bc(ln2_b[l, :], 128), reads=[], writes=[lb], sb=lb)
        xt = [cx.sbuf(ph, "xt%d" % i, [128, D]) for i in range(2)]
        yg = [cx.sbuf(ph, "yg%d" % i, [128, D]) for i in range(12)]
        acc = [cx.sbuf(ph, "acc%d" % i, [128, D]) for i in range(2)]
        lnb = ln_bufs(ph)
        def gathers(ti):
            for k in range(4):
                y_ = yg[(ti % 3) * 4 + k]
                cx.dma("pool", None, None, reads=[ybuf, IDX], writes=[y_], sb=y_,
                       fn=lambda: P.indirect_dma_start(out=y_[:], out_offset=None, in_=ybuf[:],
                                                       in_offset=bass.IndirectOffsetOnAxis(ap=IDX[:, ti, k:k + 1], axis=0)))
        gathers(0); gathers(1)
        for ti in range(NT):
            s = ti // (SEQ // 128)
            b = ti % 2
            x_t = xt[b]; a_ = acc[b]
            cx.dma("sp", x_t[:], xsrc[ti * 128:(ti + 1) * 128, :], reads=[xsrc], writes=[x_t], sb=x_t)
            if ti + 2 < NT:
                gathers(ti + 2)
            ys = [yg[(ti % 3) * 4 + k] for k in range(4)]
            cx.op("dve", lambda: V.tensor_scalar_mul(a_[:], ys[0][:], GK[:, ti, 0:1]), [ys[0], GK], [a_])
            for k in range(1, 4):
                cx.op("dve", lambda: V.scalar_tensor_tensor(a_[:], ys[k][:], GK[:, ti, k:k + 1], a_[:], ALU.mult, ALU.add), [ys[k], GK, a_], [a_])
            ln_tile(lnb, [a_], [a_[:, 0:512], a_[:, 512:1024]], x_t, mod[("gt", s)], lg, lb, xdst[ti * 128:(ti + 1) * 128, :], xdst, ti)
        cx.end_phase()
        ph.close()

    def done(tag):
        return stop_after == tag

    phase_ada()
    cur = x_in
    finished = False
    for l in range(n_layers):
        if done("ada"):
            break
        phase_proj(l, cur)
        if done("proj"): break
        phase_fprep(l)
        phase_mix(l)
        if done("mix"): break
        x1 = xres[0]
        phase_merge(l, cur, x1)
        if done("merge"): break
        phase_experts(l)
        if done("experts"): break
        last = (l == n_layers - 1)
        x2 = out_d if last else xres[1]
        phase_combine(l, x1, x2)
        cur = x2
    for name, src in (("oT", oT), ("x1", xres[0]), ("gT", gT), ("faT", faT), ("qkT", qkT[0]), ("xbuf", xbuf), ("ybuf", ybuf)):
        if name in dbg:
            ph = ExitStack()
            d = dbg[name]
            rows, cols = d.t.shape
            cw_ = min(cols, 1024)
            tmpb = [cx.sbuf(ph, "dump%d" % i, [128, cw_], src.t.dtype if hasattr(src, "t") else src.dtype) for i in range(2)]
            tmpf = [cx.sbuf(ph, "dumpf%d" % i, [128, cw_]) for i in range(2)]
            srcb = src if hasattr(src, "t") else qkT
            i = 0
            for r0 in range(0, rows, 128):
                for c0 in range(0, cols, cw_):
                    i += 1
                    n = min(128, rows - r0)
                    tb_, tf_ = tmpb[i % 2], tmpf[i % 2]
                    cx.dma("sp", tb_[0:n, :], src[r0:r0 + n, c0:c0 + cw_], reads=[srcb], writes=[tb_], sb=tb_)
                    cx.op("dve", lambda: V.tensor_copy(tf_[0:n, :], tb_[0:n, :]), [tb_], [tf_])
                    cx.dma("sp", d[r0:r0 + n, c0:c0 + cw_], tf_[0:n, :], reads=[tf_], writes=[d], sb=tf_)
            cx.end_phase()
            ph.close()
    ph = ExitStack()
    for name in ("IDX", "GK"):
        if name in dbg:
            srcb = IDX if name == "IDX" else GK
            tmpf = cx.sbuf(ph, "dumpg" + name, [128, NT * 4])
            cx.op("dve", lambda: V.tensor_copy(tmpf[:], srcb[:].rearrange("p a b -> p (a b)")), [srcb], [tmpf])
            cx.dma("sp", dbg[name][:], tmpf[:], reads=[tmpf], writes=[dbg[name]], sb=tmpf)
    cx.end_phase()
    ph.close()
    st.close()
    return nc, cx


def make_consts():
    import ml_dtypes
    k = np.arange(128)[:, None]
    q = np.arange(128)[None, :]
    masks = np.zeros((128, 8, 128), np.float32)
    masks[:, 0, :] = (k < q)
    masks[:, 1, :] = (k > q)
    masks[:, 2, :] = 1.0
    masks[:, 3, :] = np.where(k > q, NEG, 0.0)
    masks[:, 4, :] = np.where(k >= q, NEG, 0.0)
    masks[:, 5, :] = np.where(k < q, -1.0, 0.0)
    masks[:, 6, :] = np.where(k >= q, -1.0, 0.0)
    masks[:, 7, :] = -1.0
    ebase = np.broadcast_to((np.arange(NE) * CAP + 1).astype(np.float32)[None, :], (128, NE)).copy()
    return {
        "k_identb": np.eye(128, dtype=np.float32).astype(ml_dtypes.bfloat16),
        "k_identf": np.eye(128, dtype=np.float32),
        "k_masks": masks,
        "k_ebase": ebase,
    }


WEIGHT_KEYS = ["w_ada", "b_ada", "ln1_g", "ln1_b", "w_in", "b_f", "conv_w", "conv_b", "lru_wa", "lru_ba", "lru_wx",
               "lru_bx", "lru_lambda", "w_gate", "b_gate", "w_pa", "w_pb", "w_pc", "w_o", "ln2_g", "ln2_b", "w_router",
               "b_router", "w_gu", "b_gu", "w_down", "b_down"]


def make_in_maps(inputs):
    consts = make_consts()
    x = np.ascontiguousarray(np.asarray(inputs["x"], dtype=np.float32))
    c = np.ascontiguousarray(np.asarray(inputs["c"], dtype=np.float32))
    shared = {k: np.ascontiguousarray(np.asarray(inputs[k], dtype=np.float32)) for k in WEIGHT_KEYS}
    shared.update(consts)
    in_maps = []
    for i in range(NCORES):
        m = dict(shared)
        m["x"] = x[NSEQ * i:NSEQ * (i + 1)].reshape(T, D)
        m["c"] = c[NSEQ * i:NSEQ * (i + 1)]
        in_maps.append(m)
    return in_maps


def kernel(**inputs):
    nc, cx = build_program()
    in_maps = make_in_maps(inputs)
    res = run_bass_kernel_spmd(nc, in_maps, core_ids=list(range(NCORES)))
    out = np.stack([np.asarray(r["out"]).reshape(NSEQ, SEQ, D) for r in res.results], axis=0)
    return out.reshape(NCORES * NSEQ, SEQ, D).astype(np.float32)
```

```python
from contextlib import ExitStack
import numpy as np
import concourse.bass as bass
import concourse.mybir as mybir
from concourse.bass_utils import run_bass_kernel_spmd

F32 = mybir.dt.float32
F32R = mybir.dt.float32r
BF16 = mybir.dt.bfloat16
I32 = mybir.dt.int32
U32 = mybir.dt.uint32
AF = mybir.ActivationFunctionType
ALU = mybir.AluOpType

NCORES = 8
D = 1024
SEQ = 2048
NSEQ = 2
T = NSEQ * SEQ
NT = T // 128
NCH = T // 512
DEPTH = 2
H = 8
DH = 64
D_IN = 5128
NE = 32
CAP = 1024
ALPHA = (2.0 * DEPTH) ** 0.25
LN_EPS = 1e-5
NEG = -30000.0


class Slot:
    def __init__(self, sem):
        self.sem = sem
        self.count = 0


class Buf:
    def __init__(self, name, t=None):
        self.name = name
        self.t = t
        self.w = {}
        self.r = {}
        self.ds = None

    def __getitem__(self, k):
        return self.t[k]


class EngState:
    def __init__(self, name, eng, sem):
        self.name = name
        self.eng = eng
        self.sem = sem
        self.count = 0
        self.waited = {}


class Ctx:
    def __init__(self, nc, stack, n_dma_sems=72):
        self.nc = nc
        self.stack = stack
        self.E = {}
        for name, eng in (("pe", nc.tensor), ("act", nc.scalar), ("dve", nc.vector),
                          ("pool", nc.gpsimd), ("sp", nc.sync)):
            sem = stack.enter_context(nc.semaphore("s_" + name))
            self.E[name] = EngState(name, eng, sem)
        self.free_slots = [Slot(stack.enter_context(nc.semaphore("d%d" % i))) for i in range(n_dma_sems)]
        self.used_slots = []
        self.n_ins = 0
        self.n_wait = 0
        self.uid = 0

    def sbuf(self, ph, name, shape, dtype=F32):
        self.uid += 1
        t = ph.enter_context(self.nc.sbuf_tensor("%s_%d" % (name, self.uid), list(shape), dtype))
        return Buf(name, t)

    def psum(self, name, shape, dtype=F32):
        t = self.stack.enter_context(self.nc.psum_tensor(name, list(shape), dtype))
        return Buf(name, t)

    def dram(self, name, shape, dtype=F32, kind="Internal"):
        t = self.nc.dram_tensor(name, list(shape), dtype, kind=kind)
        return Buf(name, t.ap())

    def _wait(self, E, deps):
        for sid, (sem, val) in deps.items():
            if E.waited.get(sid, 0) >= val:
                continue
            E.eng.wait_ge(sem, val)
            E.waited[sid] = val
            self.n_wait += 1

    @staticmethod
    def _merge(d, src, skip=None):
        for sid, (sem, val) in src.items():
            if skip is not None and sid == skip:
                continue
            if sid not in d or d[sid][1] < val:
                d[sid] = (sem, val)

    def op(self, en, fn, reads=(), writes=()):
        E = self.E[en]
        own = id(E.sem)
        deps = {}
        for b in reads:
            self._merge(deps, b.w, skip=own if en == "pe" else None)
        for b in writes:
            self._merge(deps, b.w, skip=own)
            self._merge(deps, b.r, skip=own)
        self._wait(E, deps)
        ins = fn()
        E.count += 1
        ins.then_inc(E.sem, 1)
        tok = (E.sem, E.count)
        for b in reads:
            b.r[own] = tok
        for b in writes:
            b.w = {own: tok}
            b.r = {}
        self.n_ins += 1
        return ins

    def dma(self, qn, out, in_, reads=(), writes=(), sb=None, fn=None):
        E = self.E[qn]
        if sb.ds is None:
            sb.ds = self.free_slots.pop()
            self.used_slots.append(sb.ds)
        ds = sb.ds
        deps = {}
        if ds.count:
            deps[id(ds.sem)] = (ds.sem, ds.count)
        for b in reads:
            self._merge(deps, b.w)
        for b in writes:
            self._merge(deps, b.w)
            self._merge(deps, b.r)
        self._wait(E, deps)
        ins = E.eng.dma_start(out=out, in_=in_) if fn is None else fn()
        ds.count += 16
        ins.then_inc(ds.sem, 16)
        tok = (ds.sem, ds.count)
        sid = id(ds.sem)
        for b in reads:
            b.r[sid] = tok
        for b in writes:
            b.w = {sid: tok}
            b.r = {}
        self.n_ins += 1
        return ins

    def cond_region(self, cond, body):
        snap_c = {n: E.count for n, E in self.E.items()}
        all_slots = self.used_slots + self.free_slots
        snap_s = {id(sl): sl.count for sl in all_slots}
        snap_w = {n: dict(E.waited) for n, E in self.E.items()}
        with self.nc.If(cond):
            body()
        with self.nc.Else():
            for n, E in self.E.items():
                d = E.count - snap_c[n]
                if d:
                    if snap_c[n]:
                        E.eng.wait_ge(E.sem, snap_c[n])
                    E.eng.sem_inc(E.sem, d)
            sp = self.E["sp"]
            for sl in self.used_slots + self.free_slots:
                d = sl.count - snap_s.get(id(sl), 0)
                if d:
                    if snap_s.get(id(sl), 0):
                        sp.eng.wait_ge(sl.sem, snap_s[id(sl)])
                    sp.eng.sem_inc(sl.sem, d)
        for n, E in self.E.items():
            E.waited = snap_w[n]

    def barrier(self, only=None):
        deps = {}
        for E in self.E.values():
            if E.count:
                deps[id(E.sem)] = (E.sem, E.count)
        for s in self.used_slots:
            if s.count:
                deps[id(s.sem)] = (s.sem, s.count)
        for name, E in self.E.items():
            if only is not None and name not in only:
                continue
            d = {k: v for k, v in deps.items() if k != id(E.sem)}
            self._wait(E, d)

    def end_phase(self):
        self.barrier()
        self.free_slots.extend(self.used_slots)
        self.used_slots = []


def build_program(n_layers=DEPTH, stop_after=None, debug=()):
    nc = bass.Bass("TRN2", target_bir_lowering=False)
    st = ExitStack()
    cx = Ctx(nc, st)
    V, S, P = nc.vector, nc.scalar, nc.gpsimd

    def din(name, shape, dtype=F32):
        return cx.dram(name, shape, dtype, kind="ExternalInput")

    x_in = din("x", [T, D]); c_in = din("c", [NSEQ, D])
    w_ada = din("w_ada", [DEPTH, D, 6 * D]); b_ada = din("b_ada", [DEPTH, 6 * D])
    ln1_g = din("ln1_g", [DEPTH, D]); ln1_b = din("ln1_b", [DEPTH, D])
    w_in = din("w_in", [DEPTH, D, D_IN]); b_f = din("b_f", [DEPTH, H])
    conv_w = din("conv_w", [DEPTH, 4, D]); conv_b = din("conv_b", [DEPTH, D])
    lru_wa = din("lru_wa", [DEPTH, 16, 64, 64]); lru_ba = din("lru_ba", [DEPTH, D])
    lru_wx = din("lru_wx", [DEPTH, 16, 64, 64]); lru_bx = din("lru_bx", [DEPTH, D])
    lru_lambda = din("lru_lambda", [DEPTH, D])
    w_gate = din("w_gate", [DEPTH, D, 3 * D]); b_gate = din("b_gate", [DEPTH, 3 * D])
    w_pa = din("w_pa", [DEPTH, 512, D]); w_pb = din("w_pb", [DEPTH, 512, D])
    w_pc = din("w_pc", [DEPTH, D, D]); w_o = din("w_o", [DEPTH, D, D])
    ln2_g = din("ln2_g", [DEPTH, D]); ln2_b = din("ln2_b", [DEPTH, D])
    w_router = din("w_router", [DEPTH, D, NE]); b_router = din("b_router", [DEPTH, NE])
    w_gu = din("w_gu", [DEPTH, NE, D, 2 * D]); b_gu = din("b_gu", [DEPTH, NE, 2 * D])
    w_down = din("w_down", [DEPTH, NE, D, D]); b_down = din("b_down", [DEPTH, NE, D])
    k_identb = din("k_identb", [128, 128], BF16)
    k_identf = din("k_identf", [128, 128])
    k_masks = din("k_masks", [128, 8, 128])
    k_ebase = din("k_ebase", [128, NE])
    out_d = cx.dram("out", [T, D], F32, kind="ExternalOutput")

    adaB = cx.dram("adaB", [DEPTH, NSEQ, 6, D])
    xres = [cx.dram("xresA", [T, D]), cx.dram("xresB", [T, D])]
    qkT = cx.dram("qkT", [4, 512, T], BF16)
    vv = cx.dram("vv", [2, T, 512], BF16)
    faT = cx.dram("faT", [H, T])
    Fd = cx.dram("Fd", [6, H, T], BF16)
    xgT = cx.dram("xgT", [2, D, T])
    gT = cx.dram("gT", [3 * D, T])
    oT = cx.dram("oT", [2 * D, T], BF16)
    xbuf = cx.dram("xbuf", [(NE + 1) * CAP, D], BF16)
    ybuf = cx.dram("ybuf", [(NE + 1) * CAP, D])
    dbg = {}
    for name, shape in debug:
        dbg[name] = cx.dram("dbg_" + name, shape, F32, kind="ExternalOutput")

    pb = [cx.psum("pb%d" % i, [128, 512]) for i in range(7)]
    pbh = cx.psum("pbh", [128, 1024], BF16)

    gl = st
    identb = cx.sbuf(gl, "identb", [128, 128], BF16)
    identf = cx.sbuf(gl, "identf", [128, 128])
    masks = cx.sbuf(gl, "masks", [128, 8, 128])
    masksb = cx.sbuf(gl, "masksb", [128, 8, 128], BF16)
    masksr = cx.sbuf(gl, "masksr", [128, 8, 128], F32R)
    ebase = cx.sbuf(gl, "ebase", [128, NE])
    IDX = cx.sbuf(gl, "IDX", [128, NT, 4], I32)
    GK = cx.sbuf(gl, "GK", [128, NT, 4])
    onesb = cx.sbuf(gl, "onesb", [128, 128], BF16)
    cx.dma("sp", identb[:], k_identb[:], reads=[k_identb], writes=[identb], sb=identb)
    cx.dma("sp", identf[:], k_identf[:], reads=[k_identf], writes=[identf], sb=identf)
    cx.dma("sp", masks[:], k_masks[:], reads=[k_masks], writes=[masks], sb=masks)
    cx.dma("sp", ebase[:], k_ebase[:], reads=[k_ebase], writes=[ebase], sb=ebase)
    cx.op("dve", lambda: V.tensor_copy(masksb[:], masks[:]), [masks], [masksb])
    cx.op("dve", lambda: V.tensor_copy(masksr[:], masks[:]), [masks], [masksr])
    cx.op("dve", lambda: V.memset(onesb[:], 1.0), [], [onesb])
    M_SUT, M_TRI, M_ONES, M_NEGC, M_NEGNS, M_NSTRICT, M_NTRII, M_NONES = range(8)

    def row_bc(ap_row, n):
        return ap_row.partition_broadcast(n)

    def phase_ada():
        ph = ExitStack()
        c_col = cx.sbuf(ph, "c_col", [128, NSEQ, 8])
        cond = cx.sbuf(ph, "cond", [128, NSEQ, 8])
        with nc.allow_non_contiguous_dma(reason="tiny transposed load of c"):
            cx.dma("sp", c_col[:], c_in.t.rearrange("s (kc p) -> p s kc", p=128), reads=[c_in], writes=[c_col], sb=c_col)
        cx.op("act", lambda: S.activation(cond[:], c_col[:], AF.Silu), [c_col], [cond])
        condB = cx.sbuf(ph, "condB", [128, NSEQ, 8, 128], BF16)
        for s in range(NSEQ):
            cx.op("dve", lambda: V.tensor_copy(condB[:, s], cond[:, s, :].unsqueeze(2).to_broadcast([128, 8, 128])),
                  [cond], [condB])
        wring = [cx.sbuf(ph, "wada%d" % i, [128, 8, 512], BF16) for i in range(3)]
        brow = [cx.sbuf(ph, "brow%d" % i, [128, 512]) for i in range(2)]
        res = [cx.sbuf(ph, "ares%d" % i, [128, 512]) for i in range(3)]
        k = 0
        for l in range(n_layers):
            for n in range(12):
                wt = wring[k % 3]; br = brow[k % 2]
                cx.dma("pool", None, None, reads=[w_ada], writes=[wt], sb=wt,
                       fn=lambda: P.dma_start(out=wt[:], in_=w_ada[l, :, n * 512:(n + 1) * 512].rearrange("(kc p) n -> p kc n", p=128)))
                cx.dma("sp", br[:], row_bc(b_ada[l, n * 512:(n + 1) * 512], 128), reads=[b_ada], writes=[br], sb=br)
                which = n // 2
                for s in range(NSEQ):
                    ps = pb[(k * NSEQ + s) % 4]
                    for kc in range(8):
                        cx.op("pe", lambda: nc.tensor.matmul(ps[:], condB[:, s, kc, :], wt[:, kc, :], start=(kc == 0), stop=(kc == 7)),
                              [condB, wt], [ps])
                    r = res[(k * NSEQ + s) % 3]
                    cx.op("dve", lambda: V.tensor_tensor(r[:], ps[:], br[:], ALU.add), [ps, br], [r])
                    if which not in (0, 3):
                        cx.op("dve", lambda: V.tensor_scalar_add(r[:], r[:], 1.0), [r], [r])
                    c0_ = (n % 2) * 512
                    cx.dma("sp", adaB[l, s, which:which + 1, c0_:c0_ + 512], r[0:1, :], reads=[r], writes=[adaB], sb=r)
                k += 1
        cx.end_phase()
        ph.close()

    def load_mod(ph, l, sub, names=("sh", "sc", "gt")):
        tiles = {}
        for s in range(NSEQ):
            for nm, idx in (("sh", 3 * sub), ("sc", 3 * sub + 1), ("gt", 3 * sub + 2)):
                if nm not in names:
                    continue
                t = cx.sbuf(ph, "mod_%s%d" % (nm, s), [128, D])
                cx.dma("sp", t[:], row_bc(adaB[l, s, idx, :], 128), reads=[adaB], writes=[t], sb=t)
                tiles[(nm, s)] = t
        return tiles

    def phase_proj(l, xsrc):
        ph = ExitStack()
        TC = 1024
        NC2 = T // TC
        TPC = TC // 128
        mod = load_mod(ph, l, 0, ("sh", "sc"))
        bgate = cx.sbuf(ph, "bgate", [128, 24])
        with nc.allow_non_contiguous_dma(reason="tiny bias relayout"):
            cx.dma("sp", bgate[:], b_gate[l].rearrange("(m p) -> p m", p=128), reads=[b_gate], writes=[bgate], sb=bgate)
        xt = [cx.sbuf(ph, "xt%d" % i, [128, D]) for i in range(TPC)]
        hf = [cx.sbuf(ph, "hf%d" % i, [128, D]) for i in range(2)]
        hb = [cx.sbuf(ph, "hb%d" % i, [128, D], BF16) for i in range(2)]
        hT = [cx.sbuf(ph, "hT%d" % i, [128, 8, TC], BF16) for i in range(2)]
        wring = [cx.sbuf(ph, "win%d" % i, [128, 8, 512], BF16) for i in range(3)]
        wfa = cx.sbuf(ph, "wfa", [128, 8, 8], BF16)
        evf = [cx.sbuf(ph, "evf%d" % i, [128, 512]) for i in range(8)]
        evb = [cx.sbuf(ph, "evb%d" % i, [128, 512], BF16) for i in range(8)]
        cx.dma("pool", None, None, reads=[w_in], writes=[wfa], sb=wfa,
               fn=lambda: P.dma_start(out=wfa[:], in_=w_in[l, :, 1536:1544].rearrange("(kc p) n -> p kc n", p=128)))
        cnt = {"wk": 0, "ek": 0, "pk": 0}
        pieces = [("qa", 0), ("ka", 512), ("va", 1024), ("qb", 1544), ("kb", 2056), ("vb", 2568),
                  ("xc0", 3080), ("xc1", 3592), ("gc0", 4104), ("gc1", 4616)] + [("g%d" % i, i * 512) for i in range(6)]

        def load_x(ch):
            for tt in range(TPC):
                ti = ch * TPC + tt
                cx.dma("sp", xt[tt][:], xsrc[ti * 128:(ti + 1) * 128, :], reads=[xsrc], writes=[xt[tt]], sb=xt[tt])

        def compute_h(ch):
            s = (ch * TC) // SEQ
            hTc = hT[ch % 2]
            for tt in range(TPC):
                x_t = xt[tt]; h_f = hf[tt % 2]; h_b = hb[tt % 2]
                cx.op("dve", lambda: V.tensor_tensor(h_f[:], x_t[:], mod[("sc", s)][:], ALU.mult), [x_t, mod[("sc", s)]], [h_f])
                cx.op("dve", lambda: V.tensor_tensor(h_b[:], h_f[:], mod[("sh", s)][:], ALU.add), [h_f, mod[("sh", s)]], [h_b])
                for kc in range(8):
                    cx.op("pe", lambda: nc.tensor.transpose(pbh[:, kc * 128:(kc + 1) * 128], h_b[:, kc * 128:(kc + 1) * 128], identb[:]),
                          [h_b, identb], [pbh])
                cx.op("act", lambda: S.copy(hTc[:, :, tt * 128:(tt + 1) * 128], pbh[:].rearrange("p (kc t) -> p kc t", kc=8)),
                      [pbh], [hTc])

        def evac(kind, ps, arg=None):
            ek = cnt["ek"]; cnt["ek"] += 1
            if kind == "bf":
                ev = evb[ek % 8]
                if ek % 2:
                    cx.op("act", lambda: S.mul(ev[:], ps[:], arg), [ps], [ev])
                else:
                    cx.op("dve", lambda: V.tensor_scalar_mul(ev[:], ps[:], arg), [ps], [ev])
            elif kind == "f":
                ev = evf[ek % 8]
                if ek % 2:
                    cx.op("act", lambda: S.copy(ev[:], ps[:]), [ps], [ev])
                else:
                    cx.op("dve", lambda: V.tensor_copy(ev[:], ps[:]), [ps], [ev])
            else:
                ev = evf[ek % 8]
                cx.op("act", lambda: S.activation(ev[:], ps[:], AF.Sigmoid, bias=bgate[:, arg:arg + 1], scale=1.0), [ps, bgate], [ev])
            return ev

        def next_ps():
            ps = pb[cnt["pk"] % 6]; cnt["pk"] += 1
            return ps

        load_x(0)
        compute_h(0)
        for ch in range(NC2):
            hTc = hT[ch % 2]
            tok0 = ch * TC
            if ch + 1 < NC2:
                load_x(ch + 1)
            for half in range(TC // 512):
                ps = next_ps()
                hs = slice(half * 512, (half + 1) * 512)
                for kc in range(8):
                    cx.op("pe", lambda: nc.tensor.matmul(ps[0:8, :], wfa[:, kc, :], hTc[:, kc, hs], start=(kc == 0), stop=(kc == 7)),
                          [wfa, hTc], [ps])
                ev = evf[cnt["ek"] % 8]; cnt["ek"] += 1
                cx.op("dve", lambda: V.tensor_copy(ev[0:8, :], ps[0:8, :]), [ps], [ev])
                cx.dma("sp", faT[:, tok0 + half * 512:tok0 + (half + 1) * 512], ev[0:8, :], reads=[ev], writes=[faT], sb=ev)
            for pi_, (nm, c0) in enumerate(pieces):
                if pi_ == 8 and ch + 1 < NC2:
                    compute_h(ch + 1)
                wt = wring[cnt["wk"] % 3]; cnt["wk"] += 1
                wsrc = (w_gate if nm[0] == "g" and nm[1].isdigit() else w_in)
                cx.dma("pool", None, None, reads=[wsrc], writes=[wt], sb=wt,
                       fn=lambda: P.dma_start(out=wt[:], in_=wsrc[l, :, c0:c0 + 512].rearrange("(kc p) n -> p kc n", p=128)))
                if nm in ("va", "vb"):
                    for tt in range(TPC):
                        ps = next_ps()
                        for kc in range(8):
                            cx.op("pe", lambda: nc.tensor.matmul(ps[:], hTc[:, kc, tt * 128:(tt + 1) * 128], wt[:, kc, :], start=(kc == 0), stop=(kc == 7)),
                                  [hTc, wt], [ps])
                        ev = evac("bf", ps, 1.0)
                        cx.dma("sp", vv[0 if nm == "va" else 1, tok0 + tt * 128:tok0 + (tt + 1) * 128, :], ev[:], reads=[ev], writes=[vv], sb=ev)
                    continue
                for m in range(4):
                    for half in range(TC // 512):
                        hs = slice(half * 512, (half + 1) * 512)
                        tk = tok0 + half * 512
                        ps = next_ps()
                        for kc in range(8):
                            cx.op("pe", lambda: nc.tensor.matmul(ps[:], wt[:, kc, m * 128:(m + 1) * 128], hTc[:, kc, hs], start=(kc == 0), stop=(kc == 7)),
                                  [wt, hTc], [ps])
                        if nm in ("qa", "ka", "qb", "kb"):
                            ev = evac("bf", ps, 0.125 if nm[0] == "q" else 1.0)
                            qi = ("qa", "ka", "qb", "kb").index(nm)
                            cx.dma("sp", qkT[qi, m * 128:(m + 1) * 128, tk:tk + 512], ev[:], reads=[ev], writes=[qkT], sb=ev)
                        elif nm[0] == "x" or nm[:2] == "gc":
                            ev = evac("f", ps)
                            r0 = int(nm[2]) * 512 + m * 128
                            cx.dma("sp", xgT[0 if nm[0] == "x" else 1, r0:r0 + 128, tk:tk + 512], ev[:], reads=[ev], writes=[xgT], sb=ev)
                        else:
                            mi = int(nm[1]) * 4 + m
                            ev = evac("g", ps, mi)
                            cx.dma("sp", gT[mi * 128:(mi + 1) * 128, tk:tk + 512], ev[:], reads=[ev], writes=[gT], sb=ev)
        cx.end_phase()
        ph.close()

    def phase_fprep(l):
        ph = ExitStack()
        bf = cx.sbuf(ph, "bf", [H, 1]); nbf = cx.sbuf(ph, "nbf", [H, 1])
        with nc.allow_non_contiguous_dma(reason="tiny"):
            cx.dma("sp", bf[:], b_f[l].rearrange("(h o) -> h o", o=1), reads=[b_f], writes=[bf], sb=bf)
        cx.op("dve", lambda: V.tensor_scalar_mul(nbf[:], bf[:], -1.0), [bf], [nbf])
        ones = cx.sbuf(ph, "ones", [H, SEQ]); cx.op("dve", lambda: V.memset(ones[:], 1.0), [], [ones])
        for s in range(NSEQ):
            fa = cx.sbuf(ph, "fa%d" % s, [H, SEQ]); e = cx.sbuf(ph, "fe%d" % s, [H, SEQ]); sp_ = cx.sbuf(ph, "fs%d" % s, [H, SEQ])
            F = cx.sbuf(ph, "F%d" % s, [H, SEQ]); r1 = cx.sbuf(ph, "r1%d" % s, [H, SEQ]); r2 = cx.sbuf(ph, "r2%d" % s, [H, SEQ])
            parts = cx.sbuf(ph, "parts%d" % s, [H, 6, SEQ], BF16)
            cx.dma("sp", fa[:], faT[:, s * SEQ:(s + 1) * SEQ], reads=[faT], writes=[fa], sb=fa)
            cx.op("act", lambda: S.activation(e[:], fa[:], AF.Exp, bias=nbf[:, 0:1], scale=-1.0), [fa, nbf], [e])
            cx.op("act", lambda: S.activation(sp_[:], e[:], AF.Ln, bias=1.0, scale=1.0), [e], [sp_])
            cx.op("dve", lambda: V.tensor_tensor_scan(F[:], ones[:], sp_[:], 0.0, ALU.mult, ALU.subtract), [ones, sp_], [F])
            cx.op("dve", lambda: V.tensor_copy(parts[:, 0, :], F[:]), [F], [parts])
            cx.op("dve", lambda: V.tensor_tensor(r1[:], F[:], parts[:, 0, :], ALU.subtract), [F, parts], [r1])
            cx.op("dve", lambda: V.tensor_copy(parts[:, 1, :], r1[:]), [r1], [parts])
            cx.op("dve", lambda: V.tensor_tensor(r2[:], r1[:], parts[:, 1, :], ALU.subtract), [r1, parts], [r2])
            cx.op("dve", lambda: V.tensor_copy(parts[:, 2, :], r2[:]), [r2], [parts])
            cx.op("dve", lambda: V.tensor_scalar_mul(parts[:, 3:6, :], parts[:, 0:3, :], -1.0), [parts], [parts])
            cx.dma("sp", Fd[:, :, s * SEQ:(s + 1) * SEQ].rearrange("v h t -> h v t"), parts[:], reads=[parts], writes=[Fd], sb=parts)
        cx.end_phase()
        ph.close()

    def gen_fox(ph):
        NB = 2
        kT = [cx.sbuf(ph, "fkT%d" % i, [70, SEQ], BF16) for i in range(NB)]
        qT = [cx.sbuf(ph, "fqT%d" % i, [70, SEQ], BF16) for i in range(NB)]
        Va = [cx.sbuf(ph, "Va%d" % i, [128, 16, 128], BF16) for i in range(NB)]
        Pt = [cx.sbuf(ph, "Pt%d" % i, [128, 512], BF16) for i in range(3)]
        rec = [cx.sbuf(ph, "rec%d" % i, [128, 512]) for i in range(2)]
        ob = [cx.sbuf(ph, "fob%d" % i, [64, 512], BF16) for i in range(2)]
        for i in range(NB):
            cx.op("dve", lambda: V.memset(kT[i][64:70, :], 1.0), [], [kT[i]])
            cx.op("dve", lambda: V.memset(qT[i][64:70, :], 1.0), [], [qT[i]])
            cx.op("dve", lambda: V.memset(Va[i][:], 1.0), [], [Va[i]])
        it = 0; pk = 0; ck = 0
        units = [(s, h) for s in range(NSEQ) for h in range(H)]

        def loads(ui):
            s, h = units[ui]
            b = ui % NB
            t0 = s * SEQ
            cx.dma("sp", qT[b][0:64, :], qkT[0, h * 64:(h + 1) * 64, t0:t0 + SEQ], reads=[qkT], writes=[qT[b]], sb=qT[b])
            cx.dma("sp", qT[b][64:67, :], Fd[0:3, h, t0:t0 + SEQ], reads=[Fd], writes=[qT[b]], sb=qT[b])
            cx.dma("sp", kT[b][0:64, :], qkT[1, h * 64:(h + 1) * 64, t0:t0 + SEQ], reads=[qkT], writes=[kT[b]], sb=kT[b])
            cx.dma("sp", kT[b][67:70, :], Fd[3:6, h, t0:t0 + SEQ], reads=[Fd], writes=[kT[b]], sb=kT[b])
            with nc.allow_non_contiguous_dma(reason="v head slice, 128B runs"):
                cx.dma("sp", Va[b][:, :, 0:64], vv[0, t0:t0 + SEQ, h * 64:(h + 1) * 64].rearrange("(j p) d -> p j d", p=128),
                       reads=[vv], writes=[Va[b]], sb=Va[b])

        loads(0); loads(1)
        steps = [(ui, c, J) for ui in range(len(units)) for c in range(4) for J in range(4 * c + 4)]
        pend = {}
        for k in range(len(steps) + 2):
            if k < len(steps):
                ui, c, J = steps[k]
                s, h = units[ui]; b = ui % NB
                q0 = c * 512
                lo = 128 * max(0, J - 4 * c)
                Sb = pb[pk % 4]; Pb = Pt[pk % 3]; pk += 1
                diag = J >= 4 * c
                cx.op("pe", lambda: nc.tensor.matmul(Sb[:, lo:512], kT[b][:, J * 128:(J + 1) * 128], qT[b][:, q0 + lo:q0 + 512], start=True, stop=not diag),
                      [kT[b], qT[b]], [Sb])
                if diag:
                    cx.op("pe", lambda: nc.tensor.matmul(Sb[:, lo:lo + 128], identb[:], masksb[:, M_NEGC, :], start=False, stop=True),
                          [identb, masksb], [Sb])
                cx.op("act", lambda: S.activation(Pb[:, lo:512], Sb[:, lo:512], AF.Exp), [Sb], [Pb])
                pend[k] = (lo, Pb)
            if k >= 2:
                ui, c, J = steps[k - 2]
                s, h = units[ui]; b = ui % NB
                lo, Pb = pend.pop(k - 2)
                nJ = 4 * c + 4
                gci = ui * 4 + c
                O = pb[4 + gci % 2]
                cx.op("pe", lambda: nc.tensor.matmul(O[:, lo:512], Va[b][:, J, :], Pb[:, lo:512], start=(J == 0), stop=(J == nJ - 1)),
                      [Va[b], Pb], [O])
                if J == nJ - 1:
                    rc = rec[gci % 2]; o_ = ob[gci % 2]
                    t0 = s * SEQ; q0 = c * 512
                    cx.op("dve", lambda: V.reciprocal(rc[64:128, :], O[64:128, :]), [O], [rc])
                    cx.op("dve", lambda: V.tensor_tensor(o_[:], O[0:64, :], rc[64:128, :], ALU.mult), [O, rc], [o_])
                    cx.dma("sp", oT[h * 64:(h + 1) * 64, t0 + q0:t0 + q0 + 512], o_[:], reads=[o_], writes=[oT], sb=o_)
                    if c == 3 and ui + 2 < len(units):
                        loads(ui + 2)
            yield

    def gen_sb(ph):
        NB = 2
        kT = [cx.sbuf(ph, "kT%d" % i, [64, SEQ], BF16) for i in range(NB)]
        qT = [cx.sbuf(ph, "qT%d" % i, [64, SEQ], BF16) for i in range(NB)]
        Vb = [cx.sbuf(ph, "Vb%d" % i, [128, 16, 64], BF16) for i in range(NB)]
        Et = [cx.sbuf(ph, "Et%d" % i, [128, 512]) for i in range(3)]
        SPt = [cx.sbuf(ph, "SPt%d" % i, [128, 512], F32R) for i in range(4)]
        R = [cx.sbuf(ph, "R%d" % i, [128, 512], F32R) for i in range(2)]
        Wt = [cx.sbuf(ph, "Wt%d" % i, [128, 512], BF16) for i in range(3)]
        ob = [cx.sbuf(ph, "ob%d" % i, [64, 512], BF16) for i in range(2)]
        Zf = cx.sbuf(ph, "Zf", [128, 512])
        cx.op("dve", lambda: V.memset(Zf[:], 0.0), [], [Zf])
        it = 0; zk = 0; ak = 0; ck = 0; k3 = 0; wk = 0
        units = [(s, h) for s in range(NSEQ) for h in range(H)]

        def loads(ui):
            s, h = units[ui]
            b = ui % NB
            t0 = s * SEQ
            cx.dma("sp", qT[b][:], qkT[2, h * 64:(h + 1) * 64, t0:t0 + SEQ], reads=[qkT], writes=[qT[b]], sb=qT[b])
            cx.dma("sp", kT[b][:], qkT[3, h * 64:(h + 1) * 64, t0:t0 + SEQ], reads=[qkT], writes=[kT[b]], sb=kT[b])
            with nc.allow_non_contiguous_dma(reason="v head slice, 128B runs"):
                cx.dma("sp", Vb[b][:], vv[1, t0:t0 + SEQ, h * 64:(h + 1) * 64].rearrange("(j p) d -> p j d", p=128),
                       reads=[vv], writes=[Vb[b]], sb=Vb[b])

        loads(0); loads(1)
        steps = [(ui, c, 4 * c + 3 - i) for ui in range(len(units)) for c in range(4) for i in range(4 * c + 4)]
        st_ = {}
        for k in range(len(steps) + 2):
            if k < len(steps):
                ui, c, J = steps[k]
                s, h = units[ui]; b = ui % NB
                gci = ui * 4 + c
                q0 = c * 512
                top = (J == 4 * c + 3)
                if top:
                    cx.op("dve", lambda: V.tensor_copy(R[gci % 2][:], Zf[:]), [Zf], [R[gci % 2]])
                lo = 128 * max(0, J - 4 * c)
                diag = J >= 4 * c
                Z = pb[zk % 2]; zk += 1
                e_ = Et[k3 % 3]; sp_ = SPt[k3 % 4]; k3 += 1
                cx.op("pe", lambda: nc.tensor.matmul(Z[:, lo:512], kT[b][:, J * 128:(J + 1) * 128], qT[b][:, q0 + lo:q0 + 512], start=True, stop=True),
                      [kT[b], qT[b]], [Z])
                cx.op("act", lambda: S.activation(e_[:, lo:512], Z[:, lo:512], AF.Exp), [Z], [e_])
                cx.op("act", lambda: S.activation(sp_[:, lo:512], e_[:, lo:512], AF.Ln, bias=1.0, scale=1.0), [e_], [sp_])
                if diag:
                    cx.op("dve", lambda: V.tensor_tensor(sp_[:, lo:lo + 128], sp_[:, lo:lo + 128].bitcast(F32), masks[:, M_SUT, :], ALU.mult), [sp_, masks], [sp_])
                st_[k] = (lo, diag, top, sp_)
            if 1 <= k <= len(steps):
                ui, c, J = steps[k - 1]
                s, h = units[ui]; b = ui % NB
                gci = ui * 4 + c
                q0 = c * 512
                Rc = R[gci % 2]
                lo, diag, top, sp_ = st_[k - 1]
                Ab = pb[2 + ak % 2]; ak += 1
                w_ = Wt[wk % 3]; wk += 1
                cx.op("pe", lambda: nc.tensor.matmul(Ab[:, lo:512], kT[b][:, J * 128:(J + 1) * 128], qT[b][:, q0 + lo:q0 + 512], start=True, stop=False),
                      [kT[b], qT[b]], [Ab])
                cx.op("pe", lambda: nc.tensor.matmul(Ab[:, lo:512], masksr[:, M_NTRII, :], sp_[:, lo:512], start=False, stop=(top and not diag)),
                      [masksr, sp_], [Ab])
                if not top:
                    cx.op("pe", lambda: nc.tensor.matmul(Ab[:, lo:512], masksr[:, M_NONES, :], Rc[:, lo:512], start=False, stop=not diag),
                          [masksr, Rc], [Ab])
                if diag:
                    cx.op("pe", lambda: nc.tensor.matmul(Ab[:, lo:lo + 128], identb[:], masksb[:, M_NEGNS, :], start=False, stop=True),
                          [identb, masksb], [Ab])
                cx.op("act", lambda: S.activation(w_[:, lo:512], Ab[:, lo:512], AF.Exp), [Ab], [w_])
                if J > 0:
                    cx.op("dve", lambda: V.tensor_tensor(Rc[:, lo:512], Rc[:, lo:512].bitcast(F32), sp_[:, lo:512].bitcast(F32), ALU.add), [Rc, sp_], [Rc])
                st_[k - 1] = (lo, diag, top, sp_, w_)
            if k >= 2:
                ui, c, J = steps[k - 2]
                s, h = units[ui]; b = ui % NB
                gci = ui * 4 + c
                O = pb[4 + gci % 2]; o_ = ob[gci % 2]
                lo, diag, top, sp_, w_ = st_.pop(k - 2)
                cx.op("pe", lambda: nc.tensor.matmul(O[0:64, lo:512], Vb[b][:, J, :], w_[:, lo:512], start=top, stop=(J == 0)),
                      [Vb[b], w_], [O])
                if J == 0:
                    t0 = s * SEQ; q0 = c * 512
                    cx.op("act", lambda: S.copy(o_[:], O[0:64, :]), [O], [o_])
                    cx.dma("sp", oT[512 + h * 64:512 + (h + 1) * 64, t0 + q0:t0 + q0 + 512], o_[:], reads=[o_], writes=[oT], sb=o_)
                    if c == 3 and ui + 2 < len(units):
                        loads(ui + 2)
            yield

    def gen_lru(ph, l):
        def colvec(name, src_row):
            t = cx.sbuf(ph, name, [128, 8])
            with nc.allow_non_contiguous_dma(reason="tiny"):
                cx.dma("sp", t[:], src_row.rearrange("(m p) -> p m", p=128), reads=[], writes=[t], sb=t)
            return t
        cb = colvec("cb", conv_b[l]); ba = colvec("ba", lru_ba[l]); bx = colvec("bx", lru_bx[l]); lam = colvec("lam", lru_lambda[l])
        cw = cx.sbuf(ph, "cw", [128, 4, 8])
        with nc.allow_non_contiguous_dma(reason="tiny"):
            cx.dma("sp", cw[:], conv_w[l].rearrange("i (m p) -> p i m", p=128), reads=[], writes=[cw], sb=cw)
        el = cx.sbuf(ph, "el", [128, 8]); cA = cx.sbuf(ph, "cA", [128, 8]); cA2 = cx.sbuf(ph, "cA2", [128, 8])
        cx.op("act", lambda: S.activation(el[:], lam[:], AF.Exp, scale=-1.0), [lam], [el])
        cx.op("act", lambda: S.activation(cA[:], el[:], AF.Ln, bias=1.0, scale=1.0), [el], [cA])
        cx.op("dve", lambda: V.tensor_scalar_mul(cA2[:], cA[:], -16.0), [cA], [cA2])
        cx.op("dve", lambda: V.tensor_scalar_mul(cA[:], cA[:], -8.0), [cA], [cA])
        BDf = cx.sbuf(ph, "BDf", [128, 2, 128]); BD = [cx.sbuf(ph, "BD%d" % i, [128, 2, 128], F32R) for i in range(2)]
        cx.op("dve", lambda: V.memset(BDf[:], 0.0), [], [BDf])
        N = SEQ
        xc = [cx.sbuf(ph, "xc%d" % i, [128, 3 + N]) for i in range(2)]
        gc = [cx.sbuf(ph, "gc%d" % i, [128, N]) for i in range(2)]
        xvs = [cx.sbuf(ph, "xv%d" % i, [128, N], F32R) for i in range(2)]
        tas = [cx.sbuf(ph, "t_a%d" % i, [128, N]) for i in range(2)]
        t_b = cx.sbuf(ph, "t_b", [128, N])
        t_c = cx.sbuf(ph, "t_c", [128, N]); t_d = cx.sbuf(ph, "t_d", [128, N]); hh = cx.sbuf(ph, "hh", [128, N])
        oc = [cx.sbuf(ph, "oc%d" % i, [128, N], BF16) for i in range(2)]
        units = [(m, s) for m in range(8) for s in range(NSEQ)]

        def stage1(ui):
            m, s = units[ui]
            bd = BD[m % 2]
            if s == 0:
                for g_, wsrc in enumerate((lru_wa, lru_wx)):
                    cx.dma("sp", BDf[0:64, g_, 0:64], wsrc[l, 2 * m], reads=[], writes=[BDf], sb=BDf)
                    yield
                    cx.dma("sp", BDf[64:128, g_, 64:128], wsrc[l, 2 * m + 1], reads=[], writes=[BDf], sb=BDf)
                    yield
                cx.op("dve", lambda: V.tensor_copy(bd[:], BDf[:]), [BDf], [bd])
                yield
            x_ = xc[ui % 2]; g = gc[ui % 2]; xv = xvs[ui % 2]; t_a = tas[ui % 2]
            t0 = s * SEQ
            cx.op("pool", lambda: P.memset(x_[:, 0:3], 0.0), [], [x_])
            yield
            cx.dma("sp", x_[:, 3:3 + N], xgT[0, m * 128:(m + 1) * 128, t0:t0 + N], reads=[xgT], writes=[x_], sb=x_)
            yield
            cx.dma("sp", g[:], xgT[1, m * 128:(m + 1) * 128, t0:t0 + N], reads=[xgT], writes=[g], sb=g)
            yield
            cx.op("dve", lambda: V.tensor_scalar(t_a[:], x_[:, 0:N], cw[:, 0, m:m + 1], cb[:, m:m + 1], ALU.mult, ALU.add), [x_, cw, cb], [t_a])
            yield
            for i in (1, 2):
                cx.op("dve", lambda: V.scalar_tensor_tensor(t_a[:], x_[:, i:i + N], cw[:, i, m:m + 1], t_a[:], ALU.mult, ALU.add), [x_, cw, t_a], [t_a])
                yield
            cx.op("dve", lambda: V.scalar_tensor_tensor(t_a[:], x_[:, 3:3 + N], cw[:, 3, m:m + 1], t_a[:], ALU.mult, ALU.add), [x_, cw, t_a], [t_a])
            yield
            cx.op("act", lambda: S.copy(xv[:], t_a[:]), [t_a], [xv])
            yield
            cx.op("pool", lambda: P.tensor_tensor(t_d[:], g[:], g[:], ALU.mult), [g], [t_d])
            yield
            cx.op("pool", lambda: P.tensor_scalar(t_d[:], t_d[:], 0.044715, 1.0, ALU.mult, ALU.add), [t_d], [t_d])
            yield
            cx.op("pool", lambda: P.tensor_tensor(t_d[:], t_d[:], g[:], ALU.mult), [t_d, g], [t_d])
            yield
            cx.op("act", lambda: S.activation(t_d[:], t_d[:], AF.Sigmoid, scale=1.5957691216057308), [t_d], [t_d])
            yield
            cx.op("pool", lambda: P.tensor_tensor(g[:], t_d[:], g[:], ALU.mult), [t_d, g], [g])
            yield

        def stage2(ui):
            m, s = units[ui]
            bd = BD[m % 2]
            g = gc[ui % 2]; xv = xvs[ui % 2]; t_a = tas[ui % 2]; o_ = oc[ui % 2]
            t0 = s * SEQ
            for q in range(N // 512):
                pr = pb[6]; pi = pb[6]
                cs = slice(q * 512, (q + 1) * 512)
                cx.op("pe", lambda: nc.tensor.matmul(pr[:], bd[:, 0, :], xv[:, cs], start=True, stop=True), [bd, xv], [pr])
                yield
                cx.op("act", lambda: S.activation(t_b[:, cs], pr[:], AF.Sigmoid, bias=ba[:, m:m + 1], scale=1.0), [pr, ba], [t_b])
                cx.op("pe", lambda: nc.tensor.matmul(pi[:], bd[:, 1, :], xv[:, cs], start=True, stop=True), [bd, xv], [pi])
                yield
                cx.op("act", lambda: S.activation(t_c[:, cs], pi[:], AF.Sigmoid, bias=bx[:, m:m + 1], scale=1.0), [pi, bx], [t_c])
            cx.op("dve", lambda: V.tensor_tensor(t_c[:], t_c[:], t_a[:], ALU.mult), [t_c, t_a], [t_c])
            yield
            cx.op("act", lambda: S.activation(t_a[:], t_b[:], AF.Exp, scale=cA[:, m:m + 1]), [t_b, cA], [t_a])
            yield
            cx.op("act", lambda: S.activation(t_b[:], t_b[:], AF.Exp, scale=cA2[:, m:m + 1]), [t_b, cA2], [t_b])
            yield
            cx.op("act", lambda: S.activation(t_b[:], t_b[:], AF.Ln, bias=1.0, scale=-1.0), [t_b], [t_b])
            yield
            cx.op("act", lambda: S.activation(t_b[:], t_b[:], AF.Exp, scale=0.5), [t_b], [t_b])
            yield
            cx.op("dve", lambda: V.tensor_tensor(t_c[:], t_c[:], t_b[:], ALU.mult), [t_c, t_b], [t_c])
            cx.op("dve", lambda: V.tensor_tensor_scan(hh[:], t_a[:], t_c[:], 0.0, ALU.mult, ALU.add), [t_a, t_c], [hh])
            yield
            cx.op("dve", lambda: V.tensor_tensor(o_[:], hh[:], g[:], ALU.mult), [hh, g], [o_])
            yield
            cx.dma("sp", oT[1024 + m * 128:1024 + (m + 1) * 128, t0:t0 + N], o_[:], reads=[o_], writes=[oT], sb=o_)
            yield

        yield from stage1(0)
        for ui in range(len(units)):
            if ui + 1 < len(units):
                yield from stage1(ui + 1)
            yield from stage2(ui)

    def phase_mix(l):
        ph = ExitStack()
        def attn():
            yield from gen_fox(ph)
            yield from gen_sb(ph)
        ga = attn(); gr = gen_lru(ph, l)
        done_a = done_r = False
        while not (done_a and done_r):
            for _ in range(2):
                if not done_a:
                    try:
                        next(ga)
                    except StopIteration:
                        done_a = True
            if not done_r:
                try:
                    next(gr)
                except StopIteration:
                    done_r = True
        cx.end_phase()
        ph.close()

    def ln_tile(ph_bufs, y_src_bufs, y_ap_halves, x_t, gtile, lg, lb, dst_ap, dst_buf, k):
        tt_, st6, mv, rs, res_ = ph_bufs
        t = tt_[k % 2]; r = res_[k % 4]; s6 = st6[k % 2]; mv_ = mv[k % 2]; rs_ = rs[k % 2]
        for hf_ in range(2):
            cs = slice(hf_ * 512, (hf_ + 1) * 512)
            cx.op("dve", lambda: V.tensor_tensor(t[:, cs], y_ap_halves[hf_], gtile[:, cs], ALU.mult), list(y_src_bufs) + [gtile], [t])
        cx.op("dve", lambda: V.scalar_tensor_tensor(t[:], x_t[:], ALPHA, t[:], ALU.mult, ALU.add), [x_t, t], [t])
        for hf_ in range(2):
            cx.op("dve", lambda: V.bn_stats(s6[:, hf_, :], t[:, hf_ * 512:(hf_ + 1) * 512]), [t], [s6])
        cx.op("dve", lambda: V.bn_aggr(mv_[:], s6[:].rearrange("p a b -> p (a b)")), [s6], [mv_])
        cx.op("dve", lambda: V.tensor_scalar_add(rs_[:], mv_[:, 1:2], LN_EPS), [mv_], [rs_])
        cx.op("act", lambda: S.activation(rs_[:], rs_[:], AF.Ln), [rs_], [rs_])
        cx.op("act", lambda: S.activation(rs_[:], rs_[:], AF.Exp, scale=-0.5), [rs_], [rs_])
        cx.op("dve", lambda: V.tensor_scalar(t[:], t[:], mv_[:, 0:1], rs_[:, 0:1], ALU.subtract, ALU.mult), [t, mv_, rs_], [t])
        cx.op("pool", lambda: P.tensor_tensor(t[:], t[:], lg[:], ALU.mult), [t, lg], [t])
        cx.op("dve", lambda: V.tensor_tensor(r[:], t[:], lb[:], ALU.add), [t, lb], [r])
        cx.dma("sp", dst_ap, r[:], reads=[r], writes=[dst_buf], sb=r)
        return r

    def ln_bufs(ph):
        return ([cx.sbuf(ph, "lnt%d" % i, [128, D]) for i in range(2)],
                [cx.sbuf(ph, "lns%d" % i, [128, 2, 6]) for i in range(2)],
                [cx.sbuf(ph, "lnm%d" % i, [128, 2]) for i in range(2)],
                [cx.sbuf(ph, "lnr%d" % i, [128, 1]) for i in range(2)],
                [cx.sbuf(ph, "lno%d" % i, [128, D]) for i in range(4)])

    def phase_merge(l, xsrc, xdst):
        ph = ExitStack()
        mod = load_mod(ph, l, 0, ("gt",))
        lg = cx.sbuf(ph, "lg", [128, D]); lb = cx.sbuf(ph, "lb", [128, D])
        cx.dma("sp", lg[:], row_bc(ln1_g[l, :], 128), reads=[], writes=[lg], sb=lg)
        cx.dma("sp", lb[:], row_bc(ln1_b[l, :], 128), reads=[], writes=[lb], sb=lb)
        wpa = cx.sbuf(ph, "wpa", [128, 4, D], BF16); wpb = cx.sbuf(ph, "wpb", [128, 4, D], BF16)
        wpc = cx.sbuf(ph, "wpc", [128, 8, D], BF16); wo = cx.sbuf(ph, "wo", [128, 8, D], BF16)
        for t_, src in ((wpa, w_pa), (wpb, w_pb), (wpc, w_pc), (wo, w_o)):
            cx.dma("pool", None, None, reads=[], writes=[t_], sb=t_,
                   fn=lambda: P.dma_start(out=t_[:], in_=src[l].rearrange("(kc p) n -> p kc n", p=128)))
        oTc = [cx.sbuf(ph, "oTc%d" % i, [128, 16, 512], BF16) for i in range(1)]
        gg = [cx.sbuf(ph, "gg%d" % i, [128, 3, 512]) for i in range(2)]
        ta = [cx.sbuf(ph, "ta%d" % i, [128, 512]) for i in range(2)]
        tb = [cx.sbuf(ph, "tb%d" % i, [128, 512]) for i in range(2)]
        mT = [cx.sbuf(ph, "mT%d" % i, [128, 8, 512], BF16) for i in range(1)]
        xt = [cx.sbuf(ph, "xt%d" % i, [128, D]) for i in range(2)]
        lnb = ln_bufs(ph)
        rs = route_setup(ph, l)
        hist = []
        gk = 0; k = 0
        for ch in range(NCH):
            s = ch // (SEQ // 512)
            tok0 = ch * 512
            oc_ = oTc[0]; mT_ = mT[0]
            cx.dma("sp", oc_[:], oT[:, tok0:tok0 + 512].rearrange("(kc p) t -> p kc t", p=128), reads=[oT], writes=[oc_], sb=oc_)
            for m in range(8):
                g_ = gg[gk % 2]; a_ = ta[gk % 2]; b_ = tb[gk % 2]; gk += 1
                cx.dma("sp", g_[:], gT[:, tok0:tok0 + 512].rearrange("(j q p) t -> q p j t", j=3, p=128)[m], reads=[gT], writes=[g_], sb=g_)
                pa, pb_, pc = pb[0 + 3 * (m % 2)], pb[1 + 3 * (m % 2)], pb[2 + 3 * (m % 2)]
                for kc in range(4):
                    cx.op("pe", lambda: nc.tensor.matmul(pa[:], wpa[:, kc, m * 128:(m + 1) * 128], oc_[:, kc, :], start=(kc == 0), stop=(kc == 3)), [wpa, oc_], [pa])
                for kc in range(4):
                    cx.op("pe", lambda: nc.tensor.matmul(pb_[:], wpb[:, kc, m * 128:(m + 1) * 128], oc_[:, 4 + kc, :], start=(kc == 0), stop=(kc == 3)), [wpb, oc_], [pb_])
                for kc in range(8):
                    cx.op("pe", lambda: nc.tensor.matmul(pc[:], wpc[:, kc, m * 128:(m + 1) * 128], oc_[:, 8 + kc, :], start=(kc == 0), stop=(kc == 7)), [wpc, oc_], [pc])
                cx.op("dve", lambda: V.tensor_tensor(a_[:], pa[:], g_[:, 0, :], ALU.mult), [pa, g_], [a_])
                cx.op("dve", lambda: V.tensor_tensor(b_[:], pb_[:], g_[:, 1, :], ALU.mult), [pb_, g_], [b_])
                cx.op("dve", lambda: V.tensor_tensor(a_[:], a_[:], b_[:], ALU.add), [a_, b_], [a_])
                cx.op("dve", lambda: V.tensor_tensor(b_[:], pc[:], g_[:, 2, :], ALU.mult), [pc, g_], [b_])
                cx.op("dve", lambda: V.tensor_tensor(mT_[:, m, :], a_[:], b_[:], ALU.add), [a_, b_], [mT_])
            for tt in range(4):
                ti = ch * 4 + tt
                x_t = xt[k % 2]
                cx.dma("sp", x_t[:], xsrc[ti * 128:(ti + 1) * 128, :], reads=[xsrc], writes=[x_t], sb=x_t)
                ys = []
                for hf_ in range(2):
                    yb = pb[(k * 2 + hf_) % 6]
                    for m in range(8):
                        cx.op("pe", lambda: nc.tensor.matmul(yb[:], mT_[:, m, tt * 128:(tt + 1) * 128], wo[:, m, hf_ * 512:(hf_ + 1) * 512], start=(m == 0), stop=(m == 7)),
                              [mT_, wo], [yb])
                    ys.append(yb)
                if len(hist) >= 1:
                    route_stage(rs, *hist[-1], 1)
                if len(hist) >= 2:
                    route_stage(rs, *hist[-2], 3)
                r_ = ln_tile(lnb, ys, [ys[0][:], ys[1][:]], x_t, mod[("gt", s)], lg, lb, xdst[ti * 128:(ti + 1) * 128, :], xdst, k)
                route_stage(rs, ti, r_, 0)
                if len(hist) >= 1:
                    route_stage(rs, *hist[-1], 2)
                if len(hist) >= 2:
                    route_stage(rs, *hist[-2], 4)
                hist.append((ti, r_))
                k += 1
        route_stage(rs, *hist[-1], 1)
        route_stage(rs, *hist[-2], 3)
        route_stage(rs, *hist[-1], 2)
        route_stage(rs, *hist[-2], 4)
        route_stage(rs, *hist[-1], 3)
        route_stage(rs, *hist[-1], 4)
        if "cnt" in dbg:
            cx.dma("sp", dbg["cnt"][:], rs["cnt"][:], reads=[rs["cnt"]], writes=[dbg["cnt"]], sb=rs["cnt"])
        cx.end_phase()
        ph.close()

    def route_setup(ph, l):
        rs = {}
        rs["mod"] = load_mod(ph, l, 1, ("sh", "sc"))
        wr = cx.sbuf(ph, "wr", [128, 8, NE])
        with nc.allow_non_contiguous_dma(reason="router weights 128B runs"):
            cx.dma("sp", wr[:], w_router[l].rearrange("(kc p) e -> p kc e", p=128), reads=[], writes=[wr], sb=wr)
        brt = cx.sbuf(ph, "brt", [128, NE])
        cx.dma("sp", brt[:], row_bc(b_router[l, :], 128), reads=[], writes=[brt], sb=brt)
        cnt = cx.sbuf(ph, "cnt", [128, NE]); cx.op("dve", lambda: V.memset(cnt[:], 0.0), [], [cnt])
        rs.update(wr=wr, brt=brt, cnt=cnt)
        rs["hf"] = [cx.sbuf(ph, "rhf%d" % i, [128, D]) for i in range(3)]
        rs["hb"] = [cx.sbuf(ph, "rhb%d" % i, [128, D], BF16) for i in range(3)]
        rs["hT"] = [cx.sbuf(ph, "rhT%d" % i, [128, 8, 128]) for i in range(3)]
        rs["pp"] = pbh[:].bitcast(F32)
        rs["ppb"] = pbh
        def sm(name, w=NE):
            return [cx.sbuf(ph, "%s%d" % (name, i), [128, w]) for i in range(3)]
        for nm, w in (("lgt", NE), ("m8", 8), ("msk", NE), ("ex", NE), ("em", NE), ("ssum", 1), ("gte", NE), ("slv", NE),
                      ("s8", 8), ("nmx", 1), ("tmp", NE)):
            rs[nm] = sm(nm, w)
        return rs

    def route_stage(rs, ti, x_t, stage):
        mod = rs["mod"]; wr = rs["wr"]; brt = rs["brt"]; cnt = rs["cnt"]
        s = ti // (SEQ // 128)
        b = ti % 3
        h_f = rs["hf"][b]; h_b = rs["hb"][b]; hT_ = rs["hT"][b]
        lgt, m8, msk, ex, em, ssum, gte, slv, s8, nmx, tmp = (rs[k] for k in ("lgt", "m8", "msk", "ex", "em", "ssum", "gte", "slv", "s8", "nmx", "tmp"))
        L_ = lgt[b]; M8 = m8[b]; MK = msk[b]; SL = slv[b]
        pp = rs["pp"]
        if stage == 0:
            cx.op("dve", lambda: V.tensor_tensor(h_f[:], x_t[:], mod[("sc", s)][:], ALU.mult), [x_t, mod[("sc", s)]], [h_f])
            cx.op("dve", lambda: V.tensor_tensor(h_f[:], h_f[:], mod[("sh", s)][:], ALU.add), [h_f, mod[("sh", s)]], [h_f])
            cx.op("act", lambda: S.copy(h_b[:], h_f[:]), [h_f], [h_b])
        elif stage == 1:
            pt = pb[6]
            for half in range(2):
                for q in range(4):
                    kc = half * 4 + q
                    cx.op("pe", lambda: nc.tensor.transpose(pt[:, q * 128:(q + 1) * 128], h_f[:, kc * 128:(kc + 1) * 128], identf[:]), [h_f, identf], [pt])
                cx.op("act", lambda: S.copy(hT_[:, half * 4:half * 4 + 4, :], pt[:].rearrange("p (q t) -> p q t", q=4)), [pt], [hT_])
            for kc in range(8):
                cx.op("pe", lambda: nc.tensor.matmul(pp[:, 0:NE], hT_[:, kc, :], wr[:, kc, :], start=(kc == 0), stop=(kc == 7)), [hT_, wr], [rs["ppb"]])
        elif stage == 2:
            cx.op("dve", lambda: V.tensor_tensor(L_[:], pp[:, 0:NE], brt[:], ALU.add), [rs["ppb"], brt], [L_])
            cx.op("dve", lambda: V.max(M8[:], L_[:]), [L_], [M8])
            cx.op("dve", lambda: V.tensor_scalar(MK[:], L_[:], M8[:, 3:4], None, ALU.is_ge), [L_, M8], [MK])
            cx.op("dve", lambda: V.tensor_scalar_mul(nmx[b][:], M8[:, 0:1], -1.0), [M8], [nmx[b]])
            cx.op("act", lambda: S.activation(ex[b][:], L_[:], AF.Exp, bias=nmx[b][:, 0:1], scale=1.0), [L_, nmx[b]], [ex[b]])
            cx.op("dve", lambda: V.tensor_tensor(em[b][:], ex[b][:], MK[:], ALU.mult), [ex[b], MK], [em[b]])
            cx.op("dve", lambda: V.reduce_sum(ssum[b][:], em[b][:], mybir.AxisListType.X), [em[b]], [ssum[b]])
            cx.op("dve", lambda: V.reciprocal(ssum[b][:], ssum[b][:]), [ssum[b]], [ssum[b]])
            cx.op("dve", lambda: V.tensor_scalar_mul(gte[b][:], em[b][:], ssum[b][:, 0:1]), [em[b], ssum[b]], [gte[b]])
        elif stage == 3:
            cx.op("pe", lambda: nc.tensor.matmul(pp[:, 64:64 + NE], masks[:, M_SUT, :], MK[:], start=True, stop=True), [masks, MK], [rs["ppb"]])
            cx.op("pe", lambda: nc.tensor.matmul(pp[:, 128:128 + NE], masks[:, M_ONES, :], MK[:], start=True, stop=True), [masks, MK], [rs["ppb"]])
        else:
            cx.op("dve", lambda: V.tensor_tensor(SL[:], pp[:, 64:64 + NE], cnt[:], ALU.add), [rs["ppb"], cnt], [SL])
            cx.op("dve", lambda: V.tensor_tensor(cnt[:], pp[:, 128:128 + NE], cnt[:], ALU.add), [rs["ppb"], cnt], [cnt])
            cx.op("dve", lambda: V.tensor_tensor(SL[:], SL[:], ebase[:], ALU.add), [SL, ebase], [SL])
            cx.op("dve", lambda: V.tensor_tensor(SL[:], SL[:], MK[:], ALU.mult), [SL, MK], [SL])
            cx.op("dve", lambda: V.tensor_scalar_add(SL[:], SL[:], -1.0), [SL], [SL])
            cx.op("dve", lambda: V.max(s8[b][:], SL[:]), [SL], [s8[b]])
            cx.op("dve", lambda: V.tensor_copy(IDX[:, ti, :], s8[b][:, 0:4]), [s8[b]], [IDX])
            for k in range(4):
                cx.op("dve", lambda: V.tensor_scalar(tmp[b][:], SL[:], s8[b][:, k:k + 1], None, ALU.is_equal), [SL, s8[b]], [tmp[b]])
                cx.op("dve", lambda: V.tensor_tensor(tmp[b][:], tmp[b][:], gte[b][:], ALU.mult), [tmp[b], gte[b]], [tmp[b]])
                cx.op("dve", lambda: V.reduce_sum(GK[:, ti, k:k + 1], tmp[b][:], mybir.AxisListType.X), [tmp[b]], [GK])
            for k in range(4):
                cx.dma("pool", None, None, reads=[h_b, IDX], writes=[xbuf], sb=h_b,
                       fn=lambda: P.indirect_dma_start(out=xbuf[:], out_offset=bass.IndirectOffsetOnAxis(ap=IDX[:, ti, k:k + 1], axis=0),
                                                       in_=h_b[:], in_offset=None))

    def phase_experts(l):
        ph = ExitStack()
        wgu = [cx.sbuf(ph, "wgu%d" % i, [128, 8, 2 * D], BF16) for i in range(2)]
        wdn = [cx.sbuf(ph, "wdn%d" % i, [128, 8, D], BF16) for i in range(2)]
        bgu = [cx.sbuf(ph, "bgu%d" % i, [128, 16]) for i in range(2)]
        bdn = [cx.sbuf(ph, "bdn%d" % i, [128, D]) for i in range(2)]
        xr = [cx.sbuf(ph, "xr%d" % i, [128, 4, D], BF16) for i in range(2)]
        xTs = [cx.sbuf(ph, "xT%d" % i, [128, 8, 512], BF16) for i in range(2)]
        aTs = [cx.sbuf(ph, "aT%d" % i, [128, 8, 512], BF16) for i in range(2)]
        g1 = [cx.sbuf(ph, "g1%d" % i, [128, 512]) for i in range(2)]
        sg = [cx.sbuf(ph, "sg%d" % i, [128, 512]) for i in range(2)]
        u1 = [cx.sbuf(ph, "u1%d" % i, [128, 512]) for i in range(2)]
        yt = [cx.sbuf(ph, "yt%d" % i, [128, D]) for i in range(2)]
        jk = 0; yk = 0
        chunks = [(e, sc_) for e in range(NE) for sc_ in range(CAP // 512)]

        def load_w(e):
            b = e % 2
            cx.dma("pool", None, None, reads=[], writes=[wgu[b]], sb=wgu[b],
                   fn=lambda: P.dma_start(out=wgu[b][:], in_=w_gu[l, e].rearrange("(kc p) n -> p kc n", p=128)))
            cx.dma("pool", None, None, reads=[], writes=[wdn[b]], sb=wdn[b],
                   fn=lambda: P.dma_start(out=wdn[b][:], in_=w_down[l, e].rearrange("(kc p) n -> p kc n", p=128)))
            cx.dma("sp", bdn[b][:], row_bc(b_down[l, e, :], 128), reads=[], writes=[bdn[b]], sb=bdn[b])
            with nc.allow_non_contiguous_dma(reason="tiny"):
                cx.dma("sp", bgu[b][:], b_gu[l, e].rearrange("(m p) -> p m", p=128), reads=[], writes=[bgu[b]], sb=bgu[b])

        def load_x(ci):
            e, sc_ = chunks[ci]
            r0 = e * CAP + sc_ * 512
            xr_ = xr[ci % 2]
            cx.dma("sp", xr_[:], xbuf[r0:r0 + 512, :].rearrange("(t p) d -> p t d", p=128), reads=[xbuf], writes=[xr_], sb=xr_)

        def transp(ci, t_):
            xr_ = xr[ci % 2]; xT = xTs[ci % 2]
            for kc in range(8):
                cx.op("pe", lambda: nc.tensor.transpose(pbh[:, kc * 128:(kc + 1) * 128], xr_[:, t_, kc * 128:(kc + 1) * 128], identb[:]), [xr_, identb], [pbh])
            if t_ % 2:
                cx.op("act", lambda: S.copy(xT[:, :, t_ * 128:(t_ + 1) * 128], pbh[:].rearrange("p (kc t) -> p kc t", kc=8)), [pbh], [xT])
            else:
                cx.op("dve", lambda: V.tensor_copy(xT[:, :, t_ * 128:(t_ + 1) * 128], pbh[:].rearrange("p (kc t) -> p kc t", kc=8)), [pbh], [xT])

        load_w(0)
        load_x(0)
        for t_ in range(4):
            transp(0, t_)
        for ci, (e, sc_) in enumerate(chunks):
            b = e % 2
            r0 = e * CAP + sc_ * 512
            xT = xTs[ci % 2]; aT = aTs[ci % 2]
            if sc_ == 0 and e + 1 < NE:
                load_w(e + 1)
            if ci + 1 < len(chunks):
                load_x(ci + 1)
            for j in range(8):
                pg = pb[(jk % 2) * 2]; pu = pb[(jk % 2) * 2 + 1]
                g_ = g1[jk % 2]; s_ = sg[jk % 2]; u_ = u1[jk % 2]; jk += 1
                for kc in range(8):
                    cx.op("pe", lambda: nc.tensor.matmul(pg[:], wgu[b][:, kc, j * 128:(j + 1) * 128], xT[:, kc, :], start=(kc == 0), stop=(kc == 7)), [wgu[b], xT], [pg])
                for kc in range(8):
                    cx.op("pe", lambda: nc.tensor.matmul(pu[:], wgu[b][:, kc, D + j * 128:D + (j + 1) * 128], xT[:, kc, :], start=(kc == 0), stop=(kc == 7)), [wgu[b], xT], [pu])
                cx.op("dve", lambda: V.tensor_scalar(g_[:], pg[:], bgu[b][:, j:j + 1], 7.0, ALU.add, ALU.min), [pg, bgu[b]], [g_])
                cx.op("act", lambda: S.activation(u_[:], pu[:], AF.Identity, bias=bgu[b][:, 8 + j:9 + j], scale=1.0), [pu, bgu[b]], [u_])
                cx.op("act", lambda: S.activation(s_[:], g_[:], AF.Sigmoid, scale=1.702), [g_], [s_])
                cx.op("dve", lambda: V.tensor_scalar(u_[:], u_[:], 7.0, -7.0, ALU.min, ALU.max), [u_], [u_])
                cx.op("dve", lambda: V.tensor_tensor(g_[:], g_[:], s_[:], ALU.mult), [g_, s_], [g_])
                cx.op("dve", lambda: V.scalar_tensor_tensor(aT[:, j, :], u_[:], 1.0, g_[:], ALU.add, ALU.mult), [u_, g_], [aT])
            for t_ in range(4):
                if ci + 1 < len(chunks):
                    transp(ci + 1, t_)
                y_ = yt[yk % 2]; yk += 1
                for hf_ in range(2):
                    py = pb[4 + (yk * 2 + hf_) % 3]
                    for j in range(8):
                        cx.op("pe", lambda: nc.tensor.matmul(py[:], aT[:, j, t_ * 128:(t_ + 1) * 128], wdn[b][:, j, hf_ * 512:(hf_ + 1) * 512], start=(j == 0), stop=(j == 7)), [aT, wdn[b]], [py])
                    cx.op("dve", lambda: V.tensor_tensor(y_[:, hf_ * 512:(hf_ + 1) * 512], py[:], bdn[b][:, hf_ * 512:(hf_ + 1) * 512], ALU.add), [py, bdn[b]], [y_])
                cx.dma("sp", ybuf[r0 + t_ * 128:r0 + (t_ + 1) * 128, :], y_[:], reads=[y_], writes=[ybuf], sb=y_)
        cx.end_phase()
        ph.close()

    def phase_combine(l, xsrc, xdst):
        ph = ExitStack()
        mod = load_mod(ph, l, 1, ("gt",))
        lg = cx.sbuf(ph, "lg", [128, D]); lb = cx.sbuf(ph, "lb", [128, D])
        cx.dma("sp", lg[:], row_bc(ln2_g[l, :], 128), reads=[], writes=[lg], sb=lg)
        cx.dma("sp", lb[:], row_bc(ln2_b[l, :], 128), reads=[], writes=[lb], sb=lb)
        xt = [cx.sbuf(ph, "xt%d" % i, [128, D]) for i in range(2)]
        yg = [cx.sbuf(ph, "yg%d" % i, [128, D]) for i in range(12)]
        acc = [cx.sbuf(ph, "acc%d" % i, [128, D]) for i in range(2)]
        lnb = ln_bufs(ph)
        def gathers(ti):
            for k in range(4):
                y_ = yg[(ti % 3) * 4 + k]
                cx.dma("pool", None, None, reads=[ybuf, IDX], writes=[y_], sb=y_,
                       fn=lambda: P.indirect_dma_start(out=y_[:], out_offset=None, in_=ybuf[:],
                                                       in_offset=bass.IndirectOffsetOnAxis(ap=IDX[:, ti, k:k + 1], axis=0)))
        gathers(0); gathers(1)
        for ti in range(NT):
            s = ti // (SEQ // 128)
            b = ti % 2
            x_t = xt[b]; a_ = acc[b]
            cx.dma("sp", x_t[:], xsrc[ti * 128:(ti + 1) * 128, :], reads=[xsrc], writes=[x_t], sb=x_t)
            if ti + 2 < NT:
                gathers(ti + 2)
            ys = [yg[(ti % 3) * 4 + k] for k in range(4)]
            cx.op("dve", lambda: V.tensor_scalar_mul(a_[:], ys[0][:], GK[:, ti, 0:1]), [ys[0], GK], [a_])
            for k in range(1, 4):
                cx.op("dve", lambda: V.scalar_tensor_tensor(a_[:], ys[k][:], GK[:, ti, k:k + 1], a_[:], ALU.mult, ALU.add), [ys[k], GK, a_], [a_])
            ln_tile(lnb, [a_], [a_[:, 0:512], a_[:, 512:1024]], x_t, mod[("gt", s)], lg, lb, xdst[ti * 128:(ti + 1) * 128, :], xdst, ti)
        cx.end_phase()
        ph.close()

    def done(tag):
        return stop_after == tag

    phase_ada()
    cur = x_in
    finished = False
    for l in range(n_layers):
        if done("ada"):
            break
        phase_proj(l, cur)
        if done("proj"): break
        phase_fprep(l)
        phase_mix(l)
        if done("mix"): break
        x1 = xres[0]
        phase_merge(l, cur, x1)
        if done("merge"): break
        phase_experts(l)
        if done("experts"): break
        last = (l == n_layers - 1)
        x2 = out_d if last else xres[1]
        phase_combine(l, x1, x2)
        cur = x2
    for name, src in (("oT", oT), ("x1", xres[0]), ("gT", gT), ("faT", faT), ("qkT", qkT[0]), ("xbuf", xbuf), ("ybuf", ybuf)):
        if name in dbg:
            ph = ExitStack()
            d = dbg[name]
            rows, cols = d.t.shape
            cw_ = min(cols, 1024)
            tmpb = [cx.sbuf(ph, "dump%d" % i, [128, cw_], src.t.dtype if hasattr(src, "t") else src.dtype) for i in range(2)]
            tmpf = [cx.sbuf(ph, "dumpf%d" % i, [128, cw_]) for i in range(2)]
            srcb = src if hasattr(src, "t") else qkT
            i = 0
            for r0 in range(0, rows, 128):
                for c0 in range(0, cols, cw_):
                    i += 1
                    n = min(128, rows - r0)
                    tb_, tf_ = tmpb[i % 2], tmpf[i % 2]
                    cx.dma("sp", tb_[0:n, :], src[r0:r0 + n, c0:c0 + cw_], reads=[srcb], writes=[tb_], sb=tb_)
                    cx.op("dve", lambda: V.tensor_copy(tf_[0:n, :], tb_[0:n, :]), [tb_], [tf_])
                    cx.dma("sp", d[r0:r0 + n, c0:c0 + cw_], tf_[0:n, :], reads=[tf_], writes=[d], sb=tf_)
            cx.end_phase()
            ph.close()
    ph = ExitStack()
    for name in ("IDX", "GK"):
        if name in dbg:
            srcb = IDX if name == "IDX" else GK
            tmpf = cx.sbuf(ph, "dumpg" + name, [128, NT * 4])
            cx.op("dve", lambda: V.tensor_copy(tmpf[:], srcb[:].rearrange("p a b -> p (a b)")), [srcb], [tmpf])
            cx.dma("sp", dbg[name][:], tmpf[:], reads=[tmpf], writes=[dbg[name]], sb=tmpf)
    cx.end_phase()
    ph.close()
    st.close()
    return nc, cx


def make_consts():
    import ml_dtypes
    k = np.arange(128)[:, None]
    q = np.arange(128)[None, :]
    masks = np.zeros((128, 8, 128), np.float32)
    masks[:, 0, :] = (k < q)
    masks[:, 1, :] = (k > q)
    masks[:, 2, :] = 1.0
    masks[:, 3, :] = np.where(k > q, NEG, 0.0)
    masks[:, 4, :] = np.where(k >= q, NEG, 0.0)
    masks[:, 5, :] = np.where(k < q, -1.0, 0.0)
    masks[:, 6, :] = np.where(k >= q, -1.0, 0.0)
    masks[:, 7, :] = -1.0
    ebase = np.broadcast_to((np.arange(NE) * CAP + 1).astype(np.float32)[None, :], (128, NE)).copy()
    return {
        "k_identb": np.eye(128, dtype=np.float32).astype(ml_dtypes.bfloat16),
        "k_identf": np.eye(128, dtype=np.float32),
        "k_masks": masks,
        "k_ebase": ebase,
    }


WEIGHT_KEYS = ["w_ada", "b_ada", "ln1_g", "ln1_b", "w_in", "b_f", "conv_w", "conv_b", "lru_wa", "lru_ba", "lru_wx",
               "lru_bx", "lru_lambda", "w_gate", "b_gate", "w_pa", "w_pb", "w_pc", "w_o", "ln2_g", "ln2_b", "w_router",
               "b_router", "w_gu", "b_gu", "w_down", "b_down"]


def make_in_maps(inputs):
    consts = make_consts()
    x = np.ascontiguousarray(np.asarray(inputs["x"], dtype=np.float32))
    c = np.ascontiguousarray(np.asarray(inputs["c"], dtype=np.float32))
    shared = {k: np.ascontiguousarray(np.asarray(inputs[k], dtype=np.float32)) for k in WEIGHT_KEYS}
    shared.update(consts)
    in_maps = []
    for i in range(NCORES):
        m = dict(shared)
        m["x"] = x[NSEQ * i:NSEQ * (i + 1)].reshape(T, D)
        m["c"] = c[NSEQ * i:NSEQ * (i + 1)]
        in_maps.append(m)
    return in_maps


def kernel(**inputs):
    nc, cx = build_program()
    in_maps = make_in_maps(inputs)
    res = run_bass_kernel_spmd(nc, in_maps, core_ids=list(range(NCORES)))
    out = np.stack([np.asarray(r["out"]).reshape(NSEQ, SEQ, D) for r in res.results], axis=0)
    return out.reshape(NCORES * NSEQ, SEQ, D).astype(np.float32)
```

```python
from contextlib import ExitStack
import numpy as np
import concourse.bass as bass
import concourse.mybir as mybir
from concourse.bass_utils import run_bass_kernel_spmd

F32 = mybir.dt.float32
F32R = mybir.dt.float32r
BF16 = mybir.dt.bfloat16
I32 = mybir.dt.int32
U32 = mybir.dt.uint32
AF = mybir.ActivationFunctionType
ALU = mybir.AluOpType

NCORES = 8
D = 1024
SEQ = 2048
NSEQ = 2
T = NSEQ * SEQ
NT = T // 128
NCH = T // 512
DEPTH = 2
H = 8
DH = 64
D_IN = 5128
NE = 32
CAP = 1024
ALPHA = (2.0 * DEPTH) ** 0.25
LN_EPS = 1e-5
NEG = -30000.0


class Slot:
    def __init__(self, sem):
        self.sem = sem
        self.count = 0


class Buf:
    def __init__(self, name, t=None):
        self.name = name
        self.t = t
        self.w = {}
        self.r = {}
        self.ds = None

    def __getitem__(self, k):
        return self.t[k]


class EngState:
    def __init__(self, name, eng, sem):
        self.name = name
        self.eng = eng
        self.sem = sem
        self.count = 0
        self.waited = {}


class Ctx:
    def __init__(self, nc, stack, n_dma_sems=72):
        self.nc = nc
        self.stack = stack
        self.E = {}
        for name, eng in (("pe", nc.tensor), ("act", nc.scalar), ("dve", nc.vector),
                          ("pool", nc.gpsimd), ("sp", nc.sync)):
            sem = stack.enter_context(nc.semaphore("s_" + name))
            self.E[name] = EngState(name, eng, sem)
        self.free_slots = [Slot(stack.enter_context(nc.semaphore("d%d" % i))) for i in range(n_dma_sems)]
        self.used_slots = []
        self.n_ins = 0
        self.n_wait = 0
        self.uid = 0

    def sbuf(self, ph, name, shape, dtype=F32):
        self.uid += 1
        t = ph.enter_context(self.nc.sbuf_tensor("%s_%d" % (name, self.uid), list(shape), dtype))
        return Buf(name, t)

    def psum(self, name, shape, dtype=F32):
        t = self.stack.enter_context(self.nc.psum_tensor(name, list(shape), dtype))
        return Buf(name, t)

    def dram(self, name, shape, dtype=F32, kind="Internal"):
        t = self.nc.dram_tensor(name, list(shape), dtype, kind=kind)
        return Buf(name, t.ap())

    def _wait(self, E, deps):
        for sid, (sem, val) in deps.items():
            if E.waited.get(sid, 0) >= val:
                continue
            E.eng.wait_ge(sem, val)
            E.waited[sid] = val
            self.n_wait += 1

    @staticmethod
    def _merge(d, src, skip=None):
        for sid, (sem, val) in src.items():
            if skip is not None and sid == skip:
                continue
            if sid not in d or d[sid][1] < val:
                d[sid] = (sem, val)

    def op(self, en, fn, reads=(), writes=()):
        E = self.E[en]
        own = id(E.sem)
        deps = {}
        for b in reads:
            self._merge(deps, b.w, skip=own if en == "pe" else None)
        for b in writes:
            self._merge(deps, b.w, skip=own)
            self._merge(deps, b.r, skip=own)
        self._wait(E, deps)
        ins = fn()
        E.count += 1
        ins.then_inc(E.sem, 1)
        tok = (E.sem, E.count)
        for b in reads:
            b.r[own] = tok
        for b in writes:
            b.w = {own: tok}
            b.r = {}
        self.n_ins += 1
        return ins

    def dma(self, qn, out, in_, reads=(), writes=(), sb=None, fn=None):
        E = self.E[qn]
        if sb.ds is None:
            sb.ds = self.free_slots.pop()
            self.used_slots.append(sb.ds)
        ds = sb.ds
        deps = {}
        if ds.count:
            deps[id(ds.sem)] = (ds.sem, ds.count)
        for b in reads:
            self._merge(deps, b.w)
        for b in writes:
            self._merge(deps, b.w)
            self._merge(deps, b.r)
        self._wait(E, deps)
        ins = E.eng.dma_start(out=out, in_=in_) if fn is None else fn()
        ds.count += 16
        ins.then_inc(ds.sem, 16)
        tok = (ds.sem, ds.count)
        sid = id(ds.sem)
        for b in reads:
            b.r[sid] = tok
        for b in writes:
            b.w = {sid: tok}
            b.r = {}
        self.n_ins += 1
        return ins

    def cond_region(self, regs, thr, body):
        snap_c = {n: E.count for n, E in self.E.items()}
        all_slots = self.used_slots + self.free_slots
        snap_s = {id(sl): sl.count for sl in all_slots}
        snap_w = {n: dict(E.waited) for n, E in self.E.items()}
        with self.nc.If_cmp(regs, thr, "IS_GT"):
            body()
        with self.nc.Else():
            for n, E in self.E.items():
                d = E.count - snap_c[n]
                if d:
                    if snap_c[n]:
                        E.eng.wait_ge(E.sem, snap_c[n])
                    E.eng.sem_inc(E.sem, d)
            sp = self.E["sp"]
            for sl in self.used_slots + self.free_slots:
                d = sl.count - snap_s.get(id(sl), 0)
                if d:
                    if snap_s.get(id(sl), 0):
                        sp.eng.wait_ge(sl.sem, snap_s[id(sl)])
                    sp.eng.sem_inc(sl.sem, d)
        for n, E in self.E.items():
            E.waited = snap_w[n]

    def barrier(self, only=None):
        deps = {}
        for E in self.E.values():
            if E.count:
                deps[id(E.sem)] = (E.sem, E.count)
        for s in self.used_slots:
            if s.count:
                deps[id(s.sem)] = (s.sem, s.count)
        for name, E in self.E.items():
            if only is not None and name not in only:
                continue
            d = {k: v for k, v in deps.items() if k != id(E.sem)}
            self._wait(E, d)

    def end_phase(self):
        self.barrier()
        self.free_slots.extend(self.used_slots)
        self.used_slots = []


def build_program(n_layers=DEPTH, stop_after=None, debug=()):
    nc = bass.Bass("TRN2", target_bir_lowering=False)
    st = ExitStack()
    cx = Ctx(nc, st)
    V, S, P = nc.vector, nc.scalar, nc.gpsimd

    def din(name, shape, dtype=F32):
        return cx.dram(name, shape, dtype, kind="ExternalInput")

    x_in = din("x", [T, D]); c_in = din("c", [NSEQ, D])
    w_ada = din("w_ada", [DEPTH, D, 6 * D]); b_ada = din("b_ada", [DEPTH, 6 * D])
    ln1_g = din("ln1_g", [DEPTH, D]); ln1_b = din("ln1_b", [DEPTH, D])
    w_in = din("w_in", [DEPTH, D, D_IN]); b_f = din("b_f", [DEPTH, H])
    conv_w = din("conv_w", [DEPTH, 4, D]); conv_b = din("conv_b", [DEPTH, D])
    lru_wa = din("lru_wa", [DEPTH, 16, 64, 64]); lru_ba = din("lru_ba", [DEPTH, D])
    lru_wx = din("lru_wx", [DEPTH, 16, 64, 64]); lru_bx = din("lru_bx", [DEPTH, D])
    lru_lambda = din("lru_lambda", [DEPTH, D])
    w_gate = din("w_gate", [DEPTH, D, 3 * D]); b_gate = din("b_gate", [DEPTH, 3 * D])
    w_pa = din("w_pa", [DEPTH, 512, D]); w_pb = din("w_pb", [DEPTH, 512, D])
    w_pc = din("w_pc", [DEPTH, D, D]); w_o = din("w_o", [DEPTH, D, D])
    ln2_g = din("ln2_g", [DEPTH, D]); ln2_b = din("ln2_b", [DEPTH, D])
    w_router = din("w_router", [DEPTH, D, NE]); b_router = din("b_router", [DEPTH, NE])
    w_gu = din("w_gu", [DEPTH, NE, D, 2 * D]); b_gu = din("b_gu", [DEPTH, NE, 2 * D])
    w_down = din("w_down", [DEPTH, NE, D, D]); b_down = din("b_down", [DEPTH, NE, D])
    k_identb = din("k_identb", [128, 128], BF16)
    k_identf = din("k_identf", [128, 128])
    k_masks = din("k_masks", [128, 8, 128])
    k_ebase = din("k_ebase", [128, NE])
    out_d = cx.dram("out", [T, D], F32, kind="ExternalOutput")

    adaB = cx.dram("adaB", [DEPTH, NSEQ, 6, D])
    xres = [cx.dram("xresA", [T, D]), cx.dram("xresB", [T, D])]
    qkT = cx.dram("qkT", [4, 512, T], BF16)
    vv = cx.dram("vv", [2, T, 512], BF16)
    faT = cx.dram("faT", [H, T])
    Fd = cx.dram("Fd", [6, H, T], BF16)
    xgT = cx.dram("xgT", [2, D, T])
    gT = cx.dram("gT", [3 * D, T])
    oT = cx.dram("oT", [2 * D, T], BF16)
    xbuf = cx.dram("xbuf", [(NE + 1) * CAP, D], BF16)
    ybuf = cx.dram("ybuf", [(NE + 1) * CAP, D])
    dbg = {}
    for name, shape in debug:
        dbg[name] = cx.dram("dbg_" + name, shape, F32, kind="ExternalOutput")

    pb = [cx.psum("pb%d" % i, [128, 512]) for i in range(7)]
    pbh = cx.psum("pbh", [128, 1024], BF16)

    gl = st
    identb = cx.sbuf(gl, "identb", [128, 128], BF16)
    identf = cx.sbuf(gl, "identf", [128, 128])
    masks = cx.sbuf(gl, "masks", [128, 8, 128])
    masksb = cx.sbuf(gl, "masksb", [128, 8, 128], BF16)
    masksr = cx.sbuf(gl, "masksr", [128, 8, 128], F32R)
    ebase = cx.sbuf(gl, "ebase", [128, NE])
    IDX = cx.sbuf(gl, "IDX", [128, NT, 4], I32)
    GK = cx.sbuf(gl, "GK", [128, NT, 4])
    onesb = cx.sbuf(gl, "onesb", [128, 128], BF16)
    CNTI = cx.sbuf(gl, "CNTI", [1, NE], I32)
    cx.dma("sp", identb[:], k_identb[:], reads=[k_identb], writes=[identb], sb=identb)
    cx.dma("sp", identf[:], k_identf[:], reads=[k_identf], writes=[identf], sb=identf)
    cx.dma("sp", masks[:], k_masks[:], reads=[k_masks], writes=[masks], sb=masks)
    cx.dma("sp", ebase[:], k_ebase[:], reads=[k_ebase], writes=[ebase], sb=ebase)
    cx.op("dve", lambda: V.tensor_copy(masksb[:], masks[:]), [masks], [masksb])
    cx.op("dve", lambda: V.tensor_copy(masksr[:], masks[:]), [masks], [masksr])
    cx.op("dve", lambda: V.memset(onesb[:], 1.0), [], [onesb])
    M_SUT, M_TRI, M_ONES, M_NEGC, M_NEGNS, M_NSTRICT, M_NTRII, M_NONES = range(8)

    def row_bc(ap_row, n):
        return ap_row.partition_broadcast(n)

    def phase_ada():
        ph = ExitStack()
        c_col = cx.sbuf(ph, "c_col", [128, NSEQ, 8])
        cond = cx.sbuf(ph, "cond", [128, NSEQ, 8])
        with nc.allow_non_contiguous_dma(reason="tiny transposed load of c"):
            cx.dma("sp", c_col[:], c_in.t.rearrange("s (kc p) -> p s kc", p=128), reads=[c_in], writes=[c_col], sb=c_col)
        cx.op("act", lambda: S.activation(cond[:], c_col[:], AF.Silu), [c_col], [cond])
        condB = cx.sbuf(ph, "condB", [128, NSEQ, 8, 128], BF16)
        for s in range(NSEQ):
            cx.op("dve", lambda: V.tensor_copy(condB[:, s], cond[:, s, :].unsqueeze(2).to_broadcast([128, 8, 128])),
                  [cond], [condB])
        wring = [cx.sbuf(ph, "wada%d" % i, [128, 8, 512], BF16) for i in range(3)]
        brow = [cx.sbuf(ph, "brow%d" % i, [128, 512]) for i in range(2)]
        res = [cx.sbuf(ph, "ares%d" % i, [128, 512]) for i in range(3)]
        k = 0
        for l in range(n_layers):
            for n in range(12):
                wt = wring[k % 3]; br = brow[k % 2]
                cx.dma("pool", None, None, reads=[w_ada], writes=[wt], sb=wt,
                       fn=lambda: P.dma_start(out=wt[:], in_=w_ada[l, :, n * 512:(n + 1) * 512].rearrange("(kc p) n -> p kc n", p=128)))
                cx.dma("sp", br[:], row_bc(b_ada[l, n * 512:(n + 1) * 512], 128), reads=[b_ada], writes=[br], sb=br)
                which = n // 2
                for s in range(NSEQ):
                    ps = pb[(k * NSEQ + s) % 4]
                    for kc in range(8):
                        cx.op("pe", lambda: nc.tensor.matmul(ps[:], condB[:, s, kc, :], wt[:, kc, :], start=(kc == 0), stop=(kc == 7)),
                              [condB, wt], [ps])
                    r = res[(k * NSEQ + s) % 3]
                    cx.op("dve", lambda: V.tensor_tensor(r[:], ps[:], br[:], ALU.add), [ps, br], [r])
                    if which not in (0, 3):
                        cx.op("dve", lambda: V.tensor_scalar_add(r[:], r[:], 1.0), [r], [r])
                    c0_ = (n % 2) * 512
                    cx.dma("sp", adaB[l, s, which:which + 1, c0_:c0_ + 512], r[0:1, :], reads=[r], writes=[adaB], sb=r)
                k += 1
        cx.end_phase()
        ph.close()

    def load_mod(ph, l, sub, names=("sh", "sc", "gt")):
        tiles = {}
        for s in range(NSEQ):
            for nm, idx in (("sh", 3 * sub), ("sc", 3 * sub + 1), ("gt", 3 * sub + 2)):
                if nm not in names:
                    continue
                t = cx.sbuf(ph, "mod_%s%d" % (nm, s), [128, D])
                cx.dma("sp", t[:], row_bc(adaB[l, s, idx, :], 128), reads=[adaB], writes=[t], sb=t)
                tiles[(nm, s)] = t
        return tiles

    def phase_proj(l, xsrc):
        ph = ExitStack()
        TC = 1024
        NC2 = T // TC
        TPC = TC // 128
        mod = load_mod(ph, l, 0, ("sh", "sc"))
        bgate = cx.sbuf(ph, "bgate", [128, 24])
        with nc.allow_non_contiguous_dma(reason="tiny bias relayout"):
            cx.dma("sp", bgate[:], b_gate[l].rearrange("(m p) -> p m", p=128), reads=[b_gate], writes=[bgate], sb=bgate)
        xt = [cx.sbuf(ph, "xt%d" % i, [128, D]) for i in range(TPC)]
        hf = [cx.sbuf(ph, "hf%d" % i, [128, D]) for i in range(2)]
        hb = [cx.sbuf(ph, "hb%d" % i, [128, D], BF16) for i in range(2)]
        hT = [cx.sbuf(ph, "hT%d" % i, [128, 8, TC], BF16) for i in range(2)]
        wring = [cx.sbuf(ph, "win%d" % i, [128, 8, 512], BF16) for i in range(3)]
        wfa = cx.sbuf(ph, "wfa", [128, 8, 8], BF16)
        evf = [cx.sbuf(ph, "evf%d" % i, [128, 512]) for i in range(8)]
        evb = [cx.sbuf(ph, "evb%d" % i, [128, 512], BF16) for i in range(8)]
        cx.dma("pool", None, None, reads=[w_in], writes=[wfa], sb=wfa,
               fn=lambda: P.dma_start(out=wfa[:], in_=w_in[l, :, 1536:1544].rearrange("(kc p) n -> p kc n", p=128)))
        cnt = {"wk": 0, "ek": 0, "pk": 0}
        pieces = [("qa", 0), ("ka", 512), ("va", 1024), ("qb", 1544), ("kb", 2056), ("vb", 2568),
                  ("xc0", 3080), ("xc1", 3592), ("gc0", 4104), ("gc1", 4616)] + [("g%d" % i, i * 512) for i in range(6)]

        def load_x(ch):
            for tt in range(TPC):
                ti = ch * TPC + tt
                cx.dma("sp", xt[tt][:], xsrc[ti * 128:(ti + 1) * 128, :], reads=[xsrc], writes=[xt[tt]], sb=xt[tt])

        def compute_h(ch):
            s = (ch * TC) // SEQ
            hTc = hT[ch % 2]
            for tt in range(TPC):
                x_t = xt[tt]; h_f = hf[tt % 2]; h_b = hb[tt % 2]
                cx.op("dve", lambda: V.tensor_tensor(h_f[:], x_t[:], mod[("sc", s)][:], ALU.mult), [x_t, mod[("sc", s)]], [h_f])
                cx.op("dve", lambda: V.tensor_tensor(h_b[:], h_f[:], mod[("sh", s)][:], ALU.add), [h_f, mod[("sh", s)]], [h_b])
                for kc in range(8):
                    cx.op("pe", lambda: nc.tensor.transpose(pbh[:, kc * 128:(kc + 1) * 128], h_b[:, kc * 128:(kc + 1) * 128], identb[:]),
                          [h_b, identb], [pbh])
                cx.op("act", lambda: S.copy(hTc[:, :, tt * 128:(tt + 1) * 128], pbh[:].rearrange("p (kc t) -> p kc t", kc=8)),
                      [pbh], [hTc])

        def evac(kind, ps, arg=None):
            ek = cnt["ek"]; cnt["ek"] += 1
            if kind == "bf":
                ev = evb[ek % 8]
                if ek % 2:
                    cx.op("act", lambda: S.mul(ev[:], ps[:], arg), [ps], [ev])
                else:
                    cx.op("dve", lambda: V.tensor_scalar_mul(ev[:], ps[:], arg), [ps], [ev])
            elif kind == "f":
                ev = evf[ek % 8]
                if ek % 2:
                    cx.op("act", lambda: S.copy(ev[:], ps[:]), [ps], [ev])
                else:
                    cx.op("dve", lambda: V.tensor_copy(ev[:], ps[:]), [ps], [ev])
            else:
                ev = evf[ek % 8]
                cx.op("act", lambda: S.activation(ev[:], ps[:], AF.Sigmoid, bias=bgate[:, arg:arg + 1], scale=1.0), [ps, bgate], [ev])
            return ev

        def next_ps():
            ps = pb[cnt["pk"] % 6]; cnt["pk"] += 1
            return ps

        load_x(0)
        compute_h(0)
        for ch in range(NC2):
            hTc = hT[ch % 2]
            tok0 = ch * TC
            if ch + 1 < NC2:
                load_x(ch + 1)
            for half in range(TC // 512):
                ps = next_ps()
                hs = slice(half * 512, (half + 1) * 512)
                for kc in range(8):
                    cx.op("pe", lambda: nc.tensor.matmul(ps[0:8, :], wfa[:, kc, :], hTc[:, kc, hs], start=(kc == 0), stop=(kc == 7)),
                          [wfa, hTc], [ps])
                ev = evf[cnt["ek"] % 8]; cnt["ek"] += 1
                cx.op("dve", lambda: V.tensor_copy(ev[0:8, :], ps[0:8, :]), [ps], [ev])
                cx.dma("sp", faT[:, tok0 + half * 512:tok0 + (half + 1) * 512], ev[0:8, :], reads=[ev], writes=[faT], sb=ev)
            for pi_, (nm, c0) in enumerate(pieces):
                if pi_ == 8 and ch + 1 < NC2:
                    compute_h(ch + 1)
                wt = wring[cnt["wk"] % 3]; cnt["wk"] += 1
                wsrc = (w_gate if nm[0] == "g" and nm[1].isdigit() else w_in)
                cx.dma("pool", None, None, reads=[wsrc], writes=[wt], sb=wt,
                       fn=lambda: P.dma_start(out=wt[:], in_=wsrc[l, :, c0:c0 + 512].rearrange("(kc p) n -> p kc n", p=128)))
                if nm in ("va", "vb"):
                    for tt in range(TPC):
                        ps = next_ps()
                        for kc in range(8):
                            cx.op("pe", lambda: nc.tensor.matmul(ps[:], hTc[:, kc, tt * 128:(tt + 1) * 128], wt[:, kc, :], start=(kc == 0), stop=(kc == 7)),
                                  [hTc, wt], [ps])
                        ev = evac("bf", ps, 1.0)
                        cx.dma("sp", vv[0 if nm == "va" else 1, tok0 + tt * 128:tok0 + (tt + 1) * 128, :], ev[:], reads=[ev], writes=[vv], sb=ev)
                    continue
                for m in range(4):
                    for half in range(TC // 512):
                        hs = slice(half * 512, (half + 1) * 512)
                        tk = tok0 + half * 512
                        ps = next_ps()
                        for kc in range(8):
                            cx.op("pe", lambda: nc.tensor.matmul(ps[:], wt[:, kc, m * 128:(m + 1) * 128], hTc[:, kc, hs], start=(kc == 0), stop=(kc == 7)),
                                  [wt, hTc], [ps])
                        if nm in ("qa", "ka", "qb", "kb"):
                            ev = evac("bf", ps, 0.125 if nm[0] == "q" else 1.0)
                            qi = ("qa", "ka", "qb", "kb").index(nm)
                            cx.dma("sp", qkT[qi, m * 128:(m + 1) * 128, tk:tk + 512], ev[:], reads=[ev], writes=[qkT], sb=ev)
                        elif nm[0] == "x" or nm[:2] == "gc":
                            ev = evac("f", ps)
                            r0 = int(nm[2]) * 512 + m * 128
                            cx.dma("sp", xgT[0 if nm[0] == "x" else 1, r0:r0 + 128, tk:tk + 512], ev[:], reads=[ev], writes=[xgT], sb=ev)
                        else:
                            mi = int(nm[1]) * 4 + m
                            ev = evac("g", ps, mi)
                            cx.dma("sp", gT[mi * 128:(mi + 1) * 128, tk:tk + 512], ev[:], reads=[ev], writes=[gT], sb=ev)
        cx.end_phase()
        ph.close()

    def phase_fprep(l):
        ph = ExitStack()
        bf = cx.sbuf(ph, "bf", [H, 1]); nbf = cx.sbuf(ph, "nbf", [H, 1])
        with nc.allow_non_contiguous_dma(reason="tiny"):
            cx.dma("sp", bf[:], b_f[l].rearrange("(h o) -> h o", o=1), reads=[b_f], writes=[bf], sb=bf)
        cx.op("dve", lambda: V.tensor_scalar_mul(nbf[:], bf[:], -1.0), [bf], [nbf])
        ones = cx.sbuf(ph, "ones", [H, SEQ]); cx.op("dve", lambda: V.memset(ones[:], 1.0), [], [ones])
        for s in range(NSEQ):
            fa = cx.sbuf(ph, "fa%d" % s, [H, SEQ]); e = cx.sbuf(ph, "fe%d" % s, [H, SEQ]); sp_ = cx.sbuf(ph, "fs%d" % s, [H, SEQ])
            F = cx.sbuf(ph, "F%d" % s, [H, SEQ]); r1 = cx.sbuf(ph, "r1%d" % s, [H, SEQ]); r2 = cx.sbuf(ph, "r2%d" % s, [H, SEQ])
            parts = cx.sbuf(ph, "parts%d" % s, [H, 6, SEQ], BF16)
            cx.dma("sp", fa[:], faT[:, s * SEQ:(s + 1) * SEQ], reads=[faT], writes=[fa], sb=fa)
            cx.op("act", lambda: S.activation(e[:], fa[:], AF.Exp, bias=nbf[:, 0:1], scale=-1.0), [fa, nbf], [e])
            cx.op("act", lambda: S.activation(sp_[:], e[:], AF.Ln, bias=1.0, scale=1.0), [e], [sp_])
            cx.op("dve", lambda: V.tensor_tensor_scan(F[:], ones[:], sp_[:], 0.0, ALU.mult, ALU.subtract), [ones, sp_], [F])
            cx.op("dve", lambda: V.tensor_copy(parts[:, 0, :], F[:]), [F], [parts])
            cx.op("dve", lambda: V.tensor_tensor(r1[:], F[:], parts[:, 0, :], ALU.subtract), [F, parts], [r1])
            cx.op("dve", lambda: V.tensor_copy(parts[:, 1, :], r1[:]), [r1], [parts])
            cx.op("dve", lambda: V.tensor_tensor(r2[:], r1[:], parts[:, 1, :], ALU.subtract), [r1, parts], [r2])
            cx.op("dve", lambda: V.tensor_copy(parts[:, 2, :], r2[:]), [r2], [parts])
            cx.op("dve", lambda: V.tensor_scalar_mul(parts[:, 3:6, :], parts[:, 0:3, :], -1.0), [parts], [parts])
            cx.dma("sp", Fd[:, :, s * SEQ:(s + 1) * SEQ].rearrange("v h t -> h v t"), parts[:], reads=[parts], writes=[Fd], sb=parts)
        cx.end_phase()
        ph.close()

    def gen_fox(ph):
        NB = 2
        kT = [cx.sbuf(ph, "fkT%d" % i, [70, SEQ], BF16) for i in range(NB)]
        qT = [cx.sbuf(ph, "fqT%d" % i, [70, SEQ], BF16) for i in range(NB)]
        Va = [cx.sbuf(ph, "Va%d" % i, [128, 16, 128], BF16) for i in range(NB)]
        Pt = [cx.sbuf(ph, "Pt%d" % i, [128, 512], BF16) for i in range(3)]
        rec = [cx.sbuf(ph, "rec%d" % i, [128, 512]) for i in range(2)]
        ob = [cx.sbuf(ph, "fob%d" % i, [64, 512], BF16) for i in range(2)]
        for i in range(NB):
            cx.op("dve", lambda: V.memset(kT[i][64:70, :], 1.0), [], [kT[i]])
            cx.op("dve", lambda: V.memset(qT[i][64:70, :], 1.0), [], [qT[i]])
            cx.op("dve", lambda: V.memset(Va[i][:], 1.0), [], [Va[i]])
        it = 0; pk = 0; ck = 0
        units = [(s, h) for s in range(NSEQ) for h in range(H)]

        def loads(ui):
            s, h = units[ui]
            b = ui % NB
            t0 = s * SEQ
            cx.dma("sp", qT[b][0:64, :], qkT[0, h * 64:(h + 1) * 64, t0:t0 + SEQ], reads=[qkT], writes=[qT[b]], sb=qT[b])
            cx.dma("sp", qT[b][64:67, :], Fd[0:3, h, t0:t0 + SEQ], reads=[Fd], writes=[qT[b]], sb=qT[b])
            cx.dma("sp", kT[b][0:64, :], qkT[1, h * 64:(h + 1) * 64, t0:t0 + SEQ], reads=[qkT], writes=[kT[b]], sb=kT[b])
            cx.dma("sp", kT[b][67:70, :], Fd[3:6, h, t0:t0 + SEQ], reads=[Fd], writes=[kT[b]], sb=kT[b])
            with nc.allow_non_contiguous_dma(reason="v head slice, 128B runs"):
                cx.dma("sp", Va[b][:, :, 0:64], vv[0, t0:t0 + SEQ, h * 64:(h + 1) * 64].rearrange("(j p) d -> p j d", p=128),
                       reads=[vv], writes=[Va[b]], sb=Va[b])

        loads(0); loads(1)
        steps = [(ui, c, J) for ui in range(len(units)) for c in range(4) for J in range(4 * c + 4)]
        pend = {}
        for k in range(len(steps) + 2):
            if k < len(steps):
                ui, c, J = steps[k]
                s, h = units[ui]; b = ui % NB
                q0 = c * 512
                lo = 128 * max(0, J - 4 * c)
                Sb = pb[pk % 4]; Pb = Pt[pk % 3]; pk += 1
                diag = J >= 4 * c
                cx.op("pe", lambda: nc.tensor.matmul(Sb[:, lo:512], kT[b][:, J * 128:(J + 1) * 128], qT[b][:, q0 + lo:q0 + 512], start=True, stop=not diag),
                      [kT[b], qT[b]], [Sb])
                if diag:
                    cx.op("pe", lambda: nc.tensor.matmul(Sb[:, lo:lo + 128], identb[:], masksb[:, M_NEGC, :], start=False, stop=True),
                          [identb, masksb], [Sb])
                cx.op("act", lambda: S.activation(Pb[:, lo:512], Sb[:, lo:512], AF.Exp), [Sb], [Pb])
                pend[k] = (lo, Pb)
            if k >= 2:
                ui, c, J = steps[k - 2]
                s, h = units[ui]; b = ui % NB
                lo, Pb = pend.pop(k - 2)
                nJ = 4 * c + 4
                gci = ui * 4 + c
                O = pb[4 + gci % 2]
                cx.op("pe", lambda: nc.tensor.matmul(O[:, lo:512], Va[b][:, J, :], Pb[:, lo:512], start=(J == 0), stop=(J == nJ - 1)),
                      [Va[b], Pb], [O])
                if J == nJ - 1:
                    rc = rec[gci % 2]; o_ = ob[gci % 2]
                    t0 = s * SEQ; q0 = c * 512
                    cx.op("dve", lambda: V.reciprocal(rc[64:128, :], O[64:128, :]), [O], [rc])
                    cx.op("dve", lambda: V.tensor_tensor(o_[:], O[0:64, :], rc[64:128, :], ALU.mult), [O, rc], [o_])
                    cx.dma("sp", oT[h * 64:(h + 1) * 64, t0 + q0:t0 + q0 + 512], o_[:], reads=[o_], writes=[oT], sb=o_)
                    if c == 3 and ui + 2 < len(units):
                        loads(ui + 2)
            yield

    def gen_sb(ph):
        NB = 2
        kT = [cx.sbuf(ph, "kT%d" % i, [64, SEQ], BF16) for i in range(NB)]
        qT = [cx.sbuf(ph, "qT%d" % i, [64, SEQ], BF16) for i in range(NB)]
        Vb = [cx.sbuf(ph, "Vb%d" % i, [128, 16, 64], BF16) for i in range(NB)]
        Et = [cx.sbuf(ph, "Et%d" % i, [128, 512]) for i in range(3)]
        SPt = [cx.sbuf(ph, "SPt%d" % i, [128, 512], F32R) for i in range(4)]
        R = [cx.sbuf(ph, "R%d" % i, [128, 512], F32R) for i in range(2)]
        Wt = [cx.sbuf(ph, "Wt%d" % i, [128, 512], BF16) for i in range(3)]
        ob = [cx.sbuf(ph, "ob%d" % i, [64, 512], BF16) for i in range(2)]
        Zf = cx.sbuf(ph, "Zf", [128, 512])
        cx.op("dve", lambda: V.memset(Zf[:], 0.0), [], [Zf])
        it = 0; zk = 0; ak = 0; ck = 0; k3 = 0; wk = 0
        units = [(s, h) for s in range(NSEQ) for h in range(H)]

        def loads(ui):
            s, h = units[ui]
            b = ui % NB
            t0 = s * SEQ
            cx.dma("sp", qT[b][:], qkT[2, h * 64:(h + 1) * 64, t0:t0 + SEQ], reads=[qkT], writes=[qT[b]], sb=qT[b])
            cx.dma("sp", kT[b][:], qkT[3, h * 64:(h + 1) * 64, t0:t0 + SEQ], reads=[qkT], writes=[kT[b]], sb=kT[b])
            with nc.allow_non_contiguous_dma(reason="v head slice, 128B runs"):
                cx.dma("sp", Vb[b][:], vv[1, t0:t0 + SEQ, h * 64:(h + 1) * 64].rearrange("(j p) d -> p j d", p=128),
                       reads=[vv], writes=[Vb[b]], sb=Vb[b])

        loads(0); loads(1)
        steps = [(ui, c, 4 * c + 3 - i) for ui in range(len(units)) for c in range(4) for i in range(4 * c + 4)]
        st_ = {}
        for k in range(len(steps) + 2):
            if k < len(steps):
                ui, c, J = steps[k]
                s, h = units[ui]; b = ui % NB
                gci = ui * 4 + c
                q0 = c * 512
                top = (J == 4 * c + 3)
                if top:
                    cx.op("dve", lambda: V.tensor_copy(R[gci % 2][:], Zf[:]), [Zf], [R[gci % 2]])
                lo = 128 * max(0, J - 4 * c)
                diag = J >= 4 * c
                Z = pb[zk % 2]; zk += 1
                e_ = Et[k3 % 3]; sp_ = SPt[k3 % 4]; k3 += 1
                cx.op("pe", lambda: nc.tensor.matmul(Z[:, lo:512], kT[b][:, J * 128:(J + 1) * 128], qT[b][:, q0 + lo:q0 + 512], start=True, stop=True),
                      [kT[b], qT[b]], [Z])
                cx.op("act", lambda: S.activation(e_[:, lo:512], Z[:, lo:512], AF.Exp), [Z], [e_])
                cx.op("act", lambda: S.activation(sp_[:, lo:512], e_[:, lo:512], AF.Ln, bias=1.0, scale=1.0), [e_], [sp_])
                if diag:
                    cx.op("dve", lambda: V.tensor_tensor(sp_[:, lo:lo + 128], sp_[:, lo:lo + 128].bitcast(F32), masks[:, M_SUT, :], ALU.mult), [sp_, masks], [sp_])
                st_[k] = (lo, diag, top, sp_)
            if 1 <= k <= len(steps):
                ui, c, J = steps[k - 1]
                s, h = units[ui]; b = ui % NB
                gci = ui * 4 + c
                q0 = c * 512
                Rc = R[gci % 2]
                lo, diag, top, sp_ = st_[k - 1]
                Ab = pb[2 + ak % 2]; ak += 1
                w_ = Wt[wk % 3]; wk += 1
                cx.op("pe", lambda: nc.tensor.matmul(Ab[:, lo:512], kT[b][:, J * 128:(J + 1) * 128], qT[b][:, q0 + lo:q0 + 512], start=True, stop=False),
                      [kT[b], qT[b]], [Ab])
                cx.op("pe", lambda: nc.tensor.matmul(Ab[:, lo:512], masksr[:, M_NTRII, :], sp_[:, lo:512], start=False, stop=(top and not diag)),
                      [masksr, sp_], [Ab])
                if not top:
                    cx.op("pe", lambda: nc.tensor.matmul(Ab[:, lo:512], masksr[:, M_NONES, :], Rc[:, lo:512], start=False, stop=not diag),
                          [masksr, Rc], [Ab])
                if diag:
                    cx.op("pe", lambda: nc.tensor.matmul(Ab[:, lo:lo + 128], identb[:], masksb[:, M_NEGNS, :], start=False, stop=True),
                          [identb, masksb], [Ab])
                cx.op("act", lambda: S.activation(w_[:, lo:512], Ab[:, lo:512], AF.Exp), [Ab], [w_])
                if J > 0:
                    cx.op("dve", lambda: V.tensor_tensor(Rc[:, lo:512], Rc[:, lo:512].bitcast(F32), sp_[:, lo:512].bitcast(F32), ALU.add), [Rc, sp_], [Rc])
                st_[k - 1] = (lo, diag, top, sp_, w_)
            if k >= 2:
                ui, c, J = steps[k - 2]
                s, h = units[ui]; b = ui % NB
                gci = ui * 4 + c
                O = pb[4 + gci % 2]; o_ = ob[gci % 2]
                lo, diag, top, sp_, w_ = st_.pop(k - 2)
                cx.op("pe", lambda: nc.tensor.matmul(O[0:64, lo:512], Vb[b][:, J, :], w_[:, lo:512], start=top, stop=(J == 0)),
                      [Vb[b], w_], [O])
                if J == 0:
                    t0 = s * SEQ; q0 = c * 512
                    cx.op("act", lambda: S.copy(o_[:], O[0:64, :]), [O], [o_])
                    cx.dma("sp", oT[512 + h * 64:512 + (h + 1) * 64, t0 + q0:t0 + q0 + 512], o_[:], reads=[o_], writes=[oT], sb=o_)
                    if c == 3 and ui + 2 < len(units):
                        loads(ui + 2)
            yield

    def gen_lru(ph, l):
        def colvec(name, src_row):
            t = cx.sbuf(ph, name, [128, 8])
            with nc.allow_non_contiguous_dma(reason="tiny"):
                cx.dma("sp", t[:], src_row.rearrange("(m p) -> p m", p=128), reads=[], writes=[t], sb=t)
            return t
        cb = colvec("cb", conv_b[l]); ba = colvec("ba", lru_ba[l]); bx = colvec("bx", lru_bx[l]); lam = colvec("lam", lru_lambda[l])
        cw = cx.sbuf(ph, "cw", [128, 4, 8])
        with nc.allow_non_contiguous_dma(reason="tiny"):
            cx.dma("sp", cw[:], conv_w[l].rearrange("i (m p) -> p i m", p=128), reads=[], writes=[cw], sb=cw)
        el = cx.sbuf(ph, "el", [128, 8]); cA = cx.sbuf(ph, "cA", [128, 8]); cA2 = cx.sbuf(ph, "cA2", [128, 8])
        cx.op("act", lambda: S.activation(el[:], lam[:], AF.Exp, scale=-1.0), [lam], [el])
        cx.op("act", lambda: S.activation(cA[:], el[:], AF.Ln, bias=1.0, scale=1.0), [el], [cA])
        cx.op("dve", lambda: V.tensor_scalar_mul(cA2[:], cA[:], -16.0), [cA], [cA2])
        cx.op("dve", lambda: V.tensor_scalar_mul(cA[:], cA[:], -8.0), [cA], [cA])
        BDf = cx.sbuf(ph, "BDf", [128, 2, 128]); BD = [cx.sbuf(ph, "BD%d" % i, [128, 2, 128], F32R) for i in range(2)]
        cx.op("dve", lambda: V.memset(BDf[:], 0.0), [], [BDf])
        N = SEQ
        xc = [cx.sbuf(ph, "xc%d" % i, [128, 3 + N]) for i in range(2)]
        gc = [cx.sbuf(ph, "gc%d" % i, [128, N]) for i in range(2)]
        xvs = [cx.sbuf(ph, "xv%d" % i, [128, N], F32R) for i in range(2)]
        tas = [cx.sbuf(ph, "t_a%d" % i, [128, N]) for i in range(2)]
        t_b = cx.sbuf(ph, "t_b", [128, N])
        t_c = cx.sbuf(ph, "t_c", [128, N]); t_d = cx.sbuf(ph, "t_d", [128, N]); hh = cx.sbuf(ph, "hh", [128, N])
        oc = [cx.sbuf(ph, "oc%d" % i, [128, N], BF16) for i in range(2)]
        units = [(m, s) for m in range(8) for s in range(NSEQ)]

        def stage1(ui):
            m, s = units[ui]
            bd = BD[m % 2]
            if s == 0:
                for g_, wsrc in enumerate((lru_wa, lru_wx)):
                    cx.dma("sp", BDf[0:64, g_, 0:64], wsrc[l, 2 * m], reads=[], writes=[BDf], sb=BDf)
                    yield
                    cx.dma("sp", BDf[64:128, g_, 64:128], wsrc[l, 2 * m + 1], reads=[], writes=[BDf], sb=BDf)
                    yield
                cx.op("dve", lambda: V.tensor_copy(bd[:], BDf[:]), [BDf], [bd])
                yield
            x_ = xc[ui % 2]; g = gc[ui % 2]; xv = xvs[ui % 2]; t_a = tas[ui % 2]
            t0 = s * SEQ
            cx.op("pool", lambda: P.memset(x_[:, 0:3], 0.0), [], [x_])
            yield
            cx.dma("sp", x_[:, 3:3 + N], xgT[0, m * 128:(m + 1) * 128, t0:t0 + N], reads=[xgT], writes=[x_], sb=x_)
            yield
            cx.dma("sp", g[:], xgT[1, m * 128:(m + 1) * 128, t0:t0 + N], reads=[xgT], writes=[g], sb=g)
            yield
            cx.op("dve", lambda: V.tensor_scalar(t_a[:], x_[:, 0:N], cw[:, 0, m:m + 1], cb[:, m:m + 1], ALU.mult, ALU.add), [x_, cw, cb], [t_a])
            yield
            for i in (1, 2):
                cx.op("dve", lambda: V.scalar_tensor_tensor(t_a[:], x_[:, i:i + N], cw[:, i, m:m + 1], t_a[:], ALU.mult, ALU.add), [x_, cw, t_a], [t_a])
                yield
            cx.op("dve", lambda: V.scalar_tensor_tensor(t_a[:], x_[:, 3:3 + N], cw[:, 3, m:m + 1], t_a[:], ALU.mult, ALU.add), [x_, cw, t_a], [t_a])
            yield
            cx.op("act", lambda: S.copy(xv[:], t_a[:]), [t_a], [xv])
            yield
            cx.op("pool", lambda: P.tensor_tensor(t_d[:], g[:], g[:], ALU.mult), [g], [t_d])
            yield
            cx.op("pool", lambda: P.tensor_scalar(t_d[:], t_d[:], 0.044715, 1.0, ALU.mult, ALU.add), [t_d], [t_d])
            yield
            cx.op("pool", lambda: P.tensor_tensor(t_d[:], t_d[:], g[:], ALU.mult), [t_d, g], [t_d])
            yield
            cx.op("act", lambda: S.activation(t_d[:], t_d[:], AF.Sigmoid, scale=1.5957691216057308), [t_d], [t_d])
            yield
            cx.op("pool", lambda: P.tensor_tensor(g[:], t_d[:], g[:], ALU.mult), [t_d, g], [g])
            yield

        def stage2(ui):
            m, s = units[ui]
            bd = BD[m % 2]
            g = gc[ui % 2]; xv = xvs[ui % 2]; t_a = tas[ui % 2]; o_ = oc[ui % 2]
            t0 = s * SEQ
            for q in range(N // 512):
                pr = pb[6]; pi = pb[6]
                cs = slice(q * 512, (q + 1) * 512)
                cx.op("pe", lambda: nc.tensor.matmul(pr[:], bd[:, 0, :], xv[:, cs], start=True, stop=True), [bd, xv], [pr])
                yield
                cx.op("act", lambda: S.activation(t_b[:, cs], pr[:], AF.Sigmoid, bias=ba[:, m:m + 1], scale=1.0), [pr, ba], [t_b])
                cx.op("pe", lambda: nc.tensor.matmul(pi[:], bd[:, 1, :], xv[:, cs], start=True, stop=True), [bd, xv], [pi])
                yield
                cx.op("act", lambda: S.activation(t_c[:, cs], pi[:], AF.Sigmoid, bias=bx[:, m:m + 1], scale=1.0), [pi, bx], [t_c])
            cx.op("dve", lambda: V.tensor_tensor(t_c[:], t_c[:], t_a[:], ALU.mult), [t_c, t_a], [t_c])
            yield
            cx.op("act", lambda: S.activation(t_a[:], t_b[:], AF.Exp, scale=cA[:, m:m + 1]), [t_b, cA], [t_a])
            yield
            cx.op("act", lambda: S.activation(t_b[:], t_b[:], AF.Exp, scale=cA2[:, m:m + 1]), [t_b, cA2], [t_b])
            yield
            cx.op("act", lambda: S.activation(t_b[:], t_b[:], AF.Ln, bias=1.0, scale=-1.0), [t_b], [t_b])
            yield
            cx.op("act", lambda: S.activation(t_b[:], t_b[:], AF.Exp, scale=0.5), [t_b], [t_b])
            yield
            cx.op("dve", lambda: V.tensor_tensor(t_c[:], t_c[:], t_b[:], ALU.mult), [t_c, t_b], [t_c])
            cx.op("dve", lambda: V.tensor_tensor_scan(hh[:], t_a[:], t_c[:], 0.0, ALU.mult, ALU.add), [t_a, t_c], [hh])
            yield
            cx.op("dve", lambda: V.tensor_tensor(o_[:], hh[:], g[:], ALU.mult), [hh, g], [o_])
            yield
            cx.dma("sp", oT[1024 + m * 128:1024 + (m + 1) * 128, t0:t0 + N], o_[:], reads=[o_], writes=[oT], sb=o_)
            yield

        yield from stage1(0)
        for ui in range(len(units)):
            if ui + 1 < len(units):
                yield from stage1(ui + 1)
            yield from stage2(ui)

    def phase_mix(l):
        ph = ExitStack()
        def attn():
            yield from gen_fox(ph)
            yield from gen_sb(ph)
        ga = attn(); gr = gen_lru(ph, l)
        done_a = done_r = False
        while not (done_a and done_r):
            for _ in range(2):
                if not done_a:
                    try:
                        next(ga)
                    except StopIteration:
                        done_a = True
            if not done_r:
                try:
                    next(gr)
                except StopIteration:
                    done_r = True
        cx.end_phase()
        ph.close()

    def ln_tile(ph_bufs, y_src_bufs, y_ap_halves, x_t, gtile, lg, lb, dst_ap, dst_buf, k):
        tt_, st6, mv, rs, res_ = ph_bufs
        t = tt_[k % 2]; r = res_[k % 4]; s6 = st6[k % 2]; mv_ = mv[k % 2]; rs_ = rs[k % 2]
        for hf_ in range(2):
            cs = slice(hf_ * 512, (hf_ + 1) * 512)
            cx.op("dve", lambda: V.tensor_tensor(t[:, cs], y_ap_halves[hf_], gtile[:, cs], ALU.mult), list(y_src_bufs) + [gtile], [t])
        cx.op("dve", lambda: V.scalar_tensor_tensor(t[:], x_t[:], ALPHA, t[:], ALU.mult, ALU.add), [x_t, t], [t])
        for hf_ in range(2):
            cx.op("dve", lambda: V.bn_stats(s6[:, hf_, :], t[:, hf_ * 512:(hf_ + 1) * 512]), [t], [s6])
        cx.op("dve", lambda: V.bn_aggr(mv_[:], s6[:].rearrange("p a b -> p (a b)")), [s6], [mv_])
        cx.op("dve", lambda: V.tensor_scalar_add(rs_[:], mv_[:, 1:2], LN_EPS), [mv_], [rs_])
        cx.op("act", lambda: S.activation(rs_[:], rs_[:], AF.Ln), [rs_], [rs_])
        cx.op("act", lambda: S.activation(rs_[:], rs_[:], AF.Exp, scale=-0.5), [rs_], [rs_])
        cx.op("dve", lambda: V.tensor_scalar(t[:], t[:], mv_[:, 0:1], rs_[:, 0:1], ALU.subtract, ALU.mult), [t, mv_, rs_], [t])
        cx.op("pool", lambda: P.tensor_tensor(t[:], t[:], lg[:], ALU.mult), [t, lg], [t])
        cx.op("dve", lambda: V.tensor_tensor(r[:], t[:], lb[:], ALU.add), [t, lb], [r])
        cx.dma("sp", dst_ap, r[:], reads=[r], writes=[dst_buf], sb=r)
        return r

    def ln_bufs(ph):
        return ([cx.sbuf(ph, "lnt%d" % i, [128, D]) for i in range(2)],
                [cx.sbuf(ph, "lns%d" % i, [128, 2, 6]) for i in range(2)],
                [cx.sbuf(ph, "lnm%d" % i, [128, 2]) for i in range(2)],
                [cx.sbuf(ph, "lnr%d" % i, [128, 1]) for i in range(2)],
                [cx.sbuf(ph, "lno%d" % i, [128, D]) for i in range(4)])

    def phase_merge(l, xsrc, xdst):
        ph = ExitStack()
        mod = load_mod(ph, l, 0, ("gt",))
        lg = cx.sbuf(ph, "lg", [128, D]); lb = cx.sbuf(ph, "lb", [128, D])
        cx.dma("sp", lg[:], row_bc(ln1_g[l, :], 128), reads=[], writes=[lg], sb=lg)
        cx.dma("sp", lb[:], row_bc(ln1_b[l, :], 128), reads=[], writes=[lb], sb=lb)
        wpa = cx.sbuf(ph, "wpa", [128, 4, D], BF16); wpb = cx.sbuf(ph, "wpb", [128, 4, D], BF16)
        wpc = cx.sbuf(ph, "wpc", [128, 8, D], BF16); wo = cx.sbuf(ph, "wo", [128, 8, D], BF16)
        for t_, src in ((wpa, w_pa), (wpb, w_pb), (wpc, w_pc), (wo, w_o)):
            cx.dma("pool", None, None, reads=[], writes=[t_], sb=t_,
                   fn=lambda: P.dma_start(out=t_[:], in_=src[l].rearrange("(kc p) n -> p kc n", p=128)))
        oTc = [cx.sbuf(ph, "oTc%d" % i, [128, 16, 512], BF16) for i in range(1)]
        gg = [cx.sbuf(ph, "gg%d" % i, [128, 3, 512]) for i in range(2)]
        ta = [cx.sbuf(ph, "ta%d" % i, [128, 512]) for i in range(2)]
        tb = [cx.sbuf(ph, "tb%d" % i, [128, 512]) for i in range(2)]
        mT = [cx.sbuf(ph, "mT%d" % i, [128, 8, 512], BF16) for i in range(1)]
        xt = [cx.sbuf(ph, "xt%d" % i, [128, D]) for i in range(2)]
        lnb = ln_bufs(ph)
        rs = route_setup(ph, l)
        hist = []
        gk = 0; k = 0
        for ch in range(NCH):
            s = ch // (SEQ // 512)
            tok0 = ch * 512
            oc_ = oTc[0]; mT_ = mT[0]
            cx.dma("sp", oc_[:], oT[:, tok0:tok0 + 512].rearrange("(kc p) t -> p kc t", p=128), reads=[oT], writes=[oc_], sb=oc_)
            for m in range(8):
                g_ = gg[gk % 2]; a_ = ta[gk % 2]; b_ = tb[gk % 2]; gk += 1
                cx.dma("sp", g_[:], gT[:, tok0:tok0 + 512].rearrange("(j q p) t -> q p j t", j=3, p=128)[m], reads=[gT], writes=[g_], sb=g_)
                pa, pb_, pc = pb[0 + 3 * (m % 2)], pb[1 + 3 * (m % 2)], pb[2 + 3 * (m % 2)]
                for kc in range(4):
                    cx.op("pe", lambda: nc.tensor.matmul(pa[:], wpa[:, kc, m * 128:(m + 1) * 128], oc_[:, kc, :], start=(kc == 0), stop=(kc == 3)), [wpa, oc_], [pa])
                for kc in range(4):
                    cx.op("pe", lambda: nc.tensor.matmul(pb_[:], wpb[:, kc, m * 128:(m + 1) * 128], oc_[:, 4 + kc, :], start=(kc == 0), stop=(kc == 3)), [wpb, oc_], [pb_])
                for kc in range(8):
                    cx.op("pe", lambda: nc.tensor.matmul(pc[:], wpc[:, kc, m * 128:(m + 1) * 128], oc_[:, 8 + kc, :], start=(kc == 0), stop=(kc == 7)), [wpc, oc_], [pc])
                cx.op("dve", lambda: V.tensor_tensor(a_[:], pa[:], g_[:, 0, :], ALU.mult), [pa, g_], [a_])
                cx.op("dve", lambda: V.tensor_tensor(b_[:], pb_[:], g_[:, 1, :], ALU.mult), [pb_, g_], [b_])
                cx.op("dve", lambda: V.tensor_tensor(a_[:], a_[:], b_[:], ALU.add), [a_, b_], [a_])
                cx.op("dve", lambda: V.tensor_tensor(b_[:], pc[:], g_[:, 2, :], ALU.mult), [pc, g_], [b_])
                cx.op("dve", lambda: V.tensor_tensor(mT_[:, m, :], a_[:], b_[:], ALU.add), [a_, b_], [mT_])
            for tt in range(4):
                ti = ch * 4 + tt
                x_t = xt[k % 2]
                cx.dma("sp", x_t[:], xsrc[ti * 128:(ti + 1) * 128, :], reads=[xsrc], writes=[x_t], sb=x_t)
                ys = []
                for hf_ in range(2):
                    yb = pb[(k * 2 + hf_) % 6]
                    for m in range(8):
                        cx.op("pe", lambda: nc.tensor.matmul(yb[:], mT_[:, m, tt * 128:(tt + 1) * 128], wo[:, m, hf_ * 512:(hf_ + 1) * 512], start=(m == 0), stop=(m == 7)),
                              [mT_, wo], [yb])
                    ys.append(yb)
                if len(hist) >= 1:
                    route_stage(rs, *hist[-1], 1)
                if len(hist) >= 2:
                    route_stage(rs, *hist[-2], 3)
                r_ = ln_tile(lnb, ys, [ys[0][:], ys[1][:]], x_t, mod[("gt", s)], lg, lb, xdst[ti * 128:(ti + 1) * 128, :], xdst, k)
                route_stage(rs, ti, r_, 0)
                if len(hist) >= 1:
                    route_stage(rs, *hist[-1], 2)
                if len(hist) >= 2:
                    route_stage(rs, *hist[-2], 4)
                hist.append((ti, r_))
                k += 1
        route_stage(rs, *hist[-1], 1)
        route_stage(rs, *hist[-2], 3)
        route_stage(rs, *hist[-1], 2)
        route_stage(rs, *hist[-2], 4)
        route_stage(rs, *hist[-1], 3)
        route_stage(rs, *hist[-1], 4)
        cx.op("dve", lambda: V.tensor_copy(CNTI[0:1, :], rs["cnt"][0:1, :]), [rs["cnt"]], [CNTI])
        if "cnt" in dbg:
            cx.dma("sp", dbg["cnt"][:], rs["cnt"][:], reads=[rs["cnt"]], writes=[dbg["cnt"]], sb=rs["cnt"])
        cx.end_phase()
        ph.close()

    def route_setup(ph, l):
        rs = {}
        rs["mod"] = load_mod(ph, l, 1, ("sh", "sc"))
        wr = cx.sbuf(ph, "wr", [128, 8, NE])
        with nc.allow_non_contiguous_dma(reason="router weights 128B runs"):
            cx.dma("sp", wr[:], w_router[l].rearrange("(kc p) e -> p kc e", p=128), reads=[], writes=[wr], sb=wr)
        brt = cx.sbuf(ph, "brt", [128, NE])
        cx.dma("sp", brt[:], row_bc(b_router[l, :], 128), reads=[], writes=[brt], sb=brt)
        cnt = cx.sbuf(ph, "cnt", [128, NE]); cx.op("dve", lambda: V.memset(cnt[:], 0.0), [], [cnt])
        rs.update(wr=wr, brt=brt, cnt=cnt)
        rs["hf"] = [cx.sbuf(ph, "rhf%d" % i, [128, D]) for i in range(3)]
        rs["hb"] = [cx.sbuf(ph, "rhb%d" % i, [128, D], BF16) for i in range(3)]
        rs["hT"] = [cx.sbuf(ph, "rhT%d" % i, [128, 8, 128]) for i in range(3)]
        rs["pp"] = pbh[:].bitcast(F32)
        rs["ppb"] = pbh
        def sm(name, w=NE):
            return [cx.sbuf(ph, "%s%d" % (name, i), [128, w]) for i in range(3)]
        for nm, w in (("lgt", NE), ("m8", 8), ("msk", NE), ("ex", NE), ("em", NE), ("ssum", 1), ("gte", NE), ("slv", NE),
                      ("s8", 8), ("nmx", 1), ("tmp", NE)):
            rs[nm] = sm(nm, w)
        return rs

    def route_stage(rs, ti, x_t, stage):
        mod = rs["mod"]; wr = rs["wr"]; brt = rs["brt"]; cnt = rs["cnt"]
        s = ti // (SEQ // 128)
        b = ti % 3
        h_f = rs["hf"][b]; h_b = rs["hb"][b]; hT_ = rs["hT"][b]
        lgt, m8, msk, ex, em, ssum, gte, slv, s8, nmx, tmp = (rs[k] for k in ("lgt", "m8", "msk", "ex", "em", "ssum", "gte", "slv", "s8", "nmx", "tmp"))
        L_ = lgt[b]; M8 = m8[b]; MK = msk[b]; SL = slv[b]
        pp = rs["pp"]
        if stage == 0:
            cx.op("dve", lambda: V.tensor_tensor(h_f[:], x_t[:], mod[("sc", s)][:], ALU.mult), [x_t, mod[("sc", s)]], [h_f])
            cx.op("dve", lambda: V.tensor_tensor(h_f[:], h_f[:], mod[("sh", s)][:], ALU.add), [h_f, mod[("sh", s)]], [h_f])
            cx.op("act", lambda: S.copy(h_b[:], h_f[:]), [h_f], [h_b])
        elif stage == 1:
            pt = pb[6]
            for half in range(2):
                for q in range(4):
                    kc = half * 4 + q
                    cx.op("pe", lambda: nc.tensor.transpose(pt[:, q * 128:(q + 1) * 128], h_f[:, kc * 128:(kc + 1) * 128], identf[:]), [h_f, identf], [pt])
                cx.op("act", lambda: S.copy(hT_[:, half * 4:half * 4 + 4, :], pt[:].rearrange("p (q t) -> p q t", q=4)), [pt], [hT_])
            for kc in range(8):
                cx.op("pe", lambda: nc.tensor.matmul(pp[:, 0:NE], hT_[:, kc, :], wr[:, kc, :], start=(kc == 0), stop=(kc == 7)), [hT_, wr], [rs["ppb"]])
        elif stage == 2:
            cx.op("dve", lambda: V.tensor_tensor(L_[:], pp[:, 0:NE], brt[:], ALU.add), [rs["ppb"], brt], [L_])
            cx.op("dve", lambda: V.max(M8[:], L_[:]), [L_], [M8])
            cx.op("dve", lambda: V.tensor_scalar(MK[:], L_[:], M8[:, 3:4], None, ALU.is_ge), [L_, M8], [MK])
            cx.op("dve", lambda: V.tensor_scalar_mul(nmx[b][:], M8[:, 0:1], -1.0), [M8], [nmx[b]])
            cx.op("act", lambda: S.activation(ex[b][:], L_[:], AF.Exp, bias=nmx[b][:, 0:1], scale=1.0), [L_, nmx[b]], [ex[b]])
            cx.op("dve", lambda: V.tensor_tensor(em[b][:], ex[b][:], MK[:], ALU.mult), [ex[b], MK], [em[b]])
            cx.op("dve", lambda: V.reduce_sum(ssum[b][:], em[b][:], mybir.AxisListType.X), [em[b]], [ssum[b]])
            cx.op("dve", lambda: V.reciprocal(ssum[b][:], ssum[b][:]), [ssum[b]], [ssum[b]])
            cx.op("dve", lambda: V.tensor_scalar_mul(gte[b][:], em[b][:], ssum[b][:, 0:1]), [em[b], ssum[b]], [gte[b]])
        elif stage == 3:
            cx.op("pe", lambda: nc.tensor.matmul(pp[:, 64:64 + NE], masks[:, M_SUT, :], MK[:], start=True, stop=True), [masks, MK], [rs["ppb"]])
            cx.op("pe", lambda: nc.tensor.matmul(pp[:, 128:128 + NE], masks[:, M_ONES, :], MK[:], start=True, stop=True), [masks, MK], [rs["ppb"]])
        else:
            cx.op("dve", lambda: V.tensor_tensor(SL[:], pp[:, 64:64 + NE], cnt[:], ALU.add), [rs["ppb"], cnt], [SL])
            cx.op("dve", lambda: V.tensor_tensor(cnt[:], pp[:, 128:128 + NE], cnt[:], ALU.add), [rs["ppb"], cnt], [cnt])
            cx.op("dve", lambda: V.tensor_tensor(SL[:], SL[:], ebase[:], ALU.add), [SL, ebase], [SL])
            cx.op("dve", lambda: V.tensor_tensor(SL[:], SL[:], MK[:], ALU.mult), [SL, MK], [SL])
            cx.op("dve", lambda: V.tensor_scalar_add(SL[:], SL[:], -1.0), [SL], [SL])
            cx.op("dve", lambda: V.max(s8[b][:], SL[:]), [SL], [s8[b]])
            cx.op("dve", lambda: V.tensor_copy(IDX[:, ti, :], s8[b][:, 0:4]), [s8[b]], [IDX])
            for k in range(4):
                cx.op("dve", lambda: V.tensor_scalar(tmp[b][:], SL[:], s8[b][:, k:k + 1], None, ALU.is_equal), [SL, s8[b]], [tmp[b]])
                cx.op("dve", lambda: V.tensor_tensor(tmp[b][:], tmp[b][:], gte[b][:], ALU.mult), [tmp[b], gte[b]], [tmp[b]])
                cx.op("dve", lambda: V.reduce_sum(GK[:, ti, k:k + 1], tmp[b][:], mybir.AxisListType.X), [tmp[b]], [GK])
            for k in range(4):
                cx.dma("pool", None, None, reads=[h_b, IDX], writes=[xbuf], sb=h_b,
                       fn=lambda: P.indirect_dma_start(out=xbuf[:], out_offset=bass.IndirectOffsetOnAxis(ap=IDX[:, ti, k:k + 1], axis=0),
                                                       in_=h_b[:], in_offset=None))

    def phase_experts(l):
        ph = ExitStack()
        wgu = [cx.sbuf(ph, "wgu%d" % i, [128, 8, 2 * D], BF16) for i in range(2)]
        wdn = [cx.sbuf(ph, "wdn%d" % i, [128, 8, D], BF16) for i in range(2)]
        bgu = [cx.sbuf(ph, "bgu%d" % i, [128, 16]) for i in range(2)]
        bdn = [cx.sbuf(ph, "bdn%d" % i, [1, D], BF16) for i in range(2)]
        xrA = [cx.sbuf(ph, "xrA%d" % i, [128, 4, D], BF16) for i in range(2)]
        xrB = cx.sbuf(ph, "xrB", [128, 2, D], BF16)
        xTA = [cx.sbuf(ph, "xTA%d" % i, [128, 8, 512], BF16) for i in range(2)]
        xTB = cx.sbuf(ph, "xTB", [128, 8, 256], BF16)
        aTA = cx.sbuf(ph, "aTA", [128, 8, 512], BF16)
        aTB = cx.sbuf(ph, "aTB", [128, 8, 256], BF16)
        g1 = [cx.sbuf(ph, "g1%d" % i, [128, 512]) for i in range(2)]
        sg = [cx.sbuf(ph, "sg%d" % i, [128, 512]) for i in range(2)]
        u1 = [cx.sbuf(ph, "u1%d" % i, [128, 512]) for i in range(2)]
        yt = [cx.sbuf(ph, "yt%d" % i, [128, D]) for i in range(2)]
        k_ = {"jk": 0, "yk": 0}
        cregs = nc.alloc_registers("cnt_reg_%d" % l)

        def load_w(e):
            b = e % 2
            cx.dma("pool", None, None, reads=[], writes=[wgu[b]], sb=wgu[b],
                   fn=lambda: P.dma_start(out=wgu[b][:], in_=w_gu[l, e].rearrange("(kc p) n -> p kc n", p=128)))
            cx.dma("pool", None, None, reads=[], writes=[wdn[b]], sb=wdn[b],
                   fn=lambda: P.dma_start(out=wdn[b][:], in_=w_down[l, e].rearrange("(kc p) n -> p kc n", p=128)))
            cx.dma("pool", None, None, reads=[], writes=[bdn[b]], sb=bdn[b],
                   fn=lambda: P.dma_start(out=bdn[b][:], in_=b_down[l, e:e + 1, :]))
            with nc.allow_non_contiguous_dma(reason="tiny"):
                cx.dma("sp", bgu[b][:], b_gu[l, e].rearrange("(m p) -> p m", p=128), reads=[], writes=[bgu[b]], sb=bgu[b])

        def load_x(xr_, r0, nt):
            cx.dma("sp", xr_[:, 0:nt, :], xbuf[r0:r0 + nt * 128, :].rearrange("(t p) d -> p t d", p=128), reads=[xbuf], writes=[xr_], sb=xr_)

        def transp(xr_, xT, t_):
            for kc in range(8):
                cx.op("pe", lambda: nc.tensor.transpose(pbh[:, kc * 128:(kc + 1) * 128], xr_[:, t_, kc * 128:(kc + 1) * 128], identb[:]), [xr_, identb], [pbh])
            if t_ % 2:
                cx.op("act", lambda: S.copy(xT[:, :, t_ * 128:(t_ + 1) * 128], pbh[:].rearrange("p (kc t) -> p kc t", kc=8)), [pbh], [xT])
            else:
                cx.op("dve", lambda: V.tensor_copy(xT[:, :, t_ * 128:(t_ + 1) * 128], pbh[:].rearrange("p (kc t) -> p kc t", kc=8)), [pbh], [xT])

        def up(e, xT, aT, ns):
            b = e % 2
            for j in range(8):
                jk = k_["jk"]; k_["jk"] += 1
                pg = pb[(jk % 2) * 2]; pu = pb[(jk % 2) * 2 + 1]
                g_ = g1[jk % 2]; s_ = sg[jk % 2]; u_ = u1[jk % 2]
                for kc in range(8):
                    cx.op("pe", lambda: nc.tensor.matmul(pg[:, 0:ns], wgu[b][:, kc, j * 128:(j + 1) * 128], xT[:, kc, 0:ns], start=(kc == 0), stop=(kc == 7)), [wgu[b], xT], [pg])
                for kc in range(8):
                    cx.op("pe", lambda: nc.tensor.matmul(pu[:, 0:ns], wgu[b][:, kc, D + j * 128:D + (j + 1) * 128], xT[:, kc, 0:ns], start=(kc == 0), stop=(kc == 7)), [wgu[b], xT], [pu])
                cx.op("dve", lambda: V.tensor_scalar(g_[:, 0:ns], pg[:, 0:ns], bgu[b][:, j:j + 1], 7.0, ALU.add, ALU.min), [pg, bgu[b]], [g_])
                cx.op("act", lambda: S.activation(u_[:, 0:ns], pu[:, 0:ns], AF.Identity, bias=bgu[b][:, 8 + j:9 + j], scale=1.0), [pu, bgu[b]], [u_])
                cx.op("act", lambda: S.activation(s_[:, 0:ns], g_[:, 0:ns], AF.Sigmoid, scale=1.702), [g_], [s_])
                cx.op("dve", lambda: V.tensor_scalar(u_[:, 0:ns], u_[:, 0:ns], 7.0, -7.0, ALU.min, ALU.max), [u_], [u_])
                cx.op("dve", lambda: V.tensor_tensor(g_[:, 0:ns], g_[:, 0:ns], s_[:, 0:ns], ALU.mult), [g_, s_], [g_])
                cx.op("dve", lambda: V.scalar_tensor_tensor(aT[:, j, 0:ns], u_[:, 0:ns], 1.0, g_[:, 0:ns], ALU.add, ALU.mult), [u_, g_], [aT])

        def down_tile(e, aT, r0, t_):
            b = e % 2
            yk = k_["yk"]; k_["yk"] += 1
            y_ = yt[yk % 2]
            for hf_ in range(2):
                py = pb[4 + (yk * 2 + hf_) % 3]
                for j in range(8):
                    cx.op("pe", lambda: nc.tensor.matmul(py[:], aT[:, j, t_ * 128:(t_ + 1) * 128], wdn[b][:, j, hf_ * 512:(hf_ + 1) * 512], start=(j == 0), stop=False), [aT, wdn[b]], [py])
                cx.op("pe", lambda: nc.tensor.matmul(py[:], onesb[0:1, :], bdn[b][0:1, hf_ * 512:(hf_ + 1) * 512], start=False, stop=True), [onesb, bdn[b]], [py])
                if hf_:
                    cx.op("act", lambda: S.copy(y_[:, 512:1024], py[:]), [py], [y_])
                else:
                    cx.op("dve", lambda: V.tensor_copy(y_[:, 0:512], py[:]), [py], [y_])
            cx.dma("sp", ybuf[r0 + t_ * 128:r0 + (t_ + 1) * 128, :], y_[:], reads=[y_], writes=[ybuf], sb=y_)

        def small_group(e, off):
            r0 = e * CAP + off
            load_x(xrB, r0, 2)
            for t_ in range(2):
                transp(xrB, xTB, t_)
            up(e, xTB, aTB, 256)
            for t_ in range(2):
                down_tile(e, aTB, r0, t_)

        load_w(0)
        load_x(xrA[0], 0, 4)
        for t_ in range(4):
            transp(xrA[0], xTA[0], t_)
        for e in range(NE):
            r0 = e * CAP
            nxt = e + 1 < NE
            if nxt:
                load_w(e + 1)
                load_x(xrA[(e + 1) % 2], (e + 1) * CAP, 4)
            up(e, xTA[e % 2], aTA, 512)
            for t_ in range(4):
                if nxt:
                    transp(xrA[(e + 1) % 2], xTA[(e + 1) % 2], t_)
                down_tile(e, aTA, r0, t_)
            for E in cx.E.values():
                E.eng.reg_load(cregs[E.eng.engine], CNTI[0:1, e:e + 1])
            cx.cond_region(cregs, 512, lambda: small_group(e, 512))
            cx.cond_region(cregs, 768, lambda: small_group(e, 768))
        cx.end_phase()
        ph.close()

    def phase_combine(l, xsrc, xdst):
        ph = ExitStack()
        mod = load_mod(ph, l, 1, ("gt",))
        lg = cx.sbuf(ph, "lg", [128, D]); lb = cx.sbuf(ph, "lb", [128, D])
        cx.dma("sp", lg[:], row_bc(ln2_g[l, :], 128), reads=[], writes=[lg], sb=lg)
        cx.dma("sp", lb[:], row_bc(ln2_b[l, :], 128), reads=[], writes=[lb], sb=lb)
        xt = [cx.sbuf(ph, "xt%d" % i, [128, D]) for i in range(2)]
        yg = [cx.sbuf(ph, "yg%d" % i, [128, D]) for i in range(12)]
        acc = [cx.sbuf(ph, "acc%d" % i, [128, D]) for i in range(2)]
        lnb = ln_bufs(ph)
        def gathers(ti):
            for k in range(4):
                y_ = yg[(ti % 3) * 4 + k]
                cx.dma("pool", None, None, reads=[ybuf, IDX], writes=[y_], sb=y_,
                       fn=lambda: P.indirect_dma_start(out=y_[:], out_offset=None, in_=ybuf[:],
                                                       in_offset=bass.IndirectOffsetOnAxis(ap=IDX[:, ti, k:k + 1], axis=0)))
        gathers(0); gathers(1)
        for ti in range(NT):
            s = ti // (SEQ // 128)
            b = ti % 2
            x_t = xt[b]; a_ = acc[b]
            cx.dma("sp", x_t[:], xsrc[ti * 128:(ti + 1) * 128, :], reads=[xsrc], writes=[x_t], sb=x_t)
            if ti + 2 < NT:
                gathers(ti + 2)
            ys = [yg[(ti % 3) * 4 + k] for k in range(4)]
            cx.op("dve", lambda: V.tensor_scalar_mul(a_[:], ys[0][:], GK[:, ti, 0:1]), [ys[0], GK], [a_])
            for k in range(1, 4):
                cx.op("dve", lambda: V.scalar_tensor_tensor(a_[:], ys[k][:], GK[:, ti, k:k + 1], a_[:], ALU.mult, ALU.add), [ys[k], GK, a_], [a_])
            ln_tile(lnb, [a_], [a_[:, 0:512], a_[:, 512:1024]], x_t, mod[("gt", s)], lg, lb, xdst[ti * 128:(ti + 1) * 128, :], xdst, ti)
        cx.end_phase()
        ph.close()

    def done(tag):
        return stop_after == tag

    phase_ada()
    cur = x_in
    finished = False
    for l in range(n_layers):
        if done("ada"):
            break
        phase_proj(l, cur)
        if done("proj"): break
        phase_fprep(l)
        phase_mix(l)
        if done("mix"): break
        x1 = xres[0]
        phase_merge(l, cur, x1)
        if done("merge"): break
        phase_experts(l)
        if done("experts"): break
        last = (l == n_layers - 1)
        x2 = out_d if last else xres[1]
        phase_combine(l, x1, x2)
        cur = x2
    for name, src in (("oT", oT), ("x1", xres[0]), ("gT", gT), ("faT", faT), ("qkT", qkT[0]), ("xbuf", xbuf), ("ybuf", ybuf)):
        if name in dbg:
            ph = ExitStack()
            d = dbg[name]
            rows, cols = d.t.shape
            cw_ = min(cols, 1024)
            tmpb = [cx.sbuf(ph, "dump%d" % i, [128, cw_], src.t.dtype if hasattr(src, "t") else src.dtype) for i in range(2)]
            tmpf = [cx.sbuf(ph, "dumpf%d" % i, [128, cw_]) for i in range(2)]
            srcb = src if hasattr(src, "t") else qkT
            i = 0
            for r0 in range(0, rows, 128):
                for c0 in range(0, cols, cw_):
                    i += 1
                    n = min(128, rows - r0)
                    tb_, tf_ = tmpb[i % 2], tmpf[i % 2]
                    cx.dma("sp", tb_[0:n, :], src[r0:r0 + n, c0:c0 + cw_], reads=[srcb], writes=[tb_], sb=tb_)
                    cx.op("dve", lambda: V.tensor_copy(tf_[0:n, :], tb_[0:n, :]), [tb_], [tf_])
                    cx.dma("sp", d[r0:r0 + n, c0:c0 + cw_], tf_[0:n, :], reads=[tf_], writes=[d], sb=tf_)
            cx.end_phase()
            ph.close()
    ph = ExitStack()
    for name in ("IDX", "GK"):
        if name in dbg:
            srcb = IDX if name == "IDX" else GK
            tmpf = cx.sbuf(ph, "dumpg" + name, [128, NT * 4])
            cx.op("dve", lambda: V.tensor_copy(tmpf[:], srcb[:].rearrange("p a b -> p (a b)")), [srcb], [tmpf])
            cx.dma("sp", dbg[name][:], tmpf[:], reads=[tmpf], writes=[dbg[name]], sb=tmpf)
    cx.end_phase()
    ph.close()
    st.close()
    return nc, cx


def make_consts():
    import ml_dtypes
    k = np.arange(128)[:, None]
    q = np.arange(128)[None, :]
    masks = np.zeros((128, 8, 128), np.float32)
    masks[:, 0, :] = (k < q)
    masks[:, 1, :] = (k > q)
    masks[:, 2, :] = 1.0
    masks[:, 3, :] = np.where(k > q, NEG, 0.0)
    masks[:, 4, :] = np.where(k >= q, NEG, 0.0)
    masks[:, 5, :] = np.where(k < q, -1.0, 0.0)
    masks[:, 6, :] = np.where(k >= q, -1.0, 0.0)
    masks[:, 7, :] = -1.0
    ebase = np.broadcast_to((np.arange(NE) * CAP + 1).astype(np.float32)[None, :], (128, NE)).copy()
    return {
        "k_identb": np.eye(128, dtype=np.float32).astype(ml_dtypes.bfloat16),
        "k_identf": np.eye(128, dtype=np.float32),
        "k_masks": masks,
        "k_ebase": ebase,
    }


WEIGHT_KEYS = ["w_ada", "b_ada", "ln1_g", "ln1_b", "w_in", "b_f", "conv_w", "conv_b", "lru_wa", "lru_ba", "lru_wx",
               "lru_bx", "lru_lambda", "w_gate", "b_gate", "w_pa", "w_pb", "w_pc", "w_o", "ln2_g", "ln2_b", "w_router",
               "b_router", "w_gu", "b_gu", "w_down", "b_down"]


def make_in_maps(inputs):
    consts = make_consts()
    x = np.ascontiguousarray(np.asarray(inputs["x"], dtype=np.float32))
    c = np.ascontiguousarray(np.asarray(inputs["c"], dtype=np.float32))
    shared = {k: np.ascontiguousarray(np.asarray(inputs[k], dtype=np.float32)) for k in WEIGHT_KEYS}
    shared.update(consts)
    in_maps = []
    for i in range(NCORES):
        m = dict(shared)
        m["x"] = x[NSEQ * i:NSEQ * (i + 1)].reshape(T, D)
        m["c"] = c[NSEQ * i:NSEQ * (i + 1)]
        in_maps.append(m)
    return in_maps


def kernel(**inputs):
    nc, cx = build_program()
    in_maps = make_in_maps(inputs)
    res = run_bass_kernel_spmd(nc, in_maps, core_ids=list(range(NCORES)))
    out = np.stack([np.asarray(r["out"]).reshape(NSEQ, SEQ, D) for r in res.results], axis=0)
    return out.reshape(NCORES * NSEQ, SEQ, D).astype(np.float32)
```

```python
from contextlib import ExitStack
import numpy as np
import concourse.bass as bass
import concourse.mybir as mybir
from concourse.bass_utils import run_bass_kernel_spmd

F32 = mybir.dt.float32
F32R = mybir.dt.float32r
BF16 = mybir.dt.bfloat16
I32 = mybir.dt.int32
U32 = mybir.dt.uint32
AF = mybir.ActivationFunctionType
ALU = mybir.AluOpType

NCORES = 8
D = 1024
SEQ = 2048
NSEQ = 2
T = NSEQ * SEQ
NT = T // 128
NCH = T // 512
DEPTH = 2
H = 8
DH = 64
D_IN = 5128
NE = 32
CAP = 1024
ALPHA = (2.0 * DEPTH) ** 0.25
LN_EPS = 1e-5
NEG = -30000.0


class Slot:
    def __init__(self, sem):
        self.sem = sem
        self.count = 0


class Buf:
    def __init__(self, name, t=None):
        self.name = name
        self.t = t
        self.w = {}
        self.r = {}
        self.ds = None

    def __getitem__(self, k):
        return self.t[k]


class EngState:
    def __init__(self, name, eng, sem):
        self.name = name
        self.eng = eng
        self.sem = sem
        self.count = 0
        self.waited = {}


class Ctx:
    def __init__(self, nc, stack, n_dma_sems=72):
        self.nc = nc
        self.stack = stack
        self.E = {}
        for name, eng in (("pe", nc.tensor), ("act", nc.scalar), ("dve", nc.vector),
                          ("pool", nc.gpsimd), ("sp", nc.sync)):
            sem = stack.enter_context(nc.semaphore("s_" + name))
            self.E[name] = EngState(name, eng, sem)
        self.free_slots = [Slot(stack.enter_context(nc.semaphore("d%d" % i))) for i in range(n_dma_sems)]
        self.used_slots = []
        self.n_ins = 0
        self.n_wait = 0
        self.uid = 0

    def sbuf(self, ph, name, shape, dtype=F32):
        self.uid += 1
        t = ph.enter_context(self.nc.sbuf_tensor("%s_%d" % (name, self.uid), list(shape), dtype))
        return Buf(name, t)

    def psum(self, name, shape, dtype=F32):
        t = self.stack.enter_context(self.nc.psum_tensor(name, list(shape), dtype))
        return Buf(name, t)

    def dram(self, name, shape, dtype=F32, kind="Internal"):
        t = self.nc.dram_tensor(name, list(shape), dtype, kind=kind)
        return Buf(name, t.ap())

    def _wait(self, E, deps):
        for sid, (sem, val) in deps.items():
            if E.waited.get(sid, 0) >= val:
                continue
            E.eng.wait_ge(sem, val)
            E.waited[sid] = val
            self.n_wait += 1

    @staticmethod
    def _merge(d, src, skip=None):
        for sid, (sem, val) in src.items():
            if skip is not None and sid == skip:
                continue
            if sid not in d or d[sid][1] < val:
                d[sid] = (sem, val)

    def op(self, en, fn, reads=(), writes=()):
        E = self.E[en]
        own = id(E.sem)
        deps = {}
        for b in reads:
            self._merge(deps, b.w, skip=own if en == "pe" else None)
        for b in writes:
            self._merge(deps, b.w, skip=own)
            self._merge(deps, b.r, skip=own)
        self._wait(E, deps)
        ins = fn()
        E.count += 1
        ins.then_inc(E.sem, 1)
        tok = (E.sem, E.count)
        for b in reads:
            b.r[own] = tok
        for b in writes:
            b.w = {own: tok}
            b.r = {}
        self.n_ins += 1
        return ins

    def dma(self, qn, out, in_, reads=(), writes=(), sb=None, fn=None):
        E = self.E[qn]
        if sb.ds is None:
            sb.ds = self.free_slots.pop()
            self.used_slots.append(sb.ds)
        ds = sb.ds
        deps = {}
        if ds.count:
            deps[id(ds.sem)] = (ds.sem, ds.count)
        for b in reads:
            self._merge(deps, b.w)
        for b in writes:
            self._merge(deps, b.w)
            self._merge(deps, b.r)
        self._wait(E, deps)
        ins = E.eng.dma_start(out=out, in_=in_) if fn is None else fn()
        ds.count += 16
        ins.then_inc(ds.sem, 16)
        tok = (ds.sem, ds.count)
        sid = id(ds.sem)
        for b in reads:
            b.r[sid] = tok
        for b in writes:
            b.w = {sid: tok}
            b.r = {}
        self.n_ins += 1
        return ins

    def cond_region(self, regs, thr, body):
        snap_c = {n: E.count for n, E in self.E.items()}
        all_slots = self.used_slots + self.free_slots
        snap_s = {id(sl): sl.count for sl in all_slots}
        snap_w = {n: dict(E.waited) for n, E in self.E.items()}
        with self.nc.If_cmp(regs, thr, "IS_GT"):
            body()
        with self.nc.Else():
            for n, E in self.E.items():
                d = E.count - snap_c[n]
                if d:
                    if snap_c[n]:
                        E.eng.wait_ge(E.sem, snap_c[n])
                    E.eng.sem_inc(E.sem, d)
            sp = self.E["sp"]
            for sl in self.used_slots + self.free_slots:
                d = sl.count - snap_s.get(id(sl), 0)
                if d:
                    if snap_s.get(id(sl), 0):
                        sp.eng.wait_ge(sl.sem, snap_s[id(sl)])
                    sp.eng.sem_inc(sl.sem, d)
        for n, E in self.E.items():
            E.waited = snap_w[n]

    def barrier(self, only=None):
        deps = {}
        for E in self.E.values():
            if E.count:
                deps[id(E.sem)] = (E.sem, E.count)
        for s in self.used_slots:
            if s.count:
                deps[id(s.sem)] = (s.sem, s.count)
        for name, E in self.E.items():
            if only is not None and name not in only:
                continue
            d = {k: v for k, v in deps.items() if k != id(E.sem)}
            self._wait(E, d)

    def end_phase(self):
        self.barrier()
        self.free_slots.extend(self.used_slots)
        self.used_slots = []


def build_program(n_layers=DEPTH, stop_after=None, debug=()):
    nc = bass.Bass("TRN2", target_bir_lowering=False)
    st = ExitStack()
    cx = Ctx(nc, st)
    V, S, P = nc.vector, nc.scalar, nc.gpsimd

    def din(name, shape, dtype=F32):
        return cx.dram(name, shape, dtype, kind="ExternalInput")

    x_in = din("x", [T, D]); c_in = din("c", [NSEQ, D])
    w_ada = din("w_ada", [DEPTH, D, 6 * D]); b_ada = din("b_ada", [DEPTH, 6 * D])
    ln1_g = din("ln1_g", [DEPTH, D]); ln1_b = din("ln1_b", [DEPTH, D])
    w_in = din("w_in", [DEPTH, D, D_IN]); b_f = din("b_f", [DEPTH, H])
    conv_w = din("conv_w", [DEPTH, 4, D]); conv_b = din("conv_b", [DEPTH, D])
    lru_wa = din("lru_wa", [DEPTH, 16, 64, 64]); lru_ba = din("lru_ba", [DEPTH, D])
    lru_wx = din("lru_wx", [DEPTH, 16, 64, 64]); lru_bx = din("lru_bx", [DEPTH, D])
    lru_lambda = din("lru_lambda", [DEPTH, D])
    w_gate = din("w_gate", [DEPTH, D, 3 * D]); b_gate = din("b_gate", [DEPTH, 3 * D])
    w_pa = din("w_pa", [DEPTH, 512, D]); w_pb = din("w_pb", [DEPTH, 512, D])
    w_pc = din("w_pc", [DEPTH, D, D]); w_o = din("w_o", [DEPTH, D, D])
    ln2_g = din("ln2_g", [DEPTH, D]); ln2_b = din("ln2_b", [DEPTH, D])
    w_router = din("w_router", [DEPTH, D, NE]); b_router = din("b_router", [DEPTH, NE])
    w_gu = din("w_gu", [DEPTH, NE, D, 2 * D]); b_gu = din("b_gu", [DEPTH, NE, 2 * D])
    w_down = din("w_down", [DEPTH, NE, D, D]); b_down = din("b_down", [DEPTH, NE, D])
    k_identb = din("k_identb", [128, 128], BF16)
    k_identf = din("k_identf", [128, 128])
    k_masks = din("k_masks", [128, 8, 128])
    k_ebase = din("k_ebase", [128, NE])
    out_d = cx.dram("out", [T, D], F32, kind="ExternalOutput")

    adaB = cx.dram("adaB", [DEPTH, NSEQ, 6, D])
    xres = [cx.dram("xresA", [T, D]), cx.dram("xresB", [T, D])]
    qkT = cx.dram("qkT", [4, 512, T], BF16)
    vv = cx.dram("vv", [2, T, 512], BF16)
    faT = cx.dram("faT", [H, T])
    Fd = cx.dram("Fd", [6, H, T], BF16)
    xgT = cx.dram("xgT", [2, D, T])
    gT = cx.dram("gT", [3 * D, T])
    oT = cx.dram("oT", [2 * D, T], BF16)
    xbuf = cx.dram("xbuf", [(NE + 1) * CAP, D], BF16)
    ybuf = cx.dram("ybuf", [(NE + 1) * CAP, D])
    dbg = {}
    for name, shape in debug:
        dbg[name] = cx.dram("dbg_" + name, shape, F32, kind="ExternalOutput")

    pb = [cx.psum("pb%d" % i, [128, 512]) for i in range(7)]
    pbh = cx.psum("pbh", [128, 1024], BF16)

    gl = st
    identb = cx.sbuf(gl, "identb", [128, 128], BF16)
    identf = cx.sbuf(gl, "identf", [128, 128])
    masks = cx.sbuf(gl, "masks", [128, 8, 128])
    masksb = cx.sbuf(gl, "masksb", [128, 8, 128], BF16)
    masksr = cx.sbuf(gl, "masksr", [128, 8, 128], F32R)
    ebase = cx.sbuf(gl, "ebase", [128, NE])
    IDX = cx.sbuf(gl, "IDX", [128, NT, 4], I32)
    GK = cx.sbuf(gl, "GK", [128, NT, 4])
    onesb = cx.sbuf(gl, "onesb", [128, 128], BF16)
    CNTI = cx.sbuf(gl, "CNTI", [1, NE], I32)
    cx.dma("sp", identb[:], k_identb[:], reads=[k_identb], writes=[identb], sb=identb)
    cx.dma("sp", identf[:], k_identf[:], reads=[k_identf], writes=[identf], sb=identf)
    cx.dma("sp", masks[:], k_masks[:], reads=[k_masks], writes=[masks], sb=masks)
    cx.dma("sp", ebase[:], k_ebase[:], reads=[k_ebase], writes=[ebase], sb=ebase)
    cx.op("dve", lambda: V.tensor_copy(masksb[:], masks[:]), [masks], [masksb])
    cx.op("dve", lambda: V.tensor_copy(masksr[:], masks[:]), [masks], [masksr])
    cx.op("dve", lambda: V.memset(onesb[:], 1.0), [], [onesb])
    M_SUT, M_TRI, M_ONES, M_NEGC, M_NEGNS, M_NSTRICT, M_NTRII, M_NONES = range(8)

    def row_bc(ap_row, n):
        return ap_row.partition_broadcast(n)

    def phase_ada():
        ph = ExitStack()
        c_col = cx.sbuf(ph, "c_col", [128, NSEQ, 8])
        cond = cx.sbuf(ph, "cond", [128, NSEQ, 8])
        with nc.allow_non_contiguous_dma(reason="tiny transposed load of c"):
            cx.dma("sp", c_col[:], c_in.t.rearrange("s (kc p) -> p s kc", p=128), reads=[c_in], writes=[c_col], sb=c_col)
        cx.op("act", lambda: S.activation(cond[:], c_col[:], AF.Silu), [c_col], [cond])
        condB = cx.sbuf(ph, "condB", [128, NSEQ, 8, 128], BF16)
        for s in range(NSEQ):
            cx.op("dve", lambda: V.tensor_copy(condB[:, s], cond[:, s, :].unsqueeze(2).to_broadcast([128, 8, 128])),
                  [cond], [condB])
        wring = [cx.sbuf(ph, "wada%d" % i, [128, 8, 512], BF16) for i in range(3)]
        brow = [cx.sbuf(ph, "brow%d" % i, [128, 512]) for i in range(2)]
        res = [cx.sbuf(ph, "ares%d" % i, [128, 512]) for i in range(3)]
        k = 0
        for l in range(n_layers):
            for n in range(12):
                wt = wring[k % 3]; br = brow[k % 2]
                cx.dma("pool", None, None, reads=[w_ada], writes=[wt], sb=wt,
                       fn=lambda: P.dma_start(out=wt[:], in_=w_ada[l, :, n * 512:(n + 1) * 512].rearrange("(kc p) n -> p kc n", p=128)))
                cx.dma("sp", br[:], row_bc(b_ada[l, n * 512:(n + 1) * 512], 128), reads=[b_ada], writes=[br], sb=br)
                which = n // 2
                for s in range(NSEQ):
                    ps = pb[(k * NSEQ + s) % 4]
                    for kc in range(8):
                        cx.op("pe", lambda: nc.tensor.matmul(ps[:], condB[:, s, kc, :], wt[:, kc, :], start=(kc == 0), stop=(kc == 7)),
                              [condB, wt], [ps])
                    r = res[(k * NSEQ + s) % 3]
                    cx.op("dve", lambda: V.tensor_tensor(r[:], ps[:], br[:], ALU.add), [ps, br], [r])
                    if which not in (0, 3):
                        cx.op("dve", lambda: V.tensor_scalar_add(r[:], r[:], 1.0), [r], [r])
                    c0_ = (n % 2) * 512
                    cx.dma("sp", adaB[l, s, which:which + 1, c0_:c0_ + 512], r[0:1, :], reads=[r], writes=[adaB], sb=r)
                k += 1
        cx.end_phase()
        ph.close()

    def load_mod(ph, l, sub, names=("sh", "sc", "gt")):
        tiles = {}
        for s in range(NSEQ):
            for nm, idx in (("sh", 3 * sub), ("sc", 3 * sub + 1), ("gt", 3 * sub + 2)):
                if nm not in names:
                    continue
                t = cx.sbuf(ph, "mod_%s%d" % (nm, s), [128, D])
                cx.dma("sp", t[:], row_bc(adaB[l, s, idx, :], 128), reads=[adaB], writes=[t], sb=t)
                tiles[(nm, s)] = t
        return tiles

    def phase_proj(l, xsrc):
        ph = ExitStack()
        TC = 1024
        NC2 = T // TC
        TPC = TC // 128
        mod = load_mod(ph, l, 0, ("sh", "sc"))
        bgate = cx.sbuf(ph, "bgate", [128, 24])
        with nc.allow_non_contiguous_dma(reason="tiny bias relayout"):
            cx.dma("sp", bgate[:], b_gate[l].rearrange("(m p) -> p m", p=128), reads=[b_gate], writes=[bgate], sb=bgate)
        xt = [cx.sbuf(ph, "xt%d" % i, [128, D]) for i in range(TPC)]
        hf = [cx.sbuf(ph, "hf%d" % i, [128, D]) for i in range(2)]
        hb = [cx.sbuf(ph, "hb%d" % i, [128, D], BF16) for i in range(2)]
        hT = [cx.sbuf(ph, "hT%d" % i, [128, 8, TC], BF16) for i in range(2)]
        wring = [cx.sbuf(ph, "win%d" % i, [128, 8, 512], BF16) for i in range(3)]
        wfa = cx.sbuf(ph, "wfa", [128, 8, 8], BF16)
        evf = [cx.sbuf(ph, "evf%d" % i, [128, 512]) for i in range(8)]
        evb = [cx.sbuf(ph, "evb%d" % i, [128, 512], BF16) for i in range(8)]
        cx.dma("pool", None, None, reads=[w_in], writes=[wfa], sb=wfa,
               fn=lambda: P.dma_start(out=wfa[:], in_=w_in[l, :, 1536:1544].rearrange("(kc p) n -> p kc n", p=128)))
        cnt = {"wk": 0, "ek": 0, "pk": 0}
        pieces = [("qa", 0), ("ka", 512), ("va", 1024), ("qb", 1544), ("kb", 2056), ("vb", 2568),
                  ("xc0", 3080), ("xc1", 3592), ("gc0", 4104), ("gc1", 4616)] + [("g%d" % i, i * 512) for i in range(6)]

        def load_x(ch):
            for tt in range(TPC):
                ti = ch * TPC + tt
                cx.dma("sp", xt[tt][:], xsrc[ti * 128:(ti + 1) * 128, :], reads=[xsrc], writes=[xt[tt]], sb=xt[tt])

        def compute_h(ch):
            s = (ch * TC) // SEQ
            hTc = hT[ch % 2]
            for tt in range(TPC):
                x_t = xt[tt]; h_f = hf[tt % 2]; h_b = hb[tt % 2]
                cx.op("dve", lambda: V.tensor_tensor(h_f[:], x_t[:], mod[("sc", s)][:], ALU.mult), [x_t, mod[("sc", s)]], [h_f])
                cx.op("dve", lambda: V.tensor_tensor(h_b[:], h_f[:], mod[("sh", s)][:], ALU.add), [h_f, mod[("sh", s)]], [h_b])
                for kc in range(8):
                    cx.op("pe", lambda: nc.tensor.transpose(pbh[:, kc * 128:(kc + 1) * 128], h_b[:, kc * 128:(kc + 1) * 128], identb[:]),
                          [h_b, identb], [pbh])
                cx.op("act", lambda: S.copy(hTc[:, :, tt * 128:(tt + 1) * 128], pbh[:].rearrange("p (kc t) -> p kc t", kc=8)),
                      [pbh], [hTc])

        def evac(kind, ps, arg=None):
            ek = cnt["ek"]; cnt["ek"] += 1
            if kind == "bf":
                ev = evb[ek % 8]
                if ek % 2:
                    cx.op("act", lambda: S.mul(ev[:], ps[:], arg), [ps], [ev])
                else:
                    cx.op("dve", lambda: V.tensor_scalar_mul(ev[:], ps[:], arg), [ps], [ev])
            elif kind == "f":
                ev = evf[ek % 8]
                if ek % 2:
                    cx.op("act", lambda: S.copy(ev[:], ps[:]), [ps], [ev])
                else:
                    cx.op("dve", lambda: V.tensor_copy(ev[:], ps[:]), [ps], [ev])
            else:
                ev = evf[ek % 8]
                cx.op("act", lambda: S.activation(ev[:], ps[:], AF.Sigmoid, bias=bgate[:, arg:arg + 1], scale=1.0), [ps, bgate], [ev])
            return ev

        def next_ps():
            ps = pb[cnt["pk"] % 6]; cnt["pk"] += 1
            return ps

        load_x(0)
        compute_h(0)
        for ch in range(NC2):
            hTc = hT[ch % 2]
            tok0 = ch * TC
            if ch + 1 < NC2:
                load_x(ch + 1)
            for half in range(TC // 512):
                ps = next_ps()
                hs = slice(half * 512, (half + 1) * 512)
                for kc in range(8):
                    cx.op("pe", lambda: nc.tensor.matmul(ps[0:8, :], wfa[:, kc, :], hTc[:, kc, hs], start=(kc == 0), stop=(kc == 7)),
                          [wfa, hTc], [ps])
                ev = evf[cnt["ek"] % 8]; cnt["ek"] += 1
                cx.op("dve", lambda: V.tensor_copy(ev[0:8, :], ps[0:8, :]), [ps], [ev])
                cx.dma("sp", faT[:, tok0 + half * 512:tok0 + (half + 1) * 512], ev[0:8, :], reads=[ev], writes=[faT], sb=ev)
            for pi_, (nm, c0) in enumerate(pieces):
                if pi_ == 8 and ch + 1 < NC2:
                    compute_h(ch + 1)
                wt = wring[cnt["wk"] % 3]; cnt["wk"] += 1
                wsrc = (w_gate if nm[0] == "g" and nm[1].isdigit() else w_in)
                cx.dma("pool", None, None, reads=[wsrc], writes=[wt], sb=wt,
                       fn=lambda: P.dma_start(out=wt[:], in_=wsrc[l, :, c0:c0 + 512].rearrange("(kc p) n -> p kc n", p=128)))
                if nm in ("va", "vb"):
                    for tt in range(TPC):
                        ps = next_ps()
                        for kc in range(8):
                            cx.op("pe", lambda: nc.tensor.matmul(ps[:], hTc[:, kc, tt * 128:(tt + 1) * 128], wt[:, kc, :], start=(kc == 0), stop=(kc == 7)),
                                  [hTc, wt], [ps])
                        ev = evac("bf", ps, 1.0)
                        cx.dma("sp", vv[0 if nm == "va" else 1, tok0 + tt * 128:tok0 + (tt + 1) * 128, :], ev[:], reads=[ev], writes=[vv], sb=ev)
                    continue
                for m in range(4):
                    for half in range(TC // 512):
                        hs = slice(half * 512, (half + 1) * 512)
                        tk = tok0 + half * 512
                        ps = next_ps()
                        for kc in range(8):
                            cx.op("pe", lambda: nc.tensor.matmul(ps[:], wt[:, kc, m * 128:(m + 1) * 128], hTc[:, kc, hs], start=(kc == 0), stop=(kc == 7)),
                                  [wt, hTc], [ps])
                        if nm in ("qa", "ka", "qb", "kb"):
                            ev = evac("bf", ps, 0.125 if nm[0] == "q" else 1.0)
                            qi = ("qa", "ka", "qb", "kb").index(nm)
                            cx.dma("sp", qkT[qi, m * 128:(m + 1) * 128, tk:tk + 512], ev[:], reads=[ev], writes=[qkT], sb=ev)
                        elif nm[0] == "x" or nm[:2] == "gc":
                            ev = evac("f", ps)
                            r0 = int(nm[2]) * 512 + m * 128
                            cx.dma("sp", xgT[0 if nm[0] == "x" else 1, r0:r0 + 128, tk:tk + 512], ev[:], reads=[ev], writes=[xgT], sb=ev)
                        else:
                            mi = int(nm[1]) * 4 + m
                            ev = evac("g", ps, mi)
                            cx.dma("sp", gT[mi * 128:(mi + 1) * 128, tk:tk + 512], ev[:], reads=[ev], writes=[gT], sb=ev)
        cx.end_phase()
        ph.close()

    def phase_fprep(l):
        ph = ExitStack()
        bf = cx.sbuf(ph, "bf", [H, 1]); nbf = cx.sbuf(ph, "nbf", [H, 1])
        with nc.allow_non_contiguous_dma(reason="tiny"):
            cx.dma("sp", bf[:], b_f[l].rearrange("(h o) -> h o", o=1), reads=[b_f], writes=[bf], sb=bf)
        cx.op("dve", lambda: V.tensor_scalar_mul(nbf[:], bf[:], -1.0), [bf], [nbf])
        ones = cx.sbuf(ph, "ones", [H, SEQ]); cx.op("dve", lambda: V.memset(ones[:], 1.0), [], [ones])
        for s in range(NSEQ):
            fa = cx.sbuf(ph, "fa%d" % s, [H, SEQ]); e = cx.sbuf(ph, "fe%d" % s, [H, SEQ]); sp_ = cx.sbuf(ph, "fs%d" % s, [H, SEQ])
            F = cx.sbuf(ph, "F%d" % s, [H, SEQ]); r1 = cx.sbuf(ph, "r1%d" % s, [H, SEQ]); r2 = cx.sbuf(ph, "r2%d" % s, [H, SEQ])
            parts = cx.sbuf(ph, "parts%d" % s, [H, 6, SEQ], BF16)
            cx.dma("sp", fa[:], faT[:, s * SEQ:(s + 1) * SEQ], reads=[faT], writes=[fa], sb=fa)
            cx.op("act", lambda: S.activation(e[:], fa[:], AF.Exp, bias=nbf[:, 0:1], scale=-1.0), [fa, nbf], [e])
            cx.op("act", lambda: S.activation(sp_[:], e[:], AF.Ln, bias=1.0, scale=1.0), [e], [sp_])
            cx.op("dve", lambda: V.tensor_tensor_scan(F[:], ones[:], sp_[:], 0.0, ALU.mult, ALU.subtract), [ones, sp_], [F])
            cx.op("dve", lambda: V.tensor_copy(parts[:, 0, :], F[:]), [F], [parts])
            cx.op("dve", lambda: V.tensor_tensor(r1[:], F[:], parts[:, 0, :], ALU.subtract), [F, parts], [r1])
            cx.op("dve", lambda: V.tensor_copy(parts[:, 1, :], r1[:]), [r1], [parts])
            cx.op("dve", lambda: V.tensor_tensor(r2[:], r1[:], parts[:, 1, :], ALU.subtract), [r1, parts], [r2])
            cx.op("dve", lambda: V.tensor_copy(parts[:, 2, :], r2[:]), [r2], [parts])
            cx.op("dve", lambda: V.tensor_scalar_mul(parts[:, 3:6, :], parts[:, 0:3, :], -1.0), [parts], [parts])
            cx.dma("sp", Fd[:, :, s * SEQ:(s + 1) * SEQ].rearrange("v h t -> h v t"), parts[:], reads=[parts], writes=[Fd], sb=parts)
        cx.end_phase()
        ph.close()

    def gen_fox(ph):
        NB = 2
        kT = [cx.sbuf(ph, "fkT%d" % i, [70, SEQ], BF16) for i in range(NB)]
        qT = [cx.sbuf(ph, "fqT%d" % i, [70, SEQ], BF16) for i in range(NB)]
        Va = [cx.sbuf(ph, "Va%d" % i, [128, 16, 128], BF16) for i in range(NB)]
        Pt = [cx.sbuf(ph, "Pt%d" % i, [128, 512], BF16) for i in range(3)]
        rec = [cx.sbuf(ph, "rec%d" % i, [128, 512]) for i in range(2)]
        ob = [cx.sbuf(ph, "fob%d" % i, [64, 512], BF16) for i in range(2)]
        for i in range(NB):
            cx.op("dve", lambda: V.memset(kT[i][64:70, :], 1.0), [], [kT[i]])
            cx.op("dve", lambda: V.memset(qT[i][64:70, :], 1.0), [], [qT[i]])
            cx.op("dve", lambda: V.memset(Va[i][:], 1.0), [], [Va[i]])
        it = 0; pk = 0; ck = 0
        units = [(s, h) for s in range(NSEQ) for h in range(H)]

        def loads(ui):
            s, h = units[ui]
            b = ui % NB
            t0 = s * SEQ
            cx.dma("sp", qT[b][0:64, :], qkT[0, h * 64:(h + 1) * 64, t0:t0 + SEQ], reads=[qkT], writes=[qT[b]], sb=qT[b])
            cx.dma("sp", qT[b][64:67, :], Fd[0:3, h, t0:t0 + SEQ], reads=[Fd], writes=[qT[b]], sb=qT[b])
            cx.dma("sp", kT[b][0:64, :], qkT[1, h * 64:(h + 1) * 64, t0:t0 + SEQ], reads=[qkT], writes=[kT[b]], sb=kT[b])
            cx.dma("sp", kT[b][67:70, :], Fd[3:6, h, t0:t0 + SEQ], reads=[Fd], writes=[kT[b]], sb=kT[b])
            with nc.allow_non_contiguous_dma(reason="v head slice, 128B runs"):
                cx.dma("sp", Va[b][:, :, 0:64], vv[0, t0:t0 + SEQ, h * 64:(h + 1) * 64].rearrange("(j p) d -> p j d", p=128),
                       reads=[vv], writes=[Va[b]], sb=Va[b])

        loads(0); loads(1)
        steps = [(ui, c, J) for ui in range(len(units)) for c in range(4) for J in range(4 * c + 4)]
        pend = {}
        for k in range(len(steps) + 2):
            if k < len(steps):
                ui, c, J = steps[k]
                s, h = units[ui]; b = ui % NB
                q0 = c * 512
                lo = 128 * max(0, J - 4 * c)
                Sb = pb[pk % 4]; Pb = Pt[pk % 3]; pk += 1
                diag = J >= 4 * c
                cx.op("pe", lambda: nc.tensor.matmul(Sb[:, lo:512], kT[b][:, J * 128:(J + 1) * 128], qT[b][:, q0 + lo:q0 + 512], start=True, stop=not diag),
                      [kT[b], qT[b]], [Sb])
                if diag:
                    cx.op("pe", lambda: nc.tensor.matmul(Sb[:, lo:lo + 128], identb[:], masksb[:, M_NEGC, :], start=False, stop=True),
                          [identb, masksb], [Sb])
                cx.op("act", lambda: S.activation(Pb[:, lo:512], Sb[:, lo:512], AF.Exp), [Sb], [Pb])
                pend[k] = (lo, Pb)
            if k >= 2:
                ui, c, J = steps[k - 2]
                s, h = units[ui]; b = ui % NB
                lo, Pb = pend.pop(k - 2)
                nJ = 4 * c + 4
                gci = ui * 4 + c
                O = pb[4 + gci % 2]
                cx.op("pe", lambda: nc.tensor.matmul(O[:, lo:512], Va[b][:, J, :], Pb[:, lo:512], start=(J == 0), stop=(J == nJ - 1)),
                      [Va[b], Pb], [O])
                if J == nJ - 1:
                    rc = rec[gci % 2]; o_ = ob[gci % 2]
                    t0 = s * SEQ; q0 = c * 512
                    cx.op("dve", lambda: V.reciprocal(rc[64:128, :], O[64:128, :]), [O], [rc])
                    cx.op("dve", lambda: V.tensor_tensor(o_[:], O[0:64, :], rc[64:128, :], ALU.mult), [O, rc], [o_])
                    cx.dma("sp", oT[h * 64:(h + 1) * 64, t0 + q0:t0 + q0 + 512], o_[:], reads=[o_], writes=[oT], sb=o_)
                    if c == 3 and ui + 2 < len(units):
                        loads(ui + 2)
            yield

    def gen_sb(ph):
        NB = 2
        kT = [cx.sbuf(ph, "kT%d" % i, [64, SEQ], BF16) for i in range(NB)]
        qT = [cx.sbuf(ph, "qT%d" % i, [64, SEQ], BF16) for i in range(NB)]
        Vb = [cx.sbuf(ph, "Vb%d" % i, [128, 16, 64], BF16) for i in range(NB)]
        Et = [cx.sbuf(ph, "Et%d" % i, [128, 512]) for i in range(3)]
        SPt = [cx.sbuf(ph, "SPt%d" % i, [128, 512], F32R) for i in range(4)]
        R = [cx.sbuf(ph, "R%d" % i, [128, 512], F32R) for i in range(2)]
        Wt = [cx.sbuf(ph, "Wt%d" % i, [128, 512], BF16) for i in range(3)]
        ob = [cx.sbuf(ph, "ob%d" % i, [64, 512], BF16) for i in range(2)]
        Zf = cx.sbuf(ph, "Zf", [128, 512])
        cx.op("dve", lambda: V.memset(Zf[:], 0.0), [], [Zf])
        it = 0; zk = 0; ak = 0; ck = 0; k3 = 0; wk = 0
        units = [(s, h) for s in range(NSEQ) for h in range(H)]

        def loads(ui):
            s, h = units[ui]
            b = ui % NB
            t0 = s * SEQ
            cx.dma("sp", qT[b][:], qkT[2, h * 64:(h + 1) * 64, t0:t0 + SEQ], reads=[qkT], writes=[qT[b]], sb=qT[b])
            cx.dma("sp", kT[b][:], qkT[3, h * 64:(h + 1) * 64, t0:t0 + SEQ], reads=[qkT], writes=[kT[b]], sb=kT[b])
            with nc.allow_non_contiguous_dma(reason="v head slice, 128B runs"):
                cx.dma("sp", Vb[b][:], vv[1, t0:t0 + SEQ, h * 64:(h + 1) * 64].rearrange("(j p) d -> p j d", p=128),
                       reads=[vv], writes=[Vb[b]], sb=Vb[b])

        loads(0); loads(1)
        steps = [(ui, c, 4 * c + 3 - i) for ui in range(len(units)) for c in range(4) for i in range(4 * c + 4)]
        st_ = {}
        for k in range(len(steps) + 2):
            if k < len(steps):
                ui, c, J = steps[k]
                s, h = units[ui]; b = ui % NB
                gci = ui * 4 + c
                q0 = c * 512
                top = (J == 4 * c + 3)
                if top:
                    cx.op("dve", lambda: V.tensor_copy(R[gci % 2][:], Zf[:]), [Zf], [R[gci % 2]])
                lo = 128 * max(0, J - 4 * c)
                diag = J >= 4 * c
                Z = pb[zk % 2]; zk += 1
                e_ = Et[k3 % 3]; sp_ = SPt[k3 % 4]; k3 += 1
                cx.op("pe", lambda: nc.tensor.matmul(Z[:, lo:512], kT[b][:, J * 128:(J + 1) * 128], qT[b][:, q0 + lo:q0 + 512], start=True, stop=True),
                      [kT[b], qT[b]], [Z])
                cx.op("act", lambda: S.activation(e_[:, lo:512], Z[:, lo:512], AF.Exp), [Z], [e_])
                cx.op("act", lambda: S.activation(sp_[:, lo:512], e_[:, lo:512], AF.Ln, bias=1.0, scale=1.0), [e_], [sp_])
                if diag:
                    cx.op("dve", lambda: V.tensor_tensor(sp_[:, lo:lo + 128], sp_[:, lo:lo + 128].bitcast(F32), masks[:, M_SUT, :], ALU.mult), [sp_, masks], [sp_])
                st_[k] = (lo, diag, top, sp_)
            if 1 <= k <= len(steps):
                ui, c, J = steps[k - 1]
                s, h = units[ui]; b = ui % NB
                gci = ui * 4 + c
                q0 = c * 512
                Rc = R[gci % 2]
                lo, diag, top, sp_ = st_[k - 1]
                Ab = pb[2 + ak % 2]; ak += 1
                w_ = Wt[wk % 3]; wk += 1
                cx.op("pe", lambda: nc.tensor.matmul(Ab[:, lo:512], kT[b][:, J * 128:(J + 1) * 128], qT[b][:, q0 + lo:q0 + 512], start=True, stop=False),
                      [kT[b], qT[b]], [Ab])
                cx.op("pe", lambda: nc.tensor.matmul(Ab[:, lo:512], masksr[:, M_NTRII, :], sp_[:, lo:512], start=False, stop=(top and not diag)),
                      [masksr, sp_], [Ab])
                if not top:
                    cx.op("pe", lambda: nc.tensor.matmul(Ab[:, lo:512], masksr[:, M_NONES, :], Rc[:, lo:512], start=False, stop=not diag),
                          [masksr, Rc], [Ab])
                if diag:
                    cx.op("pe", lambda: nc.tensor.matmul(Ab[:, lo:lo + 128], identb[:], masksb[:, M_NEGNS, :], start=False, stop=True),
                          [identb, masksb], [Ab])
                cx.op("act", lambda: S.activation(w_[:, lo:512], Ab[:, lo:512], AF.Exp), [Ab], [w_])
                if J > 0:
                    cx.op("dve", lambda: V.tensor_tensor(Rc[:, lo:512], Rc[:, lo:512].bitcast(F32), sp_[:, lo:512].bitcast(F32), ALU.add), [Rc, sp_], [Rc])
                st_[k - 1] = (lo, diag, top, sp_, w_)
            if k >= 2:
                ui, c, J = steps[k - 2]
                s, h = units[ui]; b = ui % NB
                gci = ui * 4 + c
                O = pb[4 + gci % 2]; o_ = ob[gci % 2]
                lo, diag, top, sp_, w_ = st_.pop(k - 2)
                cx.op("pe", lambda: nc.tensor.matmul(O[0:64, lo:512], Vb[b][:, J, :], w_[:, lo:512], start=top, stop=(J == 0)),
                      [Vb[b], w_], [O])
                if J == 0:
                    t0 = s * SEQ; q0 = c * 512
                    cx.op("act", lambda: S.copy(o_[:], O[0:64, :]), [O], [o_])
                    cx.dma("sp", oT[512 + h * 64:512 + (h + 1) * 64, t0 + q0:t0 + q0 + 512], o_[:], reads=[o_], writes=[oT], sb=o_)
                    if c == 3 and ui + 2 < len(units):
                        loads(ui + 2)
            yield

    def gen_lru(ph, l):
        def colvec(name, src_row):
            t = cx.sbuf(ph, name, [128, 8])
            with nc.allow_non_contiguous_dma(reason="tiny"):
                cx.dma("sp", t[:], src_row.rearrange("(m p) -> p m", p=128), reads=[], writes=[t], sb=t)
            return t
        cb = colvec("cb", conv_b[l]); ba = colvec("ba", lru_ba[l]); bx = colvec("bx", lru_bx[l]); lam = colvec("lam", lru_lambda[l])
        cw = cx.sbuf(ph, "cw", [128, 4, 8])
        with nc.allow_non_contiguous_dma(reason="tiny"):
            cx.dma("sp", cw[:], conv_w[l].rearrange("i (m p) -> p i m", p=128), reads=[], writes=[cw], sb=cw)
        el = cx.sbuf(ph, "el", [128, 8]); cA = cx.sbuf(ph, "cA", [128, 8]); cA2 = cx.sbuf(ph, "cA2", [128, 8])
        cx.op("act", lambda: S.activation(el[:], lam[:], AF.Exp, scale=-1.0), [lam], [el])
        cx.op("act", lambda: S.activation(cA[:], el[:], AF.Ln, bias=1.0, scale=1.0), [el], [cA])
        cx.op("dve", lambda: V.tensor_scalar_mul(cA2[:], cA[:], -16.0), [cA], [cA2])
        cx.op("dve", lambda: V.tensor_scalar_mul(cA[:], cA[:], -8.0), [cA], [cA])
        BDf = cx.sbuf(ph, "BDf", [128, 2, 128]); BD = [cx.sbuf(ph, "BD%d" % i, [128, 2, 128], F32R) for i in range(2)]
        cx.op("dve", lambda: V.memset(BDf[:], 0.0), [], [BDf])
        N = SEQ
        xc = [cx.sbuf(ph, "xc%d" % i, [128, 3 + N]) for i in range(2)]
        gc = [cx.sbuf(ph, "gc%d" % i, [128, N]) for i in range(2)]
        xvs = [cx.sbuf(ph, "xv%d" % i, [128, N], F32R) for i in range(2)]
        tas = [cx.sbuf(ph, "t_a%d" % i, [128, N]) for i in range(2)]
        t_b = cx.sbuf(ph, "t_b", [128, N])
        t_c = cx.sbuf(ph, "t_c", [128, N]); t_d = cx.sbuf(ph, "t_d", [128, N]); hh = cx.sbuf(ph, "hh", [128, N])
        oc = [cx.sbuf(ph, "oc%d" % i, [128, N], BF16) for i in range(2)]
        units = [(m, s) for m in range(8) for s in range(NSEQ)]

        def stage1(ui):
            m, s = units[ui]
            bd = BD[m % 2]
            if s == 0:
                for g_, wsrc in enumerate((lru_wa, lru_wx)):
                    cx.dma("sp", BDf[0:64, g_, 0:64], wsrc[l, 2 * m], reads=[], writes=[BDf], sb=BDf)
                    yield
                    cx.dma("sp", BDf[64:128, g_, 64:128], wsrc[l, 2 * m + 1], reads=[], writes=[BDf], sb=BDf)
                    yield
                cx.op("dve", lambda: V.tensor_copy(bd[:], BDf[:]), [BDf], [bd])
                yield
            x_ = xc[ui % 2]; g = gc[ui % 2]; xv = xvs[ui % 2]; t_a = tas[ui % 2]
            t0 = s * SEQ
            cx.op("pool", lambda: P.memset(x_[:, 0:3], 0.0), [], [x_])
            yield
            cx.dma("sp", x_[:, 3:3 + N], xgT[0, m * 128:(m + 1) * 128, t0:t0 + N], reads=[xgT], writes=[x_], sb=x_)
            yield
            cx.dma("sp", g[:], xgT[1, m * 128:(m + 1) * 128, t0:t0 + N], reads=[xgT], writes=[g], sb=g)
            yield
            cx.op("dve", lambda: V.tensor_scalar(t_a[:], x_[:, 0:N], cw[:, 0, m:m + 1], cb[:, m:m + 1], ALU.mult, ALU.add), [x_, cw, cb], [t_a])
            yield
            for i in (1, 2):
                cx.op("dve", lambda: V.scalar_tensor_tensor(t_a[:], x_[:, i:i + N], cw[:, i, m:m + 1], t_a[:], ALU.mult, ALU.add), [x_, cw, t_a], [t_a])
                yield
            cx.op("dve", lambda: V.scalar_tensor_tensor(t_a[:], x_[:, 3:3 + N], cw[:, 3, m:m + 1], t_a[:], ALU.mult, ALU.add), [x_, cw, t_a], [t_a])
            yield
            cx.op("act", lambda: S.copy(xv[:], t_a[:]), [t_a], [xv])
            yield
            cx.op("pool", lambda: P.tensor_tensor(t_d[:], g[:], g[:], ALU.mult), [g], [t_d])
            yield
            cx.op("pool", lambda: P.tensor_scalar(t_d[:], t_d[:], 0.044715, 1.0, ALU.mult, ALU.add), [t_d], [t_d])
            yield
            cx.op("pool", lambda: P.tensor_tensor(t_d[:], t_d[:], g[:], ALU.mult), [t_d, g], [t_d])
            yield
            cx.op("act", lambda: S.activation(t_d[:], t_d[:], AF.Sigmoid, scale=1.5957691216057308), [t_d], [t_d])
            yield
            cx.op("pool", lambda: P.tensor_tensor(g[:], t_d[:], g[:], ALU.mult), [t_d, g], [g])
            yield

        def stage2(ui):
            m, s = units[ui]
            bd = BD[m % 2]
            g = gc[ui % 2]; xv = xvs[ui % 2]; t_a = tas[ui % 2]; o_ = oc[ui % 2]
            t0 = s * SEQ
            for q in range(N // 512):
                pr = pb[6]; pi = pb[6]
                cs = slice(q * 512, (q + 1) * 512)
                cx.op("pe", lambda: nc.tensor.matmul(pr[:], bd[:, 0, :], xv[:, cs], start=True, stop=True), [bd, xv], [pr])
                yield
                cx.op("act", lambda: S.activation(t_b[:, cs], pr[:], AF.Sigmoid, bias=ba[:, m:m + 1], scale=1.0), [pr, ba], [t_b])
                cx.op("pe", lambda: nc.tensor.matmul(pi[:], bd[:, 1, :], xv[:, cs], start=True, stop=True), [bd, xv], [pi])
                yield
                cx.op("act", lambda: S.activation(t_c[:, cs], pi[:], AF.Sigmoid, bias=bx[:, m:m + 1], scale=1.0), [pi, bx], [t_c])
            cx.op("dve", lambda: V.tensor_tensor(t_c[:], t_c[:], t_a[:], ALU.mult), [t_c, t_a], [t_c])
            yield
            cx.op("act", lambda: S.activation(t_a[:], t_b[:], AF.Exp, scale=cA[:, m:m + 1]), [t_b, cA], [t_a])
            yield
            cx.op("act", lambda: S.activation(t_b[:], t_b[:], AF.Exp, scale=cA2[:, m:m + 1]), [t_b, cA2], [t_b])
            yield
            cx.op("act", lambda: S.activation(t_b[:], t_b[:], AF.Ln, bias=1.0, scale=-1.0), [t_b], [t_b])
            yield
            cx.op("act", lambda: S.activation(t_b[:], t_b[:], AF.Exp, scale=0.5), [t_b], [t_b])
            yield
            cx.op("dve", lambda: V.tensor_tensor(t_c[:], t_c[:], t_b[:], ALU.mult), [t_c, t_b], [t_c])
            cx.op("dve", lambda: V.tensor_tensor_scan(hh[:], t_a[:], t_c[:], 0.0, ALU.mult, ALU.add), [t_a, t_c], [hh])
            yield
            cx.op("dve", lambda: V.tensor_tensor(o_[:], hh[:], g[:], ALU.mult), [hh, g], [o_])
            yield
            cx.dma("sp", oT[1024 + m * 128:1024 + (m + 1) * 128, t0:t0 + N], o_[:], reads=[o_], writes=[oT], sb=o_)
            yield

        yield from stage1(0)
        for ui in range(len(units)):
            if ui + 1 < len(units):
                yield from stage1(ui + 1)
            yield from stage2(ui)

    def phase_mix(l):
        ph = ExitStack()
        def attn():
            yield from gen_fox(ph)
            yield from gen_sb(ph)
        ga = attn(); gr = gen_lru(ph, l)
        done_a = done_r = False
        while not (done_a and done_r):
            for _ in range(2):
                if not done_a:
                    try:
                        next(ga)
                    except StopIteration:
                        done_a = True
            if not done_r:
                try:
                    next(gr)
                except StopIteration:
                    done_r = True
        cx.end_phase()
        ph.close()

    def ln_tile(ph_bufs, y_src_bufs, y_ap_halves, x_t, gtile, lg, lb, dst_ap, dst_buf, k):
        tt_, st6, mv, rs, res_ = ph_bufs
        t = tt_[k % 2]; r = res_[k % 4]; s6 = st6[k % 2]; mv_ = mv[k % 2]; rs_ = rs[k % 2]
        for hf_ in range(2):
            cs = slice(hf_ * 512, (hf_ + 1) * 512)
            cx.op("dve", lambda: V.tensor_tensor(t[:, cs], y_ap_halves[hf_], gtile[:, cs], ALU.mult), list(y_src_bufs) + [gtile], [t])
        cx.op("dve", lambda: V.scalar_tensor_tensor(t[:], x_t[:], ALPHA, t[:], ALU.mult, ALU.add), [x_t, t], [t])
        for hf_ in range(2):
            cx.op("dve", lambda: V.bn_stats(s6[:, hf_, :], t[:, hf_ * 512:(hf_ + 1) * 512]), [t], [s6])
        cx.op("dve", lambda: V.bn_aggr(mv_[:], s6[:].rearrange("p a b -> p (a b)")), [s6], [mv_])
        cx.op("dve", lambda: V.tensor_scalar_add(rs_[:], mv_[:, 1:2], LN_EPS), [mv_], [rs_])
        cx.op("act", lambda: S.activation(rs_[:], rs_[:], AF.Ln), [rs_], [rs_])
        cx.op("act", lambda: S.activation(rs_[:], rs_[:], AF.Exp, scale=-0.5), [rs_], [rs_])
        cx.op("dve", lambda: V.tensor_scalar(t[:], t[:], mv_[:, 0:1], rs_[:, 0:1], ALU.subtract, ALU.mult), [t, mv_, rs_], [t])
        cx.op("pool", lambda: P.tensor_tensor(t[:], t[:], lg[:], ALU.mult), [t, lg], [t])
        cx.op("dve", lambda: V.tensor_tensor(r[:], t[:], lb[:], ALU.add), [t, lb], [r])
        cx.dma("sp", dst_ap, r[:], reads=[r], writes=[dst_buf], sb=r)
        return r

    def ln_bufs(ph):
        return ([cx.sbuf(ph, "lnt%d" % i, [128, D]) for i in range(2)],
                [cx.sbuf(ph, "lns%d" % i, [128, 2, 6]) for i in range(2)],
                [cx.sbuf(ph, "lnm%d" % i, [128, 2]) for i in range(2)],
                [cx.sbuf(ph, "lnr%d" % i, [128, 1]) for i in range(2)],
                [cx.sbuf(ph, "lno%d" % i, [128, D]) for i in range(4)])

    def phase_merge(l, xsrc, xdst):
        ph = ExitStack()
        mod = load_mod(ph, l, 0, ("gt",))
        lg = cx.sbuf(ph, "lg", [128, D]); lb = cx.sbuf(ph, "lb", [128, D])
        cx.dma("sp", lg[:], row_bc(ln1_g[l, :], 128), reads=[], writes=[lg], sb=lg)
        cx.dma("sp", lb[:], row_bc(ln1_b[l, :], 128), reads=[], writes=[lb], sb=lb)
        wpa = cx.sbuf(ph, "wpa", [128, 4, D], BF16); wpb = cx.sbuf(ph, "wpb", [128, 4, D], BF16)
        wpc = cx.sbuf(ph, "wpc", [128, 8, D], BF16); wo = cx.sbuf(ph, "wo", [128, 8, D], BF16)
        for t_, src in ((wpa, w_pa), (wpb, w_pb), (wpc, w_pc), (wo, w_o)):
            cx.dma("pool", None, None, reads=[], writes=[t_], sb=t_,
                   fn=lambda: P.dma_start(out=t_[:], in_=src[l].rearrange("(kc p) n -> p kc n", p=128)))
        oTc = [cx.sbuf(ph, "oTc%d" % i, [128, 16, 512], BF16) for i in range(1)]
        gg = [cx.sbuf(ph, "gg%d" % i, [128, 3, 512]) for i in range(2)]
        ta = [cx.sbuf(ph, "ta%d" % i, [128, 512]) for i in range(2)]
        tb = [cx.sbuf(ph, "tb%d" % i, [128, 512]) for i in range(2)]
        mT = [cx.sbuf(ph, "mT%d" % i, [128, 8, 512], BF16) for i in range(1)]
        xt = [cx.sbuf(ph, "xt%d" % i, [128, D]) for i in range(2)]
        lnb = ln_bufs(ph)
        rs = route_setup(ph, l)
        hist = []
        gk = 0; k = 0
        for ch in range(NCH):
            s = ch // (SEQ // 512)
            tok0 = ch * 512
            oc_ = oTc[0]; mT_ = mT[0]
            cx.dma("sp", oc_[:], oT[:, tok0:tok0 + 512].rearrange("(kc p) t -> p kc t", p=128), reads=[oT], writes=[oc_], sb=oc_)
            for m in range(8):
                g_ = gg[gk % 2]; a_ = ta[gk % 2]; b_ = tb[gk % 2]; gk += 1
                cx.dma("sp", g_[:], gT[:, tok0:tok0 + 512].rearrange("(j q p) t -> q p j t", j=3, p=128)[m], reads=[gT], writes=[g_], sb=g_)
                pa, pb_, pc = pb[0 + 3 * (m % 2)], pb[1 + 3 * (m % 2)], pb[2 + 3 * (m % 2)]
                for kc in range(4):
                    cx.op("pe", lambda: nc.tensor.matmul(pa[:], wpa[:, kc, m * 128:(m + 1) * 128], oc_[:, kc, :], start=(kc == 0), stop=(kc == 3)), [wpa, oc_], [pa])
                for kc in range(4):
                    cx.op("pe", lambda: nc.tensor.matmul(pb_[:], wpb[:, kc, m * 128:(m + 1) * 128], oc_[:, 4 + kc, :], start=(kc == 0), stop=(kc == 3)), [wpb, oc_], [pb_])
                for kc in range(8):
                    cx.op("pe", lambda: nc.tensor.matmul(pc[:], wpc[:, kc, m * 128:(m + 1) * 128], oc_[:, 8 + kc, :], start=(kc == 0), stop=(kc == 7)), [wpc, oc_], [pc])
                cx.op("dve", lambda: V.tensor_tensor(a_[:], pa[:], g_[:, 0, :], ALU.mult), [pa, g_], [a_])
                cx.op("dve", lambda: V.tensor_tensor(b_[:], pb_[:], g_[:, 1, :], ALU.mult), [pb_, g_], [b_])
                cx.op("dve", lambda: V.tensor_tensor(a_[:], a_[:], b_[:], ALU.add), [a_, b_], [a_])
                cx.op("dve", lambda: V.tensor_tensor(b_[:], pc[:], g_[:, 2, :], ALU.mult), [pc, g_], [b_])
                cx.op("dve", lambda: V.tensor_tensor(mT_[:, m, :], a_[:], b_[:], ALU.add), [a_, b_], [mT_])
            for tt in range(4):
                ti = ch * 4 + tt
                x_t = xt[k % 2]
                cx.dma("sp", x_t[:], xsrc[ti * 128:(ti + 1) * 128, :], reads=[xsrc], writes=[x_t], sb=x_t)
                ys = []
                for hf_ in range(2):
                    yb = pb[(k * 2 + hf_) % 6]
                    for m in range(8):
                        cx.op("pe", lambda: nc.tensor.matmul(yb[:], mT_[:, m, tt * 128:(tt + 1) * 128], wo[:, m, hf_ * 512:(hf_ + 1) * 512], start=(m == 0), stop=(m == 7)),
                              [mT_, wo], [yb])
                    ys.append(yb)
                if len(hist) >= 1:
                    route_stage(rs, *hist[-1], 1)
                if len(hist) >= 2:
                    route_stage(rs, *hist[-2], 3)
                r_ = ln_tile(lnb, ys, [ys[0][:], ys[1][:]], x_t, mod[("gt", s)], lg, lb, xdst[ti * 128:(ti + 1) * 128, :], xdst, k)
                route_stage(rs, ti, r_, 0)
                if len(hist) >= 1:
                    route_stage(rs, *hist[-1], 2)
                if len(hist) >= 2:
                    route_stage(rs, *hist[-2], 4)
                hist.append((ti, r_))
                k += 1
        route_stage(rs, *hist[-1], 1)
        route_stage(rs, *hist[-2], 3)
        route_stage(rs, *hist[-1], 2)
        route_stage(rs, *hist[-2], 4)
        route_stage(rs, *hist[-1], 3)
        route_stage(rs, *hist[-1], 4)
        cx.op("dve", lambda: V.tensor_copy(CNTI[0:1, :], rs["cnt"][0:1, :]), [rs["cnt"]], [CNTI])
        if "cnt" in dbg:
            cx.dma("sp", dbg["cnt"][:], rs["cnt"][:], reads=[rs["cnt"]], writes=[dbg["cnt"]], sb=rs["cnt"])
        cx.end_phase()
        ph.close()

    def route_setup(ph, l):
        rs = {}
        rs["mod"] = load_mod(ph, l, 1, ("sh", "sc"))
        wr = cx.sbuf(ph, "wr", [128, 8, NE])
        with nc.allow_non_contiguous_dma(reason="router weights 128B runs"):
            cx.dma("sp", wr[:], w_router[l].rearrange("(kc p) e -> p kc e", p=128), reads=[], writes=[wr], sb=wr)
        brt = cx.sbuf(ph, "brt", [128, NE])
        cx.dma("sp", brt[:], row_bc(b_router[l, :], 128), reads=[], writes=[brt], sb=brt)
        cnt = cx.sbuf(ph, "cnt", [128, NE]); cx.op("dve", lambda: V.memset(cnt[:], 0.0), [], [cnt])
        rs.update(wr=wr, brt=brt, cnt=cnt)
        rs["hf"] = [cx.sbuf(ph, "rhf%d" % i, [128, D]) for i in range(3)]
        rs["hb"] = [cx.sbuf(ph, "rhb%d" % i, [128, D], BF16) for i in range(3)]
        rs["hT"] = [cx.sbuf(ph, "rhT%d" % i, [128, 8, 128]) for i in range(3)]
        rs["pp"] = pbh[:].bitcast(F32)
        rs["ppb"] = pbh
        def sm(name, w=NE):
            return [cx.sbuf(ph, "%s%d" % (name, i), [128, w]) for i in range(3)]
        for nm, w in (("lgt", NE), ("m8", 8), ("msk", NE), ("ex", NE), ("em", NE), ("ssum", 1), ("gte", NE), ("slv", NE),
                      ("s8", 8), ("nmx", 1), ("tmp", NE)):
            rs[nm] = sm(nm, w)
        return rs

    def route_stage(rs, ti, x_t, stage):
        mod = rs["mod"]; wr = rs["wr"]; brt = rs["brt"]; cnt = rs["cnt"]
        s = ti // (SEQ // 128)
        b = ti % 3
        h_f = rs["hf"][b]; h_b = rs["hb"][b]; hT_ = rs["hT"][b]
        lgt, m8, msk, ex, em, ssum, gte, slv, s8, nmx, tmp = (rs[k] for k in ("lgt", "m8", "msk", "ex", "em", "ssum", "gte", "slv", "s8", "nmx", "tmp"))
        L_ = lgt[b]; M8 = m8[b]; MK = msk[b]; SL = slv[b]
        pp = rs["pp"]
        if stage == 0:
            cx.op("dve", lambda: V.tensor_tensor(h_f[:], x_t[:], mod[("sc", s)][:], ALU.mult), [x_t, mod[("sc", s)]], [h_f])
            cx.op("dve", lambda: V.tensor_tensor(h_f[:], h_f[:], mod[("sh", s)][:], ALU.add), [h_f, mod[("sh", s)]], [h_f])
            cx.op("act", lambda: S.copy(h_b[:], h_f[:]), [h_f], [h_b])
        elif stage == 1:
            pt = pb[6]
            for half in range(2):
                for q in range(4):
                    kc = half * 4 + q
                    cx.op("pe", lambda: nc.tensor.transpose(pt[:, q * 128:(q + 1) * 128], h_f[:, kc * 128:(kc + 1) * 128], identf[:]), [h_f, identf], [pt])
                cx.op("act", lambda: S.copy(hT_[:, half * 4:half * 4 + 4, :], pt[:].rearrange("p (q t) -> p q t", q=4)), [pt], [hT_])
            for kc in range(8):
                cx.op("pe", lambda: nc.tensor.matmul(pp[:, 0:NE], hT_[:, kc, :], wr[:, kc, :], start=(kc == 0), stop=(kc == 7)), [hT_, wr], [rs["ppb"]])
        elif stage == 2:
            cx.op("dve", lambda: V.tensor_tensor(L_[:], pp[:, 0:NE], brt[:], ALU.add), [rs["ppb"], brt], [L_])
            cx.op("dve", lambda: V.max(M8[:], L_[:]), [L_], [M8])
            cx.op("dve", lambda: V.tensor_scalar(MK[:], L_[:], M8[:, 3:4], None, ALU.is_ge), [L_, M8], [MK])
            cx.op("dve", lambda: V.tensor_scalar_mul(nmx[b][:], M8[:, 0:1], -1.0), [M8], [nmx[b]])
            cx.op("act", lambda: S.activation(ex[b][:], L_[:], AF.Exp, bias=nmx[b][:, 0:1], scale=1.0), [L_, nmx[b]], [ex[b]])
            cx.op("dve", lambda: V.tensor_tensor(em[b][:], ex[b][:], MK[:], ALU.mult), [ex[b], MK], [em[b]])
            cx.op("dve", lambda: V.reduce_sum(ssum[b][:], em[b][:], mybir.AxisListType.X), [em[b]], [ssum[b]])
            cx.op("dve", lambda: V.reciprocal(ssum[b][:], ssum[b][:]), [ssum[b]], [ssum[b]])
            cx.op("dve", lambda: V.tensor_scalar_mul(gte[b][:], em[b][:], ssum[b][:, 0:1]), [em[b], ssum[b]], [gte[b]])
        elif stage == 3:
            cx.op("pe", lambda: nc.tensor.matmul(pp[:, 64:64 + NE], masks[:, M_SUT, :], MK[:], start=True, stop=True), [masks, MK], [rs["ppb"]])
            cx.op("pe", lambda: nc.tensor.matmul(pp[:, 128:128 + NE], masks[:, M_ONES, :], MK[:], start=True, stop=True), [masks, MK], [rs["ppb"]])
        else:
            cx.op("dve", lambda: V.tensor_tensor(SL[:], pp[:, 64:64 + NE], cnt[:], ALU.add), [rs["ppb"], cnt], [SL])
            cx.op("dve", lambda: V.tensor_tensor(cnt[:], pp[:, 128:128 + NE], cnt[:], ALU.add), [rs["ppb"], cnt], [cnt])
            cx.op("dve", lambda: V.tensor_tensor(SL[:], SL[:], ebase[:], ALU.add), [SL, ebase], [SL])
            cx.op("dve", lambda: V.tensor_tensor(SL[:], SL[:], MK[:], ALU.mult), [SL, MK], [SL])
            cx.op("dve", lambda: V.tensor_scalar_add(SL[:], SL[:], -1.0), [SL], [SL])
            cx.op("dve", lambda: V.max(s8[b][:], SL[:]), [SL], [s8[b]])
            cx.op("dve", lambda: V.tensor_copy(IDX[:, ti, :], s8[b][:, 0:4]), [s8[b]], [IDX])
            for k in range(4):
                cx.op("dve", lambda: V.tensor_scalar(tmp[b][:], SL[:], s8[b][:, k:k + 1], None, ALU.is_equal), [SL, s8[b]], [tmp[b]])
                cx.op("dve", lambda: V.tensor_tensor(tmp[b][:], tmp[b][:], gte[b][:], ALU.mult), [tmp[b], gte[b]], [tmp[b]])
                cx.op("dve", lambda: V.reduce_sum(GK[:, ti, k:k + 1], tmp[b][:], mybir.AxisListType.X), [tmp[b]], [GK])
            for k in range(4):
                cx.dma("pool", None, None, reads=[h_b, IDX], writes=[xbuf], sb=h_b,
                       fn=lambda: P.indirect_dma_start(out=xbuf[:], out_offset=bass.IndirectOffsetOnAxis(ap=IDX[:, ti, k:k + 1], axis=0),
                                                       in_=h_b[:], in_offset=None))

    def phase_experts(l):
        ph = ExitStack()
        wgu = [cx.sbuf(ph, "wgu%d" % i, [128, 8, 2 * D], BF16) for i in range(2)]
        wdn = [cx.sbuf(ph, "wdn%d" % i, [128, 8, D], BF16) for i in range(2)]
        bgu = [cx.sbuf(ph, "bgu%d" % i, [128, 16]) for i in range(2)]
        bdn = [cx.sbuf(ph, "bdn%d" % i, [1, D], BF16) for i in range(2)]
        xrA = [cx.sbuf(ph, "xrA%d" % i, [128, 4, D], BF16) for i in range(2)]
        xrB = [cx.sbuf(ph, "xrB%d" % i, [128, 2, D], BF16) for i in range(2)]
        xTA = [cx.sbuf(ph, "xTA%d" % i, [128, 8, 512], BF16) for i in range(2)]
        xTB = [cx.sbuf(ph, "xTB%d" % i, [128, 8, 256], BF16) for i in range(2)]
        aTA = cx.sbuf(ph, "aTA", [128, 8, 512], BF16)
        aTB = cx.sbuf(ph, "aTB", [128, 8, 256], BF16)
        g1 = [cx.sbuf(ph, "g1%d" % i, [128, 512]) for i in range(2)]
        sg = [cx.sbuf(ph, "sg%d" % i, [128, 512]) for i in range(2)]
        u1 = [cx.sbuf(ph, "u1%d" % i, [128, 512]) for i in range(2)]
        yt = [cx.sbuf(ph, "yt%d" % i, [128, D]) for i in range(2)]
        k_ = {"jk": 0, "yk": 0}
        cregs = nc.alloc_registers("cnt_reg_%d" % l)

        def load_w(e):
            b = e % 2
            cx.dma("pool", None, None, reads=[], writes=[wgu[b]], sb=wgu[b],
                   fn=lambda: P.dma_start(out=wgu[b][:], in_=w_gu[l, e].rearrange("(kc p) n -> p kc n", p=128)))
            cx.dma("pool", None, None, reads=[], writes=[wdn[b]], sb=wdn[b],
                   fn=lambda: P.dma_start(out=wdn[b][:], in_=w_down[l, e].rearrange("(kc p) n -> p kc n", p=128)))
            cx.dma("pool", None, None, reads=[], writes=[bdn[b]], sb=bdn[b],
                   fn=lambda: P.dma_start(out=bdn[b][:], in_=b_down[l, e:e + 1, :]))
            with nc.allow_non_contiguous_dma(reason="tiny"):
                cx.dma("sp", bgu[b][:], b_gu[l, e].rearrange("(m p) -> p m", p=128), reads=[], writes=[bgu[b]], sb=bgu[b])

        def load_x(xr_, r0, nt):
            cx.dma("sp", xr_[:, 0:nt, :], xbuf[r0:r0 + nt * 128, :].rearrange("(t p) d -> p t d", p=128), reads=[xbuf], writes=[xr_], sb=xr_)

        def transp(xr_, xT, t_):
            for kc in range(8):
                cx.op("pe", lambda: nc.tensor.transpose(pbh[:, kc * 128:(kc + 1) * 128], xr_[:, t_, kc * 128:(kc + 1) * 128], identb[:]), [xr_, identb], [pbh])
            if t_ % 2:
                cx.op("act", lambda: S.copy(xT[:, :, t_ * 128:(t_ + 1) * 128], pbh[:].rearrange("p (kc t) -> p kc t", kc=8)), [pbh], [xT])
            else:
                cx.op("dve", lambda: V.tensor_copy(xT[:, :, t_ * 128:(t_ + 1) * 128], pbh[:].rearrange("p (kc t) -> p kc t", kc=8)), [pbh], [xT])

        def up(e, xT, aT, ns):
            b = e % 2
            for j in range(8):
                jk = k_["jk"]; k_["jk"] += 1
                pg = pb[(jk % 2) * 2]; pu = pb[(jk % 2) * 2 + 1]
                g_ = g1[jk % 2]; s_ = sg[jk % 2]; u_ = u1[jk % 2]
                for kc in range(8):
                    cx.op("pe", lambda: nc.tensor.matmul(pg[:, 0:ns], wgu[b][:, kc, j * 128:(j + 1) * 128], xT[:, kc, 0:ns], start=(kc == 0), stop=(kc == 7)), [wgu[b], xT], [pg])
                for kc in range(8):
                    cx.op("pe", lambda: nc.tensor.matmul(pu[:, 0:ns], wgu[b][:, kc, D + j * 128:D + (j + 1) * 128], xT[:, kc, 0:ns], start=(kc == 0), stop=(kc == 7)), [wgu[b], xT], [pu])
                cx.op("dve", lambda: V.tensor_scalar(g_[:, 0:ns], pg[:, 0:ns], bgu[b][:, j:j + 1], 7.0, ALU.add, ALU.min), [pg, bgu[b]], [g_])
                cx.op("act", lambda: S.activation(u_[:, 0:ns], pu[:, 0:ns], AF.Identity, bias=bgu[b][:, 8 + j:9 + j], scale=1.0), [pu, bgu[b]], [u_])
                cx.op("act", lambda: S.activation(s_[:, 0:ns], g_[:, 0:ns], AF.Sigmoid, scale=1.702), [g_], [s_])
                cx.op("dve", lambda: V.tensor_scalar(u_[:, 0:ns], u_[:, 0:ns], 7.0, -7.0, ALU.min, ALU.max), [u_], [u_])
                cx.op("dve", lambda: V.tensor_tensor(g_[:, 0:ns], g_[:, 0:ns], s_[:, 0:ns], ALU.mult), [g_, s_], [g_])
                cx.op("dve", lambda: V.scalar_tensor_tensor(aT[:, j, 0:ns], u_[:, 0:ns], 1.0, g_[:, 0:ns], ALU.add, ALU.mult), [u_, g_], [aT])

        def down_tile(e, aT, r0, t_):
            b = e % 2
            yk = k_["yk"]; k_["yk"] += 1
            y_ = yt[yk % 2]
            for hf_ in range(2):
                py = pb[4 + (yk * 2 + hf_) % 3]
                for j in range(8):
                    cx.op("pe", lambda: nc.tensor.matmul(py[:], aT[:, j, t_ * 128:(t_ + 1) * 128], wdn[b][:, j, hf_ * 512:(hf_ + 1) * 512], start=(j == 0), stop=False), [aT, wdn[b]], [py])
                cx.op("pe", lambda: nc.tensor.matmul(py[:], onesb[0:1, :], bdn[b][0:1, hf_ * 512:(hf_ + 1) * 512], start=False, stop=True), [onesb, bdn[b]], [py])
                if hf_:
                    cx.op("act", lambda: S.copy(y_[:, 512:1024], py[:]), [py], [y_])
                else:
                    cx.op("dve", lambda: V.tensor_copy(y_[:, 0:512], py[:]), [py], [y_])
            cx.dma("sp", ybuf[r0 + t_ * 128:r0 + (t_ + 1) * 128, :], y_[:], reads=[y_], writes=[ybuf], sb=y_)

        def small_group(e, gi):
            r0 = e * CAP + 512 + 256 * gi
            up(e, xTB[gi], aTB, 256)
            for t_ in range(2):
                down_tile(e, aTB, r0, t_)

        def tail_groups(e):
            small_group(e, 0)
            cx.cond_region(cregs, 768, lambda: small_group(e, 1))

        load_w(0)
        load_x(xrA[0], 0, 4)
        for t_ in range(4):
            transp(xrA[0], xTA[0], t_)
        for e in range(NE):
            r0 = e * CAP
            nxt = e + 1 < NE
            if nxt:
                load_w(e + 1)
                load_x(xrA[(e + 1) % 2], (e + 1) * CAP, 4)
            for gi in range(2):
                load_x(xrB[gi], r0 + 512 + 256 * gi, 2)
            up(e, xTA[e % 2], aTA, 512)
            for t_ in range(4):
                if nxt:
                    transp(xrA[(e + 1) % 2], xTA[(e + 1) % 2], t_)
                else:
                    pass
                transp(xrB[t_ // 2], xTB[t_ // 2], t_ % 2)
                down_tile(e, aTA, r0, t_)
            for E in cx.E.values():
                E.eng.reg_load(cregs[E.eng.engine], CNTI[0:1, e:e + 1])
            cx.cond_region(cregs, 512, lambda: tail_groups(e))
        cx.end_phase()
        ph.close()

    def phase_combine(l, xsrc, xdst):
        ph = ExitStack()
        mod = load_mod(ph, l, 1, ("gt",))
        lg = cx.sbuf(ph, "lg", [128, D]); lb = cx.sbuf(ph, "lb", [128, D])
        cx.dma("sp", lg[:], row_bc(ln2_g[l, :], 128), reads=[], writes=[lg], sb=lg)
        cx.dma("sp", lb[:], row_bc(ln2_b[l, :], 128), reads=[], writes=[lb], sb=lb)
        xt = [cx.sbuf(ph, "xt%d" % i, [128, D]) for i in range(2)]
        yg = [cx.sbuf(ph, "yg%d" % i, [128, D]) for i in range(12)]
        acc = [cx.sbuf(ph, "acc%d" % i, [128, D]) for i in range(2)]
        lnb = ln_bufs(ph)
        def gathers(ti):
            for k in range(4):
                y_ = yg[(ti % 3) * 4 + k]
                cx.dma("pool", None, None, reads=[ybuf, IDX], writes=[y_], sb=y_,
                       fn=lambda: P.indirect_dma_start(out=y_[:], out_offset=None, in_=ybuf[:],
                                                       in_offset=bass.IndirectOffsetOnAxis(ap=IDX[:, ti, k:k + 1], axis=0)))
        gathers(0); gathers(1)
        for ti in range(NT):
            s = ti // (SEQ // 128)
            b = ti % 2
            x_t = xt[b]; a_ = acc[b]
            cx.dma("sp", x_t[:], xsrc[ti * 128:(ti + 1) * 128, :], reads=[xsrc], writes=[x_t], sb=x_t)
            if ti + 2 < NT:
                gathers(ti + 2)
            ys = [yg[(ti % 3) * 4 + k] for k in range(4)]
            cx.op("dve", lambda: V.tensor_scalar_mul(a_[:], ys[0][:], GK[:, ti, 0:1]), [ys[0], GK], [a_])
            for k in range(1, 4):
                cx.op("dve", lambda: V.scalar_tensor_tensor(a_[:], ys[k][:], GK[:, ti, k:k + 1], a_[:], ALU.mult, ALU.add), [ys[k], GK, a_], [a_])
            ln_tile(lnb, [a_], [a_[:, 0:512], a_[:, 512:1024]], x_t, mod[("gt", s)], lg, lb, xdst[ti * 128:(ti + 1) * 128, :], xdst, ti)
        cx.end_phase()
        ph.close()

    def done(tag):
        return stop_after == tag

    phase_ada()
    cur = x_in
    finished = False
    for l in range(n_layers):
        if done("ada"):
            break
        phase_proj(l, cur)
        if done("proj"): break
        phase_fprep(l)
        phase_mix(l)
        if done("mix"): break
        x1 = xres[0]
        phase_merge(l, cur, x1)
        if done("merge"): break
        phase_experts(l)
        if done("experts"): break
        last = (l == n_layers - 1)
        x2 = out_d if last else xres[1]
        phase_combine(l, x1, x2)
        cur = x2
    for name, src in (("oT", oT), ("x1", xres[0]), ("gT", gT), ("faT", faT), ("qkT", qkT[0]), ("xbuf", xbuf), ("ybuf", ybuf)):
        if name in dbg:
            ph = ExitStack()
            d = dbg[name]
            rows, cols = d.t.shape
            cw_ = min(cols, 1024)
            tmpb = [cx.sbuf(ph, "dump%d" % i, [128, cw_], src.t.dtype if hasattr(src, "t") else src.dtype) for i in range(2)]
            tmpf = [cx.sbuf(ph, "dumpf%d" % i, [128, cw_]) for i in range(2)]
            srcb = src if hasattr(src, "t") else qkT
            i = 0
            for r0 in range(0, rows, 128):
                for c0 in range(0, cols, cw_):
                    i += 1
                    n = min(128, rows - r0)
                    tb_, tf_ = tmpb[i % 2], tmpf[i % 2]
                    cx.dma("sp", tb_[0:n, :], src[r0:r0 + n, c0:c0 + cw_], reads=[srcb], writes=[tb_], sb=tb_)
                    cx.op("dve", lambda: V.tensor_copy(tf_[0:n, :], tb_[0:n, :]), [tb_], [tf_])
                    cx.dma("sp", d[r0:r0 + n, c0:c0 + cw_], tf_[0:n, :], reads=[tf_], writes=[d], sb=tf_)
            cx.end_phase()
            ph.close()
    ph = ExitStack()
    for name in ("IDX", "GK"):
        if name in dbg:
            srcb = IDX if name == "IDX" else GK
            tmpf = cx.sbuf(ph, "dumpg" + name, [128, NT * 4])
            cx.op("dve", lambda: V.tensor_copy(tmpf[:], srcb[:].rearrange("p a b -> p (a b)")), [srcb], [tmpf])
            cx.dma("sp", dbg[name][:], tmpf[:], reads=[tmpf], writes=[dbg[name]], sb=tmpf)
    cx.end_phase()
    ph.close()
    st.close()
    return nc, cx


def make_consts():
    import ml_dtypes
    k = np.arange(128)[:, None]
    q = np.arange(128)[None, :]
    masks = np.zeros((128, 8, 128), np.float32)
    masks[:, 0, :] = (k < q)
    masks[:, 1, :] = (k > q)
    masks[:, 2, :] = 1.0
    masks[:, 3, :] = np.where(k > q, NEG, 0.0)
    masks[:, 4, :] = np.where(k >= q, NEG, 0.0)
    masks[:, 5, :] = np.where(k < q, -1.0, 0.0)
    masks[:, 6, :] = np.where(k >= q, -1.0, 0.0)
    masks[:, 7, :] = -1.0
    ebase = np.broadcast_to((np.arange(NE) * CAP + 1).astype(np.float32)[None, :], (128, NE)).copy()
    return {
        "k_identb": np.eye(128, dtype=np.float32).astype(ml_dtypes.bfloat16),
        "k_identf": np.eye(128, dtype=np.float32),
        "k_masks": masks,
        "k_ebase": ebase,
    }


WEIGHT_KEYS = ["w_ada", "b_ada", "ln1_g", "ln1_b", "w_in", "b_f", "conv_w", "conv_b", "lru_wa", "lru_ba", "lru_wx",
               "lru_bx", "lru_lambda", "w_gate", "b_gate", "w_pa", "w_pb", "w_pc", "w_o", "ln2_g", "ln2_b", "w_router",
               "b_router", "w_gu", "b_gu", "w_down", "b_down"]


def make_in_maps(inputs):
    consts = make_consts()
    x = np.ascontiguousarray(np.asarray(inputs["x"], dtype=np.float32))
    c = np.ascontiguousarray(np.asarray(inputs["c"], dtype=np.float32))
    shared = {k: np.ascontiguousarray(np.asarray(inputs[k], dtype=np.float32)) for k in WEIGHT_KEYS}
    shared.update(consts)
    in_maps = []
    for i in range(NCORES):
        m = dict(shared)
        m["x"] = x[NSEQ * i:NSEQ * (i + 1)].reshape(T, D)
        m["c"] = c[NSEQ * i:NSEQ * (i + 1)]
        in_maps.append(m)
    return in_maps


def kernel(**inputs):
    nc, cx = build_program()
    in_maps = make_in_maps(inputs)
    res = run_bass_kernel_spmd(nc, in_maps, core_ids=list(range(NCORES)))
    out = np.stack([np.asarray(r["out"]).reshape(NSEQ, SEQ, D) for r in res.results], axis=0)
    return out.reshape(NCORES * NSEQ, SEQ, D).astype(np.float32)
```
